# Optimizing a Trainium2 kernel written in Bass

```python
import jax, jax.numpy as jnp
from jax import lax
import numpy as np

D_MODEL = 1024
BATCH = 32
SEQ = 2048
DEPTH = 1

MEM_LEN = 256

HEAD_DIM = 64
NSA_HEADS = D_MODEL // 128
NSA_KV_HEADS = NSA_HEADS // 4
NSA_GROUP = NSA_HEADS // NSA_KV_HEADS
NSA_WIDTH = NSA_HEADS * HEAD_DIM
NSA_KV_WIDTH = NSA_KV_HEADS * HEAD_DIM
CMP_BLOCK = 32
CMP_STRIDE = 16
CMP_HIDDEN = 2 * HEAD_DIM
SLC_BLOCK = 64
N_SELECT = 8
WINDOW = 256
NSA_QBLOCK = SLC_BLOCK

GLA_HEADS = 4
GLA_DK = 64
GLA_DV = 128
GLA_KEY_WIDTH = GLA_HEADS * GLA_DK
GLA_VAL_WIDTH = GLA_HEADS * GLA_DV
GLA_GATE_RANK = 16
GLA_TAU = 16.0
GLA_CHUNK = 64

XATTN_HEADS = 4
XATTN_HEAD_DIM = 128
XATTN_WIDTH = XATTN_HEADS * XATTN_HEAD_DIM

FFN_DIM = 2816
CONV_WIDTH = 3

RMS_EPS = 1e-6
NEG_INF = -1e30

IN_SPLITS = (NSA_WIDTH, 6 * NSA_KV_WIDTH, 3 * NSA_HEADS,
             GLA_KEY_WIDTH, GLA_KEY_WIDTH, GLA_VAL_WIDTH, GLA_VAL_WIDTH, GLA_GATE_RANK,
             2 * D_MODEL)
IN_WIDTH = (NSA_WIDTH + 6 * NSA_KV_WIDTH + 3 * NSA_HEADS + 2 * GLA_KEY_WIDTH
            + 2 * GLA_VAL_WIDTH + GLA_GATE_RANK + 2 * D_MODEL)

kernel_name = "hybrid_nsa_gla_gated_convffn"


def rms_norm(x, g):
    x32 = x.astype(jnp.float32)
    y = x32 * lax.rsqrt(jnp.mean(x32 * x32, axis=-1, keepdims=True) + RMS_EPS)
    return (y * g.astype(jnp.float32)).astype(x.dtype)


def alibi_slopes(n):
    return 2.0 ** (-8.0 * jnp.arange(1, n + 1, dtype=jnp.float32) / n)


def masked_softmax(s, mask):
    s = jnp.where(mask, s, NEG_INF)
    return jnp.where(mask, jax.nn.softmax(s, axis=-1), 0.0)


def compress_blocks(kv, pos_emb, w1, w2):
    B, S, H, dh = kv.shape
    c = kv.reshape(B, S // CMP_STRIDE, CMP_STRIDE, H, dh)
    blocks = jnp.concatenate([c[:, :-1], c[:, 1:]], axis=2) + pos_emb[None, None, :, None, :]
    n_cmp = blocks.shape[1]
    flat = blocks.transpose(0, 1, 3, 2, 4).reshape(B, n_cmp, H, CMP_BLOCK * dh)
    return jax.nn.gelu(flat @ w1) @ w2


def nsa_attention(q, kc, vc, ks, vs, kw, vw, gate_logits,
                  cmp_pos_k, cmp_w1_k, cmp_w2_k, cmp_pos_v, cmp_w1_v, cmp_w2_v):
    B, S = q.shape[:2]
    f32 = jnp.float32
    q = q.reshape(B, S, NSA_KV_HEADS, NSA_GROUP, HEAD_DIM) * (HEAD_DIM ** -0.5)
    kv_shape = (B, S, NSA_KV_HEADS, HEAD_DIM)
    kc, vc, ks, vs, kw, vw = [a.reshape(kv_shape) for a in (kc, vc, ks, vs, kw, vw)]
    gates = jax.nn.sigmoid(gate_logits.astype(f32)).reshape(B, S, NSA_KV_HEADS, NSA_GROUP, 3)
    slopes = alibi_slopes(NSA_HEADS).reshape(NSA_KV_HEADS, NSA_GROUP)

    k_cmp = compress_blocks(kc, cmp_pos_k, cmp_w1_k, cmp_w2_k)
    v_cmp = compress_blocks(vc, cmp_pos_v, cmp_w1_v, cmp_w2_v)
    n_cmp = k_cmp.shape[1]
    cmp_end = jnp.arange(n_cmp) * CMP_STRIDE + CMP_BLOCK - 1

    n_slc = S // SLC_BLOCK
    n_sel = min(N_SELECT, n_slc)
    cmp_tok = jnp.arange(n_cmp)[:, None] * CMP_STRIDE + jnp.arange(CMP_BLOCK)[None, :]
    overlap = jax.nn.one_hot(cmp_tok // SLC_BLOCK, n_slc, dtype=f32).sum(axis=1) / CMP_BLOCK

    ks_blk = ks.reshape(B, n_slc, SLC_BLOCK, NSA_KV_HEADS, HEAD_DIM).transpose(0, 3, 1, 2, 4)
    vs_blk = vs.reshape(B, n_slc, SLC_BLOCK, NSA_KV_HEADS, HEAD_DIM).transpose(0, 3, 1, 2, 4)
    kw_pad = jnp.pad(kw, ((0, 0), (WINDOW, 0), (0, 0), (0, 0)))
    vw_pad = jnp.pad(vw, ((0, 0), (WINDOW, 0), (0, 0), (0, 0)))
    b_ix = jnp.arange(B)[:, None, None, None]
    h_ix = jnp.arange(NSA_KV_HEADS)[None, :, None, None]
    forced_score = float(NSA_GROUP + 1)

    def query_block(i):
        t0 = i * NSA_QBLOCK
        qb = lax.dynamic_slice_in_dim(q, t0, NSA_QBLOCK, axis=1)
        gb = lax.dynamic_slice_in_dim(gates, t0, NSA_QBLOCK, axis=1)
        t = t0 + jnp.arange(NSA_QBLOCK)

        dist = (t[:, None] - cmp_end[None, :]).astype(f32)
        s = jnp.einsum('bqhgd,bnhd->bhgqn', qb, k_cmp).astype(f32)
        s = s - slopes[:, :, None, None] * dist
        p_cmp = masked_softmax(s, dist >= 0)
        o_cmp = jnp.einsum('bhgqn,bnhd->bqhgd', p_cmp.astype(v_cmp.dtype), v_cmp)

        imp = jnp.einsum('bhgqn,nj->bhqj', p_cmp, overlap)
        cur = t // SLC_BLOCK
        j = jnp.arange(n_slc)
        forced = (j[None, :] == 0) | (j[None, :] == cur[:, None]) | (j[None, :] == cur[:, None] - 1)
        future = j[None, :] > cur[:, None]
        imp = jnp.where(forced, forced_score, jnp.where(future, -1.0, imp))
        _, idx = lax.top_k(imp, n_sel)
        kg = ks_blk[b_ix, h_ix, idx]
        vg = vs_blk[b_ix, h_ix, idx]
        kpos = idx[..., None] * SLC_BLOCK + jnp.arange(SLC_BLOCK)
        dist = (t[:, None, None] - kpos).astype(f32)[:, :, None]
        s = jnp.einsum('bqhgd,bhqnld->bhgqnl', qb, kg).astype(f32)
        s = s - slopes[None, :, :, None, None, None] * dist
        n_keys = n_sel * SLC_BLOCK
        s = s.reshape(B, NSA_KV_HEADS, NSA_GROUP, NSA_QBLOCK, n_keys)
        mask = (dist >= 0).reshape(B, NSA_KV_HEADS, 1, NSA_QBLOCK, n_keys)
        p_slc = masked_softmax(s, mask)
        vg = vg.reshape(B, NSA_KV_HEADS, NSA_QBLOCK, n_keys, HEAD_DIM)
        o_slc = jnp.einsum('bhgqk,bhqkd->bqhgd', p_slc.astype(vg.dtype), vg)

        kwb = lax.dynamic_slice_in_dim(kw_pad, t0, NSA_QBLOCK + WINDOW, axis=1)
        vwb = lax.dynamic_slice_in_dim(vw_pad, t0, NSA_QBLOCK + WINDOW, axis=1)
        kpos = t0 - WINDOW + jnp.arange(NSA_QBLOCK + WINDOW)
        dist_i = t[:, None] - kpos[None, :]
        mask = (dist_i >= 0) & (dist_i < WINDOW) & (kpos[None, :] >= 0)
        s = jnp.einsum('bqhgd,bkhd->bhgqk', qb, kwb).astype(f32)
        s = s - slopes[:, :, None, None] * dist_i.astype(f32)
        p_win = masked_softmax(s, mask)
        o_win = jnp.einsum('bhgqk,bkhd->bqhgd', p_win.astype(vwb.dtype), vwb)

        return gb[..., 0:1] * o_cmp + gb[..., 1:2] * o_slc + gb[..., 2:3] * o_win

    out = lax.map(query_block, jnp.arange(S // NSA_QBLOCK))
    out = out.transpose(1, 0, 2, 3, 4, 5).reshape(B, S, NSA_WIDTH)
    return out.astype(q.dtype)


def gla(q, k, v, r, a_low, w_alpha2, b_alpha, norm_g):
    B, S = q.shape[:2]
    f32 = jnp.float32
    nc = S // GLA_CHUNK
    shp_k = (B, nc, GLA_CHUNK, GLA_HEADS, GLA_DK)
    shp_v = (B, nc, GLA_CHUNK, GLA_HEADS, GLA_DV)
    q = q.reshape(shp_k).astype(f32) * (GLA_DK ** -0.5)
    k = k.reshape(shp_k).astype(f32)
    v = v.reshape(shp_v).astype(f32)
    log_a = jax.nn.log_sigmoid((a_low @ w_alpha2 + b_alpha).astype(f32)) / GLA_TAU
    b = jnp.cumsum(log_a.reshape(shp_k), axis=2)
    b_last = b[:, :, -1:]
    q_d = q * jnp.exp(b)
    k_d = k * jnp.exp(-b)
    k_s = k * jnp.exp(b_last - b)
    causal = jnp.tril(jnp.ones((GLA_CHUNK, GLA_CHUNK), dtype=bool))
    att = jnp.where(causal, jnp.einsum('bnihd,bnjhd->bnhij', q_d, k_d), 0.0)
    o_intra = jnp.einsum('bnhij,bnjhe->bnihe', att, v)
    state_inc = jnp.einsum('bnjhd,bnjhe->bnhde', k_s, v)
    decay = jnp.exp(b_last[:, :, 0])

    def step(state, xs):
        dec, inc = xs
        return dec[..., None] * state + inc, state

    init = jnp.zeros((B, GLA_HEADS, GLA_DK, GLA_DV), f32)
    _, states = lax.scan(step, init, (decay.swapaxes(0, 1), state_inc.swapaxes(0, 1)))
    states = states.swapaxes(0, 1)
    o_inter = jnp.einsum('bnihd,bnhde->bnihe', q_d, states)
    o = (o_intra + o_inter).reshape(B, S, GLA_HEADS, GLA_DV)
    o = o * lax.rsqrt(jnp.mean(o * o, axis=-1, keepdims=True) + RMS_EPS) * norm_g.astype(f32)
    o = o * jax.nn.silu(r.reshape(B, S, GLA_HEADS, GLA_DV).astype(f32))
    return o.reshape(B, S, GLA_VAL_WIDTH).astype(r.dtype)


def memory_cross_attention(h, mem_n, w_xq, w_xkv, w_xo):
    B, S = h.shape[:2]
    M = mem_n.shape[1]
    q = (h @ w_xq).reshape(B, S, XATTN_HEADS, XATTN_HEAD_DIM) * (XATTN_HEAD_DIM ** -0.5)
    k, v = jnp.split(mem_n @ w_xkv, 2, axis=-1)
    k = k.reshape(B, M, XATTN_HEADS, XATTN_HEAD_DIM)
    v = v.reshape(B, M, XATTN_HEADS, XATTN_HEAD_DIM)
    p = jax.nn.softmax(jnp.einsum('bshd,bmhd->bhsm', q, k).astype(jnp.float32), axis=-1)
    o = jnp.einsum('bhsm,bmhd->bshd', p.astype(v.dtype), v).reshape(B, S, XATTN_WIDTH)
    return o @ w_xo


def conv_ffn(h, w_up, conv_w, conv_b, w_down):
    S = h.shape[1]
    u, g = jnp.split(h @ w_up, 2, axis=-1)
    u_pad = jnp.pad(u, ((0, 0), (CONV_WIDTH - 1, 0), (0, 0)))
    u = sum(conv_w[tap] * u_pad[:, tap:tap + S] for tap in range(CONV_WIDTH)) + conv_b
    return (jax.nn.gelu(u) * g) @ w_down


def setup_inputs(seed: int = 0) -> dict:
    key = jax.random.key(seed)
    ks = jax.random.split(key, 28)
    f32 = jnp.float32
    L = DEPTH

    def dense(k, shape, fan_in):
        return jax.random.normal(k, shape, f32) * fan_in ** -0.5

    def gain(k, shape):
        return 1.0 + 0.02 * jax.random.normal(k, shape, f32)

    def small(k, shape, scale):
        return scale * jax.random.normal(k, shape, f32)

    return {
        "x": jax.random.normal(ks[0], (BATCH, SEQ, D_MODEL), f32),
        "mem": jax.random.normal(ks[1], (BATCH, MEM_LEN, D_MODEL), f32),
        "ln_mix_g": gain(ks[2], (L, D_MODEL)),
        "w_in": dense(ks[3], (L, D_MODEL, IN_WIDTH), D_MODEL),
        "nsa_gate_b": small(ks[4], (L, 3 * NSA_HEADS), 0.1),
        "cmp_pos_k": small(ks[5], (L, CMP_BLOCK, HEAD_DIM), 0.1),
        "cmp_w1_k": dense(ks[6], (L, CMP_BLOCK * HEAD_DIM, CMP_HIDDEN), CMP_BLOCK * HEAD_DIM),
        "cmp_w2_k": dense(ks[7], (L, CMP_HIDDEN, HEAD_DIM), CMP_HIDDEN),
        "cmp_pos_v": small(ks[8], (L, CMP_BLOCK, HEAD_DIM), 0.1),
        "cmp_w1_v": dense(ks[9], (L, CMP_BLOCK * HEAD_DIM, CMP_HIDDEN), CMP_BLOCK * HEAD_DIM),
        "cmp_w2_v": dense(ks[10], (L, CMP_HIDDEN, HEAD_DIM), CMP_HIDDEN),
        "gla_w_alpha2": dense(ks[11], (L, GLA_GATE_RANK, GLA_KEY_WIDTH), GLA_GATE_RANK),
        "gla_b_alpha": small(ks[12], (L, GLA_KEY_WIDTH), 0.1),
        "gla_norm_g": gain(ks[13], (L, GLA_DV)),
        "w_branch_nsa": dense(ks[14], (L, NSA_WIDTH, D_MODEL), NSA_WIDTH),
        "w_branch_gla": dense(ks[15], (L, GLA_VAL_WIDTH, D_MODEL), GLA_VAL_WIDTH),
        "w_out": dense(ks[16], (L, D_MODEL, D_MODEL), D_MODEL),
        "ln_x_g": gain(ks[17], (L, D_MODEL)),
        "ln_mem_g": gain(ks[18], (L, D_MODEL)),
        "w_xq": dense(ks[19], (L, D_MODEL, XATTN_WIDTH), D_MODEL),
        "w_xkv": dense(ks[20], (L, D_MODEL, 2 * XATTN_WIDTH), D_MODEL),
        "w_xo": dense(ks[21], (L, XATTN_WIDTH, D_MODEL), XATTN_WIDTH),
        "ln_ffn_g": gain(ks[22], (L, D_MODEL)),
        "w_up": dense(ks[23], (L, D_MODEL, 2 * FFN_DIM), D_MODEL),
        "conv_w": dense(ks[24], (L, CONV_WIDTH, FFN_DIM), CONV_WIDTH),
        "conv_b": small(ks[25], (L, FFN_DIM), 0.02),
        "w_down": dense(ks[26], (L, FFN_DIM, D_MODEL), FFN_DIM),
        "ln_final_g": gain(ks[27], (D_MODEL,)),
    }


def reference(x, mem, ln_mix_g, w_in, nsa_gate_b, cmp_pos_k, cmp_w1_k, cmp_w2_k,
              cmp_pos_v, cmp_w1_v, cmp_w2_v, gla_w_alpha2, gla_b_alpha, gla_norm_g,
              w_branch_nsa, w_branch_gla, w_out, ln_x_g, ln_mem_g, w_xq, w_xkv, w_xo,
              ln_ffn_g, w_up, conv_w, conv_b, w_down, ln_final_g):
    split_points = np.cumsum(IN_SPLITS)[:-1].tolist()
    for l in range(DEPTH):
        h = rms_norm(x, ln_mix_g[l])
        q_a, kv_a, gate_a, q_b, k_b, v_b, r_b, alpha_b, merge = jnp.split(h @ w_in[l], split_points, axis=-1)
        kc, vc, ks_, vs_, kw, vw = jnp.split(kv_a, 6, axis=-1)
        o_nsa = nsa_attention(q_a, kc, vc, ks_, vs_, kw, vw, gate_a + nsa_gate_b[l],
                              cmp_pos_k[l], cmp_w1_k[l], cmp_w2_k[l],
                              cmp_pos_v[l], cmp_w1_v[l], cmp_w2_v[l])
        o_gla = gla(q_b, k_b, v_b, r_b, alpha_b, gla_w_alpha2[l], gla_b_alpha[l], gla_norm_g[l])
        g_nsa, g_gla = jnp.split(jax.nn.sigmoid(merge), 2, axis=-1)
        mix = g_nsa * (o_nsa @ w_branch_nsa[l]) + g_gla * (o_gla @ w_branch_gla[l])
        x = x + (mix @ w_out[l]).astype(x.dtype)
        mem_n = rms_norm(mem, ln_mem_g[l])
        x = x + memory_cross_attention(rms_norm(x, ln_x_g[l]), mem_n, w_xq[l], w_xkv[l], w_xo[l]).astype(x.dtype)
        x = x + conv_ffn(rms_norm(x, ln_ffn_g[l]), w_up[l], conv_w[l], conv_b[l], w_down[l]).astype(x.dtype)
    return rms_norm(x, ln_final_g)
```

```python
import numpy as np
import concourse.bass as bass
import concourse.mybir as mybir
from concourse.bass_utils import run_bass_kernel_spmd

F32 = mybir.dt.float32
BF16 = mybir.dt.bfloat16
U8 = mybir.dt.uint8
AF = mybir.ActivationFunctionType
ALU = mybir.AluOpType
AX = mybir.AxisListType

NCORES = 8
SEQ = 2048
D = 1024
NSEQ = 4
NTOK = NSEQ * SEQ
MEM = 256
FFN = 2816
NEG = -30000.0
EPS = 1e-6

STAGE = [99]


class StopBuild(Exception):
    pass


def stage(n):
    if STAGE[0] == n:
        raise StopBuild()


DEBUG = {}


class Buf:
    __slots__ = ("name", "last_w", "rd_eng", "rd_dma")

    def __init__(self, name):
        self.name = name
        self.last_w = None
        self.rd_eng = {}
        self.rd_dma = []


class Op:
    __slots__ = ("eng", "fn", "dma", "deps", "signal", "token", "prev_slot")

    def __init__(self, eng, fn, dma):
        self.eng = eng
        self.fn = fn
        self.dma = dma
        self.deps = []
        self.signal = dma
        self.token = None
        self.prev_slot = None


ENGS = ("pe", "act", "dve", "pool", "sp")
NDMA_SLOTS = 8


class Prog:
    def __init__(self):
        self.ops = {e: [] for e in ENGS}
        self.final = []
        self.fence_deps = []

    def fence(self):
        deps = []
        for e in ENGS:
            last = None
            for o in reversed(self.ops[e]):
                if not o.dma:
                    last = o
                    break
            if last is not None:
                last.signal = True
                deps.append(last)
            nd = 0
            slots = {}
            for o in self.ops[e]:
                if o.dma:
                    slots[nd % NDMA_SLOTS] = o
                    nd += 1
            deps += list(slots.values())
        self.fence_deps = deps

    def op(self, eng, fn, reads=(), writes=(), dma=False):
        o = Op(eng, fn, dma)
        raw = set()
        other = set()
        for b in reads:
            if b.last_w is not None:
                raw.add(b.last_w)
        for b in writes:
            if b.last_w is not None:
                other.add(b.last_w)
            other.update(b.rd_eng.values())
            other.update(b.rd_dma)
        for d in raw | other:
            if d is o:
                continue
            same = (not dma) and (not d.dma) and d.eng == eng
            if same and (eng == "pe" or d not in raw):
                continue
            o.deps.append(d)
            d.signal = True
        for d in self.fence_deps:
            if (not dma) and (not d.dma) and d.eng == eng:
                continue
            if d not in o.deps:
                o.deps.append(d)
        for b in reads:
            if dma:
                b.rd_dma.append(o)
            else:
                b.rd_eng[eng] = o
        for b in writes:
            b.last_w = o
            b.rd_eng = {}
            b.rd_dma = []
        self.ops[eng].append(o)
        return o

    def emit(self, nc, stack):
        sems = {e: stack.enter_context(nc.semaphore("s_" + e)) for e in ENGS}
        dsem = {e: [stack.enter_context(nc.semaphore("d_%s%d" % (e, i))) for i in range(NDMA_SLOTS)]
                for e in ("sp", "pool", "act")}
        for e in ENGS:
            cnt = 0
            nd = 0
            slot_cnt = [0] * NDMA_SLOTS
            slot_last = [None] * NDMA_SLOTS
            for o in self.ops[e]:
                if o.dma:
                    s = nd % NDMA_SLOTS
                    nd += 1
                    slot_cnt[s] += 16
                    o.prev_slot = slot_last[s]
                    o.token = (dsem[e][s], slot_cnt[s])
                    slot_last[s] = o
                elif o.signal:
                    cnt += 1
                    o.token = (sems[e], cnt)
        block = stack.enter_context(nc.Block())
        prog = self

        def body(e):
            def run(eng):
                waited = {}

                def wait(tok):
                    sem, val = tok
                    k = id(sem)
                    if waited.get(k, 0) >= val:
                        return
                    eng.wait_ge(sem, val)
                    waited[k] = val

                for o in prog.ops[e]:
                    if o.dma and o.prev_slot is not None:
                        wait(o.prev_slot.token)
                    for d in o.deps:
                        wait(d.token)
                    ins = o.fn(eng)
                    if o.token is not None:
                        ins.then_inc(o.token[0], 16 if o.dma else 1)
                if e == "sp":
                    for o in prog.final:
                        wait(o.token)
            return run

        block.tensor(body("pe"))
        block.scalar(body("act"))
        block.vector(body("dve"))
        block.gpsimd(body("pool"))
        block.sync(body("sp"))


class Arena:
    def __init__(self, ap, size):
        self.ap = ap
        self.size = size
        self.off = 0
        self.marks = []

    def alloc(self, shape, dtype, name="t"):
        esz = 4 if dtype == F32 else 2
        n = 1
        for s in shape[1:]:
            n *= s
        nbytes = (n * esz + 31) // 32 * 32
        assert self.off + nbytes <= self.size, (name, self.off, nbytes, self.size)
        a = self.ap[0:shape[0], self.off:self.off + n * esz].bitcast(dtype)
        self.off += nbytes
        if len(shape) == 3:
            a = a.rearrange("p (a b) -> p a b", a=shape[1])
        elif len(shape) == 4:
            a = a.rearrange("p (a b c) -> p a b c", a=shape[1], b=shape[2])
        return a

    def mark(self):
        self.marks.append(self.off)

    def release(self):
        self.off = self.marks.pop()


SLOPES = [2.0 ** (-(h + 1)) for h in range(8)]

C_QA, C_KC, C_VC, C_KS, C_VS, C_KW, C_VW = 0, 512, 640, 768, 896, 1024, 1152
C_GATE, C_QB, C_KB, C_VB, C_RB, C_AL, C_MG = 1280, 1304, 1560, 1816, 2328, 2840, 2856
FM_QA, FM_KC, FM_VC, FM_KS, FM_KW, FM_QB, FM_KB, FM_MG = 0, 4, 5, 6, 8, 10, 12, 14
NFM = 30
TM_VS, TM_VW, TM_KB, TM_VB, TM_RB = 0, 128, 256, 512, 1024
NTM = 1536


def _fm_cols():
    cols = []
    for c in range(4):
        cols += list(range(C_QA + 128 * c, C_QA + 128 * (c + 1)))
    cols += list(range(C_KC, C_KC + 128))
    cols += list(range(C_VC, C_VC + 128))
    for base in (C_KS, C_KW):
        for g in range(2):
            one = list(range(base + 64 * g, base + 64 * (g + 1)))
            cols += one + one
    cols += list(range(C_QB, C_QB + 256))
    cols += list(range(C_KB, C_KB + 256))
    cols += list(range(C_MG, C_MG + 2048))
    cols += list(range(C_AL, C_AL + 16))
    return np.array(cols)


def _tm_cols():
    cols = list(range(C_VS, C_VS + 128)) + list(range(C_VW, C_VW + 128))
    cols += list(range(C_KB, C_KB + 256)) + list(range(C_VB, C_VB + 512)) + list(range(C_RB, C_RB + 512))
    cols += list(range(C_GATE, C_GATE + 24))
    return np.array(cols)


def pmajor(v, nchunk):
    return np.ascontiguousarray(np.asarray(v, np.float32).reshape(nchunk, 128).T)


def host_consts():
    c = {}
    t = np.arange(SEQ)
    n = np.arange(127)
    dist = t[None, :] - (16 * n[:, None] + 31)
    c["cmpD"] = np.where(dist >= 0, -dist, -1.0e6).astype(np.float32)
    ov = np.zeros((127, 32), np.float32)
    for nn in range(127):
        for p in range(32):
            ov[nn, (16 * nn + p) // 64] += 1.0 / 32
    c["ovl"] = ov
    cur = (t // 64)
    j = np.arange(32)
    forced = (j[None, :] == 0) | (j[None, :] == cur[:, None]) | (j[None, :] == cur[:, None] - 1)
    future = j[None, :] > cur[:, None]
    mul = np.where(forced | future, 0.0, 1.0).astype(np.float32)
    add = np.where(forced, 5.0, np.where(future, -1.0, 0.0)).astype(np.float32)
    c["fmul"] = np.ascontiguousarray(mul.reshape(16, 128, 32).transpose(1, 0, 2))
    c["fadd"] = np.ascontiguousarray(add.reshape(16, 128, 32).transpose(1, 0, 2))
    c["tb"] = (64.0 * (cur[None, :] - j[:, None])).astype(np.float32)
    ea = np.zeros((34, SEQ), np.float32)
    ea[t // 64, t] = 1.0
    ea[32] = t % 64
    ea[33] = 1.0
    c["ea"] = ea
    cr = np.zeros((2, 8, 512), np.float32)
    rq = np.arange(512) % 64
    for h in range(8):
        cr[0, h] = SLOPES[h]
        cr[1, h] = -SLOPES[h] * rq
    c["crow"] = cr
    k = np.arange(128)
    cc = np.arange(896)
    c["cb"] = np.where(cc[None, :] - 384 >= k[:, None], 0.0, NEG).astype(np.float32)
    cc = np.arange(1152)
    dd = cc[None, :] - 384 - k[:, None]
    wb = np.zeros((128, 8, 1152), np.float32)
    for h in range(8):
        wb[:, h, :] = np.where((dd >= 0) & (dd < 256), -SLOPES[h] * dd, NEG)
    c["wb"] = wb
    c["ident"] = np.eye(128, dtype=np.float32)
    jj = np.arange(128)
    same = (jj[:, None] // 64) == (jj[None, :] // 64)
    c["gmask"] = (same & (jj[:, None] <= jj[None, :])).astype(np.float32)
    c["srst"] = np.broadcast_to(np.where(t % 64 == 0, 0.0, 1.0).astype(np.float32), (128, SEQ)).copy()
    c["gup"] = (same & (jj[:, None] > jj[None, :])).astype(np.float32)
    return c


CONST_SHAPES = None


def build_program(phases=("p0", "p1", "p2", "p3")):
    import contextlib
    nc = bass.Bass("TRN2", target_bir_lowering=False)
    P = Prog()
    stack = contextlib.ExitStack()

    def din(name, shape, dt=F32):
        return nc.dram_tensor(name, list(shape), dt, kind="ExternalInput").ap()

    def dscr(name, shape, dt):
        kind = "ExternalOutput" if DEBUG.get(name) else "Internal"
        return nc.dram_tensor(name, list(shape), dt, kind=kind).ap()

    consts = host_consts()
    x_d = din("x", [NTOK, D])
    mem_d = din("mem", [NSEQ * MEM, D])
    wfm_d = din("w_fm", [D, NFM * 128 + 16])
    wtm_d = din("w_tm", [D, NTM + 24])
    g_mix_d = din("g_mix", [128, 8])
    gate_b_d = din("gate_b", [128, 24])
    w1k_d = din("w1k", [2048, 128]); w1v_d = din("w1v", [2048, 128])
    w2k_d = din("w2k", [128, 128]); w2v_d = din("w2v", [128, 64])
    pek_d = din("pek", [64, 32]); pev_d = din("pev", [64, 32])
    wa2_d = din("wa2", [17, 256])
    gla_g_d = din("gla_g", [128, 128])
    ba_d = din("ba", [128, 2])
    wbn_d = din("wbn", [512, D]); wbg_d = din("wbg", [512, D]); wout_d = din("wout", [D, D])
    g_x_d = din("g_x", [128, 8]); g_mem_d = din("g_mem", [128, 8])
    wxq_d = din("wxq", [D, 512]); wxkv_d = din("wxkv", [D, 1024]); wxo_d = din("wxo", [512, D])
    g_ffn_d = din("g_ffn", [128, 8])
    wup_d = din("wup", [D, 2 * FFN]); wdn_d = din("wdn", [FFN, D])
    convw_d = din("convw", [128, 3, 22]); convb_d = din("convb", [128, 22])
    g_fin_d = din("g_fin", [128, D])
    cd = {k: din("c_" + k, v.shape) for k, v in consts.items()}
    out_d = nc.dram_tensor("out", [NTOK, D], F32, kind="ExternalOutput").ap()
    s_fm = dscr("s_fm", [NFM, 128, NTOK], BF16)
    s_al = dscr("s_al", [16, NTOK], F32)
    s_tm = dscr("s_tm", [NTOK, NTM], BF16)
    s_gate = dscr("s_gate", [NTOK, 24], F32)
    s_on = dscr("s_on", [4, 128, NTOK], BF16)
    s_og = dscr("s_og", [4, 128, NTOK], BF16)
    s_x2 = dscr("s_x2", [NTOK, D], F32)
    dbg = {}
    if DEBUG.get("gla"):
        dbg["cs"] = nc.dram_tensor("dbg_cs", [128, 2, SEQ], F32, kind="ExternalOutput").ap()
        dbg["kd"] = nc.dram_tensor("dbg_kd", [128, 2, SEQ], BF16, kind="ExternalOutput").ap()
        dbg["qd"] = nc.dram_tensor("dbg_qd", [128, 4, SEQ], BF16, kind="ExternalOutput").ap()
        dbg["rg"] = nc.dram_tensor("dbg_rg", [128, 16, 512], BF16, kind="ExternalOutput").ap()
        dbg["ogb"] = nc.dram_tensor("dbg_ogb", [128, 512], BF16, kind="ExternalOutput").ap()
        dbg["att"] = nc.dram_tensor("dbg_att", [128, 4, 128], BF16, kind="ExternalOutput").ap()
        dbg["la"] = nc.dram_tensor("dbg_la", [128, 2, SEQ], F32, kind="ExternalOutput").ap()
    if DEBUG.get("ffn"):
        dbg["aT"] = nc.dram_tensor("dbg_aT", [128, 22, 512], BF16, kind="ExternalOutput").ap()
        dbg["x3"] = nc.dram_tensor("dbg_x3", [128, 4, D], F32, kind="ExternalOutput").ap()
        dbg["hf"] = nc.dram_tensor("dbg_hf", [128, 8, 512], BF16, kind="ExternalOutput").ap()

    ARENA = 204 * 1024
    arena_t = stack.enter_context(nc.sbuf_tensor("arena", [128, ARENA], U8))
    psum_t = stack.enter_context(nc.psum_tensor("psum", [128, 4096], F32))
    A = Arena(arena_t, ARENA)
    PS = [psum_t[:, 512 * i:512 * (i + 1)] for i in range(8)]
    PSB = [Buf("ps%d" % i) for i in range(8)]

    rr = {"ev": 0, "q": 0}

    def dma(q, out, in_, reads=(), writes=(), **kw):
        return P.op(q, lambda e: e.dma_start(out=out, in_=in_, **kw), reads=reads, writes=writes, dma=True)

    def load_cast(dst, src, wb, nsplit=1):
        last = dst.shape[-1]
        step = (last + nsplit - 1) // nsplit
        for s0 in range(0, last, step):
            s1 = min(last, s0 + step)
            if len(dst.shape) == 2:
                dma("pool", dst[:, s0:s1], src[:, s0:s1], writes=[wb])
            else:
                dma("pool", dst[:, :, s0:s1], src[:, :, s0:s1], writes=[wb])

    def evac_copy(out, in_, reads, writes, scale=None):
        rr["ev"] += 1
        if rr["ev"] % 2 == 0:
            if scale is None:
                P.op("act", lambda e: e.activation(out=out, in_=in_, func=AF.Copy), reads=reads, writes=writes)
            else:
                P.op("act", lambda e: e.activation(out=out, in_=in_, func=AF.Copy, scale=scale), reads=reads, writes=writes)
        else:
            if scale is None:
                P.op("dve", lambda e: e.tensor_copy(out=out, in_=in_), reads=reads, writes=writes)
            else:
                P.op("dve", lambda e: e.tensor_scalar(out=out, in0=in_, scalar1=scale, scalar2=None, op0=ALU.mult),
                     reads=reads, writes=writes)

    def mm(out, lhsT, rhs, start, stop, reads, writes, skip=False):
        P.op("pe", lambda e: e.matmul(out, lhsT=lhsT, rhs=rhs, start=start, stop=stop, skip_group_check=skip),
             reads=reads, writes=writes)

    def transpose(out, in_, ident, reads, writes):
        P.op("pe", lambda e: e.transpose(out, in_, ident), reads=reads, writes=writes)

    ident_f = A.alloc([128, 128], F32, "ident_f")
    ident_b = A.alloc([128, 128], BF16, "ident_b")
    B_ident = Buf("ident")
    dma("sp", ident_f, cd["ident"][:, :], writes=[B_ident])
    dma("pool", ident_b, cd["ident"][:, :], writes=[B_ident])

    def rmsnorm_T(xt, B_xt, nsub, g_sb, B_g, hT, B_hT, xn, B_xn, st, B_st, psA):
        for s in range(nsub):
            P.op("act", lambda e, s=s: e.activation(out=xn[:, s, :], in_=xt[:, s, :], func=AF.Square,
                                                    accum_out=st[:, s:s + 1]),
                 reads=[B_xt], writes=[B_xn, B_st])
        P.op("act", lambda e: e.activation(out=st[:, 8:8 + nsub], in_=st[:, 0:nsub], func=AF.Sqrt, bias=EPS,
                                           scale=1.0 / D), reads=[B_st], writes=[B_st])
        P.op("dve", lambda e: e.reciprocal(out=st[:, 16:16 + nsub], in_=st[:, 8:8 + nsub]), reads=[B_st], writes=[B_st])
        for s in range(nsub):
            P.op("dve", lambda e, s=s: e.tensor_scalar(out=xn[:, s, :], in0=xt[:, s, :], scalar1=st[:, 16 + s:17 + s],
                                                       scalar2=None, op0=ALU.mult),
                 reads=[B_xt, B_st], writes=[B_xn])
        for kc in range(8):
            pi = psA[kc % len(psA)]
            pst = PS[pi].bitcast(BF16)
            for s in range(nsub):
                transpose(pst[:, s * 128:(s + 1) * 128], xn[:, s, kc * 128:(kc + 1) * 128], ident_b,
                          reads=[B_xn, B_ident], writes=[PSB[pi]])
            evac_copy(hT[:, kc, 0:nsub * 128], pst[:, 0:nsub * 128], reads=[PSB[pi], B_g], writes=[B_hT],
                      scale=g_sb[:, kc:kc + 1])

    if "p0" in phases:
        A.mark()
        wfm = A.alloc([128, 8, NFM * 128 + 16], BF16, "wfm"); B_wfm = Buf("wfm")
        wtm = A.alloc([128, 8, NTM + 24], BF16, "wtm"); B_wtm = Buf("wtm")
        load_cast(wfm, wfm_d.rearrange("(kc p) n -> p kc n", p=128), B_wfm, nsplit=4)
        load_cast(wtm, wtm_d.rearrange("(kc p) n -> p kc n", p=128), B_wtm, nsplit=2)
        gmix = A.alloc([128, 8], F32, "gmix"); B_gmix = Buf("gmix")
        dma("sp", gmix, g_mix_d[:, :], writes=[B_gmix])
        xt = A.alloc([128, 4, D], F32, "xt"); B_xt = Buf("xt")
        xn = A.alloc([128, 4, D], BF16, "xn"); B_xn = Buf("xn")
        st = A.alloc([128, 32], F32, "st"); B_st = Buf("st")
        hTs = [A.alloc([128, 8, 512], BF16, "hT%d" % i) for i in range(2)]
        B_hTs = [Buf("hT%d" % i) for i in range(2)]
        fmo = A.alloc([128, NFM, 512], BF16, "fmo")
        B_fmo = [Buf("fmo%d" % i) for i in range(3)]
        alo = A.alloc([16, 512], F32, "alo"); B_alo = Buf("alo")
        tmo = A.alloc([128, 4, NTM], BF16, "tmo"); B_tmo = Buf("tmo")
        gto = A.alloc([128, 4, 24], F32, "gto"); B_gto = Buf("gto")
        mmbank = [2, 3, 4, 5, 6, 7]
        bi = 0
        for it in range(NTOK // 512):
            t0 = it * 512
            hT = hTs[it % 2]; B_hT = B_hTs[it % 2]
            dma("sp", xt, x_d[t0:t0 + 512, :].rearrange("(s p) d -> p s d", p=128), writes=[B_xt])
            rmsnorm_T(xt, B_xt, 4, gmix, B_gmix, hT, B_hT, xn, B_xn, st, B_st, [0, 1])
            for c in range(NFM + 1):
                M = 128 if c < NFM else 16
                pi = mmbank[bi % 6]; bi += 1
                for kc in range(8):
                    mm(PS[pi][0:M, :], wfm[:, kc, c * 128:c * 128 + M], hT[:, kc, :], kc == 0, kc == 7,
                       reads=[B_wfm, B_hT], writes=[PSB[pi]])
                if c == NFM:
                    P.op("dve", lambda e, pi=pi: e.tensor_copy(out=alo, in_=PS[pi][0:16, :]),
                         reads=[PSB[pi]], writes=[B_alo])
                    continue
                bo = B_fmo[c // 10]
                if c < 4:
                    evac_copy(fmo[:, c, :], PS[pi], [PSB[pi]], [bo], scale=0.125)
                elif c >= FM_MG:
                    P.op("act", lambda e, c=c, pi=pi: e.activation(out=fmo[:, c, :], in_=PS[pi], func=AF.Sigmoid),
                         reads=[PSB[pi]], writes=[bo])
                else:
                    evac_copy(fmo[:, c, :], PS[pi], [PSB[pi]], [bo])
                if c % 10 == 9:
                    g0 = c - 9
                    dma("sp", s_fm[g0:g0 + 10, :, t0:t0 + 512].rearrange("c p t -> p c t"), fmo[:, g0:g0 + 10, :],
                        reads=[bo])
            dma("sp", s_al[:, t0:t0 + 512], alo, reads=[B_alo])
            for s in range(4):
                for (c0, c1) in ((0, 512), (512, 1024), (1024, 1536), (1536, 1560)):
                    pi = mmbank[bi % 6]; bi += 1
                    for kc in range(8):
                        mm(PS[pi][:, 0:c1 - c0], hT[:, kc, s * 128:(s + 1) * 128], wtm[:, kc, c0:c1], kc == 0, kc == 7,
                           reads=[B_wtm, B_hT], writes=[PSB[pi]])
                    if c0 < 1536:
                        evac_copy(tmo[:, s, c0:c1], PS[pi], [PSB[pi]], [B_tmo])
                    else:
                        evac_copy(gto[:, s, :], PS[pi][:, 0:24], [PSB[pi]], [B_gto])
            dma("sp", s_tm[t0:t0 + 512, :].rearrange("(s p) n -> p s n", p=128), tmo, reads=[B_tmo])
            dma("sp", s_gate[t0:t0 + 512, :].rearrange("(s p) n -> p s n", p=128), gto, reads=[B_gto])
        A.release()


    def V_tt(out, in0, in1, op, reads, writes):
        P.op("dve", lambda e: e.tensor_tensor(out=out, in0=in0, in1=in1, op=op), reads=reads, writes=writes)

    def V_ts(out, in0, s1, s2, op0, op1, reads, writes):
        if op1 is None:
            P.op("dve", lambda e: e.tensor_scalar(out=out, in0=in0, scalar1=s1, scalar2=None, op0=op0), reads=reads, writes=writes)
        else:
            P.op("dve", lambda e: e.tensor_scalar(out=out, in0=in0, scalar1=s1, scalar2=s2, op0=op0, op1=op1), reads=reads, writes=writes)

    def V_stt(out, in0, scalar, in1, op0, op1, reads, writes):
        P.op("dve", lambda e: e.scalar_tensor_tensor(out=out, in0=in0, scalar=scalar, in1=in1, op0=op0, op1=op1),
             reads=reads, writes=writes)

    def V_copy(out, in_, reads, writes):
        P.op("dve", lambda e: e.tensor_copy(out=out, in_=in_), reads=reads, writes=writes)

    def V_recip(out, in_, reads, writes):
        P.op("dve", lambda e: e.reciprocal(out=out, in_=in_), reads=reads, writes=writes)

    def V_max(out, in_, reads, writes):
        P.op("dve", lambda e: e.max(out=out, in_=in_), reads=reads, writes=writes)

    def A_act(out, in_, func, reads, writes, **kw):
        P.op("act", lambda e: e.activation(out=out, in_=in_, func=func, **kw), reads=reads, writes=writes)

    def G_memset(ap, val, writes):
        P.op("pool", lambda e: e.memset(ap, val), writes=writes)

    def G_tt(out, in0, in1, op, reads, writes):
        P.op("pool", lambda e: e.tensor_tensor(out=out, in0=in0, in1=in1, op=op), reads=reads, writes=writes)
    def phase1_nsa():
        B_c1 = Buf("c1")
        cmpD = A.alloc([128, SEQ], F32, "cmpD")
        dma("sp", cmpD[0:127, :], cd["cmpD"][:, :], writes=[B_c1])
        fmul = A.alloc([128, 16, 32], F32, "fmul"); fadd = A.alloc([128, 16, 32], F32, "fadd")
        dma("sp", fmul, cd["fmul"][:, :, :], writes=[B_c1]); dma("sp", fadd, cd["fadd"][:, :, :], writes=[B_c1])
        tbt = A.alloc([32, SEQ], F32, "tb"); dma("sp", tbt, cd["tb"][:, :], writes=[B_c1])
        ea = A.alloc([128, SEQ], BF16, "ea")
        G_memset(ea, 0.0, [B_c1])
        dma("pool", ea[0:34, :], cd["ea"][:, :], writes=[B_c1])
        cbt = A.alloc([128, 896], BF16, "cb"); dma("pool", cbt, cd["cb"][:, :], writes=[B_c1])
        wbt = A.alloc([128, 8, 1152], BF16, "wb"); dma("pool", wbt, cd["wb"][:, :, :], writes=[B_c1])
        MbA = A.alloc([128, 8, 512], BF16, "MbA"); B_mb = [Buf("mb%d" % h) for h in range(8)]
        G_memset(MbA, 0.0, B_mb)
        dma("pool", MbA[32:34, :, :], cd["crow"][:, :, :], writes=B_mb)
        W1 = {}; W2 = {}; peT = {}; cbias = {}
        B_cw = Buf("cw")
        for nm, w1d, w2d, ped in (("k", w1k_d, w2k_d, pek_d), ("v", w1v_d, w2v_d, pev_d)):
            W1[nm] = A.alloc([128, 32, 128], BF16, "w1" + nm)
            src = w1d.rearrange("(p d) h -> d p h", d=64)
            dma("pool", W1[nm][0:64, :, :], src, writes=[B_cw])
            dma("pool", W1[nm][64:128, :, :], src, writes=[B_cw])
            W2[nm] = A.alloc([128, 128 if nm == "k" else 64], BF16, "w2" + nm)
            dma("pool", W2[nm], w2d[:, :], writes=[B_cw])
            peT[nm] = A.alloc([64, 32], BF16, "pe" + nm)
            dma("pool", peT[nm], ped[:, :], writes=[B_cw])
            cbias[nm] = A.alloc([128, 1], F32, "cbias" + nm)
        B_cb = Buf("cbias")
        for nm in ("k", "v"):
            for p in range(32):
                mm(PS[7][:, 0:1], W1[nm][0:64, p, :], peT[nm][0:64, p:p + 1], p == 0, p == 31, reads=[B_cw], writes=[PSB[7]])
            V_copy(cbias[nm], PS[7][:, 0:1], [PSB[7]], [B_cb])
        gateb = A.alloc([128, 24], F32, "gateb"); dma("sp", gateb, gate_b_d[:, :], writes=[B_c1])
        stage(1)
        qa = A.alloc([128, 4, SEQ], BF16, "qa"); B_qa = Buf("qa")
        kcT = A.alloc([128, SEQ], BF16, "kcT"); vcT = A.alloc([128, SEQ], BF16, "vcT"); B_kvc = Buf("kvc")
        ksT = A.alloc([128, 4, SEQ], BF16, "ksT"); B_ks = Buf("ks")
        kwT = A.alloc([128, 4, SEQ], BF16, "kwT"); B_kw = Buf("kw")
        vsa = A.alloc([128, 16, 2, 65], BF16, "vsa"); B_vs = Buf("vs")
        vwa = A.alloc([128, 16, 2, 65], BF16, "vwa"); B_vw = Buf("vw")
        G_memset(vsa[:, :, :, 64:65], 1.0, [B_vs])
        G_memset(vwa[:, :, :, 64:65], 1.0, [B_vw])
        gsig = A.alloc([128, 16, 24], F32, "gsig"); B_gs = Buf("gs")
        hidT = A.alloc([128, 128], BF16, "hidT"); B_hid = Buf("hid")
        kcmpT = A.alloc([128, 2, 128], BF16, "kcmpT"); B_kcmp = Buf("kcmp")
        Rg = A.alloc([128, 2, 97], BF16, "Rg"); B_R = Buf("R")
        G_memset(Rg[:, :, 64:65], 1.0, [B_R])
        for g in range(2):
            dma("pool", Rg[0:127, g, 65:97], cd["ovl"][:, :], writes=[B_R])
        S_sb = A.alloc([128, 512], F32, "S_sb"); B_S = Buf("S")
        PTs = [A.alloc([128, 512], BF16, "PT%d" % i) for i in range(3)]; B_PT = [Buf("PT%d" % i) for i in range(3)]
        oacc = A.alloc([128, 4, 8, 64], F32, "oacc"); B_oa = Buf("oacc")
        otmp = A.alloc([128, 4, 64], F32, "otmp"); B_ot = Buf("otmp")
        onb = A.alloc([128, 4, 512], BF16, "onb"); B_onb = Buf("onb")
        onT = A.alloc([128, 4, 512], BF16, "onT"); B_onT = Buf("onT")
        imp = A.alloc([128, 4, 32], F32, "imp"); B_imp = Buf("imp")
        itmp = A.alloc([128, 4, 32], F32, "itmp"); B_it = Buf("itmp")
        t8 = A.alloc([128, 4, 8], F32, "t8"); B_t8 = Buf("t8")
        selb = A.alloc([128, 4, 32], F32, "selb"); B_selb = Buf("selb")
        selT = A.alloc([32, 512], F32, "selT"); B_selT = Buf("selT")
        sm = A.alloc([128, 16], F32, "sm"); B_sm = Buf("sm")
        pt_i = [0]; sc_i = [0]; acc_i = [0]

        def pv_evac(pacc, pb, qt, h, br, first):
            if br == 0:
                V_ts(sm[:, 0:4], pacc[:, :, 64], 1e-30, None, ALU.max, None, [PSB[pb]], [B_sm])
                V_recip(sm[:, 4:8], sm[:, 0:4], [B_sm], [B_sm])
            else:
                V_recip(sm[:, 4:8], pacc[:, :, 64], [PSB[pb]], [B_sm])
            V_tt(sm[:, 8:12], sm[:, 4:8], gsig[:, 4 * qt:4 * qt + 4, 3 * h + br], ALU.mult, [B_sm, B_gs], [B_sm])
            rgb = sm[:, 8:12].unsqueeze(2).to_broadcast([128, 4, 64])
            if first:
                V_tt(oacc[:, :, h, :], pacc[:, :, 0:64], rgb, ALU.mult, [PSB[pb], B_sm], [B_oa])
            else:
                V_tt(otmp, pacc[:, :, 0:64], rgb, ALU.mult, [PSB[pb], B_sm], [B_ot])
                V_tt(oacc[:, :, h, :], oacc[:, :, h, :], otmp, ALU.add, [B_oa, B_ot], [B_oa])

        for sq in range(NSEQ):
            tb0 = sq * SEQ
            dma("sp", qa, s_fm[FM_QA:FM_QA + 4, :, tb0:tb0 + SEQ].rearrange("c p t -> p c t"), writes=[B_qa])
            dma("sp", kcT, s_fm[FM_KC, :, tb0:tb0 + SEQ], writes=[B_kvc])
            dma("sp", vcT, s_fm[FM_VC, :, tb0:tb0 + SEQ], writes=[B_kvc])
            for g in range(2):
                for hf in range(2):
                    dma("sp", ksT[:, 2 * g + hf, :], s_fm[FM_KS + g, :, tb0:tb0 + SEQ], writes=[B_ks])
                    dma("sp", kwT[:, 2 * g + hf, :], s_fm[FM_KW + g, :, tb0:tb0 + SEQ], writes=[B_kw])
                    zlo = 64 * (1 - hf)
                    G_memset(ksT[zlo:zlo + 64, 2 * g + hf, :], 0.0, [B_ks])
                    G_memset(kwT[zlo:zlo + 64, 2 * g + hf, :], 0.0, [B_kw])
            for g in range(2):
                dma("sp", vsa[:, :, g, 0:64],
                    s_tm[tb0:tb0 + SEQ, TM_VS + 64 * g:TM_VS + 64 * g + 64].rearrange("(kt p) d -> p kt d", p=128),
                    writes=[B_vs])
                dma("sp", vwa[:, :, g, 0:64],
                    s_tm[tb0:tb0 + SEQ, TM_VW + 64 * g:TM_VW + 64 * g + 64].rearrange("(kt p) d -> p kt d", p=128),
                    writes=[B_vw])
            dma("sp", gsig, s_gate[tb0:tb0 + SEQ, :].rearrange("(kt p) n -> p kt n", p=128), writes=[B_gs])
            V_tt(gsig, gsig, gateb.unsqueeze(1).to_broadcast([128, 16, 24]), ALU.add, [B_gs, B_c1], [B_gs])
            A_act(gsig, gsig, AF.Sigmoid, [B_gs], [B_gs])
            stage(2)
            for nm, srcT in (("k", kcT), ("v", vcT)):
                s3 = srcT.rearrange("q (n s) -> q n s", s=16)
                for g in range(2):
                    for p in range(32):
                        mm(PS[7][:, 0:127], W1[nm][64 * g:64 * g + 64, p, :], s3[64 * g:64 * g + 64, p // 16:p // 16 + 127, p % 16],
                           p == 0, p == 31, reads=[B_cw, B_kvc], writes=[PSB[7]])
                    A_act(hidT[:, 0:127], PS[7][:, 0:127], AF.Gelu_apprx_tanh, [PSB[7], B_cb], [B_hid], bias=cbias[nm])
                    if nm == "k":
                        mm(PS[6][:, 0:127], W2["k"], hidT[:, 0:127], True, True, reads=[B_cw, B_hid], writes=[PSB[6]])
                        evac_copy(kcmpT[:, g, 0:127], PS[6][:, 0:127], [PSB[6]], [B_kcmp])
                    else:
                        mm(PS[6][0:127, 0:64], hidT[:, 0:127], W2["v"], True, True, reads=[B_cw, B_hid], writes=[PSB[6]])
                        evac_copy(Rg[0:127, g, 0:64], PS[6][0:127, 0:64], [PSB[6]], [B_R])
            stage(3)
            for qt in range(4):
                q0 = qt * 512
                for g in range(2):
                    for gi in range(4):
                        h = 4 * g + gi
                        hp = 64 * (h % 2)
                        pi = sc_i[0] % 3; sc_i[0] += 1
                        mm(PS[pi][0:127, :], kcmpT[hp:hp + 64, g, 0:127], qa[hp:hp + 64, h // 2, q0:q0 + 512], True, True,
                           reads=[B_kcmp, B_qa], writes=[PSB[pi]])
                        V_stt(S_sb[0:127, :], cmpD[0:127, q0:q0 + 512], SLOPES[h], PS[pi][0:127, :], ALU.mult, ALU.add,
                              [PSB[pi], B_c1], [B_S])
                        k = pt_i[0] % 3; pt_i[0] += 1
                        PT = PTs[k]
                        A_act(PT[0:127, :], S_sb[0:127, :], AF.Exp, [B_S], [B_PT[k]])
                        pu = PS[5][:, 0:388].rearrange("p (s c) -> p s c", s=4)
                        for sub in range(4):
                            mm(pu[:, sub, :], PT[0:127, sub * 128:(sub + 1) * 128], Rg[0:127, g, :], True, True,
                               reads=[B_PT[k], B_R], writes=[PSB[5]])
                        pv_evac(pu, 5, qt, h, 0, True)
                        rdb = sm[:, 4:8].unsqueeze(2).to_broadcast([128, 4, 32])
                        if gi == 0:
                            V_tt(imp, pu[:, :, 65:97], rdb, ALU.mult, [PSB[5], B_sm], [B_imp])
                        else:
                            V_tt(itmp, pu[:, :, 65:97], rdb, ALU.mult, [PSB[5], B_sm], [B_it])
                            V_tt(imp, imp, itmp, ALU.add, [B_imp, B_it], [B_imp])
                    V_tt(imp, imp, fmul[:, 4 * qt:4 * qt + 4, :], ALU.mult, [B_imp, B_c1], [B_imp])
                    V_tt(imp, imp, fadd[:, 4 * qt:4 * qt + 4, :], ALU.add, [B_imp, B_c1], [B_imp])
                    for sub in range(4):
                        V_max(t8[:, sub, :], imp[:, sub, :], [B_imp], [B_t8])
                    for sub in range(4):
                        V_ts(selb[:, sub, :], imp[:, sub, :], t8[:, sub, 7:8], -NEG, ALU.is_ge, ALU.mult, [B_imp, B_t8], [B_selb])
                    for sub in range(4):
                        transpose(PS[6][0:32, sub * 128:(sub + 1) * 128], selb[:, sub, :], ident_f,
                                  reads=[B_selb, B_ident], writes=[PSB[6]])
                    V_ts(selT, PS[6][0:32, :], NEG, None, ALU.add, None, [PSB[6]], [B_selT])
                    for gi in range(4):
                        h = 4 * g + gi
                        V_stt(MbA[0:32, h, :], tbt[0:32, q0:q0 + 512], -SLOPES[h], selT, ALU.mult, ALU.add,
                              [B_c1, B_selT], [B_mb[h]])
                stage(4)
                for h in range(8):
                    g = h // 4; qc = h // 2
                    for br in (1, 2):
                        pb = 3 + acc_i[0] % 2; acc_i[0] += 1
                        pacc = PS[pb][:, 0:260].rearrange("p (s c) -> p s c", s=4)
                        if br == 1:
                            kts = list(range(0, 4 * qt + 4))
                        else:
                            kts = list(range(max(0, 4 * qt - 2), 4 * qt + 4))
                        pairs = []
                        for kt in kts:
                            off = kt * 128 - q0
                            for sub in range(4):
                                dmax = 128 * sub + 127 - off
                                dmin = 128 * sub - 127 - off
                                if dmax < 0:
                                    continue
                                if br == 2 and dmin >= 256:
                                    continue
                                pairs.append((kt, sub))
                        lastkt = {}
                        for kt, sub in pairs:
                            lastkt[sub] = kt
                        first = True
                        for kt in kts:
                            off = kt * 128 - q0
                            pi = sc_i[0] % 3; sc_i[0] += 1
                            kT = ksT if br == 1 else kwT
                            Bk = B_ks if br == 1 else B_kw
                            mm(PS[pi], kT[:, 2 * g + (h % 2), kt * 128:(kt + 1) * 128], qa[:, qc, q0:q0 + 512], True, False,
                               reads=[Bk, B_qa], writes=[PSB[pi]])
                            if br == 1:
                                diag = off >= 0
                                mm(PS[pi], ea[:, kt * 128:(kt + 1) * 128], MbA[:, h, :], False, not diag,
                                   reads=[B_c1, B_mb[h]], writes=[PSB[pi]])
                                if diag:
                                    mm(PS[pi], ident_b, cbt[:, 384 - off:384 - off + 512], False, True,
                                       reads=[B_ident, B_c1], writes=[PSB[pi]])
                            else:
                                mm(PS[pi], ident_b, wbt[:, h, 384 - off:384 - off + 512], False, True,
                                   reads=[B_ident, B_c1], writes=[PSB[pi]])
                            k = pt_i[0] % 3; pt_i[0] += 1
                            PT = PTs[k]
                            A_act(PT, PS[pi], AF.Exp, [PSB[pi]], [B_PT[k]])
                            va = vsa if br == 1 else vwa
                            Bv = B_vs if br == 1 else B_vw
                            for sub in range(4):
                                if (kt, sub) not in pairs:
                                    continue
                                mm(pacc[:, sub, :], PT[:, sub * 128:(sub + 1) * 128], va[:, kt, g, :], first, lastkt[sub] == kt,
                                   reads=[B_PT[k], Bv], writes=[PSB[pb]], skip=True)
                                first = False
                        pv_evac(pacc, pb, qt, h, br, False)
                stage(5)
                A_act(onb.rearrange("p a b -> p (a b)"), oacc.rearrange("p a h d -> p (a h d)"), AF.Copy, [B_oa], [B_onb])
                for fc in range(4):
                    pst = PS[6].bitcast(BF16)
                    for sub in range(4):
                        transpose(pst[:, sub * 128:(sub + 1) * 128], onb[:, sub, fc * 128:(fc + 1) * 128], ident_b,
                                  reads=[B_onb, B_ident], writes=[PSB[6]])
                    evac_copy(onT[:, fc, :], pst[:, 0:512], [PSB[6]], [B_onT])
                dma("sp", s_on[:, :, tb0 + q0:tb0 + q0 + 512].rearrange("c p t -> p c t"), onT, reads=[B_onT])

    def phase1_gla():
        B_gc = Buf("gc")
        wa2 = A.alloc([16, 256], F32, "wa2"); dma("sp", wa2, wa2_d[0:16, :], writes=[B_gc])
        ba = A.alloc([128, 2], F32, "ba"); dma("sp", ba, ba_d[:, :], writes=[B_gc])
        nba = A.alloc([128, 2], F32, "nba"); V_ts(nba, ba, -1.0, None, ALU.mult, None, [B_gc], [B_gc])
        srst = A.alloc([128, SEQ], F32, "srst"); dma("sp", srst, cd["srst"][:, :], writes=[B_gc])
        gmask = A.alloc([128, 128], F32, "gmask"); dma("sp", gmask, cd["gmask"][:, :], writes=[B_gc])
        glag = A.alloc([128, 128], F32, "glag"); dma("sp", glag, gla_g_d[:, :], writes=[B_gc])
        alT = A.alloc([16, SEQ], F32, "alT"); B_al = Buf("al")
        qbT = A.alloc([128, 2, SEQ], BF16, "qbT"); kbT = A.alloc([128, 2, SEQ], BF16, "kbT"); B_qk = Buf("qk")
        vb = A.alloc([128, 16, 512], BF16, "vb"); B_vb = Buf("vb")
        rb = A.alloc([128, 16, 512], BF16, "rb"); B_rb = Buf("rb")
        sr = A.alloc([128, 16, 512], BF16, "sr"); B_sr = Buf("sr")
        rg = A.alloc([128, 16, 512], BF16, "rg"); B_rg = Buf("rg")
        laT = A.alloc([128, 2, SEQ], F32, "laT"); B_la = Buf("la")
        bT = A.alloc([128, 2, SEQ], F32, "bT"); B_b = Buf("b")
        ET = A.alloc([128, 2, SEQ], F32, "ET"); B_E = Buf("E")
        qd4 = A.alloc([128, 4, SEQ], BF16, "qd4"); B_qd = Buf("qd")
        kdT = A.alloc([128, 2, SEQ], BF16, "kdT"); B_kd = Buf("kd")
        dec = A.alloc([128, 2, 32], F32, "dec"); B_dec = Buf("dec")
        G_memset(qd4, 0.0, [B_qd])
        kd_ab = A.alloc([128, 2, 2, 128], BF16, "kd_ab"); B_kab = Buf("kab")
        G_memset(kd_ab, 0.0, [B_kab])
        Sf = [A.alloc([128, 128], F32, "Sf%d" % c) for c in range(2)]; B_Sf = [Buf("Sf%d" % c) for c in range(2)]
        tmpS = [A.alloc([128, 128], F32, "tS%d" % c) for c in range(2)]; B_tS = [Buf("tS%d" % c) for c in range(2)]
        Sbf = [[A.alloc([128, 128], BF16, "Sbf%d%d" % (c, a)) for a in range(2)] for c in range(2)]
        B_Sbf = [[Buf("Sbf%d%d" % (c, a)) for a in range(2)] for c in range(2)]
        att = A.alloc([128, 4, 128], BF16, "att"); B_att = Buf("att")
        ss = A.alloc([128, 16], F32, "ss"); B_ss = Buf("ss")
        junk = A.alloc([128, 128], BF16, "junk"); B_junk = Buf("junk")
        ogb = A.alloc([128, 512], BF16, "ogb"); B_ogb = Buf("ogb")
        ogT = A.alloc([128, 4, 512], BF16, "ogT"); B_ogT = Buf("ogT")
        for sq in range(NSEQ):
            tb0 = sq * SEQ
            dma("sp", alT, s_al[:, tb0:tb0 + SEQ], writes=[B_al])
            dma("sp", qbT, s_fm[FM_QB:FM_QB + 2, :, tb0:tb0 + SEQ].rearrange("c p t -> p c t"), writes=[B_qk])
            dma("sp", kbT, s_fm[FM_KB:FM_KB + 2, :, tb0:tb0 + SEQ].rearrange("c p t -> p c t"), writes=[B_qk])
            dma("sp", vb, s_tm[tb0:tb0 + SEQ, TM_VB:TM_VB + 512].rearrange("(kt p) n -> p kt n", p=128), writes=[B_vb])
            dma("sp", rb, s_tm[tb0:tb0 + SEQ, TM_RB:TM_RB + 512].rearrange("(kt p) n -> p kt n", p=128), writes=[B_rb])
            for c in range(2):
                for tt in range(4):
                    mm(PS[0], wa2[0:16, c * 128:(c + 1) * 128], alT[0:16, tt * 512:(tt + 1) * 512], True, True,
                       reads=[B_gc, B_al], writes=[PSB[0]])
                    A_act(laT[:, c, tt * 512:(tt + 1) * 512], PS[0], AF.Exp, [PSB[0], B_gc], [B_la], scale=-1.0, bias=nba[:, c:c + 1])
                A_act(laT[:, c, :], laT[:, c, :], AF.Ln, [B_la], [B_la], bias=1.0)
                P.op("dve", lambda e, c=c: e.tensor_tensor_scan(out=bT[:, c, :], data0=srst, data1=laT[:, c, :], initial=0.0,
                                                                op0=ALU.mult, op1=ALU.add), reads=[B_gc, B_la], writes=[B_b])
                A_act(ET[:, c, :], bT[:, c, :], AF.Exp, [B_b], [B_E], scale=-1.0 / 16)
                A_act(dec[:, c, :], bT[:, c, :].rearrange("p (n s) -> p n s", s=64)[:, :, 63], AF.Exp, [B_b], [B_dec], scale=-1.0 / 16)
                for hh in range(2):
                    lo = 64 * hh
                    V_stt(qd4[lo:lo + 64, 2 * c + hh, :], qbT[lo:lo + 64, c, :], 0.125, ET[lo:lo + 64, c, :], ALU.mult, ALU.mult,
                          [B_qk, B_E], [B_qd])
            for c in range(2):
                A_act(ET[:, c, :], bT[:, c, :], AF.Exp, [B_b], [B_E], scale=1.0 / 16)
                V_tt(kdT[:, c, :], kbT[:, c, :], ET[:, c, :], ALU.mult, [B_qk, B_E], [B_kd])
            A_act(sr, rb, AF.Sigmoid, [B_rb], [B_sr])
            G_tt(rg, rb, sr, ALU.mult, [B_rb, B_sr], [B_rg])
            rg4 = rg.rearrange("p k (h e) -> p (k h) e", e=128)
            G_tt(rg4, rg4, glag.unsqueeze(1).to_broadcast([128, 64, 128]), ALU.mult, [B_rg, B_gc], [B_rg])
            for c in range(2):
                P.op("dve", lambda e, c=c: e.memset(Sf[c], 0.0), writes=[B_Sf[c]])
            if "cs" in dbg and sq == 0:
                dma("sp", dbg["cs"][:, :, :], bT, reads=[B_b]); dma("sp", dbg["kd"][:, :, :], kdT, reads=[B_kd])
                dma("sp", dbg["qd"][:, :, :], qd4, reads=[B_qd]); dma("sp", dbg["rg"][:, :, :], rg, reads=[B_rg])
                dma("sp", dbg["la"][:, :, :], laT, reads=[B_la])
            for blk in range(16):
                t1 = blk * 128
                pst = PS[4].bitcast(BF16)
                for c in range(2):
                    transpose(pst[:, c * 128:(c + 1) * 128], kdT[:, c, t1:t1 + 128], ident_b, reads=[B_kd, B_ident], writes=[PSB[4]])
                for c in range(2):
                    V_copy(kd_ab[0:64, c, 0, :], pst[0:64, c * 128:(c + 1) * 128], [PSB[4]], [B_kab])
                    V_copy(kd_ab[64:128, c, 1, :], pst[64:128, c * 128:(c + 1) * 128], [PSB[4]], [B_kab])
                PSm = [PS[1][:, 0:256].rearrange("p (a e) -> p a e", a=2), PS[2][:, 0:256].rearrange("p (a e) -> p a e", a=2)]
                for c in range(2):
                    for ab in range(2):
                        for hh in range(2):
                            h = 2 * c + hh
                            mm(PSm[c][64 * hh:64 * hh + 64, ab, :], kd_ab[:, c, ab, 64 * hh:64 * hh + 64], vb[:, blk, h * 128:(h + 1) * 128],
                               True, True, reads=[B_kab, B_vb], writes=[PSB[1 + c]])
                PSa = PS[0].rearrange("p (h i) -> p h i", h=4)
                for h in range(4):
                    mm(PSa[:, h, :], kdT[:, h // 2, t1:t1 + 128], qd4[:, h, t1:t1 + 128], True, True,
                       reads=[B_kd, B_qd], writes=[PSB[0]])
                V_tt(att, PSa, gmask.unsqueeze(1).to_broadcast([128, 4, 128]), ALU.mult, [PSB[0], B_gc], [B_att])
                for c in range(2):
                    V_copy(Sbf[c][0], Sf[c], [B_Sf[c]], [B_Sbf[c][0]])
                    V_tt(tmpS[c], PSm[c][:, 0, :], Sf[c], ALU.add, [PSB[1 + c], B_Sf[c]], [B_tS[c]])
                    V_ts(Sf[c], tmpS[c], dec[:, c, 2 * blk:2 * blk + 1], None, ALU.mult, None, [B_tS[c], B_dec], [B_Sf[c]])
                    V_copy(Sbf[c][1], Sf[c], [B_Sf[c]], [B_Sbf[c][1]])
                    V_tt(tmpS[c], PSm[c][:, 1, :], Sf[c], ALU.add, [PSB[1 + c], B_Sf[c]], [B_tS[c]])
                    V_ts(Sf[c], tmpS[c], dec[:, c, 2 * blk + 1:2 * blk + 2], None, ALU.mult, None, [B_tS[c], B_dec], [B_Sf[c]])
                PSo = PS[3].rearrange("p (h e) -> p h e", h=4)
                for h in range(4):
                    c = h // 2
                    mm(PSo[:, h, :], att[:, h, :], vb[:, blk, h * 128:(h + 1) * 128], True, False,
                       reads=[B_att, B_vb], writes=[PSB[3]], skip=True)
                    mm(PSo[0:64, h, :], qd4[:, h, t1:t1 + 64], Sbf[c][0], False, False,
                       reads=[B_qd, B_Sbf[c][0]], writes=[PSB[3]], skip=True)
                    mm(PSo[64:128, h, :], qd4[:, h, t1 + 64:t1 + 128], Sbf[c][1], False, True,
                       reads=[B_qd, B_Sbf[c][1]], writes=[PSB[3]], skip=True)
                for h in range(4):
                    A_act(junk, PSo[:, h, :], AF.Square, [PSB[3]], [B_junk, B_ss], accum_out=ss[:, h:h + 1])
                A_act(ss[:, 4:8], ss[:, 0:4], AF.Sqrt, [B_ss], [B_ss], bias=EPS, scale=1.0 / 128)
                V_recip(ss[:, 8:12], ss[:, 4:8], [B_ss], [B_ss])
                for h in range(4):
                    V_stt(ogb[:, h * 128:(h + 1) * 128], PSo[:, h, :], ss[:, 8 + h:9 + h], rg[:, blk, h * 128:(h + 1) * 128],
                          ALU.mult, ALU.mult, [PSB[3], B_ss, B_rg], [B_ogb])
                if "ogb" in dbg and sq == 0 and blk == 0:
                    dma("sp", dbg["ogb"][:, :], ogb, reads=[B_ogb]); dma("sp", dbg["att"][:, :, :], att, reads=[B_att])
                pst2 = PS[5].bitcast(BF16)
                for fc in range(4):
                    transpose(pst2[:, fc * 128:(fc + 1) * 128], ogb[:, fc * 128:(fc + 1) * 128], ident_b,
                              reads=[B_ogb, B_ident], writes=[PSB[5]])
                evac_copy(ogT[:, :, (blk % 4) * 128:(blk % 4) * 128 + 128], pst2[:, 0:512].rearrange("p (f t) -> p f t", f=4),
                          [PSB[5]], [B_ogT])
                if blk % 4 == 3:
                    q0 = (blk // 4) * 512
                    dma("sp", s_og[:, :, tb0 + q0:tb0 + q0 + 512].rearrange("c p t -> p c t"), ogT, reads=[B_ogT])
    if "p1" in phases:
        P.fence()
        A.mark()
        try:
            if "nonsa" not in phases:
                phase1_nsa()
        except StopBuild:
            pass
        A.release()
        P.fence()
        A.mark()
        phase1_gla()
        A.release()
    def phase2():
        B_w2 = Buf("w2")
        wbn = A.alloc([128, 4, D], BF16, "wbn"); load_cast(wbn, wbn_d.rearrange("(kc p) n -> p kc n", p=128), B_w2)
        wbg = A.alloc([128, 4, D], BF16, "wbg"); load_cast(wbg, wbg_d.rearrange("(kc p) n -> p kc n", p=128), B_w2)
        wout = A.alloc([128, 8, D], BF16, "wout"); load_cast(wout, wout_d.rearrange("(kc p) n -> p kc n", p=128), B_w2)
        wxq = A.alloc([128, 8, 512], BF16, "wxq"); load_cast(wxq, wxq_d.rearrange("(kc p) n -> p kc n", p=128), B_w2)
        wxkv = A.alloc([128, 8, D], BF16, "wxkv"); load_cast(wxkv, wxkv_d.rearrange("(kc p) n -> p kc n", p=128), B_w2)
        wxo = A.alloc([128, 4, D], BF16, "wxo"); load_cast(wxo, wxo_d.rearrange("(kc p) n -> p kc n", p=128), B_w2)
        gx = A.alloc([128, 8], F32, "gx"); gm = A.alloc([128, 8], F32, "gm"); B_g = Buf("g2")
        dma("sp", gx, g_x_d[:, :], writes=[B_g]); dma("sp", gm, g_mem_d[:, :], writes=[B_g])
        xt = A.alloc([128, 4, D], F32, "xt"); B_xt = Buf("xt")
        xn = A.alloc([128, 4, D], BF16, "xn"); B_xn = Buf("xn")
        st = A.alloc([128, 32], F32, "st"); B_st = Buf("st")
        hxT = A.alloc([128, 8, 512], BF16, "hxT"); B_hx = Buf("hx")
        memt = A.alloc([128, 2, D], F32, "memt"); B_mem = Buf("mem")
        memT = A.alloc([128, 8, 256], BF16, "memT"); B_memT = Buf("memT")
        kxT = A.alloc([128, 4, 256], BF16, "kxT"); B_kx = Buf("kx")
        vxa = A.alloc([128, 2, 4, 129], BF16, "vxa"); B_vx = Buf("vx")
        G_memset(vxa[:, :, :, 128:129], 1.0, [B_vx])
        onT = A.alloc([128, 4, 512], BF16, "onT2"); ogT = A.alloc([128, 4, 512], BF16, "ogT2"); B_o = Buf("o2")
        sg = A.alloc([128, 16, 512], BF16, "sg"); B_sg = Buf("sg")
        mixT = A.alloc([128, 8, 512], BF16, "mixT"); B_mix = Buf("mix")
        tmp1 = [A.alloc([128, 512], F32, "tmp1%d" % i) for i in range(2)]; tmp2 = [A.alloc([128, 512], F32, "tmp2%d" % i) for i in range(2)]
        B_t1 = [Buf("t1%d" % i) for i in range(2)]; B_t2 = [Buf("t2%d" % i) for i in range(2)]
        qxT = A.alloc([128, 4, 512], BF16, "qxT"); B_qx = Buf("qx")
        PTx = [A.alloc([128, 512], BF16, "PTx%d" % i) for i in range(2)]; B_PTx = [Buf("PTx%d" % i) for i in range(2)]
        oxb = A.alloc([128, 4, 512], BF16, "oxb"); B_oxb = Buf("oxb")
        oxT = A.alloc([128, 4, 512], BF16, "oxT"); B_oxT = Buf("oxT")
        rd = A.alloc([128, 8], F32, "rd"); B_rd = Buf("rd")
        bk = [0]

        def bank():
            b = 2 + bk[0] % 4; bk[0] += 1
            return b

        for it in range(NTOK // 512):
            t0 = it * 512
            if it % 4 == 0:
                sq = it // 4
                dma("sp", memt, mem_d[sq * MEM:(sq + 1) * MEM, :].rearrange("(s p) d -> p s d", p=128), writes=[B_mem])
                rmsnorm_T(memt, B_mem, 2, gm, B_g, memT, B_memT, xn, B_xn, st, B_st, [0, 1])
                for hd in range(4):
                    pi = bank()
                    for kc in range(8):
                        mm(PS[pi][:, 0:256], wxkv[:, kc, hd * 128:(hd + 1) * 128], memT[:, kc, :], kc == 0, kc == 7,
                           reads=[B_w2, B_memT], writes=[PSB[pi]])
                    evac_copy(kxT[:, hd, :], PS[pi][:, 0:256], [PSB[pi]], [B_kx])
                for ms in range(2):
                    pi = bank()
                    for kc in range(8):
                        mm(PS[pi], memT[:, kc, ms * 128:(ms + 1) * 128], wxkv[:, kc, 512:1024], kc == 0, kc == 7,
                           reads=[B_w2, B_memT], writes=[PSB[pi]])
                    evac_copy(vxa[:, ms, :, 0:128], PS[pi].rearrange("p (h d) -> p h d", h=4), [PSB[pi]], [B_vx])
            dma("sp", xt, x_d[t0:t0 + 512, :].rearrange("(s p) d -> p s d", p=128), writes=[B_xt])
            dma("sp", onT, s_on[:, :, t0:t0 + 512].rearrange("c p t -> p c t"), writes=[B_o])
            dma("sp", ogT, s_og[:, :, t0:t0 + 512].rearrange("c p t -> p c t"), writes=[B_o])
            dma("sp", sg, s_fm[FM_MG:FM_MG + 16, :, t0:t0 + 512].rearrange("c p t -> p c t"), writes=[B_sg])
            for oc in range(8):
                p1 = bank(); p2 = bank()
                for kc in range(4):
                    mm(PS[p1], wbn[:, kc, oc * 128:(oc + 1) * 128], onT[:, kc, :], kc == 0, kc == 3, reads=[B_w2, B_o], writes=[PSB[p1]])
                for kc in range(4):
                    mm(PS[p2], wbg[:, kc, oc * 128:(oc + 1) * 128], ogT[:, kc, :], kc == 0, kc == 3, reads=[B_w2, B_o], writes=[PSB[p2]])
                j = oc % 2
                V_tt(tmp1[j], PS[p1], sg[:, oc, :], ALU.mult, [PSB[p1], B_sg], [B_t1[j]])
                V_tt(tmp2[j], PS[p2], sg[:, 8 + oc, :], ALU.mult, [PSB[p2], B_sg], [B_t2[j]])
                G_tt(mixT[:, oc, :], tmp1[j], tmp2[j], ALU.add, [B_t1[j], B_t2[j]], [B_mix])
            for sub in range(4):
                for half in range(2):
                    pi = bank()
                    for kc in range(8):
                        mm(PS[pi], mixT[:, kc, sub * 128:(sub + 1) * 128], wout[:, kc, half * 512:(half + 1) * 512], kc == 0, kc == 7,
                           reads=[B_w2, B_mix], writes=[PSB[pi]])
                    V_tt(xt[:, sub, half * 512:(half + 1) * 512], xt[:, sub, half * 512:(half + 1) * 512], PS[pi], ALU.add,
                         [B_xt, PSB[pi]], [B_xt])
            rmsnorm_T(xt, B_xt, 4, gx, B_g, hxT, B_hx, xn, B_xn, st, B_st, [0, 1])
            for hd in range(4):
                pi = bank()
                for kc in range(8):
                    mm(PS[pi], wxq[:, kc, hd * 128:(hd + 1) * 128], hxT[:, kc, :], kc == 0, kc == 7, reads=[B_w2, B_hx], writes=[PSB[pi]])
                evac_copy(qxT[:, hd, :], PS[pi], [PSB[pi]], [B_qx])
            for hd in range(4):
                for ms in range(2):
                    pi = bank()
                    mm(PS[pi], kxT[:, hd, ms * 128:(ms + 1) * 128], qxT[:, hd, :], True, True, reads=[B_kx, B_qx], writes=[PSB[pi]])
                    A_act(PTx[ms], PS[pi], AF.Exp, [PSB[pi]], [B_PTx[ms]], scale=128.0 ** -0.5)
                pa = [PS[6][:, 0:258].rearrange("p (s c) -> p s c", s=2), PS[7][:, 0:258].rearrange("p (s c) -> p s c", s=2)]
                for ms in range(2):
                    for sub in range(4):
                        mm(pa[sub // 2][:, sub % 2, :], PTx[ms][:, sub * 128:(sub + 1) * 128], vxa[:, ms, hd, :],
                           ms == 0 and sub % 2 == 0, ms == 1, reads=[B_PTx[ms], B_vx], writes=[PSB[6 + sub // 2]], skip=True)
                for bq in range(2):
                    V_recip(rd[:, 2 * bq:2 * bq + 2], pa[bq][:, :, 128], [PSB[6 + bq]], [B_rd])
                for sub in range(4):
                    V_ts(oxb[:, sub, hd * 128:(hd + 1) * 128], pa[sub // 2][:, sub % 2, 0:128], rd[:, sub:sub + 1], None, ALU.mult, None,
                         [PSB[6 + sub // 2], B_rd], [B_oxb])
            for fc in range(4):
                pi = bank()
                pst = PS[pi].bitcast(BF16)
                for sub in range(4):
                    transpose(pst[:, sub * 128:(sub + 1) * 128], oxb[:, sub, fc * 128:(fc + 1) * 128], ident_b,
                              reads=[B_oxb, B_ident], writes=[PSB[pi]])
                evac_copy(oxT[:, fc, :], pst[:, 0:512], [PSB[pi]], [B_oxT])
            for sub in range(4):
                for half in range(2):
                    pi = bank()
                    for kc in range(4):
                        mm(PS[pi], oxT[:, kc, sub * 128:(sub + 1) * 128], wxo[:, kc, half * 512:(half + 1) * 512], kc == 0, kc == 3,
                           reads=[B_w2, B_oxT], writes=[PSB[pi]])
                    V_tt(xt[:, sub, half * 512:(half + 1) * 512], xt[:, sub, half * 512:(half + 1) * 512], PS[pi], ALU.add,
                         [B_xt, PSB[pi]], [B_xt])
            dma("sp", s_x2[t0:t0 + 512, :].rearrange("(s p) d -> p s d", p=128), xt, reads=[B_xt])

    def phase3():
        B_w3 = Buf("w3")
        wup = A.alloc([128, 8, 2 * FFN], BF16, "wup"); load_cast(wup, wup_d.rearrange("(kc p) n -> p kc n", p=128), B_w3, nsplit=4)
        wdn = A.alloc([128, 22, D], BF16, "wdn"); load_cast(wdn, wdn_d.rearrange("(kc p) n -> p kc n", p=128), B_w3, nsplit=1)
        gf = A.alloc([128, 8], F32, "gf"); B_g = Buf("g3"); dma("sp", gf, g_ffn_d[:, :], writes=[B_g])
        cw = A.alloc([128, 3, 22], F32, "cw"); cbv = A.alloc([128, 22], F32, "cbv")
        dma("sp", cw, convw_d[:, :, :], writes=[B_g]); dma("sp", cbv, convb_d[:, :], writes=[B_g])
        gfin = A.alloc([128, D], F32, "gfin"); dma("sp", gfin, g_fin_d[:, :], writes=[B_g])
        xt = A.alloc([128, 4, D], F32, "xt"); B_xt = Buf("xt")
        xn = A.alloc([128, 4, D], BF16, "xn"); B_xn = Buf("xn")
        st = A.alloc([128, 32], F32, "st"); B_st = Buf("st")
        hfT = A.alloc([128, 8, 512], BF16, "hfT"); B_hf = Buf("hf")
        aT = A.alloc([128, 22, 512], BF16, "aT"); B_a = Buf("aT")
        usb = [A.alloc([128, 514], F32, "usb%d" % i) for i in range(2)]; B_u = [Buf("u%d" % i) for i in range(2)]
        acc = [A.alloc([128, 512], F32, "acc%d" % i) for i in range(2)]; B_acc = [Buf("acc%d" % i) for i in range(2)]
        carry = A.alloc([128, 22, 2], F32, "carry"); B_car = Buf("carry")
        bk = [0]

        def bank():
            b = 2 + bk[0] % 6; bk[0] += 1
            return b

        for it in range(NTOK // 512):
            t0 = it * 512
            dma("sp", xt, s_x2[t0:t0 + 512, :].rearrange("(s p) d -> p s d", p=128), writes=[B_xt])
            if it % 4 == 0:
                P.op("dve", lambda e: e.memset(carry, 0.0), writes=[B_car])
            rmsnorm_T(xt, B_xt, 4, gf, B_g, hfT, B_hf, xn, B_xn, st, B_st, [0, 1])
            for fcn in range(22):
                pu = bank(); pg = bank()
                for kc in range(8):
                    mm(PS[pu], wup[:, kc, fcn * 128:(fcn + 1) * 128], hfT[:, kc, :], kc == 0, kc == 7, reads=[B_w3, B_hf], writes=[PSB[pu]])
                for kc in range(8):
                    mm(PS[pg], wup[:, kc, FFN + fcn * 128:FFN + (fcn + 1) * 128], hfT[:, kc, :], kc == 0, kc == 7,
                       reads=[B_w3, B_hf], writes=[PSB[pg]])
                j = fcn % 2
                V_copy(usb[j][:, 0:2], carry[:, fcn, :], [B_car], [B_u[j]])
                A_act(usb[j][:, 2:514], PS[pu], AF.Copy, [PSB[pu]], [B_u[j]])
                V_copy(carry[:, fcn, :], usb[j][:, 512:514], [B_u[j]], [B_car])
                V_ts(acc[j], usb[j][:, 2:514], cw[:, 2, fcn:fcn + 1], cbv[:, fcn:fcn + 1], ALU.mult, ALU.add, [B_u[j], B_g], [B_acc[j]])
                V_stt(acc[j], usb[j][:, 1:513], cw[:, 1, fcn:fcn + 1], acc[j], ALU.mult, ALU.add, [B_u[j], B_g, B_acc[j]], [B_acc[j]])
                V_stt(acc[j], usb[j][:, 0:512], cw[:, 0, fcn:fcn + 1], acc[j], ALU.mult, ALU.add, [B_u[j], B_g, B_acc[j]], [B_acc[j]])
                A_act(acc[j], acc[j], AF.Gelu_apprx_tanh, [B_acc[j]], [B_acc[j]])
                V_tt(aT[:, fcn, :], PS[pg], acc[j], ALU.mult, [PSB[pg], B_acc[j]], [B_a])
            for sub in range(4):
                for half in range(2):
                    pi = bank()
                    for kc in range(22):
                        mm(PS[pi], aT[:, kc, sub * 128:(sub + 1) * 128], wdn[:, kc, half * 512:(half + 1) * 512], kc == 0, kc == 21,
                           reads=[B_w3, B_a], writes=[PSB[pi]])
                    V_tt(xt[:, sub, half * 512:(half + 1) * 512], xt[:, sub, half * 512:(half + 1) * 512], PS[pi], ALU.add,
                         [B_xt, PSB[pi]], [B_xt])
            if "aT" in dbg and it == 0:
                dma("sp", dbg["aT"][:, :, :], aT, reads=[B_a]); dma("sp", dbg["x3"][:, :, :], xt, reads=[B_xt])
                dma("sp", dbg["hf"][:, :, :], hfT, reads=[B_hf])
            for s in range(4):
                A_act(xn[:, s, :], xt[:, s, :], AF.Square, [B_xt], [B_xn, B_st], accum_out=st[:, s:s + 1])
            A_act(st[:, 8:12], st[:, 0:4], AF.Sqrt, [B_st], [B_st], bias=EPS, scale=1.0 / D)
            V_recip(st[:, 16:20], st[:, 8:12], [B_st], [B_st])
            for s in range(4):
                V_stt(xt[:, s, :], xt[:, s, :], st[:, 16 + s:17 + s], gfin, ALU.mult, ALU.mult, [B_xt, B_st, B_g], [B_xt])
            dma("sp", out_d[t0:t0 + 512, :].rearrange("(s p) d -> p s d", p=128), xt, reads=[B_xt])

    if "p2" in phases:
        P.fence(); A.mark(); phase2(); A.release()
    if "p3" in phases:
        P.fence(); A.mark(); phase3(); A.release()
    for e in ENGS:
        last = {}
        for o in P.ops[e]:
            if o.dma:
                last[id(o.token)] = o
        seen = {}
        nd = 0
        for o in P.ops[e]:
            if o.dma:
                seen[nd % NDMA_SLOTS] = o
                nd += 1
        P.final += list(seen.values())
    P.emit(nc, stack)
    stack.close()
    return nc, consts


def prep_inputs(inp):
    f = lambda a: np.ascontiguousarray(np.asarray(a, dtype=np.float32))
    w_in = f(inp["w_in"][0])
    shared = {
        "w_fm": f(w_in[:, _fm_cols()]),
        "w_tm": f(w_in[:, _tm_cols()]),
        "g_mix": pmajor(inp["ln_mix_g"][0], 8),
        "gate_b": f(np.broadcast_to(np.asarray(inp["nsa_gate_b"][0]).reshape(1, 24), (128, 24))),
        "w1k": f(inp["cmp_w1_k"][0]), "w1v": f(inp["cmp_w1_v"][0]),
        "w2k": f(np.concatenate([inp["cmp_w2_k"][0], inp["cmp_w2_k"][0]], axis=1)),
        "w2v": f(inp["cmp_w2_v"][0]),
        "pek": f(np.asarray(inp["cmp_pos_k"][0]).T), "pev": f(np.asarray(inp["cmp_pos_v"][0]).T),
        "wa2": f(np.concatenate([inp["gla_w_alpha2"][0], np.asarray(inp["gla_b_alpha"][0]).reshape(1, 256)], axis=0)),
        "gla_g": f(np.broadcast_to(np.asarray(inp["gla_norm_g"][0]).reshape(1, 128), (128, 128))),
        "ba": pmajor(inp["gla_b_alpha"][0], 2),
        "wbn": f(inp["w_branch_nsa"][0]), "wbg": f(inp["w_branch_gla"][0]), "wout": f(inp["w_out"][0]),
        "g_x": pmajor(inp["ln_x_g"][0], 8), "g_mem": pmajor(inp["ln_mem_g"][0], 8),
        "wxq": f(inp["w_xq"][0]), "wxkv": f(inp["w_xkv"][0]), "wxo": f(inp["w_xo"][0]),
        "g_ffn": pmajor(inp["ln_ffn_g"][0], 8),
        "wup": f(inp["w_up"][0]), "wdn": f(inp["w_down"][0]),
        "convw": f(np.asarray(inp["conv_w"][0]).reshape(3, 22, 128).transpose(2, 0, 1)),
        "convb": f(np.asarray(inp["conv_b"][0]).reshape(22, 128).T),
        "g_fin": f(np.broadcast_to(np.asarray(inp["ln_final_g"]).reshape(1, D), (128, D))),
    }
    for k, v in host_consts().items():
        shared["c_" + k] = v
    x = np.asarray(inp["x"], dtype=np.float32)
    mem = np.asarray(inp["mem"], dtype=np.float32)
    maps = []
    for c in range(NCORES):
        m = dict(shared)
        m["x"] = np.ascontiguousarray(x[c * NSEQ:(c + 1) * NSEQ].reshape(NTOK, D))
        m["mem"] = np.ascontiguousarray(mem[c * NSEQ:(c + 1) * NSEQ].reshape(NSEQ * MEM, D))
        maps.append(m)
    return maps


_CACHE = {}


def kernel(**inputs):
    if "nc" not in _CACHE:
        _CACHE["nc"] = build_program()[0]
    nc = _CACHE["nc"]
    maps = prep_inputs(inputs)
    res = run_bass_kernel_spmd(nc, maps, core_ids=list(range(NCORES)))
    out = np.stack([np.asarray(r["out"]).reshape(NSEQ, SEQ, D) for r in res.results], axis=0)
    return out.reshape(NCORES * NSEQ, SEQ, D).astype(np.float32)
```

```python
import numpy as np
import concourse.bass as bass
import concourse.mybir as mybir
from concourse.bass_utils import run_bass_kernel_spmd

F32 = mybir.dt.float32
BF16 = mybir.dt.bfloat16
U8 = mybir.dt.uint8
AF = mybir.ActivationFunctionType
ALU = mybir.AluOpType
AX = mybir.AxisListType

NCORES = 8
SEQ = 2048
D = 1024
NSEQ = 4
NTOK = NSEQ * SEQ
MEM = 256
FFN = 2816
NEG = -30000.0
EPS = 1e-6

STAGE = [99]


class StopBuild(Exception):
    pass


def stage(n):
    if STAGE[0] == n:
        raise StopBuild()


DEBUG = {}


class Buf:
    __slots__ = ("name", "last_w", "rd_eng", "rd_dma")

    def __init__(self, name):
        self.name = name
        self.last_w = None
        self.rd_eng = {}
        self.rd_dma = []


class Op:
    __slots__ = ("eng", "fn", "dma", "deps", "signal", "token", "prev_slot")

    def __init__(self, eng, fn, dma):
        self.eng = eng
        self.fn = fn
        self.dma = dma
        self.deps = []
        self.signal = dma
        self.token = None
        self.prev_slot = None


ENGS = ("pe", "act", "dve", "pool", "sp")
NDMA_SLOTS = 8


class Prog:
    def __init__(self):
        self.ops = {e: [] for e in ENGS}
        self.final = []
        self.fence_deps = []

    def fence(self):
        deps = []
        for e in ENGS:
            last = None
            for o in reversed(self.ops[e]):
                if not o.dma:
                    last = o
                    break
            if last is not None:
                last.signal = True
                deps.append(last)
            nd = 0
            slots = {}
            for o in self.ops[e]:
                if o.dma:
                    slots[nd % NDMA_SLOTS] = o
                    nd += 1
            deps += list(slots.values())
        self.fence_deps = deps

    def op(self, eng, fn, reads=(), writes=(), dma=False):
        o = Op(eng, fn, dma)
        raw = set()
        other = set()
        for b in reads:
            if b.last_w is not None:
                raw.add(b.last_w)
        for b in writes:
            if b.last_w is not None:
                other.add(b.last_w)
            other.update(b.rd_eng.values())
            other.update(b.rd_dma)
        for d in raw | other:
            if d is o:
                continue
            same = (not dma) and (not d.dma) and d.eng == eng
            if same and (eng == "pe" or d not in raw):
                continue
            o.deps.append(d)
            d.signal = True
        for d in self.fence_deps:
            if (not dma) and (not d.dma) and d.eng == eng:
                continue
            if d not in o.deps:
                o.deps.append(d)
        for b in reads:
            if dma:
                b.rd_dma.append(o)
            else:
                b.rd_eng[eng] = o
        for b in writes:
            b.last_w = o
            b.rd_eng = {}
            b.rd_dma = []
        self.ops[eng].append(o)
        return o

    def emit(self, nc, stack):
        sems = {e: stack.enter_context(nc.semaphore("s_" + e)) for e in ENGS}
        dsem = {e: [stack.enter_context(nc.semaphore("d_%s%d" % (e, i))) for i in range(NDMA_SLOTS)]
                for e in ("sp", "pool", "act")}
        for e in ENGS:
            cnt = 0
            nd = 0
            slot_cnt = [0] * NDMA_SLOTS
            slot_last = [None] * NDMA_SLOTS
            for o in self.ops[e]:
                if o.dma:
                    s = nd % NDMA_SLOTS
                    nd += 1
                    slot_cnt[s] += 16
                    o.prev_slot = slot_last[s]
                    o.token = (dsem[e][s], slot_cnt[s])
                    slot_last[s] = o
                elif o.signal:
                    cnt += 1
                    o.token = (sems[e], cnt)
        block = stack.enter_context(nc.Block())
        prog = self

        def body(e):
            def run(eng):
                waited = {}

                def wait(tok):
                    sem, val = tok
                    k = id(sem)
                    if waited.get(k, 0) >= val:
                        return
                    eng.wait_ge(sem, val)
                    waited[k] = val

                for o in prog.ops[e]:
                    if o.dma and o.prev_slot is not None:
                        wait(o.prev_slot.token)
                    for d in o.deps:
                        wait(d.token)
                    ins = o.fn(eng)
                    if o.token is not None:
                        ins.then_inc(o.token[0], 16 if o.dma else 1)
                if e == "sp":
                    for o in prog.final:
                        wait(o.token)
            return run

        block.tensor(body("pe"))
        block.scalar(body("act"))
        block.vector(body("dve"))
        block.gpsimd(body("pool"))
        block.sync(body("sp"))


class Arena:
    def __init__(self, ap, size):
        self.ap = ap
        self.size = size
        self.off = 0
        self.marks = []

    def alloc(self, shape, dtype, name="t"):
        esz = 4 if dtype == F32 else 2
        n = 1
        for s in shape[1:]:
            n *= s
        nbytes = (n * esz + 31) // 32 * 32
        assert self.off + nbytes <= self.size, (name, self.off, nbytes, self.size)
        a = self.ap[0:shape[0], self.off:self.off + n * esz].bitcast(dtype)
        self.off += nbytes
        if len(shape) == 3:
            a = a.rearrange("p (a b) -> p a b", a=shape[1])
        elif len(shape) == 4:
            a = a.rearrange("p (a b c) -> p a b c", a=shape[1], b=shape[2])
        return a

    def mark(self):
        self.marks.append(self.off)

    def release(self):
        self.off = self.marks.pop()


SLOPES = [2.0 ** (-(h + 1)) for h in range(8)]

C_QA, C_KC, C_VC, C_KS, C_VS, C_KW, C_VW = 0, 512, 640, 768, 896, 1024, 1152
C_GATE, C_QB, C_KB, C_VB, C_RB, C_AL, C_MG = 1280, 1304, 1560, 1816, 2328, 2840, 2856
FM_QA, FM_KC, FM_VC, FM_KS, FM_KW, FM_QB, FM_KB, FM_MG = 0, 4, 5, 6, 8, 10, 12, 14
NFM = 30
TM_VS, TM_VW, TM_KB, TM_VB, TM_RB = 0, 128, 256, 512, 1024
NTM = 1536


def _fm_cols():
    cols = []
    for c in range(4):
        cols += list(range(C_QA + 128 * c, C_QA + 128 * (c + 1)))
    cols += list(range(C_KC, C_KC + 128))
    cols += list(range(C_VC, C_VC + 128))
    for base in (C_KS, C_KW):
        for g in range(2):
            one = list(range(base + 64 * g, base + 64 * (g + 1)))
            cols += one + one
    cols += list(range(C_QB, C_QB + 256))
    cols += list(range(C_KB, C_KB + 256))
    cols += list(range(C_MG, C_MG + 2048))
    cols += list(range(C_AL, C_AL + 16))
    return np.array(cols)


def _tm_cols():
    cols = list(range(C_VS, C_VS + 128)) + list(range(C_VW, C_VW + 128))
    cols += list(range(C_KB, C_KB + 256)) + list(range(C_VB, C_VB + 512)) + list(range(C_RB, C_RB + 512))
    cols += list(range(C_GATE, C_GATE + 24))
    return np.array(cols)


def pmajor(v, nchunk):
    return np.ascontiguousarray(np.asarray(v, np.float32).reshape(nchunk, 128).T)


def host_consts():
    c = {}
    t = np.arange(SEQ)
    n = np.arange(127)
    dist = t[None, :] - (16 * n[:, None] + 31)
    c["cmpD"] = np.where(dist >= 0, -dist, -1.0e6).astype(np.float32)
    ov = np.zeros((127, 32), np.float32)
    for nn in range(127):
        for p in range(32):
            ov[nn, (16 * nn + p) // 64] += 1.0 / 32
    c["ovl"] = ov
    cur = (t // 64)
    j = np.arange(32)
    forced = (j[None, :] == 0) | (j[None, :] == cur[:, None]) | (j[None, :] == cur[:, None] - 1)
    future = j[None, :] > cur[:, None]
    mul = np.where(forced | future, 0.0, 1.0).astype(np.float32)
    add = np.where(forced, 5.0, np.where(future, -1.0, 0.0)).astype(np.float32)
    c["fmul"] = np.ascontiguousarray(mul.reshape(16, 128, 32).transpose(1, 0, 2))
    c["fadd"] = np.ascontiguousarray(add.reshape(16, 128, 32).transpose(1, 0, 2))
    c["tb"] = (64.0 * (cur[None, :] - j[:, None])).astype(np.float32)
    ea = np.zeros((34, SEQ), np.float32)
    ea[t // 64, t] = 1.0
    ea[32] = t % 64
    ea[33] = 1.0
    c["ea"] = ea
    cr = np.zeros((2, 8, 512), np.float32)
    rq = np.arange(512) % 64
    for h in range(8):
        cr[0, h] = SLOPES[h]
        cr[1, h] = -SLOPES[h] * rq
    c["crow"] = cr
    k = np.arange(128)
    cc = np.arange(896)
    c["cb"] = np.where(cc[None, :] - 384 >= k[:, None], 0.0, NEG).astype(np.float32)
    cc = np.arange(1152)
    dd = cc[None, :] - 384 - k[:, None]
    wb = np.zeros((128, 8, 1152), np.float32)
    for h in range(8):
        wb[:, h, :] = np.where((dd >= 0) & (dd < 256), -SLOPES[h] * dd, NEG)
    c["wb"] = wb
    c["ident"] = np.eye(128, dtype=np.float32)
    jj = np.arange(128)
    same = (jj[:, None] // 64) == (jj[None, :] // 64)
    c["gmask"] = (same & (jj[:, None] <= jj[None, :])).astype(np.float32)
    c["srst"] = np.broadcast_to(np.where(t % 64 == 0, 0.0, 1.0).astype(np.float32), (128, SEQ)).copy()
    c["gup"] = (same & (jj[:, None] > jj[None, :])).astype(np.float32)
    return c


CONST_SHAPES = None


def build_program(phases=("p0", "p1", "p2", "p3")):
    import contextlib
    nc = bass.Bass("TRN2", target_bir_lowering=False)
    P = Prog()
    stack = contextlib.ExitStack()

    def din(name, shape, dt=F32):
        return nc.dram_tensor(name, list(shape), dt, kind="ExternalInput").ap()

    def dscr(name, shape, dt):
        kind = "ExternalOutput" if DEBUG.get(name) else "Internal"
        return nc.dram_tensor(name, list(shape), dt, kind=kind).ap()

    consts = host_consts()
    x_d = din("x", [NTOK, D])
    mem_d = din("mem", [NSEQ * MEM, D])
    wfm_d = din("w_fm", [D, NFM * 128 + 16])
    wtm_d = din("w_tm", [D, NTM + 24])
    g_mix_d = din("g_mix", [128, 8])
    gate_b_d = din("gate_b", [128, 24])
    w1k_d = din("w1k", [2048, 128]); w1v_d = din("w1v", [2048, 128])
    w2k_d = din("w2k", [128, 128]); w2v_d = din("w2v", [128, 64])
    pek_d = din("pek", [64, 32]); pev_d = din("pev", [64, 32])
    wa2_d = din("wa2", [17, 256])
    gla_g_d = din("gla_g", [128, 128])
    ba_d = din("ba", [128, 2])
    wbn_d = din("wbn", [512, D]); wbg_d = din("wbg", [512, D]); wout_d = din("wout", [D, D])
    g_x_d = din("g_x", [128, 8]); g_mem_d = din("g_mem", [128, 8])
    wxq_d = din("wxq", [D, 512]); wxkv_d = din("wxkv", [D, 1024]); wxo_d = din("wxo", [512, D])
    g_ffn_d = din("g_ffn", [128, 8])
    wup_d = din("wup", [D, 2 * FFN]); wdn_d = din("wdn", [FFN, D])
    convw_d = din("convw", [128, 3, 22]); convb_d = din("convb", [128, 22])
    g_fin_d = din("g_fin", [128, D])
    cd = {k: din("c_" + k, v.shape) for k, v in consts.items()}
    out_d = nc.dram_tensor("out", [NTOK, D], F32, kind="ExternalOutput").ap()
    s_fm = dscr("s_fm", [NFM, 128, NTOK], BF16)
    s_al = dscr("s_al", [16, NTOK], F32)
    s_tm = dscr("s_tm", [NTOK, NTM], BF16)
    s_gate = dscr("s_gate", [NTOK, 24], F32)
    s_on = dscr("s_on", [4, 128, NTOK], BF16)
    s_og = dscr("s_og", [4, 128, NTOK], BF16)
    s_x2 = dscr("s_x2", [NTOK, D], F32)
    dbg = {}
    if DEBUG.get("gla"):
        dbg["cs"] = nc.dram_tensor("dbg_cs", [128, 2, SEQ], F32, kind="ExternalOutput").ap()
        dbg["kd"] = nc.dram_tensor("dbg_kd", [128, 2, SEQ], BF16, kind="ExternalOutput").ap()
        dbg["qd"] = nc.dram_tensor("dbg_qd", [128, 4, SEQ], BF16, kind="ExternalOutput").ap()
        dbg["rg"] = nc.dram_tensor("dbg_rg", [128, 16, 512], BF16, kind="ExternalOutput").ap()
        dbg["ogb"] = nc.dram_tensor("dbg_ogb", [128, 512], BF16, kind="ExternalOutput").ap()
        dbg["att"] = nc.dram_tensor("dbg_att", [128, 4, 128], BF16, kind="ExternalOutput").ap()
        dbg["la"] = nc.dram_tensor("dbg_la", [128, 2, SEQ], F32, kind="ExternalOutput").ap()
    if DEBUG.get("ffn"):
        dbg["aT"] = nc.dram_tensor("dbg_aT", [128, 22, 512], BF16, kind="ExternalOutput").ap()
        dbg["x3"] = nc.dram_tensor("dbg_x3", [128, 4, D], F32, kind="ExternalOutput").ap()
        dbg["hf"] = nc.dram_tensor("dbg_hf", [128, 8, 512], BF16, kind="ExternalOutput").ap()

    ARENA = 204 * 1024
    arena_t = stack.enter_context(nc.sbuf_tensor("arena", [128, ARENA], U8))
    psum_t = stack.enter_context(nc.psum_tensor("psum", [128, 4096], F32))
    A = Arena(arena_t, ARENA)
    PS = [psum_t[:, 512 * i:512 * (i + 1)] for i in range(8)]
    PSB = [Buf("ps%d" % i) for i in range(8)]

    rr = {"ev": 0, "q": 0}

    def dma(q, out, in_, reads=(), writes=(), **kw):
        return P.op(q, lambda e: e.dma_start(out=out, in_=in_, **kw), reads=reads, writes=writes, dma=True)

    def load_cast(dst, src, wb, nsplit=1):
        last = dst.shape[-1]
        step = (last + nsplit - 1) // nsplit
        for s0 in range(0, last, step):
            s1 = min(last, s0 + step)
            if len(dst.shape) == 2:
                dma("pool", dst[:, s0:s1], src[:, s0:s1], writes=[wb])
            else:
                dma("pool", dst[:, :, s0:s1], src[:, :, s0:s1], writes=[wb])

    def evac_copy(out, in_, reads, writes, scale=None):
        rr["ev"] += 1
        if rr["ev"] % 2 == 0:
            if scale is None:
                P.op("act", lambda e: e.activation(out=out, in_=in_, func=AF.Copy), reads=reads, writes=writes)
            else:
                P.op("act", lambda e: e.activation(out=out, in_=in_, func=AF.Copy, scale=scale), reads=reads, writes=writes)
        else:
            if scale is None:
                P.op("dve", lambda e: e.tensor_copy(out=out, in_=in_), reads=reads, writes=writes)
            else:
                P.op("dve", lambda e: e.tensor_scalar(out=out, in0=in_, scalar1=scale, scalar2=None, op0=ALU.mult),
                     reads=reads, writes=writes)

    def mm(out, lhsT, rhs, start, stop, reads, writes, skip=False):
        P.op("pe", lambda e: e.matmul(out, lhsT=lhsT, rhs=rhs, start=start, stop=stop, skip_group_check=skip),
             reads=reads, writes=writes)

    def transpose(out, in_, ident, reads, writes):
        P.op("pe", lambda e: e.transpose(out, in_, ident), reads=reads, writes=writes)

    ident_f = A.alloc([128, 128], F32, "ident_f")
    ident_b = A.alloc([128, 128], BF16, "ident_b")
    B_ident = Buf("ident")
    dma("sp", ident_f, cd["ident"][:, :], writes=[B_ident])
    dma("pool", ident_b, cd["ident"][:, :], writes=[B_ident])

    def rmsnorm_T(xt, B_xt, nsub, g_sb, B_g, hT, B_hT, xn, B_xn, st, B_st, psA):
        for s in range(nsub):
            P.op("act", lambda e, s=s: e.activation(out=xn[:, s, :], in_=xt[:, s, :], func=AF.Square,
                                                    accum_out=st[:, s:s + 1]),
                 reads=[B_xt], writes=[B_xn, B_st])
        P.op("act", lambda e: e.activation(out=st[:, 8:8 + nsub], in_=st[:, 0:nsub], func=AF.Sqrt, bias=EPS,
                                           scale=1.0 / D), reads=[B_st], writes=[B_st])
        P.op("dve", lambda e: e.reciprocal(out=st[:, 16:16 + nsub], in_=st[:, 8:8 + nsub]), reads=[B_st], writes=[B_st])
        for s in range(nsub):
            P.op("dve", lambda e, s=s: e.tensor_scalar(out=xn[:, s, :], in0=xt[:, s, :], scalar1=st[:, 16 + s:17 + s],
                                                       scalar2=None, op0=ALU.mult),
                 reads=[B_xt, B_st], writes=[B_xn])
        for kc in range(8):
            pi = psA[kc % len(psA)]
            pst = PS[pi].bitcast(BF16)
            for s in range(nsub):
                transpose(pst[:, s * 128:(s + 1) * 128], xn[:, s, kc * 128:(kc + 1) * 128], ident_b,
                          reads=[B_xn, B_ident], writes=[PSB[pi]])
            evac_copy(hT[:, kc, 0:nsub * 128], pst[:, 0:nsub * 128], reads=[PSB[pi], B_g], writes=[B_hT],
                      scale=g_sb[:, kc:kc + 1])

    if "p0" in phases:
        A.mark()
        wfm = A.alloc([128, 8, NFM * 128 + 16], BF16, "wfm"); B_wfm = Buf("wfm")
        wtm = A.alloc([128, 8, NTM + 24], BF16, "wtm"); B_wtm = Buf("wtm")
        load_cast(wfm, wfm_d.rearrange("(kc p) n -> p kc n", p=128), B_wfm, nsplit=4)
        load_cast(wtm, wtm_d.rearrange("(kc p) n -> p kc n", p=128), B_wtm, nsplit=2)
        gmix = A.alloc([128, 8], F32, "gmix"); B_gmix = Buf("gmix")
        dma("sp", gmix, g_mix_d[:, :], writes=[B_gmix])
        xt = A.alloc([128, 4, D], F32, "xt"); B_xt = Buf("xt")
        xn = A.alloc([128, 4, D], BF16, "xn"); B_xn = Buf("xn")
        st = A.alloc([128, 32], F32, "st"); B_st = Buf("st")
        hTs = [A.alloc([128, 8, 512], BF16, "hT%d" % i) for i in range(2)]
        B_hTs = [Buf("hT%d" % i) for i in range(2)]
        fmo = A.alloc([128, NFM, 512], BF16, "fmo")
        B_fmo = [Buf("fmo%d" % i) for i in range(3)]
        alo = A.alloc([16, 512], F32, "alo"); B_alo = Buf("alo")
        tmo = A.alloc([128, 4, NTM], BF16, "tmo"); B_tmo = Buf("tmo")
        gto = A.alloc([128, 4, 24], F32, "gto"); B_gto = Buf("gto")
        mmbank = [2, 3, 4, 5, 6, 7]
        bi = 0
        for it in range(NTOK // 512):
            t0 = it * 512
            hT = hTs[it % 2]; B_hT = B_hTs[it % 2]
            dma("sp", xt, x_d[t0:t0 + 512, :].rearrange("(s p) d -> p s d", p=128), writes=[B_xt])
            rmsnorm_T(xt, B_xt, 4, gmix, B_gmix, hT, B_hT, xn, B_xn, st, B_st, [0, 1])
            for c in range(NFM + 1):
                M = 128 if c < NFM else 16
                pi = mmbank[bi % 6]; bi += 1
                for kc in range(8):
                    mm(PS[pi][0:M, :], wfm[:, kc, c * 128:c * 128 + M], hT[:, kc, :], kc == 0, kc == 7,
                       reads=[B_wfm, B_hT], writes=[PSB[pi]])
                if c == NFM:
                    P.op("dve", lambda e, pi=pi: e.tensor_copy(out=alo, in_=PS[pi][0:16, :]),
                         reads=[PSB[pi]], writes=[B_alo])
                    continue
                bo = B_fmo[c // 10]
                if c < 4:
                    evac_copy(fmo[:, c, :], PS[pi], [PSB[pi]], [bo], scale=0.125)
                elif c >= FM_MG:
                    P.op("act", lambda e, c=c, pi=pi: e.activation(out=fmo[:, c, :], in_=PS[pi], func=AF.Sigmoid),
                         reads=[PSB[pi]], writes=[bo])
                else:
                    evac_copy(fmo[:, c, :], PS[pi], [PSB[pi]], [bo])
                if c % 10 == 9:
                    g0 = c - 9
                    dma("sp", s_fm[g0:g0 + 10, :, t0:t0 + 512].rearrange("c p t -> p c t"), fmo[:, g0:g0 + 10, :],
                        reads=[bo])
            dma("sp", s_al[:, t0:t0 + 512], alo, reads=[B_alo])
            for s in range(4):
                for (c0, c1) in ((0, 512), (512, 1024), (1024, 1536), (1536, 1560)):
                    pi = mmbank[bi % 6]; bi += 1
                    for kc in range(8):
                        mm(PS[pi][:, 0:c1 - c0], hT[:, kc, s * 128:(s + 1) * 128], wtm[:, kc, c0:c1], kc == 0, kc == 7,
                           reads=[B_wtm, B_hT], writes=[PSB[pi]])
                    if c0 < 1536:
                        evac_copy(tmo[:, s, c0:c1], PS[pi], [PSB[pi]], [B_tmo])
                    else:
                        evac_copy(gto[:, s, :], PS[pi][:, 0:24], [PSB[pi]], [B_gto])
            dma("sp", s_tm[t0:t0 + 512, :].rearrange("(s p) n -> p s n", p=128), tmo, reads=[B_tmo])
            dma("sp", s_gate[t0:t0 + 512, :].rearrange("(s p) n -> p s n", p=128), gto, reads=[B_gto])
        A.release()


    def V_tt(out, in0, in1, op, reads, writes):
        P.op("dve", lambda e: e.tensor_tensor(out=out, in0=in0, in1=in1, op=op), reads=reads, writes=writes)

    def V_ts(out, in0, s1, s2, op0, op1, reads, writes):
        if op1 is None:
            P.op("dve", lambda e: e.tensor_scalar(out=out, in0=in0, scalar1=s1, scalar2=None, op0=op0), reads=reads, writes=writes)
        else:
            P.op("dve", lambda e: e.tensor_scalar(out=out, in0=in0, scalar1=s1, scalar2=s2, op0=op0, op1=op1), reads=reads, writes=writes)

    def V_stt(out, in0, scalar, in1, op0, op1, reads, writes):
        P.op("dve", lambda e: e.scalar_tensor_tensor(out=out, in0=in0, scalar=scalar, in1=in1, op0=op0, op1=op1),
             reads=reads, writes=writes)

    def V_copy(out, in_, reads, writes):
        P.op("dve", lambda e: e.tensor_copy(out=out, in_=in_), reads=reads, writes=writes)

    def V_recip(out, in_, reads, writes):
        P.op("dve", lambda e: e.reciprocal(out=out, in_=in_), reads=reads, writes=writes)

    def V_max(out, in_, reads, writes):
        P.op("dve", lambda e: e.max(out=out, in_=in_), reads=reads, writes=writes)

    def A_act(out, in_, func, reads, writes, **kw):
        P.op("act", lambda e: e.activation(out=out, in_=in_, func=func, **kw), reads=reads, writes=writes)

    def G_memset(ap, val, writes):
        P.op("pool", lambda e: e.memset(ap, val), writes=writes)

    def G_tt(out, in0, in1, op, reads, writes):
        P.op("pool", lambda e: e.tensor_tensor(out=out, in0=in0, in1=in1, op=op), reads=reads, writes=writes)
    def phase1_nsa():
        B_c1 = Buf("c1")
        cmpD = A.alloc([128, SEQ], F32, "cmpD")
        dma("sp", cmpD[0:127, :], cd["cmpD"][:, :], writes=[B_c1])
        fmul = A.alloc([128, 16, 32], F32, "fmul"); fadd = A.alloc([128, 16, 32], F32, "fadd")
        dma("sp", fmul, cd["fmul"][:, :, :], writes=[B_c1]); dma("sp", fadd, cd["fadd"][:, :, :], writes=[B_c1])
        tbt = A.alloc([32, SEQ], F32, "tb"); dma("sp", tbt, cd["tb"][:, :], writes=[B_c1])
        ea = A.alloc([128, SEQ], BF16, "ea")
        G_memset(ea, 0.0, [B_c1])
        dma("pool", ea[0:34, :], cd["ea"][:, :], writes=[B_c1])
        cbt = A.alloc([128, 896], BF16, "cb"); dma("pool", cbt, cd["cb"][:, :], writes=[B_c1])
        wbt = A.alloc([128, 8, 1152], BF16, "wb"); dma("pool", wbt, cd["wb"][:, :, :], writes=[B_c1])
        MbA = A.alloc([128, 8, 512], BF16, "MbA"); B_mb = [Buf("mb%d" % h) for h in range(8)]
        G_memset(MbA, 0.0, B_mb)
        dma("pool", MbA[32:34, :, :], cd["crow"][:, :, :], writes=B_mb)
        W1 = {}; W2 = {}; peT = {}; cbias = {}
        B_cw = Buf("cw")
        for nm, w1d, w2d, ped in (("k", w1k_d, w2k_d, pek_d), ("v", w1v_d, w2v_d, pev_d)):
            W1[nm] = A.alloc([128, 32, 128], BF16, "w1" + nm)
            src = w1d.rearrange("(p d) h -> d p h", d=64)
            dma("pool", W1[nm][0:64, :, :], src, writes=[B_cw])
            dma("pool", W1[nm][64:128, :, :], src, writes=[B_cw])
            W2[nm] = A.alloc([128, 128 if nm == "k" else 64], BF16, "w2" + nm)
            dma("pool", W2[nm], w2d[:, :], writes=[B_cw])
            peT[nm] = A.alloc([64, 32], BF16, "pe" + nm)
            dma("pool", peT[nm], ped[:, :], writes=[B_cw])
            cbias[nm] = A.alloc([128, 1], F32, "cbias" + nm)
        B_cb = Buf("cbias")
        for nm in ("k", "v"):
            for p in range(32):
                mm(PS[7][:, 0:1], W1[nm][0:64, p, :], peT[nm][0:64, p:p + 1], p == 0, p == 31, reads=[B_cw], writes=[PSB[7]])
            V_copy(cbias[nm], PS[7][:, 0:1], [PSB[7]], [B_cb])
        gateb = A.alloc([128, 24], F32, "gateb"); dma("sp", gateb, gate_b_d[:, :], writes=[B_c1])
        stage(1)
        qa = A.alloc([128, 4, SEQ], BF16, "qa"); B_qa = Buf("qa")
        kcT = A.alloc([128, SEQ], BF16, "kcT"); vcT = A.alloc([128, SEQ], BF16, "vcT"); B_kvc = Buf("kvc")
        ksT = A.alloc([128, 4, SEQ], BF16, "ksT"); B_ks = Buf("ks")
        kwT = A.alloc([128, 4, SEQ], BF16, "kwT"); B_kw = Buf("kw")
        vsa = A.alloc([128, 16, 2, 65], BF16, "vsa"); B_vs = Buf("vs")
        vwa = A.alloc([128, 16, 2, 65], BF16, "vwa"); B_vw = Buf("vw")
        G_memset(vsa[:, :, :, 64:65], 1.0, [B_vs])
        G_memset(vwa[:, :, :, 64:65], 1.0, [B_vw])
        gsig = A.alloc([128, 16, 24], F32, "gsig"); B_gs = Buf("gs")
        hidT = A.alloc([128, 128], BF16, "hidT"); B_hid = Buf("hid")
        kcmpT = A.alloc([128, 2, 128], BF16, "kcmpT"); B_kcmp = Buf("kcmp")
        Rg = A.alloc([128, 2, 97], BF16, "Rg"); B_R = Buf("R")
        G_memset(Rg[:, :, 64:65], 1.0, [B_R])
        for g in range(2):
            dma("pool", Rg[0:127, g, 65:97], cd["ovl"][:, :], writes=[B_R])
        S_sb = A.alloc([128, 512], F32, "S_sb"); B_S = Buf("S")
        PTs = [A.alloc([128, 512], BF16, "PT%d" % i) for i in range(3)]; B_PT = [Buf("PT%d" % i) for i in range(3)]
        oacc = A.alloc([128, 4, 8, 64], F32, "oacc"); B_oa = Buf("oacc")
        otmp = A.alloc([128, 4, 64], F32, "otmp"); B_ot = Buf("otmp")
        onb = A.alloc([128, 4, 512], BF16, "onb"); B_onb = Buf("onb")
        onT = A.alloc([128, 4, 512], BF16, "onT"); B_onT = Buf("onT")
        imp = A.alloc([128, 4, 32], F32, "imp"); B_imp = Buf("imp")
        itmp = A.alloc([128, 4, 32], F32, "itmp"); B_it = Buf("itmp")
        t8 = A.alloc([128, 4, 8], F32, "t8"); B_t8 = Buf("t8")
        selb = A.alloc([128, 4, 32], F32, "selb"); B_selb = Buf("selb")
        selT = A.alloc([32, 512], F32, "selT"); B_selT = Buf("selT")
        sm = A.alloc([128, 16], F32, "sm"); B_sm = Buf("sm")
        pt_i = [0]; sc_i = [0]; acc_i = [0]

        def pv_evac(pacc, pb, qt, h, br, first):
            if br == 0:
                V_ts(sm[:, 0:4], pacc[:, :, 64], 1e-30, None, ALU.max, None, [PSB[pb]], [B_sm])
                V_recip(sm[:, 4:8], sm[:, 0:4], [B_sm], [B_sm])
            else:
                V_recip(sm[:, 4:8], pacc[:, :, 64], [PSB[pb]], [B_sm])
            V_tt(sm[:, 8:12], sm[:, 4:8], gsig[:, 4 * qt:4 * qt + 4, 3 * h + br], ALU.mult, [B_sm, B_gs], [B_sm])
            rgb = sm[:, 8:12].unsqueeze(2).to_broadcast([128, 4, 64])
            if first:
                V_tt(oacc[:, :, h, :], pacc[:, :, 0:64], rgb, ALU.mult, [PSB[pb], B_sm], [B_oa])
            else:
                V_tt(otmp, pacc[:, :, 0:64], rgb, ALU.mult, [PSB[pb], B_sm], [B_ot])
                V_tt(oacc[:, :, h, :], oacc[:, :, h, :], otmp, ALU.add, [B_oa, B_ot], [B_oa])

        for sq in range(NSEQ):
            tb0 = sq * SEQ
            dma("sp", qa, s_fm[FM_QA:FM_QA + 4, :, tb0:tb0 + SEQ].rearrange("c p t -> p c t"), writes=[B_qa])
            dma("sp", kcT, s_fm[FM_KC, :, tb0:tb0 + SEQ], writes=[B_kvc])
            dma("sp", vcT, s_fm[FM_VC, :, tb0:tb0 + SEQ], writes=[B_kvc])
            for g in range(2):
                for hf in range(2):
                    dma("sp", ksT[:, 2 * g + hf, :], s_fm[FM_KS + g, :, tb0:tb0 + SEQ], writes=[B_ks])
                    dma("sp", kwT[:, 2 * g + hf, :], s_fm[FM_KW + g, :, tb0:tb0 + SEQ], writes=[B_kw])
                    zlo = 64 * (1 - hf)
                    G_memset(ksT[zlo:zlo + 64, 2 * g + hf, :], 0.0, [B_ks])
                    G_memset(kwT[zlo:zlo + 64, 2 * g + hf, :], 0.0, [B_kw])
            for g in range(2):
                dma("sp", vsa[:, :, g, 0:64],
                    s_tm[tb0:tb0 + SEQ, TM_VS + 64 * g:TM_VS + 64 * g + 64].rearrange("(kt p) d -> p kt d", p=128),
                    writes=[B_vs])
                dma("sp", vwa[:, :, g, 0:64],
                    s_tm[tb0:tb0 + SEQ, TM_VW + 64 * g:TM_VW + 64 * g + 64].rearrange("(kt p) d -> p kt d", p=128),
                    writes=[B_vw])
            dma("sp", gsig, s_gate[tb0:tb0 + SEQ, :].rearrange("(kt p) n -> p kt n", p=128), writes=[B_gs])
            V_tt(gsig, gsig, gateb.unsqueeze(1).to_broadcast([128, 16, 24]), ALU.add, [B_gs, B_c1], [B_gs])
            A_act(gsig, gsig, AF.Sigmoid, [B_gs], [B_gs])
            stage(2)
            for nm, srcT in (("k", kcT), ("v", vcT)):
                s3 = srcT.rearrange("q (n s) -> q n s", s=16)
                for g in range(2):
                    for p in range(32):
                        mm(PS[7][:, 0:127], W1[nm][64 * g:64 * g + 64, p, :], s3[64 * g:64 * g + 64, p // 16:p // 16 + 127, p % 16],
                           p == 0, p == 31, reads=[B_cw, B_kvc], writes=[PSB[7]])
                    A_act(hidT[:, 0:127], PS[7][:, 0:127], AF.Gelu_apprx_tanh, [PSB[7], B_cb], [B_hid], bias=cbias[nm])
                    if nm == "k":
                        mm(PS[6][:, 0:127], W2["k"], hidT[:, 0:127], True, True, reads=[B_cw, B_hid], writes=[PSB[6]])
                        evac_copy(kcmpT[:, g, 0:127], PS[6][:, 0:127], [PSB[6]], [B_kcmp])
                    else:
                        mm(PS[6][0:127, 0:64], hidT[:, 0:127], W2["v"], True, True, reads=[B_cw, B_hid], writes=[PSB[6]])
                        evac_copy(Rg[0:127, g, 0:64], PS[6][0:127, 0:64], [PSB[6]], [B_R])
            stage(3)
            for qt in range(4):
                q0 = qt * 512
                for g in range(2):
                    for gi in range(4):
                        h = 4 * g + gi
                        hp = 64 * (h % 2)
                        pi = sc_i[0] % 3; sc_i[0] += 1
                        mm(PS[pi][0:127, :], kcmpT[hp:hp + 64, g, 0:127], qa[hp:hp + 64, h // 2, q0:q0 + 512], True, True,
                           reads=[B_kcmp, B_qa], writes=[PSB[pi]])
                        V_stt(S_sb[0:127, :], cmpD[0:127, q0:q0 + 512], SLOPES[h], PS[pi][0:127, :], ALU.mult, ALU.add,
                              [PSB[pi], B_c1], [B_S])
                        k = pt_i[0] % 3; pt_i[0] += 1
                        PT = PTs[k]
                        A_act(PT[0:127, :], S_sb[0:127, :], AF.Exp, [B_S], [B_PT[k]])
                        pu = PS[5][:, 0:388].rearrange("p (s c) -> p s c", s=4)
                        for sub in range(4):
                            mm(pu[:, sub, :], PT[0:127, sub * 128:(sub + 1) * 128], Rg[0:127, g, :], True, True,
                               reads=[B_PT[k], B_R], writes=[PSB[5]])
                        pv_evac(pu, 5, qt, h, 0, True)
                        rdb = sm[:, 4:8].unsqueeze(2).to_broadcast([128, 4, 32])
                        if gi == 0:
                            V_tt(imp, pu[:, :, 65:97], rdb, ALU.mult, [PSB[5], B_sm], [B_imp])
                        else:
                            V_tt(itmp, pu[:, :, 65:97], rdb, ALU.mult, [PSB[5], B_sm], [B_it])
                            V_tt(imp, imp, itmp, ALU.add, [B_imp, B_it], [B_imp])
                    V_tt(imp, imp, fmul[:, 4 * qt:4 * qt + 4, :], ALU.mult, [B_imp, B_c1], [B_imp])
                    V_tt(imp, imp, fadd[:, 4 * qt:4 * qt + 4, :], ALU.add, [B_imp, B_c1], [B_imp])
                    for sub in range(4):
                        V_max(t8[:, sub, :], imp[:, sub, :], [B_imp], [B_t8])
                    for sub in range(4):
                        V_ts(selb[:, sub, :], imp[:, sub, :], t8[:, sub, 7:8], -NEG, ALU.is_ge, ALU.mult, [B_imp, B_t8], [B_selb])
                    for sub in range(4):
                        transpose(PS[6][0:32, sub * 128:(sub + 1) * 128], selb[:, sub, :], ident_f,
                                  reads=[B_selb, B_ident], writes=[PSB[6]])
                    V_ts(selT, PS[6][0:32, :], NEG, None, ALU.add, None, [PSB[6]], [B_selT])
                    for gi in range(4):
                        h = 4 * g + gi
                        V_stt(MbA[0:32, h, :], tbt[0:32, q0:q0 + 512], -SLOPES[h], selT, ALU.mult, ALU.add,
                              [B_c1, B_selT], [B_mb[h]])
                stage(4)
                items = []
                for h in range(8):
                    g = h // 4; qc = h // 2
                    for br in (1, 2):
                        pb = 3 + acc_i[0] % 2; acc_i[0] += 1
                        pacc = PS[pb][:, 0:260].rearrange("p (s c) -> p s c", s=4)
                        if br == 1:
                            kts = list(range(0, 4 * qt + 4))
                        else:
                            kts = list(range(max(0, 4 * qt - 2), 4 * qt + 4))
                        pairs = []
                        for kt in kts:
                            off = kt * 128 - q0
                            for sub in range(4):
                                dmax = 128 * sub + 127 - off
                                dmin = 128 * sub - 127 - off
                                if dmax < 0:
                                    continue
                                if br == 2 and dmin >= 256:
                                    continue
                                pairs.append((kt, sub))
                        lastkt = {}
                        for kt, sub in pairs:
                            lastkt[sub] = kt
                        for kt in kts:
                            items.append(dict(h=h, g=g, qc=qc, br=br, kt=kt, pb=pb, pacc=pacc, pairs=pairs, lastkt=lastkt,
                                              firstkt=(kt == kts[0]), lastk=(kt == kts[-1])))

                def emit_scores(it):
                    h = it["h"]; g = it["g"]; br = it["br"]; kt = it["kt"]
                    off = kt * 128 - q0
                    pi = sc_i[0] % 3; sc_i[0] += 1
                    kT = ksT if br == 1 else kwT
                    Bk = B_ks if br == 1 else B_kw
                    mm(PS[pi], kT[:, 2 * g + (h % 2), kt * 128:(kt + 1) * 128], qa[:, it["qc"], q0:q0 + 512], True, False,
                       reads=[Bk, B_qa], writes=[PSB[pi]])
                    if br == 1:
                        diag = off >= 0
                        mm(PS[pi], ea[:, kt * 128:(kt + 1) * 128], MbA[:, h, :], False, not diag,
                           reads=[B_c1, B_mb[h]], writes=[PSB[pi]])
                        if diag:
                            mm(PS[pi], ident_b, cbt[:, 384 - off:384 - off + 512], False, True,
                               reads=[B_ident, B_c1], writes=[PSB[pi]])
                    else:
                        mm(PS[pi], ident_b, wbt[:, h, 384 - off:384 - off + 512], False, True,
                           reads=[B_ident, B_c1], writes=[PSB[pi]])
                    k = pt_i[0] % 3; pt_i[0] += 1
                    it["k"] = k
                    A_act(PTs[k], PS[pi], AF.Exp, [PSB[pi]], [B_PT[k]])

                def emit_pv(it):
                    h = it["h"]; g = it["g"]; br = it["br"]; kt = it["kt"]; k = it["k"]; pb = it["pb"]
                    va = vsa if br == 1 else vwa
                    Bv = B_vs if br == 1 else B_vw
                    first = it["firstkt"]
                    for sub in range(4):
                        if (kt, sub) not in it["pairs"]:
                            continue
                        mm(it["pacc"][:, sub, :], PTs[k][:, sub * 128:(sub + 1) * 128], va[:, kt, g, :], first, it["lastkt"][sub] == kt,
                           reads=[B_PT[k], Bv], writes=[PSB[pb]], skip=True)
                        first = False
                    if it["lastk"]:
                        pv_evac(it["pacc"], pb, qt, h, br, False)

                LA = 2
                for i in range(len(items) + LA):
                    if i < len(items):
                        emit_scores(items[i])
                    if i >= LA:
                        emit_pv(items[i - LA])
                stage(5)
                A_act(onb.rearrange("p a b -> p (a b)"), oacc.rearrange("p a h d -> p (a h d)"), AF.Copy, [B_oa], [B_onb])
                for fc in range(4):
                    pst = PS[6].bitcast(BF16)
                    for sub in range(4):
                        transpose(pst[:, sub * 128:(sub + 1) * 128], onb[:, sub, fc * 128:(fc + 1) * 128], ident_b,
                                  reads=[B_onb, B_ident], writes=[PSB[6]])
                    evac_copy(onT[:, fc, :], pst[:, 0:512], [PSB[6]], [B_onT])
                dma("sp", s_on[:, :, tb0 + q0:tb0 + q0 + 512].rearrange("c p t -> p c t"), onT, reads=[B_onT])

    def phase1_gla():
        B_gc = Buf("gc")
        wa2 = A.alloc([16, 256], F32, "wa2"); dma("sp", wa2, wa2_d[0:16, :], writes=[B_gc])
        ba = A.alloc([128, 2], F32, "ba"); dma("sp", ba, ba_d[:, :], writes=[B_gc])
        nba = A.alloc([128, 2], F32, "nba"); V_ts(nba, ba, -1.0, None, ALU.mult, None, [B_gc], [B_gc])
        srst = A.alloc([128, SEQ], F32, "srst"); dma("sp", srst, cd["srst"][:, :], writes=[B_gc])
        gmask = A.alloc([128, 128], F32, "gmask"); dma("sp", gmask, cd["gmask"][:, :], writes=[B_gc])
        glag = A.alloc([128, 128], F32, "glag"); dma("sp", glag, gla_g_d[:, :], writes=[B_gc])
        alT = A.alloc([16, SEQ], F32, "alT"); B_al = Buf("al")
        qbT = A.alloc([128, 2, SEQ], BF16, "qbT"); kbT = A.alloc([128, 2, SEQ], BF16, "kbT"); B_qk = Buf("qk")
        vb = A.alloc([128, 16, 512], BF16, "vb"); B_vb = Buf("vb")
        rb = A.alloc([128, 16, 512], BF16, "rb"); B_rb = Buf("rb")
        sr = A.alloc([128, 16, 512], BF16, "sr"); B_sr = Buf("sr")
        rg = A.alloc([128, 16, 512], BF16, "rg"); B_rg = Buf("rg")
        laT = A.alloc([128, 2, SEQ], F32, "laT"); B_la = Buf("la")
        bT = A.alloc([128, 2, SEQ], F32, "bT"); B_b = Buf("b")
        ET = A.alloc([128, 2, SEQ], F32, "ET"); B_E = Buf("E")
        qd4 = A.alloc([128, 4, SEQ], BF16, "qd4"); B_qd = Buf("qd")
        kdT = A.alloc([128, 2, SEQ], BF16, "kdT"); B_kd = Buf("kd")
        dec = A.alloc([128, 2, 32], F32, "dec"); B_dec = Buf("dec")
        G_memset(qd4, 0.0, [B_qd])
        kd_ab = A.alloc([128, 2, 2, 128], BF16, "kd_ab"); B_kab = Buf("kab")
        G_memset(kd_ab, 0.0, [B_kab])
        Sf = [A.alloc([128, 128], F32, "Sf%d" % c) for c in range(2)]; B_Sf = [Buf("Sf%d" % c) for c in range(2)]
        tmpS = [A.alloc([128, 128], F32, "tS%d" % c) for c in range(2)]; B_tS = [Buf("tS%d" % c) for c in range(2)]
        Sbf = [[A.alloc([128, 128], BF16, "Sbf%d%d" % (c, a)) for a in range(2)] for c in range(2)]
        B_Sbf = [[Buf("Sbf%d%d" % (c, a)) for a in range(2)] for c in range(2)]
        att = A.alloc([128, 4, 128], BF16, "att"); B_att = Buf("att")
        ss = A.alloc([128, 16], F32, "ss"); B_ss = Buf("ss")
        junk = A.alloc([128, 128], BF16, "junk"); B_junk = Buf("junk")
        ogb = A.alloc([128, 512], BF16, "ogb"); B_ogb = Buf("ogb")
        ogT = A.alloc([128, 4, 512], BF16, "ogT"); B_ogT = Buf("ogT")
        for sq in range(NSEQ):
            tb0 = sq * SEQ
            dma("sp", alT, s_al[:, tb0:tb0 + SEQ], writes=[B_al])
            dma("sp", qbT, s_fm[FM_QB:FM_QB + 2, :, tb0:tb0 + SEQ].rearrange("c p t -> p c t"), writes=[B_qk])
            dma("sp", kbT, s_fm[FM_KB:FM_KB + 2, :, tb0:tb0 + SEQ].rearrange("c p t -> p c t"), writes=[B_qk])
            dma("sp", vb, s_tm[tb0:tb0 + SEQ, TM_VB:TM_VB + 512].rearrange("(kt p) n -> p kt n", p=128), writes=[B_vb])
            dma("sp", rb, s_tm[tb0:tb0 + SEQ, TM_RB:TM_RB + 512].rearrange("(kt p) n -> p kt n", p=128), writes=[B_rb])
            for c in range(2):
                for tt in range(4):
                    mm(PS[0], wa2[0:16, c * 128:(c + 1) * 128], alT[0:16, tt * 512:(tt + 1) * 512], True, True,
                       reads=[B_gc, B_al], writes=[PSB[0]])
                    A_act(laT[:, c, tt * 512:(tt + 1) * 512], PS[0], AF.Exp, [PSB[0], B_gc], [B_la], scale=-1.0, bias=nba[:, c:c + 1])
                A_act(laT[:, c, :], laT[:, c, :], AF.Ln, [B_la], [B_la], bias=1.0)
                P.op("dve", lambda e, c=c: e.tensor_tensor_scan(out=bT[:, c, :], data0=srst, data1=laT[:, c, :], initial=0.0,
                                                                op0=ALU.mult, op1=ALU.add), reads=[B_gc, B_la], writes=[B_b])
                A_act(ET[:, c, :], bT[:, c, :], AF.Exp, [B_b], [B_E], scale=-1.0 / 16)
                A_act(dec[:, c, :], bT[:, c, :].rearrange("p (n s) -> p n s", s=64)[:, :, 63], AF.Exp, [B_b], [B_dec], scale=-1.0 / 16)
                for hh in range(2):
                    lo = 64 * hh
                    V_stt(qd4[lo:lo + 64, 2 * c + hh, :], qbT[lo:lo + 64, c, :], 0.125, ET[lo:lo + 64, c, :], ALU.mult, ALU.mult,
                          [B_qk, B_E], [B_qd])
            for c in range(2):
                A_act(ET[:, c, :], bT[:, c, :], AF.Exp, [B_b], [B_E], scale=1.0 / 16)
                V_tt(kdT[:, c, :], kbT[:, c, :], ET[:, c, :], ALU.mult, [B_qk, B_E], [B_kd])
            A_act(sr, rb, AF.Sigmoid, [B_rb], [B_sr])
            G_tt(rg, rb, sr, ALU.mult, [B_rb, B_sr], [B_rg])
            rg4 = rg.rearrange("p k (h e) -> p (k h) e", e=128)
            G_tt(rg4, rg4, glag.unsqueeze(1).to_broadcast([128, 64, 128]), ALU.mult, [B_rg, B_gc], [B_rg])
            for c in range(2):
                P.op("dve", lambda e, c=c: e.memset(Sf[c], 0.0), writes=[B_Sf[c]])
            if "cs" in dbg and sq == 0:
                dma("sp", dbg["cs"][:, :, :], bT, reads=[B_b]); dma("sp", dbg["kd"][:, :, :], kdT, reads=[B_kd])
                dma("sp", dbg["qd"][:, :, :], qd4, reads=[B_qd]); dma("sp", dbg["rg"][:, :, :], rg, reads=[B_rg])
                dma("sp", dbg["la"][:, :, :], laT, reads=[B_la])
            for blk in range(16):
                t1 = blk * 128
                pst = PS[4].bitcast(BF16)
                for c in range(2):
                    transpose(pst[:, c * 128:(c + 1) * 128], kdT[:, c, t1:t1 + 128], ident_b, reads=[B_kd, B_ident], writes=[PSB[4]])
                for c in range(2):
                    V_copy(kd_ab[0:64, c, 0, :], pst[0:64, c * 128:(c + 1) * 128], [PSB[4]], [B_kab])
                    V_copy(kd_ab[64:128, c, 1, :], pst[64:128, c * 128:(c + 1) * 128], [PSB[4]], [B_kab])
                PSm = [PS[1][:, 0:256].rearrange("p (a e) -> p a e", a=2), PS[2][:, 0:256].rearrange("p (a e) -> p a e", a=2)]
                for c in range(2):
                    for ab in range(2):
                        for hh in range(2):
                            h = 2 * c + hh
                            mm(PSm[c][64 * hh:64 * hh + 64, ab, :], kd_ab[:, c, ab, 64 * hh:64 * hh + 64], vb[:, blk, h * 128:(h + 1) * 128],
                               True, True, reads=[B_kab, B_vb], writes=[PSB[1 + c]])
                PSa = PS[0].rearrange("p (h i) -> p h i", h=4)
                for h in range(4):
                    mm(PSa[:, h, :], kdT[:, h // 2, t1:t1 + 128], qd4[:, h, t1:t1 + 128], True, True,
                       reads=[B_kd, B_qd], writes=[PSB[0]])
                V_tt(att, PSa, gmask.unsqueeze(1).to_broadcast([128, 4, 128]), ALU.mult, [PSB[0], B_gc], [B_att])
                for c in range(2):
                    V_copy(Sbf[c][0], Sf[c], [B_Sf[c]], [B_Sbf[c][0]])
                    V_tt(tmpS[c], PSm[c][:, 0, :], Sf[c], ALU.add, [PSB[1 + c], B_Sf[c]], [B_tS[c]])
                    V_ts(Sf[c], tmpS[c], dec[:, c, 2 * blk:2 * blk + 1], None, ALU.mult, None, [B_tS[c], B_dec], [B_Sf[c]])
                    V_copy(Sbf[c][1], Sf[c], [B_Sf[c]], [B_Sbf[c][1]])
                    V_tt(tmpS[c], PSm[c][:, 1, :], Sf[c], ALU.add, [PSB[1 + c], B_Sf[c]], [B_tS[c]])
                    V_ts(Sf[c], tmpS[c], dec[:, c, 2 * blk + 1:2 * blk + 2], None, ALU.mult, None, [B_tS[c], B_dec], [B_Sf[c]])
                PSo = PS[3].rearrange("p (h e) -> p h e", h=4)
                for h in range(4):
                    c = h // 2
                    mm(PSo[:, h, :], att[:, h, :], vb[:, blk, h * 128:(h + 1) * 128], True, False,
                       reads=[B_att, B_vb], writes=[PSB[3]], skip=True)
                    mm(PSo[0:64, h, :], qd4[:, h, t1:t1 + 64], Sbf[c][0], False, False,
                       reads=[B_qd, B_Sbf[c][0]], writes=[PSB[3]], skip=True)
                    mm(PSo[64:128, h, :], qd4[:, h, t1 + 64:t1 + 128], Sbf[c][1], False, True,
                       reads=[B_qd, B_Sbf[c][1]], writes=[PSB[3]], skip=True)
                for h in range(4):
                    A_act(junk, PSo[:, h, :], AF.Square, [PSB[3]], [B_junk, B_ss], accum_out=ss[:, h:h + 1])
                A_act(ss[:, 4:8], ss[:, 0:4], AF.Sqrt, [B_ss], [B_ss], bias=EPS, scale=1.0 / 128)
                V_recip(ss[:, 8:12], ss[:, 4:8], [B_ss], [B_ss])
                for h in range(4):
                    V_stt(ogb[:, h * 128:(h + 1) * 128], PSo[:, h, :], ss[:, 8 + h:9 + h], rg[:, blk, h * 128:(h + 1) * 128],
                          ALU.mult, ALU.mult, [PSB[3], B_ss, B_rg], [B_ogb])
                if "ogb" in dbg and sq == 0 and blk == 0:
                    dma("sp", dbg["ogb"][:, :], ogb, reads=[B_ogb]); dma("sp", dbg["att"][:, :, :], att, reads=[B_att])
                pst2 = PS[5].bitcast(BF16)
                for fc in range(4):
                    transpose(pst2[:, fc * 128:(fc + 1) * 128], ogb[:, fc * 128:(fc + 1) * 128], ident_b,
                              reads=[B_ogb, B_ident], writes=[PSB[5]])
                evac_copy(ogT[:, :, (blk % 4) * 128:(blk % 4) * 128 + 128], pst2[:, 0:512].rearrange("p (f t) -> p f t", f=4),
                          [PSB[5]], [B_ogT])
                if blk % 4 == 3:
                    q0 = (blk // 4) * 512
                    dma("sp", s_og[:, :, tb0 + q0:tb0 + q0 + 512].rearrange("c p t -> p c t"), ogT, reads=[B_ogT])
    if "p1" in phases:
        P.fence()
        A.mark()
        try:
            if "nonsa" not in phases:
                phase1_nsa()
        except StopBuild:
            pass
        A.release()
        P.fence()
        A.mark()
        phase1_gla()
        A.release()
    def phase2():
        B_w2 = Buf("w2")
        wbn = A.alloc([128, 4, D], BF16, "wbn"); load_cast(wbn, wbn_d.rearrange("(kc p) n -> p kc n", p=128), B_w2)
        wbg = A.alloc([128, 4, D], BF16, "wbg"); load_cast(wbg, wbg_d.rearrange("(kc p) n -> p kc n", p=128), B_w2)
        wout = A.alloc([128, 8, D], BF16, "wout"); load_cast(wout, wout_d.rearrange("(kc p) n -> p kc n", p=128), B_w2)
        wxq = A.alloc([128, 8, 512], BF16, "wxq"); load_cast(wxq, wxq_d.rearrange("(kc p) n -> p kc n", p=128), B_w2)
        wxkv = A.alloc([128, 8, D], BF16, "wxkv"); load_cast(wxkv, wxkv_d.rearrange("(kc p) n -> p kc n", p=128), B_w2)
        wxo = A.alloc([128, 4, D], BF16, "wxo"); load_cast(wxo, wxo_d.rearrange("(kc p) n -> p kc n", p=128), B_w2)
        gx = A.alloc([128, 8], F32, "gx"); gm = A.alloc([128, 8], F32, "gm"); B_g = Buf("g2")
        dma("sp", gx, g_x_d[:, :], writes=[B_g]); dma("sp", gm, g_mem_d[:, :], writes=[B_g])
        xt = A.alloc([128, 4, D], F32, "xt"); B_xt = Buf("xt")
        xn = A.alloc([128, 4, D], BF16, "xn"); B_xn = Buf("xn")
        st = A.alloc([128, 32], F32, "st"); B_st = Buf("st")
        hxT = A.alloc([128, 8, 512], BF16, "hxT"); B_hx = Buf("hx")
        memt = A.alloc([128, 2, D], F32, "memt"); B_mem = Buf("mem")
        memT = A.alloc([128, 8, 256], BF16, "memT"); B_memT = Buf("memT")
        kxT = A.alloc([128, 4, 256], BF16, "kxT"); B_kx = Buf("kx")
        vxa = A.alloc([128, 2, 4, 129], BF16, "vxa"); B_vx = Buf("vx")
        G_memset(vxa[:, :, :, 128:129], 1.0, [B_vx])
        onT = A.alloc([128, 4, 512], BF16, "onT2"); ogT = A.alloc([128, 4, 512], BF16, "ogT2"); B_o = Buf("o2")
        sg = A.alloc([128, 16, 512], BF16, "sg"); B_sg = Buf("sg")
        mixT = A.alloc([128, 8, 512], BF16, "mixT"); B_mix = Buf("mix")
        tmp1 = [A.alloc([128, 512], F32, "tmp1%d" % i) for i in range(2)]; tmp2 = [A.alloc([128, 512], F32, "tmp2%d" % i) for i in range(2)]
        B_t1 = [Buf("t1%d" % i) for i in range(2)]; B_t2 = [Buf("t2%d" % i) for i in range(2)]
        qxT = A.alloc([128, 4, 512], BF16, "qxT"); B_qx = Buf("qx")
        PTx = [A.alloc([128, 512], BF16, "PTx%d" % i) for i in range(2)]; B_PTx = [Buf("PTx%d" % i) for i in range(2)]
        oxb = A.alloc([128, 4, 512], BF16, "oxb"); B_oxb = Buf("oxb")
        oxT = A.alloc([128, 4, 512], BF16, "oxT"); B_oxT = Buf("oxT")
        rd = A.alloc([128, 8], F32, "rd"); B_rd = Buf("rd")
        bk = [0]

        def bank():
            b = 2 + bk[0] % 4; bk[0] += 1
            return b

        for it in range(NTOK // 512):
            t0 = it * 512
            if it % 4 == 0:
                sq = it // 4
                dma("sp", memt, mem_d[sq * MEM:(sq + 1) * MEM, :].rearrange("(s p) d -> p s d", p=128), writes=[B_mem])
                rmsnorm_T(memt, B_mem, 2, gm, B_g, memT, B_memT, xn, B_xn, st, B_st, [0, 1])
                for hd in range(4):
                    pi = bank()
                    for kc in range(8):
                        mm(PS[pi][:, 0:256], wxkv[:, kc, hd * 128:(hd + 1) * 128], memT[:, kc, :], kc == 0, kc == 7,
                           reads=[B_w2, B_memT], writes=[PSB[pi]])
                    evac_copy(kxT[:, hd, :], PS[pi][:, 0:256], [PSB[pi]], [B_kx])
                for ms in range(2):
                    pi = bank()
                    for kc in range(8):
                        mm(PS[pi], memT[:, kc, ms * 128:(ms + 1) * 128], wxkv[:, kc, 512:1024], kc == 0, kc == 7,
                           reads=[B_w2, B_memT], writes=[PSB[pi]])
                    evac_copy(vxa[:, ms, :, 0:128], PS[pi].rearrange("p (h d) -> p h d", h=4), [PSB[pi]], [B_vx])
            dma("sp", xt, x_d[t0:t0 + 512, :].rearrange("(s p) d -> p s d", p=128), writes=[B_xt])
            dma("sp", onT, s_on[:, :, t0:t0 + 512].rearrange("c p t -> p c t"), writes=[B_o])
            dma("sp", ogT, s_og[:, :, t0:t0 + 512].rearrange("c p t -> p c t"), writes=[B_o])
            dma("sp", sg, s_fm[FM_MG:FM_MG + 16, :, t0:t0 + 512].rearrange("c p t -> p c t"), writes=[B_sg])
            for oc in range(8):
                p1 = bank(); p2 = bank()
                for kc in range(4):
                    mm(PS[p1], wbn[:, kc, oc * 128:(oc + 1) * 128], onT[:, kc, :], kc == 0, kc == 3, reads=[B_w2, B_o], writes=[PSB[p1]])
                for kc in range(4):
                    mm(PS[p2], wbg[:, kc, oc * 128:(oc + 1) * 128], ogT[:, kc, :], kc == 0, kc == 3, reads=[B_w2, B_o], writes=[PSB[p2]])
                j = oc % 2
                V_tt(tmp1[j], PS[p1], sg[:, oc, :], ALU.mult, [PSB[p1], B_sg], [B_t1[j]])
                V_tt(tmp2[j], PS[p2], sg[:, 8 + oc, :], ALU.mult, [PSB[p2], B_sg], [B_t2[j]])
                G_tt(mixT[:, oc, :], tmp1[j], tmp2[j], ALU.add, [B_t1[j], B_t2[j]], [B_mix])
            for sub in range(4):
                for half in range(2):
                    pi = bank()
                    for kc in range(8):
                        mm(PS[pi], mixT[:, kc, sub * 128:(sub + 1) * 128], wout[:, kc, half * 512:(half + 1) * 512], kc == 0, kc == 7,
                           reads=[B_w2, B_mix], writes=[PSB[pi]])
                    V_tt(xt[:, sub, half * 512:(half + 1) * 512], xt[:, sub, half * 512:(half + 1) * 512], PS[pi], ALU.add,
                         [B_xt, PSB[pi]], [B_xt])
            rmsnorm_T(xt, B_xt, 4, gx, B_g, hxT, B_hx, xn, B_xn, st, B_st, [0, 1])
            for hd in range(4):
                pi = bank()
                for kc in range(8):
                    mm(PS[pi], wxq[:, kc, hd * 128:(hd + 1) * 128], hxT[:, kc, :], kc == 0, kc == 7, reads=[B_w2, B_hx], writes=[PSB[pi]])
                evac_copy(qxT[:, hd, :], PS[pi], [PSB[pi]], [B_qx])
            for hd in range(4):
                for ms in range(2):
                    pi = bank()
                    mm(PS[pi], kxT[:, hd, ms * 128:(ms + 1) * 128], qxT[:, hd, :], True, True, reads=[B_kx, B_qx], writes=[PSB[pi]])
                    A_act(PTx[ms], PS[pi], AF.Exp, [PSB[pi]], [B_PTx[ms]], scale=128.0 ** -0.5)
                pa = [PS[6][:, 0:258].rearrange("p (s c) -> p s c", s=2), PS[7][:, 0:258].rearrange("p (s c) -> p s c", s=2)]
                for ms in range(2):
                    for sub in range(4):
                        mm(pa[sub // 2][:, sub % 2, :], PTx[ms][:, sub * 128:(sub + 1) * 128], vxa[:, ms, hd, :],
                           ms == 0 and sub % 2 == 0, ms == 1, reads=[B_PTx[ms], B_vx], writes=[PSB[6 + sub // 2]], skip=True)
                for bq in range(2):
                    V_recip(rd[:, 2 * bq:2 * bq + 2], pa[bq][:, :, 128], [PSB[6 + bq]], [B_rd])
                for sub in range(4):
                    V_ts(oxb[:, sub, hd * 128:(hd + 1) * 128], pa[sub // 2][:, sub % 2, 0:128], rd[:, sub:sub + 1], None, ALU.mult, None,
                         [PSB[6 + sub // 2], B_rd], [B_oxb])
            for fc in range(4):
                pi = bank()
                pst = PS[pi].bitcast(BF16)
                for sub in range(4):
                    transpose(pst[:, sub * 128:(sub + 1) * 128], oxb[:, sub, fc * 128:(fc + 1) * 128], ident_b,
                              reads=[B_oxb, B_ident], writes=[PSB[pi]])
                evac_copy(oxT[:, fc, :], pst[:, 0:512], [PSB[pi]], [B_oxT])
            for sub in range(4):
                for half in range(2):
                    pi = bank()
                    for kc in range(4):
                        mm(PS[pi], oxT[:, kc, sub * 128:(sub + 1) * 128], wxo[:, kc, half * 512:(half + 1) * 512], kc == 0, kc == 3,
                           reads=[B_w2, B_oxT], writes=[PSB[pi]])
                    V_tt(xt[:, sub, half * 512:(half + 1) * 512], xt[:, sub, half * 512:(half + 1) * 512], PS[pi], ALU.add,
                         [B_xt, PSB[pi]], [B_xt])
            dma("sp", s_x2[t0:t0 + 512, :].rearrange("(s p) d -> p s d", p=128), xt, reads=[B_xt])

    def phase3():
        B_w3 = Buf("w3")
        wup = A.alloc([128, 8, 2 * FFN], BF16, "wup"); load_cast(wup, wup_d.rearrange("(kc p) n -> p kc n", p=128), B_w3, nsplit=4)
        wdn = A.alloc([128, 22, D], BF16, "wdn"); load_cast(wdn, wdn_d.rearrange("(kc p) n -> p kc n", p=128), B_w3, nsplit=1)
        gf = A.alloc([128, 8], F32, "gf"); B_g = Buf("g3"); dma("sp", gf, g_ffn_d[:, :], writes=[B_g])
        cw = A.alloc([128, 3, 22], F32, "cw"); cbv = A.alloc([128, 22], F32, "cbv")
        dma("sp", cw, convw_d[:, :, :], writes=[B_g]); dma("sp", cbv, convb_d[:, :], writes=[B_g])
        gfin = A.alloc([128, D], F32, "gfin"); dma("sp", gfin, g_fin_d[:, :], writes=[B_g])
        xt = A.alloc([128, 4, D], F32, "xt"); B_xt = Buf("xt")
        xn = A.alloc([128, 4, D], BF16, "xn"); B_xn = Buf("xn")
        st = A.alloc([128, 32], F32, "st"); B_st = Buf("st")
        hfT = A.alloc([128, 8, 512], BF16, "hfT"); B_hf = Buf("hf")
        aT = A.alloc([128, 22, 512], BF16, "aT"); B_a = Buf("aT")
        usb = [A.alloc([128, 514], F32, "usb%d" % i) for i in range(2)]; B_u = [Buf("u%d" % i) for i in range(2)]
        acc = [A.alloc([128, 512], F32, "acc%d" % i) for i in range(2)]; B_acc = [Buf("acc%d" % i) for i in range(2)]
        carry = A.alloc([128, 22, 2], F32, "carry"); B_car = Buf("carry")
        bk = [0]

        def bank():
            b = 2 + bk[0] % 6; bk[0] += 1
            return b

        for it in range(NTOK // 512):
            t0 = it * 512
            dma("sp", xt, s_x2[t0:t0 + 512, :].rearrange("(s p) d -> p s d", p=128), writes=[B_xt])
            if it % 4 == 0:
                P.op("dve", lambda e: e.memset(carry, 0.0), writes=[B_car])
            rmsnorm_T(xt, B_xt, 4, gf, B_g, hfT, B_hf, xn, B_xn, st, B_st, [0, 1])
            for fcn in range(22):
                pu = bank(); pg = bank()
                for kc in range(8):
                    mm(PS[pu], wup[:, kc, fcn * 128:(fcn + 1) * 128], hfT[:, kc, :], kc == 0, kc == 7, reads=[B_w3, B_hf], writes=[PSB[pu]])
                for kc in range(8):
                    mm(PS[pg], wup[:, kc, FFN + fcn * 128:FFN + (fcn + 1) * 128], hfT[:, kc, :], kc == 0, kc == 7,
                       reads=[B_w3, B_hf], writes=[PSB[pg]])
                j = fcn % 2
                V_copy(usb[j][:, 0:2], carry[:, fcn, :], [B_car], [B_u[j]])
                A_act(usb[j][:, 2:514], PS[pu], AF.Copy, [PSB[pu]], [B_u[j]])
                V_copy(carry[:, fcn, :], usb[j][:, 512:514], [B_u[j]], [B_car])
                V_ts(acc[j], usb[j][:, 2:514], cw[:, 2, fcn:fcn + 1], cbv[:, fcn:fcn + 1], ALU.mult, ALU.add, [B_u[j], B_g], [B_acc[j]])
                V_stt(acc[j], usb[j][:, 1:513], cw[:, 1, fcn:fcn + 1], acc[j], ALU.mult, ALU.add, [B_u[j], B_g, B_acc[j]], [B_acc[j]])
                V_stt(acc[j], usb[j][:, 0:512], cw[:, 0, fcn:fcn + 1], acc[j], ALU.mult, ALU.add, [B_u[j], B_g, B_acc[j]], [B_acc[j]])
                A_act(acc[j], acc[j], AF.Gelu_apprx_tanh, [B_acc[j]], [B_acc[j]])
                V_tt(aT[:, fcn, :], PS[pg], acc[j], ALU.mult, [PSB[pg], B_acc[j]], [B_a])
            for sub in range(4):
                for half in range(2):
                    pi = bank()
                    for kc in range(22):
                        mm(PS[pi], aT[:, kc, sub * 128:(sub + 1) * 128], wdn[:, kc, half * 512:(half + 1) * 512], kc == 0, kc == 21,
                           reads=[B_w3, B_a], writes=[PSB[pi]])
                    V_tt(xt[:, sub, half * 512:(half + 1) * 512], xt[:, sub, half * 512:(half + 1) * 512], PS[pi], ALU.add,
                         [B_xt, PSB[pi]], [B_xt])
            if "aT" in dbg and it == 0:
                dma("sp", dbg["aT"][:, :, :], aT, reads=[B_a]); dma("sp", dbg["x3"][:, :, :], xt, reads=[B_xt])
                dma("sp", dbg["hf"][:, :, :], hfT, reads=[B_hf])
            for s in range(4):
                A_act(xn[:, s, :], xt[:, s, :], AF.Square, [B_xt], [B_xn, B_st], accum_out=st[:, s:s + 1])
            A_act(st[:, 8:12], st[:, 0:4], AF.Sqrt, [B_st], [B_st], bias=EPS, scale=1.0 / D)
            V_recip(st[:, 16:20], st[:, 8:12], [B_st], [B_st])
            for s in range(4):
                V_stt(xt[:, s, :], xt[:, s, :], st[:, 16 + s:17 + s], gfin, ALU.mult, ALU.mult, [B_xt, B_st, B_g], [B_xt])
            dma("sp", out_d[t0:t0 + 512, :].rearrange("(s p) d -> p s d", p=128), xt, reads=[B_xt])

    if "p2" in phases:
        P.fence(); A.mark(); phase2(); A.release()
    if "p3" in phases:
        P.fence(); A.mark(); phase3(); A.release()
    for e in ENGS:
        last = {}
        for o in P.ops[e]:
            if o.dma:
                last[id(o.token)] = o
        seen = {}
        nd = 0
        for o in P.ops[e]:
            if o.dma:
                seen[nd % NDMA_SLOTS] = o
                nd += 1
        P.final += list(seen.values())
    P.emit(nc, stack)
    stack.close()
    return nc, consts


def prep_inputs(inp):
    f = lambda a: np.ascontiguousarray(np.asarray(a, dtype=np.float32))
    w_in = f(inp["w_in"][0])
    shared = {
        "w_fm": f(w_in[:, _fm_cols()]),
        "w_tm": f(w_in[:, _tm_cols()]),
        "g_mix": pmajor(inp["ln_mix_g"][0], 8),
        "gate_b": f(np.broadcast_to(np.asarray(inp["nsa_gate_b"][0]).reshape(1, 24), (128, 24))),
        "w1k": f(inp["cmp_w1_k"][0]), "w1v": f(inp["cmp_w1_v"][0]),
        "w2k": f(np.concatenate([inp["cmp_w2_k"][0], inp["cmp_w2_k"][0]], axis=1)),
        "w2v": f(inp["cmp_w2_v"][0]),
        "pek": f(np.asarray(inp["cmp_pos_k"][0]).T), "pev": f(np.asarray(inp["cmp_pos_v"][0]).T),
        "wa2": f(np.concatenate([inp["gla_w_alpha2"][0], np.asarray(inp["gla_b_alpha"][0]).reshape(1, 256)], axis=0)),
        "gla_g": f(np.broadcast_to(np.asarray(inp["gla_norm_g"][0]).reshape(1, 128), (128, 128))),
        "ba": pmajor(inp["gla_b_alpha"][0], 2),
        "wbn": f(inp["w_branch_nsa"][0]), "wbg": f(inp["w_branch_gla"][0]), "wout": f(inp["w_out"][0]),
        "g_x": pmajor(inp["ln_x_g"][0], 8), "g_mem": pmajor(inp["ln_mem_g"][0], 8),
        "wxq": f(inp["w_xq"][0]), "wxkv": f(inp["w_xkv"][0]), "wxo": f(inp["w_xo"][0]),
        "g_ffn": pmajor(inp["ln_ffn_g"][0], 8),
        "wup": f(inp["w_up"][0]), "wdn": f(inp["w_down"][0]),
        "convw": f(np.asarray(inp["conv_w"][0]).reshape(3, 22, 128).transpose(2, 0, 1)),
        "convb": f(np.asarray(inp["conv_b"][0]).reshape(22, 128).T),
        "g_fin": f(np.broadcast_to(np.asarray(inp["ln_final_g"]).reshape(1, D), (128, D))),
    }
    for k, v in host_consts().items():
        shared["c_" + k] = v
    x = np.asarray(inp["x"], dtype=np.float32)
    mem = np.asarray(inp["mem"], dtype=np.float32)
    maps = []
    for c in range(NCORES):
        m = dict(shared)
        m["x"] = np.ascontiguousarray(x[c * NSEQ:(c + 1) * NSEQ].reshape(NTOK, D))
        m["mem"] = np.ascontiguousarray(mem[c * NSEQ:(c + 1) * NSEQ].reshape(NSEQ * MEM, D))
        maps.append(m)
    return maps


_CACHE = {}


def kernel(**inputs):
    if "nc" not in _CACHE:
        _CACHE["nc"] = build_program()[0]
    nc = _CACHE["nc"]
    maps = prep_inputs(inputs)
    res = run_bass_kernel_spmd(nc, maps, core_ids=list(range(NCORES)))
    out = np.stack([np.asarray(r["out"]).reshape(NSEQ, SEQ, D) for r in res.results], axis=0)
    return out.reshape(NCORES * NSEQ, SEQ, D).astype(np.float32)
```

```python
import numpy as np
import concourse.bass as bass
import concourse.mybir as mybir
from concourse.bass_utils import run_bass_kernel_spmd

F32 = mybir.dt.float32
BF16 = mybir.dt.bfloat16
U8 = mybir.dt.uint8
AF = mybir.ActivationFunctionType
ALU = mybir.AluOpType
AX = mybir.AxisListType

NCORES = 8
SEQ = 2048
D = 1024
NSEQ = 4
NTOK = NSEQ * SEQ
MEM = 256
FFN = 2816
NEG = -30000.0
EPS = 1e-6

STAGE = [99]


class StopBuild(Exception):
    pass


def stage(n):
    if STAGE[0] == n:
        raise StopBuild()


DEBUG = {}


class Buf:
    __slots__ = ("name", "last_w", "rd_eng", "rd_dma")

    def __init__(self, name):
        self.name = name
        self.last_w = None
        self.rd_eng = {}
        self.rd_dma = []


class Op:
    __slots__ = ("eng", "fn", "dma", "deps", "signal", "token", "prev_slot")

    def __init__(self, eng, fn, dma):
        self.eng = eng
        self.fn = fn
        self.dma = dma
        self.deps = []
        self.signal = dma
        self.token = None
        self.prev_slot = None


ENGS = ("pe", "act", "dve", "pool", "sp")
NDMA_SLOTS = 8


class Prog:
    def __init__(self):
        self.ops = {e: [] for e in ENGS}
        self.final = []
        self.fence_deps = []

    def fence(self):
        deps = []
        for e in ENGS:
            last = None
            for o in reversed(self.ops[e]):
                if not o.dma:
                    last = o
                    break
            if last is not None:
                last.signal = True
                deps.append(last)
            nd = 0
            slots = {}
            for o in self.ops[e]:
                if o.dma:
                    slots[nd % NDMA_SLOTS] = o
                    nd += 1
            deps += list(slots.values())
        self.fence_deps = deps

    def op(self, eng, fn, reads=(), writes=(), dma=False):
        o = Op(eng, fn, dma)
        raw = set()
        other = set()
        for b in reads:
            if b.last_w is not None:
                raw.add(b.last_w)
        for b in writes:
            if b.last_w is not None:
                other.add(b.last_w)
            other.update(b.rd_eng.values())
            other.update(b.rd_dma)
        for d in raw | other:
            if d is o:
                continue
            same = (not dma) and (not d.dma) and d.eng == eng
            if same and (eng == "pe" or d not in raw):
                continue
            o.deps.append(d)
            d.signal = True
        for d in self.fence_deps:
            if (not dma) and (not d.dma) and d.eng == eng:
                continue
            if d not in o.deps:
                o.deps.append(d)
        for b in reads:
            if dma:
                b.rd_dma.append(o)
            else:
                b.rd_eng[eng] = o
        for b in writes:
            b.last_w = o
            b.rd_eng = {}
            b.rd_dma = []
        self.ops[eng].append(o)
        return o

    def emit(self, nc, stack):
        sems = {e: stack.enter_context(nc.semaphore("s_" + e)) for e in ENGS}
        dsem = {e: [stack.enter_context(nc.semaphore("d_%s%d" % (e, i))) for i in range(NDMA_SLOTS)]
                for e in ("sp", "pool", "act")}
        for e in ENGS:
            cnt = 0
            nd = 0
            slot_cnt = [0] * NDMA_SLOTS
            slot_last = [None] * NDMA_SLOTS
            for o in self.ops[e]:
                if o.dma:
                    s = nd % NDMA_SLOTS
                    nd += 1
                    slot_cnt[s] += 16
                    o.prev_slot = slot_last[s]
                    o.token = (dsem[e][s], slot_cnt[s])
                    slot_last[s] = o
                elif o.signal:
                    cnt += 1
                    o.token = (sems[e], cnt)
        block = stack.enter_context(nc.Block())
        prog = self

        def body(e):
            def run(eng):
                waited = {}

                def wait(tok):
                    sem, val = tok
                    k = id(sem)
                    if waited.get(k, 0) >= val:
                        return
                    eng.wait_ge(sem, val)
                    waited[k] = val

                for o in prog.ops[e]:
                    if o.dma and o.prev_slot is not None:
                        wait(o.prev_slot.token)
                    for d in o.deps:
                        wait(d.token)
                    ins = o.fn(eng)
                    if o.token is not None:
                        ins.then_inc(o.token[0], 16 if o.dma else 1)
                if e == "sp":
                    for o in prog.final:
                        wait(o.token)
            return run

        block.tensor(body("pe"))
        block.scalar(body("act"))
        block.vector(body("dve"))
        block.gpsimd(body("pool"))
        block.sync(body("sp"))


class Arena:
    def __init__(self, ap, size):
        self.ap = ap
        self.size = size
        self.off = 0
        self.marks = []

    def alloc(self, shape, dtype, name="t"):
        esz = 4 if dtype == F32 else 2
        n = 1
        for s in shape[1:]:
            n *= s
        nbytes = (n * esz + 31) // 32 * 32
        assert self.off + nbytes <= self.size, (name, self.off, nbytes, self.size)
        a = self.ap[0:shape[0], self.off:self.off + n * esz].bitcast(dtype)
        self.off += nbytes
        if len(shape) == 3:
            a = a.rearrange("p (a b) -> p a b", a=shape[1])
        elif len(shape) == 4:
            a = a.rearrange("p (a b c) -> p a b c", a=shape[1], b=shape[2])
        return a

    def mark(self):
        self.marks.append(self.off)

    def release(self):
        self.off = self.marks.pop()


SLOPES = [2.0 ** (-(h + 1)) for h in range(8)]

C_QA, C_KC, C_VC, C_KS, C_VS, C_KW, C_VW = 0, 512, 640, 768, 896, 1024, 1152
C_GATE, C_QB, C_KB, C_VB, C_RB, C_AL, C_MG = 1280, 1304, 1560, 1816, 2328, 2840, 2856
FM_QA, FM_KC, FM_VC, FM_KS, FM_KW, FM_QB, FM_KB, FM_MG = 0, 4, 5, 6, 8, 10, 12, 14
NFM = 30
TM_VS, TM_VW, TM_KB, TM_VB, TM_RB = 0, 128, 256, 512, 1024
NTM = 1536


def _fm_cols():
    cols = []
    for c in range(4):
        cols += list(range(C_QA + 128 * c, C_QA + 128 * (c + 1)))
    cols += list(range(C_KC, C_KC + 128))
    cols += list(range(C_VC, C_VC + 128))
    for base in (C_KS, C_KW):
        for g in range(2):
            one = list(range(base + 64 * g, base + 64 * (g + 1)))
            cols += one + one
    cols += list(range(C_QB, C_QB + 256))
    cols += list(range(C_KB, C_KB + 256))
    cols += list(range(C_MG, C_MG + 2048))
    cols += list(range(C_AL, C_AL + 16))
    return np.array(cols)


def _tm_cols():
    cols = list(range(C_VS, C_VS + 128)) + list(range(C_VW, C_VW + 128))
    cols += list(range(C_KB, C_KB + 256)) + list(range(C_VB, C_VB + 512)) + list(range(C_RB, C_RB + 512))
    cols += list(range(C_GATE, C_GATE + 24))
    return np.array(cols)


def pmajor(v, nchunk):
    return np.ascontiguousarray(np.asarray(v, np.float32).reshape(nchunk, 128).T)


def host_consts():
    c = {}
    t = np.arange(SEQ)
    n = np.arange(127)
    dist = t[None, :] - (16 * n[:, None] + 31)
    c["cmpD"] = np.where(dist >= 0, -dist, -1.0e6).astype(np.float32)
    ov = np.zeros((127, 32), np.float32)
    for nn in range(127):
        for p in range(32):
            ov[nn, (16 * nn + p) // 64] += 1.0 / 32
    c["ovl"] = ov
    cur = (t // 64)
    j = np.arange(32)
    forced = (j[None, :] == 0) | (j[None, :] == cur[:, None]) | (j[None, :] == cur[:, None] - 1)
    future = j[None, :] > cur[:, None]
    mul = np.where(forced | future, 0.0, 1.0).astype(np.float32)
    add = np.where(forced, 5.0, np.where(future, -1.0, 0.0)).astype(np.float32)
    c["fmul"] = np.ascontiguousarray(mul.reshape(16, 128, 32).transpose(1, 0, 2))
    c["fadd"] = np.ascontiguousarray(add.reshape(16, 128, 32).transpose(1, 0, 2))
    c["tb"] = (64.0 * (cur[None, :] - j[:, None])).astype(np.float32)
    ea = np.zeros((34, SEQ), np.float32)
    ea[t // 64, t] = 1.0
    ea[32] = t % 64
    ea[33] = 1.0
    c["ea"] = ea
    cr = np.zeros((2, 8, 512), np.float32)
    rq = np.arange(512) % 64
    for h in range(8):
        cr[0, h] = SLOPES[h]
        cr[1, h] = -SLOPES[h] * rq
    c["crow"] = cr
    k = np.arange(128)
    cc = np.arange(896)
    c["cb"] = np.where(cc[None, :] - 384 >= k[:, None], 0.0, NEG).astype(np.float32)
    cc = np.arange(1152)
    dd = cc[None, :] - 384 - k[:, None]
    wb = np.zeros((128, 8, 1152), np.float32)
    for h in range(8):
        wb[:, h, :] = np.where((dd >= 0) & (dd < 256), -SLOPES[h] * dd, NEG)
    c["wb"] = wb
    c["ident"] = np.eye(128, dtype=np.float32)
    jj = np.arange(128)
    same = (jj[:, None] // 64) == (jj[None, :] // 64)
    c["gmask"] = (same & (jj[:, None] <= jj[None, :])).astype(np.float32)
    c["srst"] = np.broadcast_to(np.where(t % 64 == 0, 0.0, 1.0).astype(np.float32), (128, SEQ)).copy()
    c["gup"] = (same & (jj[:, None] > jj[None, :])).astype(np.float32)
    return c


CONST_SHAPES = None


def build_program(phases=("p0", "p1", "p2", "p3")):
    import contextlib
    nc = bass.Bass("TRN2", target_bir_lowering=False)
    P = Prog()
    stack = contextlib.ExitStack()

    def din(name, shape, dt=F32):
        return nc.dram_tensor(name, list(shape), dt, kind="ExternalInput").ap()

    def dscr(name, shape, dt):
        kind = "ExternalOutput" if DEBUG.get(name) else "Internal"
        return nc.dram_tensor(name, list(shape), dt, kind=kind).ap()

    consts = host_consts()
    x_d = din("x", [NTOK, D])
    mem_d = din("mem", [NSEQ * MEM, D])
    wfm_d = din("w_fm", [D, NFM * 128 + 16])
    wtm_d = din("w_tm", [D, NTM + 24])
    g_mix_d = din("g_mix", [128, 8])
    gate_b_d = din("gate_b", [128, 24])
    w1k_d = din("w1k", [2048, 128]); w1v_d = din("w1v", [2048, 128])
    w2k_d = din("w2k", [128, 128]); w2v_d = din("w2v", [128, 64])
    pek_d = din("pek", [64, 32]); pev_d = din("pev", [64, 32])
    wa2_d = din("wa2", [17, 256])
    gla_g_d = din("gla_g", [128, 128])
    ba_d = din("ba", [128, 2])
    wbn_d = din("wbn", [512, D]); wbg_d = din("wbg", [512, D]); wout_d = din("wout", [D, D])
    g_x_d = din("g_x", [128, 8]); g_mem_d = din("g_mem", [128, 8])
    wxq_d = din("wxq", [D, 512]); wxkv_d = din("wxkv", [D, 1024]); wxo_d = din("wxo", [512, D])
    g_ffn_d = din("g_ffn", [128, 8])
    wup_d = din("wup", [D, 2 * FFN]); wdn_d = din("wdn", [FFN, D])
    convw_d = din("convw", [128, 3, 22]); convb_d = din("convb", [128, 22])
    g_fin_d = din("g_fin", [128, D])
    cd = {k: din("c_" + k, v.shape) for k, v in consts.items()}
    out_d = nc.dram_tensor("out", [NTOK, D], F32, kind="ExternalOutput").ap()
    s_fm = dscr("s_fm", [NFM, 128, NTOK], BF16)
    s_al = dscr("s_al", [16, NTOK], F32)
    s_tm = dscr("s_tm", [NTOK, NTM], BF16)
    s_gate = dscr("s_gate", [NTOK, 24], F32)
    s_on = dscr("s_on", [4, 128, NTOK], BF16)
    s_og = dscr("s_og", [4, 128, NTOK], BF16)
    s_x2 = dscr("s_x2", [NTOK, D], F32)
    dbg = {}
    if DEBUG.get("gla"):
        dbg["cs"] = nc.dram_tensor("dbg_cs", [128, 2, SEQ], F32, kind="ExternalOutput").ap()
        dbg["kd"] = nc.dram_tensor("dbg_kd", [128, 2, SEQ], BF16, kind="ExternalOutput").ap()
        dbg["qd"] = nc.dram_tensor("dbg_qd", [128, 4, SEQ], BF16, kind="ExternalOutput").ap()
        dbg["rg"] = nc.dram_tensor("dbg_rg", [128, 16, 512], BF16, kind="ExternalOutput").ap()
        dbg["ogb"] = nc.dram_tensor("dbg_ogb", [128, 512], BF16, kind="ExternalOutput").ap()
        dbg["att"] = nc.dram_tensor("dbg_att", [128, 4, 128], BF16, kind="ExternalOutput").ap()
        dbg["la"] = nc.dram_tensor("dbg_la", [128, 2, SEQ], F32, kind="ExternalOutput").ap()
    if DEBUG.get("ffn"):
        dbg["aT"] = nc.dram_tensor("dbg_aT", [128, 22, 512], BF16, kind="ExternalOutput").ap()
        dbg["x3"] = nc.dram_tensor("dbg_x3", [128, 4, D], F32, kind="ExternalOutput").ap()
        dbg["hf"] = nc.dram_tensor("dbg_hf", [128, 8, 512], BF16, kind="ExternalOutput").ap()

    ARENA = 204 * 1024
    arena_t = stack.enter_context(nc.sbuf_tensor("arena", [128, ARENA], U8))
    psum_t = stack.enter_context(nc.psum_tensor("psum", [128, 4096], F32))
    A = Arena(arena_t, ARENA)
    PS = [psum_t[:, 512 * i:512 * (i + 1)] for i in range(8)]
    PSB = [Buf("ps%d" % i) for i in range(8)]

    rr = {"ev": 0, "q": 0}

    def dma(q, out, in_, reads=(), writes=(), **kw):
        return P.op(q, lambda e: e.dma_start(out=out, in_=in_, **kw), reads=reads, writes=writes, dma=True)

    def load_cast(dst, src, wb, nsplit=1):
        last = dst.shape[-1]
        step = (last + nsplit - 1) // nsplit
        for s0 in range(0, last, step):
            s1 = min(last, s0 + step)
            if len(dst.shape) == 2:
                dma("pool", dst[:, s0:s1], src[:, s0:s1], writes=[wb])
            else:
                dma("pool", dst[:, :, s0:s1], src[:, :, s0:s1], writes=[wb])

    def evac_copy(out, in_, reads, writes, scale=None):
        rr["ev"] += 1
        if rr["ev"] % 2 == 0:
            if scale is None:
                P.op("act", lambda e: e.activation(out=out, in_=in_, func=AF.Copy), reads=reads, writes=writes)
            else:
                P.op("act", lambda e: e.activation(out=out, in_=in_, func=AF.Copy, scale=scale), reads=reads, writes=writes)
        else:
            if scale is None:
                P.op("dve", lambda e: e.tensor_copy(out=out, in_=in_), reads=reads, writes=writes)
            else:
                P.op("dve", lambda e: e.tensor_scalar(out=out, in0=in_, scalar1=scale, scalar2=None, op0=ALU.mult),
                     reads=reads, writes=writes)

    def mm(out, lhsT, rhs, start, stop, reads, writes, skip=False):
        P.op("pe", lambda e: e.matmul(out, lhsT=lhsT, rhs=rhs, start=start, stop=stop, skip_group_check=skip),
             reads=reads, writes=writes)

    def transpose(out, in_, ident, reads, writes):
        P.op("pe", lambda e: e.transpose(out, in_, ident), reads=reads, writes=writes)

    ident_f = A.alloc([128, 128], F32, "ident_f")
    ident_b = A.alloc([128, 128], BF16, "ident_b")
    B_ident = Buf("ident")
    dma("sp", ident_f, cd["ident"][:, :], writes=[B_ident])
    dma("pool", ident_b, cd["ident"][:, :], writes=[B_ident])

    def rmsnorm_T(xt, B_xt, nsub, g_sb, B_g, hT, B_hT, xn, B_xn, st, B_st, psA):
        for s in range(nsub):
            P.op("act", lambda e, s=s: e.activation(out=xn[:, s, :], in_=xt[:, s, :], func=AF.Square,
                                                    accum_out=st[:, s:s + 1]),
                 reads=[B_xt], writes=[B_xn, B_st])
        P.op("act", lambda e: e.activation(out=st[:, 8:8 + nsub], in_=st[:, 0:nsub], func=AF.Sqrt, bias=EPS,
                                           scale=1.0 / D), reads=[B_st], writes=[B_st])
        P.op("dve", lambda e: e.reciprocal(out=st[:, 16:16 + nsub], in_=st[:, 8:8 + nsub]), reads=[B_st], writes=[B_st])
        for s in range(nsub):
            P.op("dve", lambda e, s=s: e.tensor_scalar(out=xn[:, s, :], in0=xt[:, s, :], scalar1=st[:, 16 + s:17 + s],
                                                       scalar2=None, op0=ALU.mult),
                 reads=[B_xt, B_st], writes=[B_xn])
        for kc in range(8):
            pi = psA[kc % len(psA)]
            pst = PS[pi].bitcast(BF16)
            for s in range(nsub):
                transpose(pst[:, s * 128:(s + 1) * 128], xn[:, s, kc * 128:(kc + 1) * 128], ident_b,
                          reads=[B_xn, B_ident], writes=[PSB[pi]])
            evac_copy(hT[:, kc, 0:nsub * 128], pst[:, 0:nsub * 128], reads=[PSB[pi], B_g], writes=[B_hT],
                      scale=g_sb[:, kc:kc + 1])

    if "p0" in phases:
        A.mark()
        wfm = A.alloc([128, 8, NFM * 128 + 16], BF16, "wfm"); B_wfm = Buf("wfm")
        wtm = A.alloc([128, 8, NTM + 24], BF16, "wtm"); B_wtm = Buf("wtm")
        load_cast(wfm, wfm_d.rearrange("(kc p) n -> p kc n", p=128), B_wfm, nsplit=4)
        load_cast(wtm, wtm_d.rearrange("(kc p) n -> p kc n", p=128), B_wtm, nsplit=2)
        gmix = A.alloc([128, 8], F32, "gmix"); B_gmix = Buf("gmix")
        dma("sp", gmix, g_mix_d[:, :], writes=[B_gmix])
        xt = A.alloc([128, 4, D], F32, "xt"); B_xt = Buf("xt")
        xn = A.alloc([128, 4, D], BF16, "xn"); B_xn = Buf("xn")
        st = A.alloc([128, 32], F32, "st"); B_st = Buf("st")
        hTs = [A.alloc([128, 8, 512], BF16, "hT%d" % i) for i in range(2)]
        B_hTs = [Buf("hT%d" % i) for i in range(2)]
        fmo = A.alloc([128, NFM, 512], BF16, "fmo")
        B_fmo = [Buf("fmo%d" % i) for i in range(3)]
        alo = A.alloc([16, 512], F32, "alo"); B_alo = Buf("alo")
        tmo = A.alloc([128, 4, NTM], BF16, "tmo"); B_tmo = Buf("tmo")
        gto = A.alloc([128, 4, 24], F32, "gto"); B_gto = Buf("gto")
        mmbank = [2, 3, 4, 5, 6, 7]
        bi = 0
        for it in range(NTOK // 512):
            t0 = it * 512
            hT = hTs[it % 2]; B_hT = B_hTs[it % 2]
            dma("sp", xt, x_d[t0:t0 + 512, :].rearrange("(s p) d -> p s d", p=128), writes=[B_xt])
            rmsnorm_T(xt, B_xt, 4, gmix, B_gmix, hT, B_hT, xn, B_xn, st, B_st, [0, 1])
            for c in range(NFM + 1):
                M = 128 if c < NFM else 16
                pi = mmbank[bi % 6]; bi += 1
                for kc in range(8):
                    mm(PS[pi][0:M, :], wfm[:, kc, c * 128:c * 128 + M], hT[:, kc, :], kc == 0, kc == 7,
                       reads=[B_wfm, B_hT], writes=[PSB[pi]])
                if c == NFM:
                    P.op("dve", lambda e, pi=pi: e.tensor_copy(out=alo, in_=PS[pi][0:16, :]),
                         reads=[PSB[pi]], writes=[B_alo])
                    continue
                bo = B_fmo[c // 10]
                if c < 4:
                    evac_copy(fmo[:, c, :], PS[pi], [PSB[pi]], [bo], scale=0.125)
                elif c >= FM_MG:
                    P.op("act", lambda e, c=c, pi=pi: e.activation(out=fmo[:, c, :], in_=PS[pi], func=AF.Sigmoid),
                         reads=[PSB[pi]], writes=[bo])
                else:
                    evac_copy(fmo[:, c, :], PS[pi], [PSB[pi]], [bo])
                if c % 10 == 9:
                    g0 = c - 9
                    dma("sp", s_fm[g0:g0 + 10, :, t0:t0 + 512].rearrange("c p t -> p c t"), fmo[:, g0:g0 + 10, :],
                        reads=[bo])
            dma("sp", s_al[:, t0:t0 + 512], alo, reads=[B_alo])
            for s in range(4):
                for (c0, c1) in ((0, 512), (512, 1024), (1024, 1536), (1536, 1560)):
                    pi = mmbank[bi % 6]; bi += 1
                    for kc in range(8):
                        mm(PS[pi][:, 0:c1 - c0], hT[:, kc, s * 128:(s + 1) * 128], wtm[:, kc, c0:c1], kc == 0, kc == 7,
                           reads=[B_wtm, B_hT], writes=[PSB[pi]])
                    if c0 < 1536:
                        evac_copy(tmo[:, s, c0:c1], PS[pi], [PSB[pi]], [B_tmo])
                    else:
                        evac_copy(gto[:, s, :], PS[pi][:, 0:24], [PSB[pi]], [B_gto])
            dma("sp", s_tm[t0:t0 + 512, :].rearrange("(s p) n -> p s n", p=128), tmo, reads=[B_tmo])
            dma("sp", s_gate[t0:t0 + 512, :].rearrange("(s p) n -> p s n", p=128), gto, reads=[B_gto])
        A.release()


    def V_tt(out, in0, in1, op, reads, writes):
        P.op("dve", lambda e: e.tensor_tensor(out=out, in0=in0, in1=in1, op=op), reads=reads, writes=writes)

    def V_ts(out, in0, s1, s2, op0, op1, reads, writes):
        if op1 is None:
            P.op("dve", lambda e: e.tensor_scalar(out=out, in0=in0, scalar1=s1, scalar2=None, op0=op0), reads=reads, writes=writes)
        else:
            P.op("dve", lambda e: e.tensor_scalar(out=out, in0=in0, scalar1=s1, scalar2=s2, op0=op0, op1=op1), reads=reads, writes=writes)

    def V_stt(out, in0, scalar, in1, op0, op1, reads, writes):
        P.op("dve", lambda e: e.scalar_tensor_tensor(out=out, in0=in0, scalar=scalar, in1=in1, op0=op0, op1=op1),
             reads=reads, writes=writes)

    def V_copy(out, in_, reads, writes):
        P.op("dve", lambda e: e.tensor_copy(out=out, in_=in_), reads=reads, writes=writes)

    def V_recip(out, in_, reads, writes):
        P.op("dve", lambda e: e.reciprocal(out=out, in_=in_), reads=reads, writes=writes)

    def V_max(out, in_, reads, writes):
        P.op("dve", lambda e: e.max(out=out, in_=in_), reads=reads, writes=writes)

    def A_act(out, in_, func, reads, writes, **kw):
        P.op("act", lambda e: e.activation(out=out, in_=in_, func=func, **kw), reads=reads, writes=writes)

    def G_memset(ap, val, writes):
        P.op("pool", lambda e: e.memset(ap, val), writes=writes)

    def G_tt(out, in0, in1, op, reads, writes):
        P.op("pool", lambda e: e.tensor_tensor(out=out, in0=in0, in1=in1, op=op), reads=reads, writes=writes)
    def phase1_nsa():
        B_c1 = Buf("c1")
        cmpD = A.alloc([128, SEQ], F32, "cmpD")
        dma("sp", cmpD[0:127, :], cd["cmpD"][:, :], writes=[B_c1])
        fmul = A.alloc([128, 16, 32], F32, "fmul"); fadd = A.alloc([128, 16, 32], F32, "fadd")
        dma("sp", fmul, cd["fmul"][:, :, :], writes=[B_c1]); dma("sp", fadd, cd["fadd"][:, :, :], writes=[B_c1])
        tbt = A.alloc([32, SEQ], F32, "tb"); dma("sp", tbt, cd["tb"][:, :], writes=[B_c1])
        ea = A.alloc([128, SEQ], BF16, "ea")
        G_memset(ea, 0.0, [B_c1])
        dma("pool", ea[0:34, :], cd["ea"][:, :], writes=[B_c1])
        cbt = A.alloc([128, 896], BF16, "cb"); dma("pool", cbt, cd["cb"][:, :], writes=[B_c1])
        wbt = A.alloc([128, 8, 1152], BF16, "wb"); dma("pool", wbt, cd["wb"][:, :, :], writes=[B_c1])
        MbAs = [A.alloc([128, 8, 512], BF16, "MbA%d" % i) for i in range(2)]
        B_mbs = [[Buf("mb%d_%d" % (i, h)) for h in range(8)] for i in range(2)]
        for i in range(2):
            G_memset(MbAs[i], 0.0, B_mbs[i])
            dma("pool", MbAs[i][32:34, :, :], cd["crow"][:, :, :], writes=B_mbs[i])
        W1 = {}; W2 = {}; peT = {}; cbias = {}
        B_cw = Buf("cw")
        for nm, w1d, w2d, ped in (("k", w1k_d, w2k_d, pek_d), ("v", w1v_d, w2v_d, pev_d)):
            W1[nm] = A.alloc([128, 32, 128], BF16, "w1" + nm)
            src = w1d.rearrange("(p d) h -> d p h", d=64)
            dma("pool", W1[nm][0:64, :, :], src, writes=[B_cw])
            dma("pool", W1[nm][64:128, :, :], src, writes=[B_cw])
            W2[nm] = A.alloc([128, 128 if nm == "k" else 64], BF16, "w2" + nm)
            dma("pool", W2[nm], w2d[:, :], writes=[B_cw])
            peT[nm] = A.alloc([64, 32], BF16, "pe" + nm)
            dma("pool", peT[nm], ped[:, :], writes=[B_cw])
            cbias[nm] = A.alloc([128, 1], F32, "cbias" + nm)
        B_cb = Buf("cbias")
        for nm in ("k", "v"):
            for p in range(32):
                mm(PS[7][:, 0:1], W1[nm][0:64, p, :], peT[nm][0:64, p:p + 1], p == 0, p == 31, reads=[B_cw], writes=[PSB[7]])
            V_copy(cbias[nm], PS[7][:, 0:1], [PSB[7]], [B_cb])
        gateb = A.alloc([128, 24], F32, "gateb"); dma("sp", gateb, gate_b_d[:, :], writes=[B_c1])
        stage(1)
        qa = A.alloc([128, 4, SEQ], BF16, "qa"); B_qa = Buf("qa")
        kcT = A.alloc([128, SEQ], BF16, "kcT"); vcT = A.alloc([128, SEQ], BF16, "vcT"); B_kvc = Buf("kvc")
        ksT = A.alloc([128, 4, SEQ], BF16, "ksT"); B_ks = Buf("ks")
        kwT = A.alloc([128, 4, SEQ], BF16, "kwT"); B_kw = Buf("kw")
        vsa = A.alloc([128, 16, 2, 65], BF16, "vsa"); B_vs = Buf("vs")
        vwa = A.alloc([128, 16, 2, 65], BF16, "vwa"); B_vw = Buf("vw")
        G_memset(vsa[:, :, :, 64:65], 1.0, [B_vs])
        G_memset(vwa[:, :, :, 64:65], 1.0, [B_vw])
        gsig = A.alloc([128, 16, 24], F32, "gsig"); B_gs = Buf("gs")
        hidT = A.alloc([128, 128], BF16, "hidT"); B_hid = Buf("hid")
        kcmpT = A.alloc([128, 2, 128], BF16, "kcmpT"); B_kcmp = Buf("kcmp")
        Rg = A.alloc([128, 2, 97], BF16, "Rg"); B_R = Buf("R")
        G_memset(Rg[:, :, 64:65], 1.0, [B_R])
        for g in range(2):
            dma("pool", Rg[0:127, g, 65:97], cd["ovl"][:, :], writes=[B_R])
        S_sb = A.alloc([128, 512], F32, "S_sb"); B_S = Buf("S")
        PTs = [A.alloc([128, 512], BF16, "PT%d" % i) for i in range(3)]; B_PT = [Buf("PT%d" % i) for i in range(3)]
        oaccs = [A.alloc([128, 4, 8, 64], F32, "oacc%d" % i) for i in range(2)]; B_oas = [Buf("oacc%d" % i) for i in range(2)]
        otmp = A.alloc([128, 4, 64], F32, "otmp"); B_ot = Buf("otmp")
        onb = A.alloc([128, 4, 512], BF16, "onb"); B_onb = Buf("onb")
        onT = A.alloc([128, 4, 512], BF16, "onT"); B_onT = Buf("onT")
        imp = A.alloc([128, 4, 32], F32, "imp"); B_imp = Buf("imp")
        itmp = A.alloc([128, 4, 32], F32, "itmp"); B_it = Buf("itmp")
        t8 = A.alloc([128, 4, 8], F32, "t8"); B_t8 = Buf("t8")
        selb = A.alloc([128, 4, 32], F32, "selb"); B_selb = Buf("selb")
        selT = A.alloc([32, 512], F32, "selT"); B_selT = Buf("selT")
        sm = A.alloc([128, 16], F32, "sm"); B_sm = Buf("sm")
        pt_i = [0]; sc_i = [0]; acc_i = [0]; ptc_i = [0]
        PTc = [A.alloc([128, 512], BF16, "PTc%d" % i) for i in range(2)]; B_PTc = [Buf("PTc%d" % i) for i in range(2)]

        def pv_evac(pacc, pb, qt, h, br, first, par):
            oacc = oaccs[par]; B_oa = B_oas[par]
            if br == 0:
                V_ts(sm[:, 0:4], pacc[:, :, 64], 1e-30, None, ALU.max, None, [PSB[pb]], [B_sm])
                V_recip(sm[:, 4:8], sm[:, 0:4], [B_sm], [B_sm])
            else:
                V_recip(sm[:, 4:8], pacc[:, :, 64], [PSB[pb]], [B_sm])
            V_tt(sm[:, 8:12], sm[:, 4:8], gsig[:, 4 * qt:4 * qt + 4, 3 * h + br], ALU.mult, [B_sm, B_gs], [B_sm])
            rgb = sm[:, 8:12].unsqueeze(2).to_broadcast([128, 4, 64])
            if first:
                V_tt(oacc[:, :, h, :], pacc[:, :, 0:64], rgb, ALU.mult, [PSB[pb], B_sm], [B_oa])
            else:
                V_tt(otmp, pacc[:, :, 0:64], rgb, ALU.mult, [PSB[pb], B_sm], [B_ot])
                V_tt(oacc[:, :, h, :], oacc[:, :, h, :], otmp, ALU.add, [B_oa, B_ot], [B_oa])

        for sq in range(NSEQ):
            tb0 = sq * SEQ
            dma("sp", qa, s_fm[FM_QA:FM_QA + 4, :, tb0:tb0 + SEQ].rearrange("c p t -> p c t"), writes=[B_qa])
            dma("sp", kcT, s_fm[FM_KC, :, tb0:tb0 + SEQ], writes=[B_kvc])
            dma("sp", vcT, s_fm[FM_VC, :, tb0:tb0 + SEQ], writes=[B_kvc])
            for g in range(2):
                for hf in range(2):
                    dma("sp", ksT[:, 2 * g + hf, :], s_fm[FM_KS + g, :, tb0:tb0 + SEQ], writes=[B_ks])
                    dma("sp", kwT[:, 2 * g + hf, :], s_fm[FM_KW + g, :, tb0:tb0 + SEQ], writes=[B_kw])
                    zlo = 64 * (1 - hf)
                    G_memset(ksT[zlo:zlo + 64, 2 * g + hf, :], 0.0, [B_ks])
                    G_memset(kwT[zlo:zlo + 64, 2 * g + hf, :], 0.0, [B_kw])
            for g in range(2):
                dma("sp", vsa[:, :, g, 0:64],
                    s_tm[tb0:tb0 + SEQ, TM_VS + 64 * g:TM_VS + 64 * g + 64].rearrange("(kt p) d -> p kt d", p=128),
                    writes=[B_vs])
                dma("sp", vwa[:, :, g, 0:64],
                    s_tm[tb0:tb0 + SEQ, TM_VW + 64 * g:TM_VW + 64 * g + 64].rearrange("(kt p) d -> p kt d", p=128),
                    writes=[B_vw])
            dma("sp", gsig, s_gate[tb0:tb0 + SEQ, :].rearrange("(kt p) n -> p kt n", p=128), writes=[B_gs])
            V_tt(gsig, gsig, gateb.unsqueeze(1).to_broadcast([128, 16, 24]), ALU.add, [B_gs, B_c1], [B_gs])
            A_act(gsig, gsig, AF.Sigmoid, [B_gs], [B_gs])
            stage(2)
            for nm, srcT in (("k", kcT), ("v", vcT)):
                s3 = srcT.rearrange("q (n s) -> q n s", s=16)
                for g in range(2):
                    for p in range(32):
                        mm(PS[7][:, 0:127], W1[nm][64 * g:64 * g + 64, p, :], s3[64 * g:64 * g + 64, p // 16:p // 16 + 127, p % 16],
                           p == 0, p == 31, reads=[B_cw, B_kvc], writes=[PSB[7]])
                    A_act(hidT[:, 0:127], PS[7][:, 0:127], AF.Gelu_apprx_tanh, [PSB[7], B_cb], [B_hid], bias=cbias[nm])
                    if nm == "k":
                        mm(PS[6][:, 0:127], W2["k"], hidT[:, 0:127], True, True, reads=[B_cw, B_hid], writes=[PSB[6]])
                        evac_copy(kcmpT[:, g, 0:127], PS[6][:, 0:127], [PSB[6]], [B_kcmp])
                    else:
                        mm(PS[6][0:127, 0:64], hidT[:, 0:127], W2["v"], True, True, reads=[B_cw, B_hid], writes=[PSB[6]])
                        evac_copy(Rg[0:127, g, 0:64], PS[6][0:127, 0:64], [PSB[6]], [B_R])
            stage(3)
            def cmp_stages(qt):
                q0 = qt * 512
                par = qt % 2
                st_list = []
                for g in range(2):
                    for gi in range(4):
                        h = 4 * g + gi
                        hp = 64 * (h % 2)
                        box = {}

                        def s1(h=h, hp=hp, g=g, box=box):
                            pi = sc_i[0] % 3; sc_i[0] += 1
                            mm(PS[pi][0:127, :], kcmpT[hp:hp + 64, g, 0:127], qa[hp:hp + 64, h // 2, q0:q0 + 512], True, True,
                               reads=[B_kcmp, B_qa], writes=[PSB[pi]])
                            V_stt(S_sb[0:127, :], cmpD[0:127, q0:q0 + 512], SLOPES[h], PS[pi][0:127, :], ALU.mult, ALU.add,
                                  [PSB[pi], B_c1], [B_S])
                            k = ptc_i[0] % 2; ptc_i[0] += 1
                            box["k"] = k
                            A_act(PTc[k][0:127, :], S_sb[0:127, :], AF.Exp, [B_S], [B_PTc[k]])

                        def s2(h=h, g=g, gi=gi, box=box):
                            k = box["k"]
                            pu = PS[5][:, 0:388].rearrange("p (s c) -> p s c", s=4)
                            for sub in range(4):
                                mm(pu[:, sub, :], PTc[k][0:127, sub * 128:(sub + 1) * 128], Rg[0:127, g, :], True, True,
                                   reads=[B_PTc[k], B_R], writes=[PSB[5]])
                            pv_evac(pu, 5, qt, h, 0, True, par)
                            rdb = sm[:, 4:8].unsqueeze(2).to_broadcast([128, 4, 32])
                            if gi == 0:
                                V_tt(imp, pu[:, :, 65:97], rdb, ALU.mult, [PSB[5], B_sm], [B_imp])
                            else:
                                V_tt(itmp, pu[:, :, 65:97], rdb, ALU.mult, [PSB[5], B_sm], [B_it])
                                V_tt(imp, imp, itmp, ALU.add, [B_imp, B_it], [B_imp])
                            if gi == 3:
                                V_tt(imp, imp, fmul[:, 4 * qt:4 * qt + 4, :], ALU.mult, [B_imp, B_c1], [B_imp])
                                V_tt(imp, imp, fadd[:, 4 * qt:4 * qt + 4, :], ALU.add, [B_imp, B_c1], [B_imp])
                                for sub in range(4):
                                    V_max(t8[:, sub, :], imp[:, sub, :], [B_imp], [B_t8])
                                for sub in range(4):
                                    V_ts(selb[:, sub, :], imp[:, sub, :], t8[:, sub, 7:8], -NEG, ALU.is_ge, ALU.mult,
                                         [B_imp, B_t8], [B_selb])

                        st_list.append(s1)
                        st_list.append(s2)

                    def s3(g=g):
                        for sub in range(4):
                            transpose(PS[6][0:32, sub * 128:(sub + 1) * 128], selb[:, sub, :], ident_f,
                                      reads=[B_selb, B_ident], writes=[PSB[6]])
                        V_ts(selT, PS[6][0:32, :], NEG, None, ALU.add, None, [PSB[6]], [B_selT])
                        for gi in range(4):
                            h = 4 * g + gi
                            V_stt(MbAs[par][0:32, h, :], tbt[0:32, q0:q0 + 512], -SLOPES[h], selT, ALU.mult, ALU.add,
                                  [B_c1, B_selT], [B_mbs[par][h]])

                    st_list.append(s3)
                return st_list

            for fn in cmp_stages(0):
                fn()
            for qt in range(4):
                q0 = qt * 512
                par = qt % 2
                nxt = cmp_stages(qt + 1) if qt < 3 else []
                items = []
                for h in range(8):
                    g = h // 4; qc = h // 2
                    for br in (1, 2):
                        pb = 3 + acc_i[0] % 2; acc_i[0] += 1
                        pacc = PS[pb][:, 0:260].rearrange("p (s c) -> p s c", s=4)
                        if br == 1:
                            kts = list(range(0, 4 * qt + 4))
                        else:
                            kts = list(range(max(0, 4 * qt - 2), 4 * qt + 4))
                        pairs = []
                        for kt in kts:
                            off = kt * 128 - q0
                            for sub in range(4):
                                dmax = 128 * sub + 127 - off
                                dmin = 128 * sub - 127 - off
                                if dmax < 0:
                                    continue
                                if br == 2 and dmin >= 256:
                                    continue
                                pairs.append((kt, sub))
                        lastkt = {}
                        for kt, sub in pairs:
                            lastkt[sub] = kt
                        for kt in kts:
                            items.append(dict(h=h, g=g, qc=qc, br=br, kt=kt, pb=pb, pacc=pacc, pairs=pairs, lastkt=lastkt,
                                              firstkt=(kt == kts[0]), lastk=(kt == kts[-1])))

                def emit_scores(it):
                    h = it["h"]; g = it["g"]; br = it["br"]; kt = it["kt"]
                    off = kt * 128 - q0
                    pi = sc_i[0] % 3; sc_i[0] += 1
                    kT = ksT if br == 1 else kwT
                    Bk = B_ks if br == 1 else B_kw
                    mm(PS[pi], kT[:, 2 * g + (h % 2), kt * 128:(kt + 1) * 128], qa[:, it["qc"], q0:q0 + 512], True, False,
                       reads=[Bk, B_qa], writes=[PSB[pi]])
                    if br == 1:
                        diag = off >= 0
                        mm(PS[pi], ea[:, kt * 128:(kt + 1) * 128], MbAs[par][:, h, :], False, not diag,
                           reads=[B_c1, B_mbs[par][h]], writes=[PSB[pi]])
                        if diag:
                            mm(PS[pi], ident_b, cbt[:, 384 - off:384 - off + 512], False, True,
                               reads=[B_ident, B_c1], writes=[PSB[pi]])
                    else:
                        mm(PS[pi], ident_b, wbt[:, h, 384 - off:384 - off + 512], False, True,
                           reads=[B_ident, B_c1], writes=[PSB[pi]])
                    k = pt_i[0] % 3; pt_i[0] += 1
                    it["k"] = k
                    A_act(PTs[k], PS[pi], AF.Exp, [PSB[pi]], [B_PT[k]])

                def emit_pv(it):
                    h = it["h"]; g = it["g"]; br = it["br"]; kt = it["kt"]; k = it["k"]; pb = it["pb"]
                    va = vsa if br == 1 else vwa
                    Bv = B_vs if br == 1 else B_vw
                    first = it["firstkt"]
                    for sub in range(4):
                        if (kt, sub) not in it["pairs"]:
                            continue
                        mm(it["pacc"][:, sub, :], PTs[k][:, sub * 128:(sub + 1) * 128], va[:, kt, g, :], first, it["lastkt"][sub] == kt,
                           reads=[B_PT[k], Bv], writes=[PSB[pb]], skip=True)
                        first = False
                    if it["lastk"]:
                        pv_evac(it["pacc"], pb, qt, h, br, False, par)

                LA = 2
                step = max(1, len(items) // (len(nxt) + 1))
                for i in range(len(items) + LA):
                    if i < len(items):
                        emit_scores(items[i])
                    if i >= LA:
                        emit_pv(items[i - LA])
                    if nxt and i % step == step - 1:
                        nxt.pop(0)()
                while nxt:
                    nxt.pop(0)()
                oacc_p = oaccs[par]
                A_act(onb.rearrange("p a b -> p (a b)"), oacc_p.rearrange("p a h d -> p (a h d)"), AF.Copy, [B_oas[par]], [B_onb])
                for fc in range(4):
                    pst = PS[6].bitcast(BF16)
                    for sub in range(4):
                        transpose(pst[:, sub * 128:(sub + 1) * 128], onb[:, sub, fc * 128:(fc + 1) * 128], ident_b,
                                  reads=[B_onb, B_ident], writes=[PSB[6]])
                    evac_copy(onT[:, fc, :], pst[:, 0:512], [PSB[6]], [B_onT])
                dma("sp", s_on[:, :, tb0 + q0:tb0 + q0 + 512].rearrange("c p t -> p c t"), onT, reads=[B_onT])

    def phase1_gla():
        B_gc = Buf("gc")
        wa2 = A.alloc([16, 256], F32, "wa2"); dma("sp", wa2, wa2_d[0:16, :], writes=[B_gc])
        ba = A.alloc([128, 2], F32, "ba"); dma("sp", ba, ba_d[:, :], writes=[B_gc])
        nba = A.alloc([128, 2], F32, "nba"); V_ts(nba, ba, -1.0, None, ALU.mult, None, [B_gc], [B_gc])
        srst = A.alloc([128, SEQ], F32, "srst"); dma("sp", srst, cd["srst"][:, :], writes=[B_gc])
        gmask = A.alloc([128, 128], F32, "gmask"); dma("sp", gmask, cd["gmask"][:, :], writes=[B_gc])
        glag = A.alloc([128, 128], F32, "glag"); dma("sp", glag, gla_g_d[:, :], writes=[B_gc])
        alT = A.alloc([16, SEQ], F32, "alT"); B_al = Buf("al")
        qbT = A.alloc([128, 2, SEQ], BF16, "qbT"); kbT = A.alloc([128, 2, SEQ], BF16, "kbT"); B_qk = Buf("qk")
        vb = A.alloc([128, 16, 512], BF16, "vb"); B_vb = Buf("vb")
        rb = A.alloc([128, 16, 512], BF16, "rb"); B_rb = Buf("rb")
        sr = A.alloc([128, 16, 512], BF16, "sr"); B_sr = Buf("sr")
        rg = A.alloc([128, 16, 512], BF16, "rg"); B_rg = Buf("rg")
        laT = A.alloc([128, 2, SEQ], F32, "laT"); B_la = Buf("la")
        bT = A.alloc([128, 2, SEQ], F32, "bT"); B_b = Buf("b")
        ET = A.alloc([128, 2, SEQ], F32, "ET"); B_E = Buf("E")
        qd4 = A.alloc([128, 4, SEQ], BF16, "qd4"); B_qd = Buf("qd")
        kdT = A.alloc([128, 2, SEQ], BF16, "kdT"); B_kd = Buf("kd")
        dec = A.alloc([128, 2, 32], F32, "dec"); B_dec = Buf("dec")
        G_memset(qd4, 0.0, [B_qd])
        kd_ab = A.alloc([128, 2, 2, 128], BF16, "kd_ab"); B_kab = Buf("kab")
        G_memset(kd_ab, 0.0, [B_kab])
        Sf = [A.alloc([128, 128], F32, "Sf%d" % c) for c in range(2)]; B_Sf = [Buf("Sf%d" % c) for c in range(2)]
        tmpS = [A.alloc([128, 128], F32, "tS%d" % c) for c in range(2)]; B_tS = [Buf("tS%d" % c) for c in range(2)]
        Sbf = [[A.alloc([128, 128], BF16, "Sbf%d%d" % (c, a)) for a in range(2)] for c in range(2)]
        B_Sbf = [[Buf("Sbf%d%d" % (c, a)) for a in range(2)] for c in range(2)]
        att = A.alloc([128, 4, 128], BF16, "att"); B_att = Buf("att")
        ss = A.alloc([128, 16], F32, "ss"); B_ss = Buf("ss")
        junk = A.alloc([128, 128], BF16, "junk"); B_junk = Buf("junk")
        ogb = A.alloc([128, 512], BF16, "ogb"); B_ogb = Buf("ogb")
        ogT = A.alloc([128, 4, 512], BF16, "ogT"); B_ogT = Buf("ogT")
        for sq in range(NSEQ):
            tb0 = sq * SEQ
            dma("sp", alT, s_al[:, tb0:tb0 + SEQ], writes=[B_al])
            dma("sp", qbT, s_fm[FM_QB:FM_QB + 2, :, tb0:tb0 + SEQ].rearrange("c p t -> p c t"), writes=[B_qk])
            dma("sp", kbT, s_fm[FM_KB:FM_KB + 2, :, tb0:tb0 + SEQ].rearrange("c p t -> p c t"), writes=[B_qk])
            dma("sp", vb, s_tm[tb0:tb0 + SEQ, TM_VB:TM_VB + 512].rearrange("(kt p) n -> p kt n", p=128), writes=[B_vb])
            dma("sp", rb, s_tm[tb0:tb0 + SEQ, TM_RB:TM_RB + 512].rearrange("(kt p) n -> p kt n", p=128), writes=[B_rb])
            for c in range(2):
                for tt in range(4):
                    mm(PS[0], wa2[0:16, c * 128:(c + 1) * 128], alT[0:16, tt * 512:(tt + 1) * 512], True, True,
                       reads=[B_gc, B_al], writes=[PSB[0]])
                    A_act(laT[:, c, tt * 512:(tt + 1) * 512], PS[0], AF.Exp, [PSB[0], B_gc], [B_la], scale=-1.0, bias=nba[:, c:c + 1])
                A_act(laT[:, c, :], laT[:, c, :], AF.Ln, [B_la], [B_la], bias=1.0)
                P.op("dve", lambda e, c=c: e.tensor_tensor_scan(out=bT[:, c, :], data0=srst, data1=laT[:, c, :], initial=0.0,
                                                                op0=ALU.mult, op1=ALU.add), reads=[B_gc, B_la], writes=[B_b])
                A_act(ET[:, c, :], bT[:, c, :], AF.Exp, [B_b], [B_E], scale=-1.0 / 16)
                A_act(dec[:, c, :], bT[:, c, :].rearrange("p (n s) -> p n s", s=64)[:, :, 63], AF.Exp, [B_b], [B_dec], scale=-1.0 / 16)
                for hh in range(2):
                    lo = 64 * hh
                    V_stt(qd4[lo:lo + 64, 2 * c + hh, :], qbT[lo:lo + 64, c, :], 0.125, ET[lo:lo + 64, c, :], ALU.mult, ALU.mult,
                          [B_qk, B_E], [B_qd])
            for c in range(2):
                A_act(ET[:, c, :], bT[:, c, :], AF.Exp, [B_b], [B_E], scale=1.0 / 16)
                V_tt(kdT[:, c, :], kbT[:, c, :], ET[:, c, :], ALU.mult, [B_qk, B_E], [B_kd])
            A_act(sr, rb, AF.Sigmoid, [B_rb], [B_sr])
            G_tt(rg, rb, sr, ALU.mult, [B_rb, B_sr], [B_rg])
            rg4 = rg.rearrange("p k (h e) -> p (k h) e", e=128)
            G_tt(rg4, rg4, glag.unsqueeze(1).to_broadcast([128, 64, 128]), ALU.mult, [B_rg, B_gc], [B_rg])
            for c in range(2):
                P.op("dve", lambda e, c=c: e.memset(Sf[c], 0.0), writes=[B_Sf[c]])
            if "cs" in dbg and sq == 0:
                dma("sp", dbg["cs"][:, :, :], bT, reads=[B_b]); dma("sp", dbg["kd"][:, :, :], kdT, reads=[B_kd])
                dma("sp", dbg["qd"][:, :, :], qd4, reads=[B_qd]); dma("sp", dbg["rg"][:, :, :], rg, reads=[B_rg])
                dma("sp", dbg["la"][:, :, :], laT, reads=[B_la])
            for blk in range(16):
                t1 = blk * 128
                pst = PS[4].bitcast(BF16)
                for c in range(2):
                    transpose(pst[:, c * 128:(c + 1) * 128], kdT[:, c, t1:t1 + 128], ident_b, reads=[B_kd, B_ident], writes=[PSB[4]])
                for c in range(2):
                    V_copy(kd_ab[0:64, c, 0, :], pst[0:64, c * 128:(c + 1) * 128], [PSB[4]], [B_kab])
                    V_copy(kd_ab[64:128, c, 1, :], pst[64:128, c * 128:(c + 1) * 128], [PSB[4]], [B_kab])
                PSm = [PS[1][:, 0:256].rearrange("p (a e) -> p a e", a=2), PS[2][:, 0:256].rearrange("p (a e) -> p a e", a=2)]
                for c in range(2):
                    for ab in range(2):
                        for hh in range(2):
                            h = 2 * c + hh
                            mm(PSm[c][64 * hh:64 * hh + 64, ab, :], kd_ab[:, c, ab, 64 * hh:64 * hh + 64], vb[:, blk, h * 128:(h + 1) * 128],
                               True, True, reads=[B_kab, B_vb], writes=[PSB[1 + c]])
                PSa = PS[0].rearrange("p (h i) -> p h i", h=4)
                for h in range(4):
                    mm(PSa[:, h, :], kdT[:, h // 2, t1:t1 + 128], qd4[:, h, t1:t1 + 128], True, True,
                       reads=[B_kd, B_qd], writes=[PSB[0]])
                V_tt(att, PSa, gmask.unsqueeze(1).to_broadcast([128, 4, 128]), ALU.mult, [PSB[0], B_gc], [B_att])
                for c in range(2):
                    V_copy(Sbf[c][0], Sf[c], [B_Sf[c]], [B_Sbf[c][0]])
                    V_tt(tmpS[c], PSm[c][:, 0, :], Sf[c], ALU.add, [PSB[1 + c], B_Sf[c]], [B_tS[c]])
                    V_ts(Sf[c], tmpS[c], dec[:, c, 2 * blk:2 * blk + 1], None, ALU.mult, None, [B_tS[c], B_dec], [B_Sf[c]])
                    V_copy(Sbf[c][1], Sf[c], [B_Sf[c]], [B_Sbf[c][1]])
                    V_tt(tmpS[c], PSm[c][:, 1, :], Sf[c], ALU.add, [PSB[1 + c], B_Sf[c]], [B_tS[c]])
                    V_ts(Sf[c], tmpS[c], dec[:, c, 2 * blk + 1:2 * blk + 2], None, ALU.mult, None, [B_tS[c], B_dec], [B_Sf[c]])
                PSo = PS[3].rearrange("p (h e) -> p h e", h=4)
                for h in range(4):
                    c = h // 2
                    mm(PSo[:, h, :], att[:, h, :], vb[:, blk, h * 128:(h + 1) * 128], True, False,
                       reads=[B_att, B_vb], writes=[PSB[3]], skip=True)
                    mm(PSo[0:64, h, :], qd4[:, h, t1:t1 + 64], Sbf[c][0], False, False,
                       reads=[B_qd, B_Sbf[c][0]], writes=[PSB[3]], skip=True)
                    mm(PSo[64:128, h, :], qd4[:, h, t1 + 64:t1 + 128], Sbf[c][1], False, True,
                       reads=[B_qd, B_Sbf[c][1]], writes=[PSB[3]], skip=True)
                for h in range(4):
                    A_act(junk, PSo[:, h, :], AF.Square, [PSB[3]], [B_junk, B_ss], accum_out=ss[:, h:h + 1])
                A_act(ss[:, 4:8], ss[:, 0:4], AF.Sqrt, [B_ss], [B_ss], bias=EPS, scale=1.0 / 128)
                V_recip(ss[:, 8:12], ss[:, 4:8], [B_ss], [B_ss])
                for h in range(4):
                    V_stt(ogb[:, h * 128:(h + 1) * 128], PSo[:, h, :], ss[:, 8 + h:9 + h], rg[:, blk, h * 128:(h + 1) * 128],
                          ALU.mult, ALU.mult, [PSB[3], B_ss, B_rg], [B_ogb])
                if "ogb" in dbg and sq == 0 and blk == 0:
                    dma("sp", dbg["ogb"][:, :], ogb, reads=[B_ogb]); dma("sp", dbg["att"][:, :, :], att, reads=[B_att])
                pst2 = PS[5].bitcast(BF16)
                for fc in range(4):
                    transpose(pst2[:, fc * 128:(fc + 1) * 128], ogb[:, fc * 128:(fc + 1) * 128], ident_b,
                              reads=[B_ogb, B_ident], writes=[PSB[5]])
                evac_copy(ogT[:, :, (blk % 4) * 128:(blk % 4) * 128 + 128], pst2[:, 0:512].rearrange("p (f t) -> p f t", f=4),
                          [PSB[5]], [B_ogT])
                if blk % 4 == 3:
                    q0 = (blk // 4) * 512
                    dma("sp", s_og[:, :, tb0 + q0:tb0 + q0 + 512].rearrange("c p t -> p c t"), ogT, reads=[B_ogT])
    if "p1" in phases:
        P.fence()
        A.mark()
        try:
            if "nonsa" not in phases:
                phase1_nsa()
        except StopBuild:
            pass
        A.release()
        P.fence()
        A.mark()
        phase1_gla()
        A.release()
    def phase2():
        B_w2 = Buf("w2")
        wbn = A.alloc([128, 4, D], BF16, "wbn"); load_cast(wbn, wbn_d.rearrange("(kc p) n -> p kc n", p=128), B_w2)
        wbg = A.alloc([128, 4, D], BF16, "wbg"); load_cast(wbg, wbg_d.rearrange("(kc p) n -> p kc n", p=128), B_w2)
        wout = A.alloc([128, 8, D], BF16, "wout"); load_cast(wout, wout_d.rearrange("(kc p) n -> p kc n", p=128), B_w2)
        wxq = A.alloc([128, 8, 512], BF16, "wxq"); load_cast(wxq, wxq_d.rearrange("(kc p) n -> p kc n", p=128), B_w2)
        wxkv = A.alloc([128, 8, D], BF16, "wxkv"); load_cast(wxkv, wxkv_d.rearrange("(kc p) n -> p kc n", p=128), B_w2)
        wxo = A.alloc([128, 4, D], BF16, "wxo"); load_cast(wxo, wxo_d.rearrange("(kc p) n -> p kc n", p=128), B_w2)
        gx = A.alloc([128, 8], F32, "gx"); gm = A.alloc([128, 8], F32, "gm"); B_g = Buf("g2")
        dma("sp", gx, g_x_d[:, :], writes=[B_g]); dma("sp", gm, g_mem_d[:, :], writes=[B_g])
        xt = A.alloc([128, 4, D], F32, "xt"); B_xt = Buf("xt")
        xn = A.alloc([128, 4, D], BF16, "xn"); B_xn = Buf("xn")
        st = A.alloc([128, 32], F32, "st"); B_st = Buf("st")
        hxT = A.alloc([128, 8, 512], BF16, "hxT"); B_hx = Buf("hx")
        memt = A.alloc([128, 2, D], F32, "memt"); B_mem = Buf("mem")
        memT = A.alloc([128, 8, 256], BF16, "memT"); B_memT = Buf("memT")
        kxT = A.alloc([128, 4, 256], BF16, "kxT"); B_kx = Buf("kx")
        vxa = A.alloc([128, 2, 4, 129], BF16, "vxa"); B_vx = Buf("vx")
        G_memset(vxa[:, :, :, 128:129], 1.0, [B_vx])
        onT = A.alloc([128, 4, 512], BF16, "onT2"); ogT = A.alloc([128, 4, 512], BF16, "ogT2"); B_o = Buf("o2")
        sg = A.alloc([128, 16, 512], BF16, "sg"); B_sg = Buf("sg")
        mixT = A.alloc([128, 8, 512], BF16, "mixT"); B_mix = Buf("mix")
        tmp1 = [A.alloc([128, 512], F32, "tmp1%d" % i) for i in range(2)]; tmp2 = [A.alloc([128, 512], F32, "tmp2%d" % i) for i in range(2)]
        B_t1 = [Buf("t1%d" % i) for i in range(2)]; B_t2 = [Buf("t2%d" % i) for i in range(2)]
        qxT = A.alloc([128, 4, 512], BF16, "qxT"); B_qx = Buf("qx")
        PTx = [A.alloc([128, 512], BF16, "PTx%d" % i) for i in range(2)]; B_PTx = [Buf("PTx%d" % i) for i in range(2)]
        oxb = A.alloc([128, 4, 512], BF16, "oxb"); B_oxb = Buf("oxb")
        oxT = A.alloc([128, 4, 512], BF16, "oxT"); B_oxT = Buf("oxT")
        rd = A.alloc([128, 8], F32, "rd"); B_rd = Buf("rd")
        bk = [0]

        def bank():
            b = 2 + bk[0] % 4; bk[0] += 1
            return b

        for it in range(NTOK // 512):
            t0 = it * 512
            if it % 4 == 0:
                sq = it // 4
                dma("sp", memt, mem_d[sq * MEM:(sq + 1) * MEM, :].rearrange("(s p) d -> p s d", p=128), writes=[B_mem])
                rmsnorm_T(memt, B_mem, 2, gm, B_g, memT, B_memT, xn, B_xn, st, B_st, [0, 1])
                for hd in range(4):
                    pi = bank()
                    for kc in range(8):
                        mm(PS[pi][:, 0:256], wxkv[:, kc, hd * 128:(hd + 1) * 128], memT[:, kc, :], kc == 0, kc == 7,
                           reads=[B_w2, B_memT], writes=[PSB[pi]])
                    evac_copy(kxT[:, hd, :], PS[pi][:, 0:256], [PSB[pi]], [B_kx])
                for ms in range(2):
                    pi = bank()
                    for kc in range(8):
                        mm(PS[pi], memT[:, kc, ms * 128:(ms + 1) * 128], wxkv[:, kc, 512:1024], kc == 0, kc == 7,
                           reads=[B_w2, B_memT], writes=[PSB[pi]])
                    evac_copy(vxa[:, ms, :, 0:128], PS[pi].rearrange("p (h d) -> p h d", h=4), [PSB[pi]], [B_vx])
            dma("sp", xt, x_d[t0:t0 + 512, :].rearrange("(s p) d -> p s d", p=128), writes=[B_xt])
            dma("sp", onT, s_on[:, :, t0:t0 + 512].rearrange("c p t -> p c t"), writes=[B_o])
            dma("sp", ogT, s_og[:, :, t0:t0 + 512].rearrange("c p t -> p c t"), writes=[B_o])
            dma("sp", sg, s_fm[FM_MG:FM_MG + 16, :, t0:t0 + 512].rearrange("c p t -> p c t"), writes=[B_sg])
            for oc in range(8):
                p1 = bank(); p2 = bank()
                for kc in range(4):
                    mm(PS[p1], wbn[:, kc, oc * 128:(oc + 1) * 128], onT[:, kc, :], kc == 0, kc == 3, reads=[B_w2, B_o], writes=[PSB[p1]])
                for kc in range(4):
                    mm(PS[p2], wbg[:, kc, oc * 128:(oc + 1) * 128], ogT[:, kc, :], kc == 0, kc == 3, reads=[B_w2, B_o], writes=[PSB[p2]])
                j = oc % 2
                V_tt(tmp1[j], PS[p1], sg[:, oc, :], ALU.mult, [PSB[p1], B_sg], [B_t1[j]])
                V_tt(tmp2[j], PS[p2], sg[:, 8 + oc, :], ALU.mult, [PSB[p2], B_sg], [B_t2[j]])
                G_tt(mixT[:, oc, :], tmp1[j], tmp2[j], ALU.add, [B_t1[j], B_t2[j]], [B_mix])
            for sub in range(4):
                for half in range(2):
                    pi = bank()
                    for kc in range(8):
                        mm(PS[pi], mixT[:, kc, sub * 128:(sub + 1) * 128], wout[:, kc, half * 512:(half + 1) * 512], kc == 0, kc == 7,
                           reads=[B_w2, B_mix], writes=[PSB[pi]])
                    V_tt(xt[:, sub, half * 512:(half + 1) * 512], xt[:, sub, half * 512:(half + 1) * 512], PS[pi], ALU.add,
                         [B_xt, PSB[pi]], [B_xt])
            rmsnorm_T(xt, B_xt, 4, gx, B_g, hxT, B_hx, xn, B_xn, st, B_st, [0, 1])
            for hd in range(4):
                pi = bank()
                for kc in range(8):
                    mm(PS[pi], wxq[:, kc, hd * 128:(hd + 1) * 128], hxT[:, kc, :], kc == 0, kc == 7, reads=[B_w2, B_hx], writes=[PSB[pi]])
                evac_copy(qxT[:, hd, :], PS[pi], [PSB[pi]], [B_qx])
            for hd in range(4):
                for ms in range(2):
                    pi = bank()
                    mm(PS[pi], kxT[:, hd, ms * 128:(ms + 1) * 128], qxT[:, hd, :], True, True, reads=[B_kx, B_qx], writes=[PSB[pi]])
                    A_act(PTx[ms], PS[pi], AF.Exp, [PSB[pi]], [B_PTx[ms]], scale=128.0 ** -0.5)
                pa = [PS[6][:, 0:258].rearrange("p (s c) -> p s c", s=2), PS[7][:, 0:258].rearrange("p (s c) -> p s c", s=2)]
                for ms in range(2):
                    for sub in range(4):
                        mm(pa[sub // 2][:, sub % 2, :], PTx[ms][:, sub * 128:(sub + 1) * 128], vxa[:, ms, hd, :],
                           ms == 0 and sub % 2 == 0, ms == 1, reads=[B_PTx[ms], B_vx], writes=[PSB[6 + sub // 2]], skip=True)
                for bq in range(2):
                    V_recip(rd[:, 2 * bq:2 * bq + 2], pa[bq][:, :, 128], [PSB[6 + bq]], [B_rd])
                for sub in range(4):
                    V_ts(oxb[:, sub, hd * 128:(hd + 1) * 128], pa[sub // 2][:, sub % 2, 0:128], rd[:, sub:sub + 1], None, ALU.mult, None,
                         [PSB[6 + sub // 2], B_rd], [B_oxb])
            for fc in range(4):
                pi = bank()
                pst = PS[pi].bitcast(BF16)
                for sub in range(4):
                    transpose(pst[:, sub * 128:(sub + 1) * 128], oxb[:, sub, fc * 128:(fc + 1) * 128], ident_b,
                              reads=[B_oxb, B_ident], writes=[PSB[pi]])
                evac_copy(oxT[:, fc, :], pst[:, 0:512], [PSB[pi]], [B_oxT])
            for sub in range(4):
                for half in range(2):
                    pi = bank()
                    for kc in range(4):
                        mm(PS[pi], oxT[:, kc, sub * 128:(sub + 1) * 128], wxo[:, kc, half * 512:(half + 1) * 512], kc == 0, kc == 3,
                           reads=[B_w2, B_oxT], writes=[PSB[pi]])
                    V_tt(xt[:, sub, half * 512:(half + 1) * 512], xt[:, sub, half * 512:(half + 1) * 512], PS[pi], ALU.add,
                         [B_xt, PSB[pi]], [B_xt])
            dma("sp", s_x2[t0:t0 + 512, :].rearrange("(s p) d -> p s d", p=128), xt, reads=[B_xt])

    def phase3():
        B_w3 = Buf("w3")
        wup = A.alloc([128, 8, 2 * FFN], BF16, "wup"); load_cast(wup, wup_d.rearrange("(kc p) n -> p kc n", p=128), B_w3, nsplit=4)
        wdn = A.alloc([128, 22, D], BF16, "wdn"); load_cast(wdn, wdn_d.rearrange("(kc p) n -> p kc n", p=128), B_w3, nsplit=1)
        gf = A.alloc([128, 8], F32, "gf"); B_g = Buf("g3"); dma("sp", gf, g_ffn_d[:, :], writes=[B_g])
        cw = A.alloc([128, 3, 22], F32, "cw"); cbv = A.alloc([128, 22], F32, "cbv")
        dma("sp", cw, convw_d[:, :, :], writes=[B_g]); dma("sp", cbv, convb_d[:, :], writes=[B_g])
        gfin = A.alloc([128, D], F32, "gfin"); dma("sp", gfin, g_fin_d[:, :], writes=[B_g])
        xt = A.alloc([128, 4, D], F32, "xt"); B_xt = Buf("xt")
        xn = A.alloc([128, 4, D], BF16, "xn"); B_xn = Buf("xn")
        st = A.alloc([128, 32], F32, "st"); B_st = Buf("st")
        hfT = A.alloc([128, 8, 512], BF16, "hfT"); B_hf = Buf("hf")
        aT = A.alloc([128, 22, 512], BF16, "aT"); B_a = Buf("aT")
        usb = [A.alloc([128, 514], F32, "usb%d" % i) for i in range(2)]; B_u = [Buf("u%d" % i) for i in range(2)]
        acc = [A.alloc([128, 512], F32, "acc%d" % i) for i in range(2)]; B_acc = [Buf("acc%d" % i) for i in range(2)]
        carry = A.alloc([128, 22, 2], F32, "carry"); B_car = Buf("carry")
        bk = [0]

        def bank():
            b = (2 + bk[0]) % 8; bk[0] += 1
            return b

        B_ac = [Buf("aT%d" % i) for i in range(22)]
        for it in range(NTOK // 512):
            t0 = it * 512
            dma("sp", xt, s_x2[t0:t0 + 512, :].rearrange("(s p) d -> p s d", p=128), writes=[B_xt])
            if it % 4 == 0:
                P.op("dve", lambda e: e.memset(carry, 0.0), writes=[B_car])
            rmsnorm_T(xt, B_xt, 4, gf, B_g, hfT, B_hf, xn, B_xn, st, B_st, [0, 1])
            for fcn in range(22):
                pu = bank(); pg = bank()
                for kc in range(8):
                    mm(PS[pu], wup[:, kc, fcn * 128:(fcn + 1) * 128], hfT[:, kc, :], kc == 0, kc == 7, reads=[B_w3, B_hf], writes=[PSB[pu]])
                for kc in range(8):
                    mm(PS[pg], wup[:, kc, FFN + fcn * 128:FFN + (fcn + 1) * 128], hfT[:, kc, :], kc == 0, kc == 7,
                       reads=[B_w3, B_hf], writes=[PSB[pg]])
                j = fcn % 2
                V_copy(usb[j][:, 0:2], carry[:, fcn, :], [B_car], [B_u[j]])
                A_act(usb[j][:, 2:514], PS[pu], AF.Copy, [PSB[pu]], [B_u[j]])
                V_copy(carry[:, fcn, :], usb[j][:, 512:514], [B_u[j]], [B_car])
                V_ts(acc[j], usb[j][:, 2:514], cw[:, 2, fcn:fcn + 1], cbv[:, fcn:fcn + 1], ALU.mult, ALU.add, [B_u[j], B_g], [B_acc[j]])
                V_stt(acc[j], usb[j][:, 1:513], cw[:, 1, fcn:fcn + 1], acc[j], ALU.mult, ALU.add, [B_u[j], B_g, B_acc[j]], [B_acc[j]])
                V_stt(acc[j], usb[j][:, 0:512], cw[:, 0, fcn:fcn + 1], acc[j], ALU.mult, ALU.add, [B_u[j], B_g, B_acc[j]], [B_acc[j]])
                A_act(acc[j], acc[j], AF.Gelu_apprx_tanh, [B_acc[j]], [B_acc[j]])
                V_tt(aT[:, fcn, :], PS[pg], acc[j], ALU.mult, [PSB[pg], B_acc[j]], [B_ac[fcn]])
            for sub in range(4):
                for half in range(2):
                    pi = bank()
                    for kc in range(22):
                        mm(PS[pi], aT[:, kc, sub * 128:(sub + 1) * 128], wdn[:, kc, half * 512:(half + 1) * 512], kc == 0, kc == 21,
                           reads=[B_w3, B_ac[kc]], writes=[PSB[pi]])
                    V_tt(xt[:, sub, half * 512:(half + 1) * 512], xt[:, sub, half * 512:(half + 1) * 512], PS[pi], ALU.add,
                         [B_xt, PSB[pi]], [B_xt])
            if "aT" in dbg and it == 0:
                dma("sp", dbg["aT"][:, :, :], aT, reads=B_ac); dma("sp", dbg["x3"][:, :, :], xt, reads=[B_xt])
                dma("sp", dbg["hf"][:, :, :], hfT, reads=[B_hf])
            for s in range(4):
                A_act(xn[:, s, :], xt[:, s, :], AF.Square, [B_xt], [B_xn, B_st], accum_out=st[:, s:s + 1])
            A_act(st[:, 8:12], st[:, 0:4], AF.Sqrt, [B_st], [B_st], bias=EPS, scale=1.0 / D)
            V_recip(st[:, 16:20], st[:, 8:12], [B_st], [B_st])
            for s in range(4):
                V_stt(xt[:, s, :], xt[:, s, :], st[:, 16 + s:17 + s], gfin, ALU.mult, ALU.mult, [B_xt, B_st, B_g], [B_xt])
            dma("sp", out_d[t0:t0 + 512, :].rearrange("(s p) d -> p s d", p=128), xt, reads=[B_xt])

    if "p2" in phases:
        P.fence(); A.mark(); phase2(); A.release()
    if "p3" in phases:
        P.fence(); A.mark(); phase3(); A.release()
    for e in ENGS:
        last = {}
        for o in P.ops[e]:
            if o.dma:
                last[id(o.token)] = o
        seen = {}
        nd = 0
        for o in P.ops[e]:
            if o.dma:
                seen[nd % NDMA_SLOTS] = o
                nd += 1
        P.final += list(seen.values())
    P.emit(nc, stack)
    stack.close()
    return nc, consts


def prep_inputs(inp):
    f = lambda a: np.ascontiguousarray(np.asarray(a, dtype=np.float32))
    w_in = f(inp["w_in"][0])
    shared = {
        "w_fm": f(w_in[:, _fm_cols()]),
        "w_tm": f(w_in[:, _tm_cols()]),
        "g_mix": pmajor(inp["ln_mix_g"][0], 8),
        "gate_b": f(np.broadcast_to(np.asarray(inp["nsa_gate_b"][0]).reshape(1, 24), (128, 24))),
        "w1k": f(inp["cmp_w1_k"][0]), "w1v": f(inp["cmp_w1_v"][0]),
        "w2k": f(np.concatenate([inp["cmp_w2_k"][0], inp["cmp_w2_k"][0]], axis=1)),
        "w2v": f(inp["cmp_w2_v"][0]),
        "pek": f(np.asarray(inp["cmp_pos_k"][0]).T), "pev": f(np.asarray(inp["cmp_pos_v"][0]).T),
        "wa2": f(np.concatenate([inp["gla_w_alpha2"][0], np.asarray(inp["gla_b_alpha"][0]).reshape(1, 256)], axis=0)),
        "gla_g": f(np.broadcast_to(np.asarray(inp["gla_norm_g"][0]).reshape(1, 128), (128, 128))),
        "ba": pmajor(inp["gla_b_alpha"][0], 2),
        "wbn": f(inp["w_branch_nsa"][0]), "wbg": f(inp["w_branch_gla"][0]), "wout": f(inp["w_out"][0]),
        "g_x": pmajor(inp["ln_x_g"][0], 8), "g_mem": pmajor(inp["ln_mem_g"][0], 8),
        "wxq": f(inp["w_xq"][0]), "wxkv": f(inp["w_xkv"][0]), "wxo": f(inp["w_xo"][0]),
        "g_ffn": pmajor(inp["ln_ffn_g"][0], 8),
        "wup": f(inp["w_up"][0]), "wdn": f(inp["w_down"][0]),
        "convw": f(np.asarray(inp["conv_w"][0]).reshape(3, 22, 128).transpose(2, 0, 1)),
        "convb": f(np.asarray(inp["conv_b"][0]).reshape(22, 128).T),
        "g_fin": f(np.broadcast_to(np.asarray(inp["ln_final_g"]).reshape(1, D), (128, D))),
    }
    for k, v in host_consts().items():
        shared["c_" + k] = v
    x = np.asarray(inp["x"], dtype=np.float32)
    mem = np.asarray(inp["mem"], dtype=np.float32)
    maps = []
    for c in range(NCORES):
        m = dict(shared)
        m["x"] = np.ascontiguousarray(x[c * NSEQ:(c + 1) * NSEQ].reshape(NTOK, D))
        m["mem"] = np.ascontiguousarray(mem[c * NSEQ:(c + 1) * NSEQ].reshape(NSEQ * MEM, D))
        maps.append(m)
    return maps


_CACHE = {}


def kernel(**inputs):
    if "nc" not in _CACHE:
        _CACHE["nc"] = build_program()[0]
    nc = _CACHE["nc"]
    maps = prep_inputs(inputs)
    res = run_bass_kernel_spmd(nc, maps, core_ids=list(range(NCORES)))
    out = np.stack([np.asarray(r["out"]).reshape(NSEQ, SEQ, D) for r in res.results], axis=0)
    return out.reshape(NCORES * NSEQ, SEQ, D).astype(np.float32)
```

```python
import numpy as np
import concourse.bass as bass
import concourse.mybir as mybir
from concourse.bass_utils import run_bass_kernel_spmd

F32 = mybir.dt.float32
BF16 = mybir.dt.bfloat16
U8 = mybir.dt.uint8
AF = mybir.ActivationFunctionType
ALU = mybir.AluOpType
AX = mybir.AxisListType

NCORES = 8
SEQ = 2048
D = 1024
NSEQ = 4
NTOK = NSEQ * SEQ
MEM = 256
FFN = 2816
NEG = -30000.0
EPS = 1e-6

STAGE = [99]


class StopBuild(Exception):
    pass


def stage(n):
    if STAGE[0] == n:
        raise StopBuild()


DEBUG = {}


class Buf:
    __slots__ = ("name", "last_w", "rd_eng", "rd_dma")

    def __init__(self, name):
        self.name = name
        self.last_w = None
        self.rd_eng = {}
        self.rd_dma = []


class Op:
    __slots__ = ("eng", "fn", "dma", "deps", "signal", "token", "prev_slot")

    def __init__(self, eng, fn, dma):
        self.eng = eng
        self.fn = fn
        self.dma = dma
        self.deps = []
        self.signal = dma
        self.token = None
        self.prev_slot = None


ENGS = ("pe", "act", "dve", "pool", "sp")
NDMA_SLOTS = 8


class Prog:
    def __init__(self):
        self.ops = {e: [] for e in ENGS}
        self.final = []
        self.fence_deps = []

    def fence(self):
        deps = []
        for e in ENGS:
            last = None
            for o in reversed(self.ops[e]):
                if not o.dma:
                    last = o
                    break
            if last is not None:
                last.signal = True
                deps.append(last)
            nd = 0
            slots = {}
            for o in self.ops[e]:
                if o.dma:
                    slots[nd % NDMA_SLOTS] = o
                    nd += 1
            deps += list(slots.values())
        self.fence_deps = deps

    def op(self, eng, fn, reads=(), writes=(), dma=False):
        o = Op(eng, fn, dma)
        raw = set()
        other = set()
        for b in reads:
            if b.last_w is not None:
                raw.add(b.last_w)
        for b in writes:
            if b.last_w is not None:
                other.add(b.last_w)
            other.update(b.rd_eng.values())
            other.update(b.rd_dma)
        for d in raw | other:
            if d is o:
                continue
            same = (not dma) and (not d.dma) and d.eng == eng
            if same and (eng == "pe" or d not in raw):
                continue
            o.deps.append(d)
            d.signal = True
        for d in self.fence_deps:
            if (not dma) and (not d.dma) and d.eng == eng:
                continue
            if d not in o.deps:
                o.deps.append(d)
        for b in reads:
            if dma:
                b.rd_dma.append(o)
            else:
                b.rd_eng[eng] = o
        for b in writes:
            b.last_w = o
            b.rd_eng = {}
            b.rd_dma = []
        self.ops[eng].append(o)
        return o

    def emit(self, nc, stack):
        sems = {e: stack.enter_context(nc.semaphore("s_" + e)) for e in ENGS}
        dsem = {e: [stack.enter_context(nc.semaphore("d_%s%d" % (e, i))) for i in range(NDMA_SLOTS)]
                for e in ("sp", "pool", "act")}
        for e in ENGS:
            cnt = 0
            nd = 0
            slot_cnt = [0] * NDMA_SLOTS
            slot_last = [None] * NDMA_SLOTS
            for o in self.ops[e]:
                if o.dma:
                    s = nd % NDMA_SLOTS
                    nd += 1
                    slot_cnt[s] += 16
                    o.prev_slot = slot_last[s]
                    o.token = (dsem[e][s], slot_cnt[s])
                    slot_last[s] = o
                elif o.signal:
                    cnt += 1
                    o.token = (sems[e], cnt)
        block = stack.enter_context(nc.Block())
        prog = self

        def body(e):
            def run(eng):
                waited = {}

                def wait(tok):
                    sem, val = tok
                    k = id(sem)
                    if waited.get(k, 0) >= val:
                        return
                    eng.wait_ge(sem, val)
                    waited[k] = val

                for o in prog.ops[e]:
                    if o.dma and o.prev_slot is not None:
                        wait(o.prev_slot.token)
                    for d in o.deps:
                        wait(d.token)
                    ins = o.fn(eng)
                    if o.token is not None:
                        ins.then_inc(o.token[0], 16 if o.dma else 1)
                if e == "sp":
                    for o in prog.final:
                        wait(o.token)
            return run

        block.tensor(body("pe"))
        block.scalar(body("act"))
        block.vector(body("dve"))
        block.gpsimd(body("pool"))
        block.sync(body("sp"))


class Arena:
    def __init__(self, ap, size):
        self.ap = ap
        self.size = size
        self.off = 0
        self.marks = []

    def alloc(self, shape, dtype, name="t"):
        esz = 4 if dtype == F32 else 2
        n = 1
        for s in shape[1:]:
            n *= s
        nbytes = (n * esz + 31) // 32 * 32
        assert self.off + nbytes <= self.size, (name, self.off, nbytes, self.size)
        a = self.ap[0:shape[0], self.off:self.off + n * esz].bitcast(dtype)
        self.off += nbytes
        if len(shape) == 3:
            a = a.rearrange("p (a b) -> p a b", a=shape[1])
        elif len(shape) == 4:
            a = a.rearrange("p (a b c) -> p a b c", a=shape[1], b=shape[2])
        return a

    def mark(self):
        self.marks.append(self.off)

    def release(self):
        self.off = self.marks.pop()


SLOPES = [2.0 ** (-(h + 1)) for h in range(8)]

C_QA, C_KC, C_VC, C_KS, C_VS, C_KW, C_VW = 0, 512, 640, 768, 896, 1024, 1152
C_GATE, C_QB, C_KB, C_VB, C_RB, C_AL, C_MG = 1280, 1304, 1560, 1816, 2328, 2840, 2856
FM_QA, FM_KC, FM_VC, FM_KS, FM_KW, FM_QB, FM_KB, FM_MG = 0, 4, 5, 6, 8, 10, 12, 14
NFM = 30
TM_VS, TM_VW, TM_KB, TM_VB, TM_RB = 0, 128, 256, 512, 1024
NTM = 1536


def _fm_cols():
    cols = []
    for c in range(4):
        cols += list(range(C_QA + 128 * c, C_QA + 128 * (c + 1)))
    cols += list(range(C_KC, C_KC + 128))
    cols += list(range(C_VC, C_VC + 128))
    for base in (C_KS, C_KW):
        for g in range(2):
            one = list(range(base + 64 * g, base + 64 * (g + 1)))
            cols += one + one
    cols += list(range(C_QB, C_QB + 256))
    cols += list(range(C_KB, C_KB + 256))
    cols += list(range(C_MG, C_MG + 2048))
    cols += list(range(C_AL, C_AL + 16))
    return np.array(cols)


def _tm_cols():
    cols = list(range(C_VS, C_VS + 128)) + list(range(C_VW, C_VW + 128))
    cols += list(range(C_KB, C_KB + 256)) + list(range(C_VB, C_VB + 512)) + list(range(C_RB, C_RB + 512))
    cols += list(range(C_GATE, C_GATE + 24))
    return np.array(cols)


def pmajor(v, nchunk):
    return np.ascontiguousarray(np.asarray(v, np.float32).reshape(nchunk, 128).T)


def host_consts():
    c = {}
    t = np.arange(SEQ)
    n = np.arange(127)
    dist = t[None, :] - (16 * n[:, None] + 31)
    c["cmpD"] = np.where(dist >= 0, -dist, -1.0e6).astype(np.float32)
    ov = np.zeros((127, 32), np.float32)
    for nn in range(127):
        for p in range(32):
            ov[nn, (16 * nn + p) // 64] += 1.0 / 32
    c["ovl"] = ov
    cur = (t // 64)
    j = np.arange(32)
    forced = (j[None, :] == 0) | (j[None, :] == cur[:, None]) | (j[None, :] == cur[:, None] - 1)
    future = j[None, :] > cur[:, None]
    mul = np.where(forced | future, 0.0, 1.0).astype(np.float32)
    add = np.where(forced, 5.0, np.where(future, -1.0, 0.0)).astype(np.float32)
    c["fmul"] = np.ascontiguousarray(mul.reshape(16, 128, 32).transpose(1, 0, 2))
    c["fadd"] = np.ascontiguousarray(add.reshape(16, 128, 32).transpose(1, 0, 2))
    c["tb"] = (64.0 * (cur[None, :] - j[:, None])).astype(np.float32)
    ea = np.zeros((34, SEQ), np.float32)
    ea[t // 64, t] = 1.0
    ea[32] = t % 64
    ea[33] = 1.0
    c["ea"] = ea
    cr = np.zeros((2, 8, 512), np.float32)
    rq = np.arange(512) % 64
    for h in range(8):
        cr[0, h] = SLOPES[h]
        cr[1, h] = -SLOPES[h] * rq
    c["crow"] = cr
    k = np.arange(128)
    cc = np.arange(896)
    c["cb"] = np.where(cc[None, :] - 384 >= k[:, None], 0.0, NEG).astype(np.float32)
    cc = np.arange(1152)
    dd = cc[None, :] - 384 - k[:, None]
    wb = np.zeros((128, 8, 1152), np.float32)
    for h in range(8):
        wb[:, h, :] = np.where((dd >= 0) & (dd < 256), -SLOPES[h] * dd, NEG)
    c["wb"] = wb
    c["ident"] = np.eye(128, dtype=np.float32)
    jj = np.arange(128)
    same = (jj[:, None] // 64) == (jj[None, :] // 64)
    c["gmask"] = (same & (jj[:, None] <= jj[None, :])).astype(np.float32)
    c["srst"] = np.broadcast_to(np.where(t % 64 == 0, 0.0, 1.0).astype(np.float32), (128, SEQ)).copy()
    c["gup"] = (same & (jj[:, None] > jj[None, :])).astype(np.float32)
    return c


CONST_SHAPES = None


def build_program(phases=("p0", "p1", "p2", "p3")):
    import contextlib
    nc = bass.Bass("TRN2", target_bir_lowering=False)
    P = Prog()
    stack = contextlib.ExitStack()

    def din(name, shape, dt=F32):
        return nc.dram_tensor(name, list(shape), dt, kind="ExternalInput").ap()

    def dscr(name, shape, dt):
        kind = "ExternalOutput" if DEBUG.get(name) else "Internal"
        return nc.dram_tensor(name, list(shape), dt, kind=kind).ap()

    consts = host_consts()
    x_d = din("x", [NTOK, D])
    mem_d = din("mem", [NSEQ * MEM, D])
    wfm_d = din("w_fm", [D, NFM * 128 + 16])
    wtm_d = din("w_tm", [D, NTM + 24])
    g_mix_d = din("g_mix", [128, 8])
    gate_b_d = din("gate_b", [128, 24])
    w1k_d = din("w1k", [2048, 128]); w1v_d = din("w1v", [2048, 128])
    w2k_d = din("w2k", [128, 128]); w2v_d = din("w2v", [128, 64])
    pek_d = din("pek", [64, 32]); pev_d = din("pev", [64, 32])
    wa2_d = din("wa2", [17, 256])
    gla_g_d = din("gla_g", [128, 128])
    ba_d = din("ba", [128, 2])
    wbn_d = din("wbn", [512, D]); wbg_d = din("wbg", [512, D]); wout_d = din("wout", [D, D])
    g_x_d = din("g_x", [128, 8]); g_mem_d = din("g_mem", [128, 8])
    wxq_d = din("wxq", [D, 512]); wxkv_d = din("wxkv", [D, 1024]); wxo_d = din("wxo", [512, D])
    g_ffn_d = din("g_ffn", [128, 8])
    wup_d = din("wup", [D, 2 * FFN]); wdn_d = din("wdn", [FFN, D])
    convw_d = din("convw", [128, 3, 22]); convb_d = din("convb", [128, 22])
    g_fin_d = din("g_fin", [128, D])
    cd = {k: din("c_" + k, v.shape) for k, v in consts.items()}
    out_d = nc.dram_tensor("out", [NTOK, D], F32, kind="ExternalOutput").ap()
    s_fm = dscr("s_fm", [NFM, 128, NTOK], BF16)
    s_al = dscr("s_al", [16, NTOK], F32)
    s_tm = dscr("s_tm", [NTOK, NTM], BF16)
    s_gate = dscr("s_gate", [NTOK, 24], F32)
    s_on = dscr("s_on", [4, 128, NTOK], BF16)
    s_og = dscr("s_og", [4, 128, NTOK], BF16)
    s_x2 = dscr("s_x2", [NTOK, D], F32)
    dbg = {}
    if DEBUG.get("gla"):
        dbg["cs"] = nc.dram_tensor("dbg_cs", [128, 2, SEQ], F32, kind="ExternalOutput").ap()
        dbg["kd"] = nc.dram_tensor("dbg_kd", [128, 2, SEQ], BF16, kind="ExternalOutput").ap()
        dbg["qd"] = nc.dram_tensor("dbg_qd", [128, 4, SEQ], BF16, kind="ExternalOutput").ap()
        dbg["rg"] = nc.dram_tensor("dbg_rg", [128, 16, 512], BF16, kind="ExternalOutput").ap()
        dbg["ogb"] = nc.dram_tensor("dbg_ogb", [128, 512], BF16, kind="ExternalOutput").ap()
        dbg["att"] = nc.dram_tensor("dbg_att", [128, 4, 128], BF16, kind="ExternalOutput").ap()
        dbg["la"] = nc.dram_tensor("dbg_la", [128, 2, SEQ], F32, kind="ExternalOutput").ap()
    if DEBUG.get("ffn"):
        dbg["aT"] = nc.dram_tensor("dbg_aT", [128, 22, 512], BF16, kind="ExternalOutput").ap()
        dbg["x3"] = nc.dram_tensor("dbg_x3", [128, 4, D], F32, kind="ExternalOutput").ap()
        dbg["hf"] = nc.dram_tensor("dbg_hf", [128, 8, 512], BF16, kind="ExternalOutput").ap()

    ARENA = 204 * 1024
    arena_t = stack.enter_context(nc.sbuf_tensor("arena", [128, ARENA], U8))
    psum_t = stack.enter_context(nc.psum_tensor("psum", [128, 4096], F32))
    A = Arena(arena_t, ARENA)
    PS = [psum_t[:, 512 * i:512 * (i + 1)] for i in range(8)]
    PSB = [Buf("ps%d" % i) for i in range(8)]

    rr = {"ev": 0, "q": 0}

    def dma(q, out, in_, reads=(), writes=(), **kw):
        return P.op(q, lambda e: e.dma_start(out=out, in_=in_, **kw), reads=reads, writes=writes, dma=True)

    def load_cast(dst, src, wb, nsplit=1):
        last = dst.shape[-1]
        step = (last + nsplit - 1) // nsplit
        for s0 in range(0, last, step):
            s1 = min(last, s0 + step)
            if len(dst.shape) == 2:
                dma("pool", dst[:, s0:s1], src[:, s0:s1], writes=[wb])
            else:
                dma("pool", dst[:, :, s0:s1], src[:, :, s0:s1], writes=[wb])

    def evac_copy(out, in_, reads, writes, scale=None):
        rr["ev"] += 1
        if rr["ev"] % 2 == 0:
            if scale is None:
                P.op("act", lambda e: e.activation(out=out, in_=in_, func=AF.Copy), reads=reads, writes=writes)
            else:
                P.op("act", lambda e: e.activation(out=out, in_=in_, func=AF.Copy, scale=scale), reads=reads, writes=writes)
        else:
            if scale is None:
                P.op("dve", lambda e: e.tensor_copy(out=out, in_=in_), reads=reads, writes=writes)
            else:
                P.op("dve", lambda e: e.tensor_scalar(out=out, in0=in_, scalar1=scale, scalar2=None, op0=ALU.mult),
                     reads=reads, writes=writes)

    def mm(out, lhsT, rhs, start, stop, reads, writes, skip=False):
        P.op("pe", lambda e: e.matmul(out, lhsT=lhsT, rhs=rhs, start=start, stop=stop, skip_group_check=skip),
             reads=reads, writes=writes)

    def transpose(out, in_, ident, reads, writes):
        P.op("pe", lambda e: e.transpose(out, in_, ident), reads=reads, writes=writes)

    ident_f = A.alloc([128, 128], F32, "ident_f")
    ident_b = A.alloc([128, 128], BF16, "ident_b")
    B_ident = Buf("ident")
    dma("sp", ident_f, cd["ident"][:, :], writes=[B_ident])
    dma("pool", ident_b, cd["ident"][:, :], writes=[B_ident])

    def rmsnorm_T(xt, B_xt, nsub, g_sb, B_g, hT, B_hT, xn, B_xn, st, B_st, psA):
        for s in range(nsub):
            P.op("act", lambda e, s=s: e.activation(out=xn[:, s, :], in_=xt[:, s, :], func=AF.Square,
                                                    accum_out=st[:, s:s + 1]),
                 reads=[B_xt], writes=[B_xn, B_st])
        P.op("act", lambda e: e.activation(out=st[:, 8:8 + nsub], in_=st[:, 0:nsub], func=AF.Sqrt, bias=EPS,
                                           scale=1.0 / D), reads=[B_st], writes=[B_st])
        P.op("dve", lambda e: e.reciprocal(out=st[:, 16:16 + nsub], in_=st[:, 8:8 + nsub]), reads=[B_st], writes=[B_st])
        for s in range(nsub):
            P.op("dve", lambda e, s=s: e.tensor_scalar(out=xn[:, s, :], in0=xt[:, s, :], scalar1=st[:, 16 + s:17 + s],
                                                       scalar2=None, op0=ALU.mult),
                 reads=[B_xt, B_st], writes=[B_xn])
        for kc in range(8):
            pi = psA[kc % len(psA)]
            pst = PS[pi].bitcast(BF16)
            for s in range(nsub):
                transpose(pst[:, s * 128:(s + 1) * 128], xn[:, s, kc * 128:(kc + 1) * 128], ident_b,
                          reads=[B_xn, B_ident], writes=[PSB[pi]])
            evac_copy(hT[:, kc, 0:nsub * 128], pst[:, 0:nsub * 128], reads=[PSB[pi], B_g], writes=[B_hT],
                      scale=g_sb[:, kc:kc + 1])

    if "p0" in phases:
        A.mark()
        wfm = A.alloc([128, 8, NFM * 128 + 16], BF16, "wfm"); B_wfm = Buf("wfm")
        wtm = A.alloc([128, 8, NTM + 24], BF16, "wtm"); B_wtm = Buf("wtm")
        load_cast(wfm, wfm_d.rearrange("(kc p) n -> p kc n", p=128), B_wfm, nsplit=4)
        load_cast(wtm, wtm_d.rearrange("(kc p) n -> p kc n", p=128), B_wtm, nsplit=2)
        gmix = A.alloc([128, 8], F32, "gmix"); B_gmix = Buf("gmix")
        dma("sp", gmix, g_mix_d[:, :], writes=[B_gmix])
        xt = A.alloc([128, 4, D], F32, "xt"); B_xt = Buf("xt")
        xn = A.alloc([128, 4, D], BF16, "xn"); B_xn = Buf("xn")
        st = A.alloc([128, 32], F32, "st"); B_st = Buf("st")
        hTs = [A.alloc([128, 8, 512], BF16, "hT%d" % i) for i in range(2)]
        B_hTs = [Buf("hT%d" % i) for i in range(2)]
        fmo = A.alloc([128, NFM, 512], BF16, "fmo")
        B_fmo = [Buf("fmo%d" % i) for i in range(3)]
        alo = A.alloc([16, 512], F32, "alo"); B_alo = Buf("alo")
        tmo = A.alloc([128, 4, NTM], BF16, "tmo"); B_tmo = Buf("tmo")
        gto = A.alloc([128, 4, 24], F32, "gto"); B_gto = Buf("gto")
        mmbank = [2, 3, 4, 5, 6, 7]
        bi = 0
        for it in range(NTOK // 512):
            t0 = it * 512
            hT = hTs[it % 2]; B_hT = B_hTs[it % 2]
            dma("sp", xt, x_d[t0:t0 + 512, :].rearrange("(s p) d -> p s d", p=128), writes=[B_xt])
            rmsnorm_T(xt, B_xt, 4, gmix, B_gmix, hT, B_hT, xn, B_xn, st, B_st, [0, 1])
            for c in range(NFM + 1):
                M = 128 if c < NFM else 16
                pi = mmbank[bi % 6]; bi += 1
                for kc in range(8):
                    mm(PS[pi][0:M, :], wfm[:, kc, c * 128:c * 128 + M], hT[:, kc, :], kc == 0, kc == 7,
                       reads=[B_wfm, B_hT], writes=[PSB[pi]])
                if c == NFM:
                    P.op("dve", lambda e, pi=pi: e.tensor_copy(out=alo, in_=PS[pi][0:16, :]),
                         reads=[PSB[pi]], writes=[B_alo])
                    continue
                bo = B_fmo[c // 10]
                if c < 4:
                    evac_copy(fmo[:, c, :], PS[pi], [PSB[pi]], [bo], scale=0.125)
                elif c >= FM_MG:
                    P.op("act", lambda e, c=c, pi=pi: e.activation(out=fmo[:, c, :], in_=PS[pi], func=AF.Sigmoid),
                         reads=[PSB[pi]], writes=[bo])
                else:
                    evac_copy(fmo[:, c, :], PS[pi], [PSB[pi]], [bo])
                if c % 10 == 9:
                    g0 = c - 9
                    dma("sp", s_fm[g0:g0 + 10, :, t0:t0 + 512].rearrange("c p t -> p c t"), fmo[:, g0:g0 + 10, :],
                        reads=[bo])
            dma("sp", s_al[:, t0:t0 + 512], alo, reads=[B_alo])
            for s in range(4):
                for (c0, c1) in ((0, 512), (512, 1024), (1024, 1536), (1536, 1560)):
                    pi = mmbank[bi % 6]; bi += 1
                    for kc in range(8):
                        mm(PS[pi][:, 0:c1 - c0], hT[:, kc, s * 128:(s + 1) * 128], wtm[:, kc, c0:c1], kc == 0, kc == 7,
                           reads=[B_wtm, B_hT], writes=[PSB[pi]])
                    if c0 < 1536:
                        evac_copy(tmo[:, s, c0:c1], PS[pi], [PSB[pi]], [B_tmo])
                    else:
                        evac_copy(gto[:, s, :], PS[pi][:, 0:24], [PSB[pi]], [B_gto])
            dma("sp", s_tm[t0:t0 + 512, :].rearrange("(s p) n -> p s n", p=128), tmo, reads=[B_tmo])
            dma("sp", s_gate[t0:t0 + 512, :].rearrange("(s p) n -> p s n", p=128), gto, reads=[B_gto])
        A.release()


    def V_tt(out, in0, in1, op, reads, writes):
        P.op("dve", lambda e: e.tensor_tensor(out=out, in0=in0, in1=in1, op=op), reads=reads, writes=writes)

    def V_ts(out, in0, s1, s2, op0, op1, reads, writes):
        if op1 is None:
            P.op("dve", lambda e: e.tensor_scalar(out=out, in0=in0, scalar1=s1, scalar2=None, op0=op0), reads=reads, writes=writes)
        else:
            P.op("dve", lambda e: e.tensor_scalar(out=out, in0=in0, scalar1=s1, scalar2=s2, op0=op0, op1=op1), reads=reads, writes=writes)

    def V_stt(out, in0, scalar, in1, op0, op1, reads, writes):
        P.op("dve", lambda e: e.scalar_tensor_tensor(out=out, in0=in0, scalar=scalar, in1=in1, op0=op0, op1=op1),
             reads=reads, writes=writes)

    def V_copy(out, in_, reads, writes):
        P.op("dve", lambda e: e.tensor_copy(out=out, in_=in_), reads=reads, writes=writes)

    def V_recip(out, in_, reads, writes):
        P.op("dve", lambda e: e.reciprocal(out=out, in_=in_), reads=reads, writes=writes)

    def V_max(out, in_, reads, writes):
        P.op("dve", lambda e: e.max(out=out, in_=in_), reads=reads, writes=writes)

    def A_act(out, in_, func, reads, writes, **kw):
        P.op("act", lambda e: e.activation(out=out, in_=in_, func=func, **kw), reads=reads, writes=writes)

    def G_memset(ap, val, writes):
        P.op("pool", lambda e: e.memset(ap, val), writes=writes)

    def G_tt(out, in0, in1, op, reads, writes):
        P.op("pool", lambda e: e.tensor_tensor(out=out, in0=in0, in1=in1, op=op), reads=reads, writes=writes)
    def phase1_nsa():
        B_c1 = Buf("c1")
        cmpD = A.alloc([128, SEQ], F32, "cmpD")
        dma("sp", cmpD[0:127, :], cd["cmpD"][:, :], writes=[B_c1])
        fmul = A.alloc([128, 16, 32], F32, "fmul"); fadd = A.alloc([128, 16, 32], F32, "fadd")
        dma("sp", fmul, cd["fmul"][:, :, :], writes=[B_c1]); dma("sp", fadd, cd["fadd"][:, :, :], writes=[B_c1])
        tbt = A.alloc([32, SEQ], F32, "tb"); dma("sp", tbt, cd["tb"][:, :], writes=[B_c1])
        ea = A.alloc([128, SEQ], BF16, "ea")
        G_memset(ea, 0.0, [B_c1])
        dma("pool", ea[0:34, :], cd["ea"][:, :], writes=[B_c1])
        cbt = A.alloc([128, 896], BF16, "cb"); dma("pool", cbt, cd["cb"][:, :], writes=[B_c1])
        wbt = A.alloc([128, 8, 1152], BF16, "wb"); dma("pool", wbt, cd["wb"][:, :, :], writes=[B_c1])
        MbAs = [A.alloc([128, 8, 512], BF16, "MbA%d" % i) for i in range(2)]
        B_mbs = [[Buf("mb%d_%d" % (i, h)) for h in range(8)] for i in range(2)]
        for i in range(2):
            G_memset(MbAs[i], 0.0, B_mbs[i])
            dma("pool", MbAs[i][32:34, :, :], cd["crow"][:, :, :], writes=B_mbs[i])
        W1 = {}; W2 = {}; peT = {}; cbias = {}
        B_cw = Buf("cw")
        for nm, w1d, w2d, ped in (("k", w1k_d, w2k_d, pek_d), ("v", w1v_d, w2v_d, pev_d)):
            W1[nm] = A.alloc([128, 32, 128], BF16, "w1" + nm)
            src = w1d.rearrange("(p d) h -> d p h", d=64)
            dma("pool", W1[nm][0:64, :, :], src, writes=[B_cw])
            dma("pool", W1[nm][64:128, :, :], src, writes=[B_cw])
            W2[nm] = A.alloc([128, 128 if nm == "k" else 64], BF16, "w2" + nm)
            dma("pool", W2[nm], w2d[:, :], writes=[B_cw])
            peT[nm] = A.alloc([64, 32], BF16, "pe" + nm)
            dma("pool", peT[nm], ped[:, :], writes=[B_cw])
            cbias[nm] = A.alloc([128, 1], F32, "cbias" + nm)
        B_cb = Buf("cbias")
        for nm in ("k", "v"):
            for p in range(32):
                mm(PS[7][:, 0:1], W1[nm][0:64, p, :], peT[nm][0:64, p:p + 1], p == 0, p == 31, reads=[B_cw], writes=[PSB[7]])
            V_copy(cbias[nm], PS[7][:, 0:1], [PSB[7]], [B_cb])
        gateb = A.alloc([128, 24], F32, "gateb"); dma("sp", gateb, gate_b_d[:, :], writes=[B_c1])
        stage(1)
        qa = A.alloc([128, 4, SEQ], BF16, "qa"); B_qa = Buf("qa")
        kcT = A.alloc([128, SEQ], BF16, "kcT"); vcT = A.alloc([128, SEQ], BF16, "vcT"); B_kvc = Buf("kvc")
        ksT = A.alloc([128, 4, SEQ], BF16, "ksT"); B_ks = Buf("ks")
        kwT = A.alloc([128, 4, SEQ], BF16, "kwT"); B_kw = Buf("kw")
        vsa = A.alloc([128, 16, 2, 65], BF16, "vsa"); B_vs = Buf("vs")
        vwa = A.alloc([128, 16, 2, 65], BF16, "vwa"); B_vw = Buf("vw")
        G_memset(vsa[:, :, :, 64:65], 1.0, [B_vs])
        G_memset(vwa[:, :, :, 64:65], 1.0, [B_vw])
        gsig = A.alloc([128, 16, 24], F32, "gsig"); B_gs = Buf("gs")
        hidT = A.alloc([128, 128], BF16, "hidT"); B_hid = Buf("hid")
        kcmpT = A.alloc([128, 2, 128], BF16, "kcmpT"); B_kcmp = Buf("kcmp")
        Rg = A.alloc([128, 2, 97], BF16, "Rg"); B_R = Buf("R")
        G_memset(Rg[:, :, 64:65], 1.0, [B_R])
        for g in range(2):
            dma("pool", Rg[0:127, g, 65:97], cd["ovl"][:, :], writes=[B_R])
        S_sb = A.alloc([128, 512], F32, "S_sb"); B_S = Buf("S")
        PTs = [A.alloc([128, 512], BF16, "PT%d" % i) for i in range(3)]; B_PT = [Buf("PT%d" % i) for i in range(3)]
        oaccs = [A.alloc([128, 4, 8, 64], F32, "oacc%d" % i) for i in range(2)]; B_oas = [Buf("oacc%d" % i) for i in range(2)]
        otmp = A.alloc([128, 4, 64], F32, "otmp"); B_ot = Buf("otmp")
        onb = A.alloc([128, 4, 512], BF16, "onb"); B_onb = Buf("onb")
        onT = A.alloc([128, 4, 512], BF16, "onT"); B_onT = Buf("onT")
        imp = A.alloc([128, 4, 32], F32, "imp"); B_imp = Buf("imp")
        itmp = A.alloc([128, 4, 32], F32, "itmp"); B_it = Buf("itmp")
        t8 = A.alloc([128, 4, 8], F32, "t8"); B_t8 = Buf("t8")
        selb = A.alloc([128, 4, 32], F32, "selb"); B_selb = Buf("selb")
        selT = A.alloc([32, 512], F32, "selT"); B_selT = Buf("selT")
        sm = A.alloc([128, 16], F32, "sm"); B_sm = Buf("sm")
        pt_i = [0]; sc_i = [0]; acc_i = [0]; ptc_i = [0]
        PTc = [A.alloc([128, 512], BF16, "PTc%d" % i) for i in range(2)]; B_PTc = [Buf("PTc%d" % i) for i in range(2)]

        def pv_evac(pacc, pb, qt, h, br, first, par):
            oacc = oaccs[par]; B_oa = B_oas[par]
            if br == 0:
                V_ts(sm[:, 0:4], pacc[:, :, 64], 1e-30, None, ALU.max, None, [PSB[pb]], [B_sm])
                V_recip(sm[:, 4:8], sm[:, 0:4], [B_sm], [B_sm])
            else:
                V_recip(sm[:, 4:8], pacc[:, :, 64], [PSB[pb]], [B_sm])
            V_tt(sm[:, 8:12], sm[:, 4:8], gsig[:, 4 * qt:4 * qt + 4, 3 * h + br], ALU.mult, [B_sm, B_gs], [B_sm])
            rgb = sm[:, 8:12].unsqueeze(2).to_broadcast([128, 4, 64])
            if first:
                V_tt(oacc[:, :, h, :], pacc[:, :, 0:64], rgb, ALU.mult, [PSB[pb], B_sm], [B_oa])
            else:
                V_tt(otmp, pacc[:, :, 0:64], rgb, ALU.mult, [PSB[pb], B_sm], [B_ot])
                V_tt(oacc[:, :, h, :], oacc[:, :, h, :], otmp, ALU.add, [B_oa, B_ot], [B_oa])

        for sq in range(NSEQ):
            tb0 = sq * SEQ
            dma("sp", qa, s_fm[FM_QA:FM_QA + 4, :, tb0:tb0 + SEQ].rearrange("c p t -> p c t"), writes=[B_qa])
            dma("sp", kcT, s_fm[FM_KC, :, tb0:tb0 + SEQ], writes=[B_kvc])
            dma("sp", vcT, s_fm[FM_VC, :, tb0:tb0 + SEQ], writes=[B_kvc])
            for g in range(2):
                for hf in range(2):
                    dma("sp", ksT[:, 2 * g + hf, :], s_fm[FM_KS + g, :, tb0:tb0 + SEQ], writes=[B_ks])
                    dma("sp", kwT[:, 2 * g + hf, :], s_fm[FM_KW + g, :, tb0:tb0 + SEQ], writes=[B_kw])
                    zlo = 64 * (1 - hf)
                    G_memset(ksT[zlo:zlo + 64, 2 * g + hf, :], 0.0, [B_ks])
                    G_memset(kwT[zlo:zlo + 64, 2 * g + hf, :], 0.0, [B_kw])
            for g in range(2):
                dma("sp", vsa[:, :, g, 0:64],
                    s_tm[tb0:tb0 + SEQ, TM_VS + 64 * g:TM_VS + 64 * g + 64].rearrange("(kt p) d -> p kt d", p=128),
                    writes=[B_vs])
                dma("sp", vwa[:, :, g, 0:64],
                    s_tm[tb0:tb0 + SEQ, TM_VW + 64 * g:TM_VW + 64 * g + 64].rearrange("(kt p) d -> p kt d", p=128),
                    writes=[B_vw])
            dma("sp", gsig, s_gate[tb0:tb0 + SEQ, :].rearrange("(kt p) n -> p kt n", p=128), writes=[B_gs])
            V_tt(gsig, gsig, gateb.unsqueeze(1).to_broadcast([128, 16, 24]), ALU.add, [B_gs, B_c1], [B_gs])
            A_act(gsig, gsig, AF.Sigmoid, [B_gs], [B_gs])
            stage(2)
            for nm, srcT in (("k", kcT), ("v", vcT)):
                s3 = srcT.rearrange("q (n s) -> q n s", s=16)
                for g in range(2):
                    for p in range(32):
                        mm(PS[7][:, 0:127], W1[nm][64 * g:64 * g + 64, p, :], s3[64 * g:64 * g + 64, p // 16:p // 16 + 127, p % 16],
                           p == 0, p == 31, reads=[B_cw, B_kvc], writes=[PSB[7]])
                    A_act(hidT[:, 0:127], PS[7][:, 0:127], AF.Gelu_apprx_tanh, [PSB[7], B_cb], [B_hid], bias=cbias[nm])
                    if nm == "k":
                        mm(PS[6][:, 0:127], W2["k"], hidT[:, 0:127], True, True, reads=[B_cw, B_hid], writes=[PSB[6]])
                        evac_copy(kcmpT[:, g, 0:127], PS[6][:, 0:127], [PSB[6]], [B_kcmp])
                    else:
                        mm(PS[6][0:127, 0:64], hidT[:, 0:127], W2["v"], True, True, reads=[B_cw, B_hid], writes=[PSB[6]])
                        evac_copy(Rg[0:127, g, 0:64], PS[6][0:127, 0:64], [PSB[6]], [B_R])
            stage(3)
            def cmp_stages(qt):
                q0 = qt * 512
                par = qt % 2
                st_list = []
                for g in range(2):
                    for gi in range(4):
                        h = 4 * g + gi
                        hp = 64 * (h % 2)
                        box = {}

                        def s1(h=h, hp=hp, g=g, box=box):
                            pi = sc_i[0] % 3; sc_i[0] += 1
                            mm(PS[pi][0:127, :], kcmpT[hp:hp + 64, g, 0:127], qa[hp:hp + 64, h // 2, q0:q0 + 512], True, True,
                               reads=[B_kcmp, B_qa], writes=[PSB[pi]])
                            V_stt(S_sb[0:127, :], cmpD[0:127, q0:q0 + 512], SLOPES[h], PS[pi][0:127, :], ALU.mult, ALU.add,
                                  [PSB[pi], B_c1], [B_S])
                            k = ptc_i[0] % 2; ptc_i[0] += 1
                            box["k"] = k
                            A_act(PTc[k][0:127, :], S_sb[0:127, :], AF.Exp, [B_S], [B_PTc[k]])

                        def s2(h=h, g=g, gi=gi, box=box):
                            k = box["k"]
                            pu = PS[5][:, 0:388].rearrange("p (s c) -> p s c", s=4)
                            for sub in range(4):
                                mm(pu[:, sub, :], PTc[k][0:127, sub * 128:(sub + 1) * 128], Rg[0:127, g, :], True, True,
                                   reads=[B_PTc[k], B_R], writes=[PSB[5]])
                            pv_evac(pu, 5, qt, h, 0, True, par)
                            rdb = sm[:, 4:8].unsqueeze(2).to_broadcast([128, 4, 32])
                            if gi == 0:
                                V_tt(imp, pu[:, :, 65:97], rdb, ALU.mult, [PSB[5], B_sm], [B_imp])
                            else:
                                V_tt(itmp, pu[:, :, 65:97], rdb, ALU.mult, [PSB[5], B_sm], [B_it])
                                V_tt(imp, imp, itmp, ALU.add, [B_imp, B_it], [B_imp])
                            if gi == 3:
                                V_tt(imp, imp, fmul[:, 4 * qt:4 * qt + 4, :], ALU.mult, [B_imp, B_c1], [B_imp])
                                V_tt(imp, imp, fadd[:, 4 * qt:4 * qt + 4, :], ALU.add, [B_imp, B_c1], [B_imp])
                                for sub in range(4):
                                    V_max(t8[:, sub, :], imp[:, sub, :], [B_imp], [B_t8])
                                for sub in range(4):
                                    V_ts(selb[:, sub, :], imp[:, sub, :], t8[:, sub, 7:8], -NEG, ALU.is_ge, ALU.mult,
                                         [B_imp, B_t8], [B_selb])

                        st_list.append(s1)
                        st_list.append(s2)

                    def s3(g=g):
                        for sub in range(4):
                            transpose(PS[6][0:32, sub * 128:(sub + 1) * 128], selb[:, sub, :], ident_f,
                                      reads=[B_selb, B_ident], writes=[PSB[6]])
                        V_ts(selT, PS[6][0:32, :], NEG, None, ALU.add, None, [PSB[6]], [B_selT])
                        for gi in range(4):
                            h = 4 * g + gi
                            V_stt(MbAs[par][0:32, h, :], tbt[0:32, q0:q0 + 512], -SLOPES[h], selT, ALU.mult, ALU.add,
                                  [B_c1, B_selT], [B_mbs[par][h]])

                    st_list.append(s3)
                return st_list

            for fn in cmp_stages(0):
                fn()
            for qt in range(4):
                q0 = qt * 512
                par = qt % 2
                nxt = cmp_stages(qt + 1) if qt < 3 else []
                items = []
                for h in range(8):
                    g = h // 4; qc = h // 2
                    for br in (1, 2):
                        pb = 3 + acc_i[0] % 2; acc_i[0] += 1
                        pacc = PS[pb][:, 0:260].rearrange("p (s c) -> p s c", s=4)
                        if br == 1:
                            kts = list(range(0, 4 * qt + 4))
                        else:
                            kts = list(range(max(0, 4 * qt - 2), 4 * qt + 4))
                        pairs = []
                        for kt in kts:
                            off = kt * 128 - q0
                            for sub in range(4):
                                dmax = 128 * sub + 127 - off
                                dmin = 128 * sub - 127 - off
                                if dmax < 0:
                                    continue
                                if br == 2 and dmin >= 256:
                                    continue
                                pairs.append((kt, sub))
                        lastkt = {}
                        for kt, sub in pairs:
                            lastkt[sub] = kt
                        for kt in kts:
                            items.append(dict(h=h, g=g, qc=qc, br=br, kt=kt, pb=pb, pacc=pacc, pairs=pairs, lastkt=lastkt,
                                              firstkt=(kt == kts[0]), lastk=(kt == kts[-1])))

                def emit_scores(it):
                    h = it["h"]; g = it["g"]; br = it["br"]; kt = it["kt"]
                    off = kt * 128 - q0
                    pi = sc_i[0] % 3; sc_i[0] += 1
                    kT = ksT if br == 1 else kwT
                    Bk = B_ks if br == 1 else B_kw
                    mm(PS[pi], kT[:, 2 * g + (h % 2), kt * 128:(kt + 1) * 128], qa[:, it["qc"], q0:q0 + 512], True, False,
                       reads=[Bk, B_qa], writes=[PSB[pi]])
                    if br == 1:
                        diag = off >= 0
                        mm(PS[pi], ea[:, kt * 128:(kt + 1) * 128], MbAs[par][:, h, :], False, not diag,
                           reads=[B_c1, B_mbs[par][h]], writes=[PSB[pi]])
                        if diag:
                            mm(PS[pi], ident_b, cbt[:, 384 - off:384 - off + 512], False, True,
                               reads=[B_ident, B_c1], writes=[PSB[pi]])
                    else:
                        mm(PS[pi], ident_b, wbt[:, h, 384 - off:384 - off + 512], False, True,
                           reads=[B_ident, B_c1], writes=[PSB[pi]])
                    k = pt_i[0] % 3; pt_i[0] += 1
                    it["k"] = k
                    A_act(PTs[k], PS[pi], AF.Exp, [PSB[pi]], [B_PT[k]])

                def emit_pv(it):
                    h = it["h"]; g = it["g"]; br = it["br"]; kt = it["kt"]; k = it["k"]; pb = it["pb"]
                    va = vsa if br == 1 else vwa
                    Bv = B_vs if br == 1 else B_vw
                    first = it["firstkt"]
                    for sub in range(4):
                        if (kt, sub) not in it["pairs"]:
                            continue
                        mm(it["pacc"][:, sub, :], PTs[k][:, sub * 128:(sub + 1) * 128], va[:, kt, g, :], first, it["lastkt"][sub] == kt,
                           reads=[B_PT[k], Bv], writes=[PSB[pb]], skip=True)
                        first = False
                    if it["lastk"]:
                        pv_evac(it["pacc"], pb, qt, h, br, False, par)

                LA = 2
                step = max(1, len(items) // (len(nxt) + 1))
                for i in range(len(items) + LA):
                    if i < len(items):
                        emit_scores(items[i])
                    if i >= LA:
                        emit_pv(items[i - LA])
                    if nxt and i % step == step - 1:
                        nxt.pop(0)()
                while nxt:
                    nxt.pop(0)()
                oacc_p = oaccs[par]
                A_act(onb.rearrange("p a b -> p (a b)"), oacc_p.rearrange("p a h d -> p (a h d)"), AF.Copy, [B_oas[par]], [B_onb])
                for fc in range(4):
                    pst = PS[6].bitcast(BF16)
                    for sub in range(4):
                        transpose(pst[:, sub * 128:(sub + 1) * 128], onb[:, sub, fc * 128:(fc + 1) * 128], ident_b,
                                  reads=[B_onb, B_ident], writes=[PSB[6]])
                    evac_copy(onT[:, fc, :], pst[:, 0:512], [PSB[6]], [B_onT])
                dma("sp", s_on[:, :, tb0 + q0:tb0 + q0 + 512].rearrange("c p t -> p c t"), onT, reads=[B_onT])

    def phase1_gla():
        B_gc = Buf("gc")
        wa2 = A.alloc([16, 256], F32, "wa2"); dma("sp", wa2, wa2_d[0:16, :], writes=[B_gc])
        ba = A.alloc([128, 2], F32, "ba"); dma("sp", ba, ba_d[:, :], writes=[B_gc])
        nba = A.alloc([128, 2], F32, "nba"); V_ts(nba, ba, -1.0, None, ALU.mult, None, [B_gc], [B_gc])
        srst = A.alloc([128, SEQ], F32, "srst"); dma("sp", srst, cd["srst"][:, :], writes=[B_gc])
        gmask = A.alloc([128, 128], F32, "gmask"); dma("sp", gmask, cd["gmask"][:, :], writes=[B_gc])
        glag = A.alloc([128, 128], F32, "glag"); dma("sp", glag, gla_g_d[:, :], writes=[B_gc])
        alT = A.alloc([16, SEQ], F32, "alT"); B_al = Buf("al")
        qbT = A.alloc([128, 2, SEQ], BF16, "qbT"); kbT = A.alloc([128, 2, SEQ], BF16, "kbT"); B_qk = Buf("qk")
        vb = A.alloc([128, 16, 512], BF16, "vb"); B_vb = Buf("vb")
        rb = A.alloc([128, 16, 512], BF16, "rb"); B_rb = Buf("rb")
        sr = A.alloc([128, 16, 512], BF16, "sr"); B_sr = Buf("sr")
        rg = A.alloc([128, 16, 512], BF16, "rg"); B_rg = Buf("rg")
        laT = A.alloc([128, 2, SEQ], F32, "laT"); B_la = Buf("la")
        bT = A.alloc([128, 2, SEQ], F32, "bT"); B_b = Buf("b")
        ET = A.alloc([128, 2, SEQ], F32, "ET"); B_E = Buf("E")
        qd4 = A.alloc([128, 4, SEQ], BF16, "qd4"); B_qd = Buf("qd")
        kdT = A.alloc([128, 2, SEQ], BF16, "kdT"); B_kd = Buf("kd")
        dec = A.alloc([128, 2, 32], F32, "dec"); B_dec = Buf("dec")
        G_memset(qd4, 0.0, [B_qd])
        kd_ab = A.alloc([128, 2, 2, 128], BF16, "kd_ab"); B_kab = Buf("kab")
        G_memset(kd_ab, 0.0, [B_kab])
        Sf = [A.alloc([128, 128], F32, "Sf%d" % c) for c in range(2)]; B_Sf = [Buf("Sf%d" % c) for c in range(2)]
        tmpS = [A.alloc([128, 128], F32, "tS%d" % c) for c in range(2)]; B_tS = [Buf("tS%d" % c) for c in range(2)]
        Sbf = [[A.alloc([128, 128], BF16, "Sbf%d%d" % (c, a)) for a in range(2)] for c in range(2)]
        B_Sbf = [[Buf("Sbf%d%d" % (c, a)) for a in range(2)] for c in range(2)]
        att = A.alloc([128, 4, 128], BF16, "att"); B_att = Buf("att")
        ss = A.alloc([128, 16], F32, "ss"); B_ss = Buf("ss")
        junk = A.alloc([128, 128], BF16, "junk"); B_junk = Buf("junk")
        ogb = A.alloc([128, 512], BF16, "ogb"); B_ogb = Buf("ogb")
        ogT = A.alloc([128, 4, 512], BF16, "ogT"); B_ogT = Buf("ogT")
        for sq in range(NSEQ):
            tb0 = sq * SEQ
            dma("sp", alT, s_al[:, tb0:tb0 + SEQ], writes=[B_al])
            dma("sp", qbT, s_fm[FM_QB:FM_QB + 2, :, tb0:tb0 + SEQ].rearrange("c p t -> p c t"), writes=[B_qk])
            dma("sp", kbT, s_fm[FM_KB:FM_KB + 2, :, tb0:tb0 + SEQ].rearrange("c p t -> p c t"), writes=[B_qk])
            dma("sp", vb, s_tm[tb0:tb0 + SEQ, TM_VB:TM_VB + 512].rearrange("(kt p) n -> p kt n", p=128), writes=[B_vb])
            dma("sp", rb, s_tm[tb0:tb0 + SEQ, TM_RB:TM_RB + 512].rearrange("(kt p) n -> p kt n", p=128), writes=[B_rb])
            for c in range(2):
                for tt in range(4):
                    mm(PS[0], wa2[0:16, c * 128:(c + 1) * 128], alT[0:16, tt * 512:(tt + 1) * 512], True, True,
                       reads=[B_gc, B_al], writes=[PSB[0]])
                    A_act(laT[:, c, tt * 512:(tt + 1) * 512], PS[0], AF.Exp, [PSB[0], B_gc], [B_la], scale=-1.0, bias=nba[:, c:c + 1])
                A_act(laT[:, c, :], laT[:, c, :], AF.Ln, [B_la], [B_la], bias=1.0)
                P.op("dve", lambda e, c=c: e.tensor_tensor_scan(out=bT[:, c, :], data0=srst, data1=laT[:, c, :], initial=0.0,
                                                                op0=ALU.mult, op1=ALU.add), reads=[B_gc, B_la], writes=[B_b])
                A_act(ET[:, c, :], bT[:, c, :], AF.Exp, [B_b], [B_E], scale=-1.0 / 16)
                A_act(dec[:, c, :], bT[:, c, :].rearrange("p (n s) -> p n s", s=64)[:, :, 63], AF.Exp, [B_b], [B_dec], scale=-1.0 / 16)
                for hh in range(2):
                    lo = 64 * hh
                    V_stt(qd4[lo:lo + 64, 2 * c + hh, :], qbT[lo:lo + 64, c, :], 0.125, ET[lo:lo + 64, c, :], ALU.mult, ALU.mult,
                          [B_qk, B_E], [B_qd])
            for c in range(2):
                A_act(ET[:, c, :], bT[:, c, :], AF.Exp, [B_b], [B_E], scale=1.0 / 16)
                V_tt(kdT[:, c, :], kbT[:, c, :], ET[:, c, :], ALU.mult, [B_qk, B_E], [B_kd])
            A_act(sr, rb, AF.Sigmoid, [B_rb], [B_sr])
            G_tt(rg, rb, sr, ALU.mult, [B_rb, B_sr], [B_rg])
            rg4 = rg.rearrange("p k (h e) -> p (k h) e", e=128)
            G_tt(rg4, rg4, glag.unsqueeze(1).to_broadcast([128, 64, 128]), ALU.mult, [B_rg, B_gc], [B_rg])
            for c in range(2):
                P.op("dve", lambda e, c=c: e.memset(Sf[c], 0.0), writes=[B_Sf[c]])
            if "cs" in dbg and sq == 0:
                dma("sp", dbg["cs"][:, :, :], bT, reads=[B_b]); dma("sp", dbg["kd"][:, :, :], kdT, reads=[B_kd])
                dma("sp", dbg["qd"][:, :, :], qd4, reads=[B_qd]); dma("sp", dbg["rg"][:, :, :], rg, reads=[B_rg])
                dma("sp", dbg["la"][:, :, :], laT, reads=[B_la])
            for blk in range(16):
                t1 = blk * 128
                pst = PS[4].bitcast(BF16)
                for c in range(2):
                    transpose(pst[:, c * 128:(c + 1) * 128], kdT[:, c, t1:t1 + 128], ident_b, reads=[B_kd, B_ident], writes=[PSB[4]])
                for c in range(2):
                    V_copy(kd_ab[0:64, c, 0, :], pst[0:64, c * 128:(c + 1) * 128], [PSB[4]], [B_kab])
                    V_copy(kd_ab[64:128, c, 1, :], pst[64:128, c * 128:(c + 1) * 128], [PSB[4]], [B_kab])
                PSm = [PS[1][:, 0:256].rearrange("p (a e) -> p a e", a=2), PS[2][:, 0:256].rearrange("p (a e) -> p a e", a=2)]
                for c in range(2):
                    for ab in range(2):
                        for hh in range(2):
                            h = 2 * c + hh
                            mm(PSm[c][64 * hh:64 * hh + 64, ab, :], kd_ab[:, c, ab, 64 * hh:64 * hh + 64], vb[:, blk, h * 128:(h + 1) * 128],
                               True, True, reads=[B_kab, B_vb], writes=[PSB[1 + c]])
                PSa = PS[0].rearrange("p (h i) -> p h i", h=4)
                for h in range(4):
                    mm(PSa[:, h, :], kdT[:, h // 2, t1:t1 + 128], qd4[:, h, t1:t1 + 128], True, True,
                       reads=[B_kd, B_qd], writes=[PSB[0]])
                V_tt(att, PSa, gmask.unsqueeze(1).to_broadcast([128, 4, 128]), ALU.mult, [PSB[0], B_gc], [B_att])
                for c in range(2):
                    V_copy(Sbf[c][0], Sf[c], [B_Sf[c]], [B_Sbf[c][0]])
                    V_tt(tmpS[c], PSm[c][:, 0, :], Sf[c], ALU.add, [PSB[1 + c], B_Sf[c]], [B_tS[c]])
                    V_ts(Sf[c], tmpS[c], dec[:, c, 2 * blk:2 * blk + 1], None, ALU.mult, None, [B_tS[c], B_dec], [B_Sf[c]])
                    V_copy(Sbf[c][1], Sf[c], [B_Sf[c]], [B_Sbf[c][1]])
                    V_tt(tmpS[c], PSm[c][:, 1, :], Sf[c], ALU.add, [PSB[1 + c], B_Sf[c]], [B_tS[c]])
                    V_ts(Sf[c], tmpS[c], dec[:, c, 2 * blk + 1:2 * blk + 2], None, ALU.mult, None, [B_tS[c], B_dec], [B_Sf[c]])
                PSo = PS[3].rearrange("p (h e) -> p h e", h=4)
                for h in range(4):
                    c = h // 2
                    mm(PSo[:, h, :], att[:, h, :], vb[:, blk, h * 128:(h + 1) * 128], True, False,
                       reads=[B_att, B_vb], writes=[PSB[3]], skip=True)
                    mm(PSo[0:64, h, :], qd4[:, h, t1:t1 + 64], Sbf[c][0], False, False,
                       reads=[B_qd, B_Sbf[c][0]], writes=[PSB[3]], skip=True)
                    mm(PSo[64:128, h, :], qd4[:, h, t1 + 64:t1 + 128], Sbf[c][1], False, True,
                       reads=[B_qd, B_Sbf[c][1]], writes=[PSB[3]], skip=True)
                for h in range(4):
                    A_act(junk, PSo[:, h, :], AF.Square, [PSB[3]], [B_junk, B_ss], accum_out=ss[:, h:h + 1])
                A_act(ss[:, 4:8], ss[:, 0:4], AF.Sqrt, [B_ss], [B_ss], bias=EPS, scale=1.0 / 128)
                V_recip(ss[:, 8:12], ss[:, 4:8], [B_ss], [B_ss])
                for h in range(4):
                    V_stt(ogb[:, h * 128:(h + 1) * 128], PSo[:, h, :], ss[:, 8 + h:9 + h], rg[:, blk, h * 128:(h + 1) * 128],
                          ALU.mult, ALU.mult, [PSB[3], B_ss, B_rg], [B_ogb])
                if "ogb" in dbg and sq == 0 and blk == 0:
                    dma("sp", dbg["ogb"][:, :], ogb, reads=[B_ogb]); dma("sp", dbg["att"][:, :, :], att, reads=[B_att])
                pst2 = PS[5].bitcast(BF16)
                for fc in range(4):
                    transpose(pst2[:, fc * 128:(fc + 1) * 128], ogb[:, fc * 128:(fc + 1) * 128], ident_b,
                              reads=[B_ogb, B_ident], writes=[PSB[5]])
                evac_copy(ogT[:, :, (blk % 4) * 128:(blk % 4) * 128 + 128], pst2[:, 0:512].rearrange("p (f t) -> p f t", f=4),
                          [PSB[5]], [B_ogT])
                if blk % 4 == 3:
                    q0 = (blk // 4) * 512
                    dma("sp", s_og[:, :, tb0 + q0:tb0 + q0 + 512].rearrange("c p t -> p c t"), ogT, reads=[B_ogT])
    if "p1" in phases:
        P.fence()
        A.mark()
        try:
            if "nonsa" not in phases:
                phase1_nsa()
        except StopBuild:
            pass
        A.release()
        P.fence()
        A.mark()
        phase1_gla()
        A.release()
    def phase2():
        B_w2 = Buf("w2")
        wbn = A.alloc([128, 4, D], BF16, "wbn"); load_cast(wbn, wbn_d.rearrange("(kc p) n -> p kc n", p=128), B_w2)
        wbg = A.alloc([128, 4, D], BF16, "wbg"); load_cast(wbg, wbg_d.rearrange("(kc p) n -> p kc n", p=128), B_w2)
        wout = A.alloc([128, 8, D], BF16, "wout"); load_cast(wout, wout_d.rearrange("(kc p) n -> p kc n", p=128), B_w2)
        wxq = A.alloc([128, 8, 512], BF16, "wxq"); load_cast(wxq, wxq_d.rearrange("(kc p) n -> p kc n", p=128), B_w2)
        wxkv = A.alloc([128, 8, D], BF16, "wxkv"); load_cast(wxkv, wxkv_d.rearrange("(kc p) n -> p kc n", p=128), B_w2)
        wxo = A.alloc([128, 4, D], BF16, "wxo"); load_cast(wxo, wxo_d.rearrange("(kc p) n -> p kc n", p=128), B_w2)
        gx = A.alloc([128, 8], F32, "gx"); gm = A.alloc([128, 8], F32, "gm"); B_g = Buf("g2")
        dma("sp", gx, g_x_d[:, :], writes=[B_g]); dma("sp", gm, g_mem_d[:, :], writes=[B_g])
        xt = A.alloc([128, 4, D], F32, "xt"); B_xt = Buf("xt")
        xn = A.alloc([128, 4, D], BF16, "xn"); B_xn = Buf("xn")
        st = A.alloc([128, 32], F32, "st"); B_st = Buf("st")
        hxT = A.alloc([128, 8, 512], BF16, "hxT"); B_hx = Buf("hx")
        memt = A.alloc([128, 2, D], F32, "memt"); B_mem = Buf("mem")
        memT = A.alloc([128, 8, 256], BF16, "memT"); B_memT = Buf("memT")
        kxT = A.alloc([128, 4, 256], BF16, "kxT"); B_kx = Buf("kx")
        vxa = A.alloc([128, 2, 4, 129], BF16, "vxa"); B_vx = Buf("vx")
        G_memset(vxa[:, :, :, 128:129], 1.0, [B_vx])
        onT = A.alloc([128, 4, 512], BF16, "onT2"); ogT = A.alloc([128, 4, 512], BF16, "ogT2"); B_o = Buf("o2")
        sg = A.alloc([128, 16, 512], BF16, "sg"); B_sg = Buf("sg")
        mixT = A.alloc([128, 8, 512], BF16, "mixT"); B_mix = Buf("mix")
        tmp1 = [A.alloc([128, 512], F32, "tmp1%d" % i) for i in range(2)]; tmp2 = [A.alloc([128, 512], F32, "tmp2%d" % i) for i in range(2)]
        B_t1 = [Buf("t1%d" % i) for i in range(2)]; B_t2 = [Buf("t2%d" % i) for i in range(2)]
        qxT = A.alloc([128, 4, 512], BF16, "qxT"); B_qx = Buf("qx")
        PTx = [A.alloc([128, 512], BF16, "PTx%d" % i) for i in range(2)]; B_PTx = [Buf("PTx%d" % i) for i in range(2)]
        oxb = A.alloc([128, 4, 512], BF16, "oxb"); B_oxb = Buf("oxb")
        oxT = A.alloc([128, 4, 512], BF16, "oxT"); B_oxT = Buf("oxT")
        rd = A.alloc([128, 8], F32, "rd"); B_rd = Buf("rd")
        bk = [0]

        def bank():
            b = 2 + bk[0] % 4; bk[0] += 1
            return b

        for it in range(NTOK // 512):
            t0 = it * 512
            if it % 4 == 0:
                sq = it // 4
                dma("sp", memt, mem_d[sq * MEM:(sq + 1) * MEM, :].rearrange("(s p) d -> p s d", p=128), writes=[B_mem])
                rmsnorm_T(memt, B_mem, 2, gm, B_g, memT, B_memT, xn, B_xn, st, B_st, [0, 1])
                for hd in range(4):
                    pi = bank()
                    for kc in range(8):
                        mm(PS[pi][:, 0:256], wxkv[:, kc, hd * 128:(hd + 1) * 128], memT[:, kc, :], kc == 0, kc == 7,
                           reads=[B_w2, B_memT], writes=[PSB[pi]])
                    evac_copy(kxT[:, hd, :], PS[pi][:, 0:256], [PSB[pi]], [B_kx])
                for ms in range(2):
                    pi = bank()
                    for kc in range(8):
                        mm(PS[pi], memT[:, kc, ms * 128:(ms + 1) * 128], wxkv[:, kc, 512:1024], kc == 0, kc == 7,
                           reads=[B_w2, B_memT], writes=[PSB[pi]])
                    evac_copy(vxa[:, ms, :, 0:128], PS[pi].rearrange("p (h d) -> p h d", h=4), [PSB[pi]], [B_vx])
            dma("sp", xt, x_d[t0:t0 + 512, :].rearrange("(s p) d -> p s d", p=128), writes=[B_xt])
            dma("sp", onT, s_on[:, :, t0:t0 + 512].rearrange("c p t -> p c t"), writes=[B_o])
            dma("sp", ogT, s_og[:, :, t0:t0 + 512].rearrange("c p t -> p c t"), writes=[B_o])
            dma("sp", sg, s_fm[FM_MG:FM_MG + 16, :, t0:t0 + 512].rearrange("c p t -> p c t"), writes=[B_sg])
            for oc in range(8):
                p1 = bank(); p2 = bank()
                for kc in range(4):
                    mm(PS[p1], wbn[:, kc, oc * 128:(oc + 1) * 128], onT[:, kc, :], kc == 0, kc == 3, reads=[B_w2, B_o], writes=[PSB[p1]])
                for kc in range(4):
                    mm(PS[p2], wbg[:, kc, oc * 128:(oc + 1) * 128], ogT[:, kc, :], kc == 0, kc == 3, reads=[B_w2, B_o], writes=[PSB[p2]])
                j = oc % 2
                V_tt(tmp1[j], PS[p1], sg[:, oc, :], ALU.mult, [PSB[p1], B_sg], [B_t1[j]])
                V_tt(tmp2[j], PS[p2], sg[:, 8 + oc, :], ALU.mult, [PSB[p2], B_sg], [B_t2[j]])
                G_tt(mixT[:, oc, :], tmp1[j], tmp2[j], ALU.add, [B_t1[j], B_t2[j]], [B_mix])
            for sub in range(4):
                for half in range(2):
                    pi = bank()
                    for kc in range(8):
                        mm(PS[pi], mixT[:, kc, sub * 128:(sub + 1) * 128], wout[:, kc, half * 512:(half + 1) * 512], kc == 0, kc == 7,
                           reads=[B_w2, B_mix], writes=[PSB[pi]])
                    V_tt(xt[:, sub, half * 512:(half + 1) * 512], xt[:, sub, half * 512:(half + 1) * 512], PS[pi], ALU.add,
                         [B_xt, PSB[pi]], [B_xt])
            rmsnorm_T(xt, B_xt, 4, gx, B_g, hxT, B_hx, xn, B_xn, st, B_st, [0, 1])
            for hd in range(4):
                pi = bank()
                for kc in range(8):
                    mm(PS[pi], wxq[:, kc, hd * 128:(hd + 1) * 128], hxT[:, kc, :], kc == 0, kc == 7, reads=[B_w2, B_hx], writes=[PSB[pi]])
                evac_copy(qxT[:, hd, :], PS[pi], [PSB[pi]], [B_qx])
            for hd in range(4):
                for ms in range(2):
                    pi = bank()
                    mm(PS[pi], kxT[:, hd, ms * 128:(ms + 1) * 128], qxT[:, hd, :], True, True, reads=[B_kx, B_qx], writes=[PSB[pi]])
                    A_act(PTx[ms], PS[pi], AF.Exp, [PSB[pi]], [B_PTx[ms]], scale=128.0 ** -0.5)
                pa = [PS[6][:, 0:258].rearrange("p (s c) -> p s c", s=2), PS[7][:, 0:258].rearrange("p (s c) -> p s c", s=2)]
                for ms in range(2):
                    for sub in range(4):
                        mm(pa[sub // 2][:, sub % 2, :], PTx[ms][:, sub * 128:(sub + 1) * 128], vxa[:, ms, hd, :],
                           ms == 0 and sub % 2 == 0, ms == 1, reads=[B_PTx[ms], B_vx], writes=[PSB[6 + sub // 2]], skip=True)
                for bq in range(2):
                    V_recip(rd[:, 2 * bq:2 * bq + 2], pa[bq][:, :, 128], [PSB[6 + bq]], [B_rd])
                for sub in range(4):
                    V_ts(oxb[:, sub, hd * 128:(hd + 1) * 128], pa[sub // 2][:, sub % 2, 0:128], rd[:, sub:sub + 1], None, ALU.mult, None,
                         [PSB[6 + sub // 2], B_rd], [B_oxb])
            for fc in range(4):
                pi = bank()
                pst = PS[pi].bitcast(BF16)
                for sub in range(4):
                    transpose(pst[:, sub * 128:(sub + 1) * 128], oxb[:, sub, fc * 128:(fc + 1) * 128], ident_b,
                              reads=[B_oxb, B_ident], writes=[PSB[pi]])
                evac_copy(oxT[:, fc, :], pst[:, 0:512], [PSB[pi]], [B_oxT])
            for sub in range(4):
                for half in range(2):
                    pi = bank()
                    for kc in range(4):
                        mm(PS[pi], oxT[:, kc, sub * 128:(sub + 1) * 128], wxo[:, kc, half * 512:(half + 1) * 512], kc == 0, kc == 3,
                           reads=[B_w2, B_oxT], writes=[PSB[pi]])
                    V_tt(xt[:, sub, half * 512:(half + 1) * 512], xt[:, sub, half * 512:(half + 1) * 512], PS[pi], ALU.add,
                         [B_xt, PSB[pi]], [B_xt])
            dma("sp", s_x2[t0:t0 + 512, :].rearrange("(s p) d -> p s d", p=128), xt, reads=[B_xt])

    def phase3():
        B_w3 = Buf("w3")
        wup = A.alloc([128, 8, 2 * FFN], BF16, "wup"); load_cast(wup, wup_d.rearrange("(kc p) n -> p kc n", p=128), B_w3, nsplit=4)
        wdn = A.alloc([128, 22, D], BF16, "wdn"); load_cast(wdn, wdn_d.rearrange("(kc p) n -> p kc n", p=128), B_w3, nsplit=1)
        gf = A.alloc([128, 8], F32, "gf"); B_g = Buf("g3"); dma("sp", gf, g_ffn_d[:, :], writes=[B_g])
        cw = A.alloc([128, 3, 22], F32, "cw"); cbv = A.alloc([128, 22], F32, "cbv")
        dma("sp", cw, convw_d[:, :, :], writes=[B_g]); dma("sp", cbv, convb_d[:, :], writes=[B_g])
        gfin = A.alloc([128, D], F32, "gfin"); dma("sp", gfin, g_fin_d[:, :], writes=[B_g])
        xt = A.alloc([128, 4, D], F32, "xt"); B_xt = Buf("xt")
        xn = A.alloc([128, 4, D], BF16, "xn"); B_xn = Buf("xn")
        st = A.alloc([128, 32], F32, "st"); B_st = Buf("st")
        hfT = A.alloc([128, 8, 512], BF16, "hfT"); B_hf = Buf("hf")
        aT = A.alloc([128, 22, 512], BF16, "aT"); B_a = Buf("aT")
        usb = [A.alloc([128, 514], F32, "usb%d" % i) for i in range(2)]; B_u = [Buf("u%d" % i) for i in range(2)]
        acc = [A.alloc([128, 512], F32, "acc%d" % i) for i in range(2)]; B_acc = [Buf("acc%d" % i) for i in range(2)]
        carry = A.alloc([128, 22, 2], F32, "carry"); B_car = Buf("carry")
        bk = [0]

        def bank():
            b = (2 + bk[0]) % 8; bk[0] += 1
            return b

        B_ac = [Buf("aT%d" % i) for i in range(22)]
        for it in range(NTOK // 512):
            t0 = it * 512
            dma("sp", xt, s_x2[t0:t0 + 512, :].rearrange("(s p) d -> p s d", p=128), writes=[B_xt])
            if it % 4 == 0:
                P.op("dve", lambda e: e.memset(carry, 0.0), writes=[B_car])
            rmsnorm_T(xt, B_xt, 4, gf, B_g, hfT, B_hf, xn, B_xn, st, B_st, [0, 1])
            pend = []
            for fcn in range(22):
                pu = bank(); pg = bank()
                for kc in range(8):
                    mm(PS[pu], wup[:, kc, fcn * 128:(fcn + 1) * 128], hfT[:, kc, :], kc == 0, kc == 7, reads=[B_w3, B_hf], writes=[PSB[pu]])
                for kc in range(8):
                    mm(PS[pg], wup[:, kc, FFN + fcn * 128:FFN + (fcn + 1) * 128], hfT[:, kc, :], kc == 0, kc == 7,
                       reads=[B_w3, B_hf], writes=[PSB[pg]])
                j = fcn % 2
                V_copy(usb[j][:, 0:2], carry[:, fcn, :], [B_car], [B_u[j]])
                A_act(usb[j][:, 2:514], PS[pu], AF.Copy, [PSB[pu]], [B_u[j]])
                V_copy(carry[:, fcn, :], usb[j][:, 512:514], [B_u[j]], [B_car])
                V_ts(acc[j], usb[j][:, 2:514], cw[:, 2, fcn:fcn + 1], cbv[:, fcn:fcn + 1], ALU.mult, ALU.add, [B_u[j], B_g], [B_acc[j]])
                V_stt(acc[j], usb[j][:, 1:513], cw[:, 1, fcn:fcn + 1], acc[j], ALU.mult, ALU.add, [B_u[j], B_g, B_acc[j]], [B_acc[j]])
                V_stt(acc[j], usb[j][:, 0:512], cw[:, 0, fcn:fcn + 1], acc[j], ALU.mult, ALU.add, [B_u[j], B_g, B_acc[j]], [B_acc[j]])
                A_act(acc[j], acc[j], AF.Gelu_apprx_tanh, [B_acc[j]], [B_acc[j]])
                if pend:
                    pend.pop(0)()
                pend.append(lambda fcn=fcn, pg=pg, j=j: V_tt(aT[:, fcn, :], PS[pg], acc[j], ALU.mult, [PSB[pg], B_acc[j]], [B_ac[fcn]]))
            while pend:
                pend.pop(0)()
            for sub in range(4):
                for half in range(2):
                    pi = bank()
                    for kc in range(22):
                        mm(PS[pi], aT[:, kc, sub * 128:(sub + 1) * 128], wdn[:, kc, half * 512:(half + 1) * 512], kc == 0, kc == 21,
                           reads=[B_w3, B_ac[kc]], writes=[PSB[pi]])
                    V_tt(xt[:, sub, half * 512:(half + 1) * 512], xt[:, sub, half * 512:(half + 1) * 512], PS[pi], ALU.add,
                         [B_xt, PSB[pi]], [B_xt])
            if "aT" in dbg and it == 0:
                dma("sp", dbg["aT"][:, :, :], aT, reads=B_ac); dma("sp", dbg["x3"][:, :, :], xt, reads=[B_xt])
                dma("sp", dbg["hf"][:, :, :], hfT, reads=[B_hf])
            for s in range(4):
                A_act(xn[:, s, :], xt[:, s, :], AF.Square, [B_xt], [B_xn, B_st], accum_out=st[:, s:s + 1])
            A_act(st[:, 8:12], st[:, 0:4], AF.Sqrt, [B_st], [B_st], bias=EPS, scale=1.0 / D)
            V_recip(st[:, 16:20], st[:, 8:12], [B_st], [B_st])
            for s in range(4):
                V_stt(xt[:, s, :], xt[:, s, :], st[:, 16 + s:17 + s], gfin, ALU.mult, ALU.mult, [B_xt, B_st, B_g], [B_xt])
            dma("sp", out_d[t0:t0 + 512, :].rearrange("(s p) d -> p s d", p=128), xt, reads=[B_xt])

    if "p2" in phases:
        P.fence(); A.mark(); phase2(); A.release()
    if "p3" in phases:
        P.fence(); A.mark(); phase3(); A.release()
    for e in ENGS:
        last = {}
        for o in P.ops[e]:
            if o.dma:
                last[id(o.token)] = o
        seen = {}
        nd = 0
        for o in P.ops[e]:
            if o.dma:
                seen[nd % NDMA_SLOTS] = o
                nd += 1
        P.final += list(seen.values())
    P.emit(nc, stack)
    stack.close()
    return nc, consts


def prep_inputs(inp):
    f = lambda a: np.ascontiguousarray(np.asarray(a, dtype=np.float32))
    w_in = f(inp["w_in"][0])
    shared = {
        "w_fm": f(w_in[:, _fm_cols()]),
        "w_tm": f(w_in[:, _tm_cols()]),
        "g_mix": pmajor(inp["ln_mix_g"][0], 8),
        "gate_b": f(np.broadcast_to(np.asarray(inp["nsa_gate_b"][0]).reshape(1, 24), (128, 24))),
        "w1k": f(inp["cmp_w1_k"][0]), "w1v": f(inp["cmp_w1_v"][0]),
        "w2k": f(np.concatenate([inp["cmp_w2_k"][0], inp["cmp_w2_k"][0]], axis=1)),
        "w2v": f(inp["cmp_w2_v"][0]),
        "pek": f(np.asarray(inp["cmp_pos_k"][0]).T), "pev": f(np.asarray(inp["cmp_pos_v"][0]).T),
        "wa2": f(np.concatenate([inp["gla_w_alpha2"][0], np.asarray(inp["gla_b_alpha"][0]).reshape(1, 256)], axis=0)),
        "gla_g": f(np.broadcast_to(np.asarray(inp["gla_norm_g"][0]).reshape(1, 128), (128, 128))),
        "ba": pmajor(inp["gla_b_alpha"][0], 2),
        "wbn": f(inp["w_branch_nsa"][0]), "wbg": f(inp["w_branch_gla"][0]), "wout": f(inp["w_out"][0]),
        "g_x": pmajor(inp["ln_x_g"][0], 8), "g_mem": pmajor(inp["ln_mem_g"][0], 8),
        "wxq": f(inp["w_xq"][0]), "wxkv": f(inp["w_xkv"][0]), "wxo": f(inp["w_xo"][0]),
        "g_ffn": pmajor(inp["ln_ffn_g"][0], 8),
        "wup": f(inp["w_up"][0]), "wdn": f(inp["w_down"][0]),
        "convw": f(np.asarray(inp["conv_w"][0]).reshape(3, 22, 128).transpose(2, 0, 1)),
        "convb": f(np.asarray(inp["conv_b"][0]).reshape(22, 128).T),
        "g_fin": f(np.broadcast_to(np.asarray(inp["ln_final_g"]).reshape(1, D), (128, D))),
    }
    for k, v in host_consts().items():
        shared["c_" + k] = v
    x = np.asarray(inp["x"], dtype=np.float32)
    mem = np.asarray(inp["mem"], dtype=np.float32)
    maps = []
    for c in range(NCORES):
        m = dict(shared)
        m["x"] = np.ascontiguousarray(x[c * NSEQ:(c + 1) * NSEQ].reshape(NTOK, D))
        m["mem"] = np.ascontiguousarray(mem[c * NSEQ:(c + 1) * NSEQ].reshape(NSEQ * MEM, D))
        maps.append(m)
    return maps


_CACHE = {}


def kernel(**inputs):
    if "nc" not in _CACHE:
        _CACHE["nc"] = build_program()[0]
    nc = _CACHE["nc"]
    maps = prep_inputs(inputs)
    res = run_bass_kernel_spmd(nc, maps, core_ids=list(range(NCORES)))
    out = np.stack([np.asarray(r["out"]).reshape(NSEQ, SEQ, D) for r in res.results], axis=0)
    return out.reshape(NCORES * NSEQ, SEQ, D).astype(np.float32)
```

```python
import numpy as np
import concourse.bass as bass
import concourse.mybir as mybir
from concourse.bass_utils import run_bass_kernel_spmd

F32 = mybir.dt.float32
BF16 = mybir.dt.bfloat16
U8 = mybir.dt.uint8
AF = mybir.ActivationFunctionType
ALU = mybir.AluOpType
AX = mybir.AxisListType

NCORES = 8
SEQ = 2048
D = 1024
NSEQ = 4
NTOK = NSEQ * SEQ
MEM = 256
FFN = 2816
NEG = -30000.0
EPS = 1e-6

STAGE = [99]


class StopBuild(Exception):
    pass


def stage(n):
    if STAGE[0] == n:
        raise StopBuild()


DEBUG = {}


class Buf:
    __slots__ = ("name", "last_w", "rd_eng", "rd_dma")

    def __init__(self, name):
        self.name = name
        self.last_w = None
        self.rd_eng = {}
        self.rd_dma = []


class Op:
    __slots__ = ("eng", "fn", "dma", "deps", "signal", "token", "prev_slot")

    def __init__(self, eng, fn, dma):
        self.eng = eng
        self.fn = fn
        self.dma = dma
        self.deps = []
        self.signal = dma
        self.token = None
        self.prev_slot = None


ENGS = ("pe", "act", "dve", "pool", "sp")
NDMA_SLOTS = 8


class Prog:
    def __init__(self):
        self.ops = {e: [] for e in ENGS}
        self.final = []
        self.fence_deps = []

    def fence(self):
        deps = []
        for e in ENGS:
            last = None
            for o in reversed(self.ops[e]):
                if not o.dma:
                    last = o
                    break
            if last is not None:
                last.signal = True
                deps.append(last)
            nd = 0
            slots = {}
            for o in self.ops[e]:
                if o.dma:
                    slots[nd % NDMA_SLOTS] = o
                    nd += 1
            deps += list(slots.values())
        self.fence_deps = deps

    def op(self, eng, fn, reads=(), writes=(), dma=False):
        o = Op(eng, fn, dma)
        raw = set()
        other = set()
        for b in reads:
            if b.last_w is not None:
                raw.add(b.last_w)
        for b in writes:
            if b.last_w is not None:
                other.add(b.last_w)
            other.update(b.rd_eng.values())
            other.update(b.rd_dma)
        for d in raw | other:
            if d is o:
                continue
            same = (not dma) and (not d.dma) and d.eng == eng
            if same and (eng == "pe" or d not in raw):
                continue
            o.deps.append(d)
            d.signal = True
        for d in self.fence_deps:
            if (not dma) and (not d.dma) and d.eng == eng:
                continue
            if d not in o.deps:
                o.deps.append(d)
        for b in reads:
            if dma:
                b.rd_dma.append(o)
            else:
                b.rd_eng[eng] = o
        for b in writes:
            b.last_w = o
            b.rd_eng = {}
            b.rd_dma = []
        self.ops[eng].append(o)
        return o

    def emit(self, nc, stack):
        sems = {e: stack.enter_context(nc.semaphore("s_" + e)) for e in ENGS}
        dsem = {e: [stack.enter_context(nc.semaphore("d_%s%d" % (e, i))) for i in range(NDMA_SLOTS)]
                for e in ("sp", "pool", "act")}
        for e in ENGS:
            cnt = 0
            nd = 0
            slot_cnt = [0] * NDMA_SLOTS
            slot_last = [None] * NDMA_SLOTS
            for o in self.ops[e]:
                if o.dma:
                    s = nd % NDMA_SLOTS
                    nd += 1
                    slot_cnt[s] += 16
                    o.prev_slot = slot_last[s]
                    o.token = (dsem[e][s], slot_cnt[s])
                    slot_last[s] = o
                elif o.signal:
                    cnt += 1
                    o.token = (sems[e], cnt)
        block = stack.enter_context(nc.Block())
        prog = self

        def body(e):
            def run(eng):
                waited = {}

                def wait(tok):
                    sem, val = tok
                    k = id(sem)
                    if waited.get(k, 0) >= val:
                        return
                    eng.wait_ge(sem, val)
                    waited[k] = val

                for o in prog.ops[e]:
                    if o.dma and o.prev_slot is not None:
                        wait(o.prev_slot.token)
                    for d in o.deps:
                        wait(d.token)
                    ins = o.fn(eng)
                    if o.token is not None:
                        ins.then_inc(o.token[0], 16 if o.dma else 1)
                if e == "sp":
                    for o in prog.final:
                        wait(o.token)
            return run

        block.tensor(body("pe"))
        block.scalar(body("act"))
        block.vector(body("dve"))
        block.gpsimd(body("pool"))
        block.sync(body("sp"))


class Arena:
    def __init__(self, ap, size):
        self.ap = ap
        self.size = size
        self.off = 0
        self.marks = []

    def alloc(self, shape, dtype, name="t"):
        esz = 4 if dtype == F32 else 2
        n = 1
        for s in shape[1:]:
            n *= s
        nbytes = (n * esz + 31) // 32 * 32
        assert self.off + nbytes <= self.size, (name, self.off, nbytes, self.size)
        a = self.ap[0:shape[0], self.off:self.off + n * esz].bitcast(dtype)
        self.off += nbytes
        if len(shape) == 3:
            a = a.rearrange("p (a b) -> p a b", a=shape[1])
        elif len(shape) == 4:
            a = a.rearrange("p (a b c) -> p a b c", a=shape[1], b=shape[2])
        return a

    def mark(self):
        self.marks.append(self.off)

    def release(self):
        self.off = self.marks.pop()


SLOPES = [2.0 ** (-(h + 1)) for h in range(8)]

C_QA, C_KC, C_VC, C_KS, C_VS, C_KW, C_VW = 0, 512, 640, 768, 896, 1024, 1152
C_GATE, C_QB, C_KB, C_VB, C_RB, C_AL, C_MG = 1280, 1304, 1560, 1816, 2328, 2840, 2856
FM_QA, FM_KC, FM_VC, FM_KS, FM_KW, FM_QB, FM_KB, FM_MG = 0, 4, 5, 6, 8, 10, 12, 14
NFM = 30
TM_VS, TM_VW, TM_KB, TM_VB, TM_RB = 0, 128, 256, 512, 1024
NTM = 1536


def _fm_cols():
    cols = []
    for c in range(4):
        cols += list(range(C_QA + 128 * c, C_QA + 128 * (c + 1)))
    cols += list(range(C_KC, C_KC + 128))
    cols += list(range(C_VC, C_VC + 128))
    for base in (C_KS, C_KW):
        for g in range(2):
            one = list(range(base + 64 * g, base + 64 * (g + 1)))
            cols += one + one
    cols += list(range(C_QB, C_QB + 256))
    cols += list(range(C_KB, C_KB + 256))
    cols += list(range(C_MG, C_MG + 2048))
    cols += list(range(C_AL, C_AL + 16))
    return np.array(cols)


def _tm_cols():
    cols = list(range(C_VS, C_VS + 128)) + list(range(C_VW, C_VW + 128))
    cols += list(range(C_KB, C_KB + 256)) + list(range(C_VB, C_VB + 512)) + list(range(C_RB, C_RB + 512))
    cols += list(range(C_GATE, C_GATE + 24))
    return np.array(cols)


def pmajor(v, nchunk):
    return np.ascontiguousarray(np.asarray(v, np.float32).reshape(nchunk, 128).T)


def host_consts():
    c = {}
    t = np.arange(SEQ)
    n = np.arange(127)
    dist = t[None, :] - (16 * n[:, None] + 31)
    c["cmpD"] = np.where(dist >= 0, -dist, -1.0e6).astype(np.float32)
    ov = np.zeros((127, 32), np.float32)
    for nn in range(127):
        for p in range(32):
            ov[nn, (16 * nn + p) // 64] += 1.0 / 32
    c["ovl"] = ov
    cur = (t // 64)
    j = np.arange(32)
    forced = (j[None, :] == 0) | (j[None, :] == cur[:, None]) | (j[None, :] == cur[:, None] - 1)
    future = j[None, :] > cur[:, None]
    mul = np.where(forced | future, 0.0, 1.0).astype(np.float32)
    add = np.where(forced, 5.0, np.where(future, -1.0, 0.0)).astype(np.float32)
    c["fmul"] = np.ascontiguousarray(mul.reshape(16, 128, 32).transpose(1, 0, 2))
    c["fadd"] = np.ascontiguousarray(add.reshape(16, 128, 32).transpose(1, 0, 2))
    c["tb"] = (64.0 * (cur[None, :] - j[:, None])).astype(np.float32)
    ea = np.zeros((34, SEQ), np.float32)
    ea[t // 64, t] = 1.0
    ea[32] = t % 64
    ea[33] = 1.0
    c["ea"] = ea
    cr = np.zeros((2, 8, 512), np.float32)
    rq = np.arange(512) % 64
    for h in range(8):
        cr[0, h] = SLOPES[h]
        cr[1, h] = -SLOPES[h] * rq
    c["crow"] = cr
    k = np.arange(128)
    cc = np.arange(896)
    c["cb"] = np.where(cc[None, :] - 384 >= k[:, None], 0.0, NEG).astype(np.float32)
    cc = np.arange(1152)
    dd = cc[None, :] - 384 - k[:, None]
    wb = np.zeros((128, 8, 1152), np.float32)
    for h in range(8):
        wb[:, h, :] = np.where((dd >= 0) & (dd < 256), -SLOPES[h] * dd, NEG)
    c["wb"] = wb
    c["ident"] = np.eye(128, dtype=np.float32)
    jj = np.arange(128)
    same = (jj[:, None] // 64) == (jj[None, :] // 64)
    c["gmask"] = (same & (jj[:, None] <= jj[None, :])).astype(np.float32)
    c["srst"] = np.broadcast_to(np.where(t % 64 == 0, 0.0, 1.0).astype(np.float32), (128, SEQ)).copy()
    c["gup"] = (same & (jj[:, None] > jj[None, :])).astype(np.float32)
    return c


CONST_SHAPES = None


def build_program(phases=("p0", "p1", "p2", "p3")):
    import contextlib
    nc = bass.Bass("TRN2", target_bir_lowering=False)
    P = Prog()
    stack = contextlib.ExitStack()

    def din(name, shape, dt=F32):
        return nc.dram_tensor(name, list(shape), dt, kind="ExternalInput").ap()

    def dscr(name, shape, dt):
        kind = "ExternalOutput" if DEBUG.get(name) else "Internal"
        return nc.dram_tensor(name, list(shape), dt, kind=kind).ap()

    consts = host_consts()
    x_d = din("x", [NTOK, D])
    mem_d = din("mem", [NSEQ * MEM, D])
    wfm_d = din("w_fm", [D, NFM * 128 + 16])
    wtm_d = din("w_tm", [D, NTM + 24])
    g_mix_d = din("g_mix", [128, 8])
    gate_b_d = din("gate_b", [128, 24])
    w1k_d = din("w1k", [2048, 128]); w1v_d = din("w1v", [2048, 128])
    w2k_d = din("w2k", [128, 128]); w2v_d = din("w2v", [128, 64])
    pek_d = din("pek", [64, 32]); pev_d = din("pev", [64, 32])
    wa2_d = din("wa2", [17, 256])
    gla_g_d = din("gla_g", [128, 128])
    ba_d = din("ba", [128, 2])
    wbn_d = din("wbn", [512, D]); wbg_d = din("wbg", [512, D]); wout_d = din("wout", [D, D])
    g_x_d = din("g_x", [128, 8]); g_mem_d = din("g_mem", [128, 8])
    wxq_d = din("wxq", [D, 512]); wxkv_d = din("wxkv", [D, 1024]); wxo_d = din("wxo", [512, D])
    g_ffn_d = din("g_ffn", [128, 8])
    wup_d = din("wup", [D, 2 * FFN]); wdn_d = din("wdn", [FFN, D])
    convw_d = din("convw", [128, 3, 22]); convb_d = din("convb", [128, 22])
    g_fin_d = din("g_fin", [128, D])
    cd = {k: din("c_" + k, v.shape) for k, v in consts.items()}
    out_d = nc.dram_tensor("out", [NTOK, D], F32, kind="ExternalOutput").ap()
    s_fm = dscr("s_fm", [NFM, 128, NTOK], BF16)
    s_al = dscr("s_al", [16, NTOK], F32)
    s_tm = dscr("s_tm", [NTOK, NTM], BF16)
    s_gate = dscr("s_gate", [NTOK, 24], F32)
    s_on = dscr("s_on", [4, 128, NTOK], BF16)
    s_og = dscr("s_og", [4, 128, NTOK], BF16)
    s_x2 = dscr("s_x2", [NTOK, D], F32)
    dbg = {}
    if DEBUG.get("gla"):
        dbg["cs"] = nc.dram_tensor("dbg_cs", [128, 2, SEQ], F32, kind="ExternalOutput").ap()
        dbg["kd"] = nc.dram_tensor("dbg_kd", [128, 2, SEQ], BF16, kind="ExternalOutput").ap()
        dbg["qd"] = nc.dram_tensor("dbg_qd", [128, 4, SEQ], BF16, kind="ExternalOutput").ap()
        dbg["rg"] = nc.dram_tensor("dbg_rg", [128, 16, 512], BF16, kind="ExternalOutput").ap()
        dbg["ogb"] = nc.dram_tensor("dbg_ogb", [128, 512], BF16, kind="ExternalOutput").ap()
        dbg["att"] = nc.dram_tensor("dbg_att", [128, 4, 128], BF16, kind="ExternalOutput").ap()
        dbg["la"] = nc.dram_tensor("dbg_la", [128, 2, SEQ], F32, kind="ExternalOutput").ap()
    if DEBUG.get("ffn"):
        dbg["aT"] = nc.dram_tensor("dbg_aT", [128, 22, 512], BF16, kind="ExternalOutput").ap()
        dbg["x3"] = nc.dram_tensor("dbg_x3", [128, 4, D], F32, kind="ExternalOutput").ap()
        dbg["hf"] = nc.dram_tensor("dbg_hf", [128, 8, 512], BF16, kind="ExternalOutput").ap()

    ARENA = 204 * 1024
    arena_t = stack.enter_context(nc.sbuf_tensor("arena", [128, ARENA], U8))
    psum_t = stack.enter_context(nc.psum_tensor("psum", [128, 4096], F32))
    A = Arena(arena_t, ARENA)
    PS = [psum_t[:, 512 * i:512 * (i + 1)] for i in range(8)]
    PSB = [Buf("ps%d" % i) for i in range(8)]

    rr = {"ev": 0, "q": 0}

    def dma(q, out, in_, reads=(), writes=(), **kw):
        return P.op(q, lambda e: e.dma_start(out=out, in_=in_, **kw), reads=reads, writes=writes, dma=True)

    def load_cast(dst, src, wb, nsplit=1):
        last = dst.shape[-1]
        step = (last + nsplit - 1) // nsplit
        for s0 in range(0, last, step):
            s1 = min(last, s0 + step)
            if len(dst.shape) == 2:
                dma("pool", dst[:, s0:s1], src[:, s0:s1], writes=[wb])
            else:
                dma("pool", dst[:, :, s0:s1], src[:, :, s0:s1], writes=[wb])

    def evac_copy(out, in_, reads, writes, scale=None):
        rr["ev"] += 1
        if rr["ev"] % 2 == 0:
            if scale is None:
                P.op("act", lambda e: e.activation(out=out, in_=in_, func=AF.Copy), reads=reads, writes=writes)
            else:
                P.op("act", lambda e: e.activation(out=out, in_=in_, func=AF.Copy, scale=scale), reads=reads, writes=writes)
        else:
            if scale is None:
                P.op("dve", lambda e: e.tensor_copy(out=out, in_=in_), reads=reads, writes=writes)
            else:
                P.op("dve", lambda e: e.tensor_scalar(out=out, in0=in_, scalar1=scale, scalar2=None, op0=ALU.mult),
                     reads=reads, writes=writes)

    def mm(out, lhsT, rhs, start, stop, reads, writes, skip=False):
        P.op("pe", lambda e: e.matmul(out, lhsT=lhsT, rhs=rhs, start=start, stop=stop, skip_group_check=skip),
             reads=reads, writes=writes)

    def transpose(out, in_, ident, reads, writes):
        P.op("pe", lambda e: e.transpose(out, in_, ident), reads=reads, writes=writes)

    ident_f = A.alloc([128, 128], F32, "ident_f")
    ident_b = A.alloc([128, 128], BF16, "ident_b")
    B_ident = Buf("ident")
    dma("sp", ident_f, cd["ident"][:, :], writes=[B_ident])
    dma("pool", ident_b, cd["ident"][:, :], writes=[B_ident])

    def V_tt(out, in0, in1, op, reads, writes):
        P.op("dve", lambda e: e.tensor_tensor(out=out, in0=in0, in1=in1, op=op), reads=reads, writes=writes)

    def V_ts(out, in0, s1, s2, op0, op1, reads, writes):
        if op1 is None:
            P.op("dve", lambda e: e.tensor_scalar(out=out, in0=in0, scalar1=s1, scalar2=None, op0=op0), reads=reads, writes=writes)
        else:
            P.op("dve", lambda e: e.tensor_scalar(out=out, in0=in0, scalar1=s1, scalar2=s2, op0=op0, op1=op1), reads=reads, writes=writes)

    def V_stt(out, in0, scalar, in1, op0, op1, reads, writes):
        P.op("dve", lambda e: e.scalar_tensor_tensor(out=out, in0=in0, scalar=scalar, in1=in1, op0=op0, op1=op1),
             reads=reads, writes=writes)

    def V_copy(out, in_, reads, writes):
        P.op("dve", lambda e: e.tensor_copy(out=out, in_=in_), reads=reads, writes=writes)

    def V_recip(out, in_, reads, writes):
        P.op("dve", lambda e: e.reciprocal(out=out, in_=in_), reads=reads, writes=writes)

    def V_max(out, in_, reads, writes):
        P.op("dve", lambda e: e.max(out=out, in_=in_), reads=reads, writes=writes)

    def A_act(out, in_, func, reads, writes, **kw):
        P.op("act", lambda e: e.activation(out=out, in_=in_, func=func, **kw), reads=reads, writes=writes)

    def G_memset(ap, val, writes):
        P.op("pool", lambda e: e.memset(ap, val), writes=writes)

    def G_tt(out, in0, in1, op, reads, writes):
        P.op("pool", lambda e: e.tensor_tensor(out=out, in0=in0, in1=in1, op=op), reads=reads, writes=writes)

    def rms_a(xt, B_xt, nsub, xn, B_xn, st, B_st):
        for s in range(nsub):
            A_act(xn[:, s, :], xt[:, s, :], AF.Square, [B_xt], [B_xn, B_st], accum_out=st[:, s:s + 1])
        A_act(st[:, 8:8 + nsub], st[:, 0:nsub], AF.Sqrt, [B_st], [B_st], bias=EPS, scale=1.0 / D)
        V_recip(st[:, 16:16 + nsub], st[:, 8:8 + nsub], [B_st], [B_st])
        for s in range(nsub):
            V_ts(xn[:, s, :], xt[:, s, :], st[:, 16 + s:17 + s], None, ALU.mult, None, [B_xt, B_st], [B_xn])

    def rms_b(nsub, g_sb, B_g, hT, B_hT, xn, B_xn, psA):
        for kc in range(8):
            pi = psA[kc % len(psA)]
            pst = PS[pi].bitcast(BF16)
            for s in range(nsub):
                transpose(pst[:, s * 128:(s + 1) * 128], xn[:, s, kc * 128:(kc + 1) * 128], ident_b,
                          reads=[B_xn, B_ident], writes=[PSB[pi]])
            evac_copy(hT[:, kc, 0:nsub * 128], pst[:, 0:nsub * 128], reads=[PSB[pi], B_g], writes=[B_hT],
                      scale=g_sb[:, kc:kc + 1])

    def rmsnorm_T(xt, B_xt, nsub, g_sb, B_g, hT, B_hT, xn, B_xn, st, B_st, psA):
        rms_a(xt, B_xt, nsub, xn, B_xn, st, B_st)
        rms_b(nsub, g_sb, B_g, hT, B_hT, xn, B_xn, psA)

    if "p0" in phases:
        A.mark()
        wfm = A.alloc([128, 8, NFM * 128 + 16], BF16, "wfm"); B_wfm = Buf("wfm")
        wtm = A.alloc([128, 8, NTM + 24], BF16, "wtm"); B_wtm = Buf("wtm")
        load_cast(wfm, wfm_d.rearrange("(kc p) n -> p kc n", p=128), B_wfm, nsplit=4)
        load_cast(wtm, wtm_d.rearrange("(kc p) n -> p kc n", p=128), B_wtm, nsplit=2)
        gmix = A.alloc([128, 8], F32, "gmix"); B_gmix = Buf("gmix")
        dma("sp", gmix, g_mix_d[:, :], writes=[B_gmix])
        xt = A.alloc([128, 4, D], F32, "xt"); B_xt = Buf("xt")
        xn = A.alloc([128, 4, D], BF16, "xn"); B_xn = Buf("xn")
        st = A.alloc([128, 32], F32, "st"); B_st = Buf("st")
        hTs = [A.alloc([128, 8, 512], BF16, "hT%d" % i) for i in range(2)]
        B_hTs = [Buf("hT%d" % i) for i in range(2)]
        fmo = A.alloc([128, NFM, 512], BF16, "fmo")
        B_fmo = [Buf("fmo%d" % i) for i in range(3)]
        alo = A.alloc([16, 512], F32, "alo"); B_alo = Buf("alo")
        tmo = A.alloc([128, 4, NTM], BF16, "tmo"); B_tmo = Buf("tmo")
        gto = A.alloc([128, 4, 24], F32, "gto"); B_gto = Buf("gto")
        mmbank = [2, 3, 4, 5, 6, 7]
        bi = 0
        NT0 = NTOK // 512

        def p0_load_a(it):
            dma("sp", xt, x_d[it * 512:it * 512 + 512, :].rearrange("(s p) d -> p s d", p=128), writes=[B_xt])
            rms_a(xt, B_xt, 4, xn, B_xn, st, B_st)

        def p0_b(it):
            rms_b(4, gmix, B_gmix, hTs[it % 2], B_hTs[it % 2], xn, B_xn, [0, 1])

        p0_load_a(0)
        p0_b(0)
        for it in range(NT0):
            t0 = it * 512
            hT = hTs[it % 2]; B_hT = B_hTs[it % 2]
            for c in range(NFM + 1):
                if c == 8 and it + 1 < NT0:
                    p0_load_a(it + 1)
                if c == 26 and it + 1 < NT0:
                    p0_b(it + 1)
                M = 128 if c < NFM else 16
                pi = mmbank[bi % 6]; bi += 1
                for kc in range(8):
                    mm(PS[pi][0:M, :], wfm[:, kc, c * 128:c * 128 + M], hT[:, kc, :], kc == 0, kc == 7,
                       reads=[B_wfm, B_hT], writes=[PSB[pi]])
                if c == NFM:
                    P.op("dve", lambda e, pi=pi: e.tensor_copy(out=alo, in_=PS[pi][0:16, :]),
                         reads=[PSB[pi]], writes=[B_alo])
                    continue
                bo = B_fmo[c // 10]
                if c < 4:
                    evac_copy(fmo[:, c, :], PS[pi], [PSB[pi]], [bo], scale=0.125)
                elif c >= FM_MG:
                    P.op("act", lambda e, c=c, pi=pi: e.activation(out=fmo[:, c, :], in_=PS[pi], func=AF.Sigmoid),
                         reads=[PSB[pi]], writes=[bo])
                else:
                    evac_copy(fmo[:, c, :], PS[pi], [PSB[pi]], [bo])
                if c % 10 == 9:
                    g0 = c - 9
                    dma("sp", s_fm[g0:g0 + 10, :, t0:t0 + 512].rearrange("c p t -> p c t"), fmo[:, g0:g0 + 10, :],
                        reads=[bo])
            dma("sp", s_al[:, t0:t0 + 512], alo, reads=[B_alo])
            for s in range(4):
                for (c0, c1) in ((0, 512), (512, 1024), (1024, 1536), (1536, 1560)):
                    pi = mmbank[bi % 6]; bi += 1
                    for kc in range(8):
                        mm(PS[pi][:, 0:c1 - c0], hT[:, kc, s * 128:(s + 1) * 128], wtm[:, kc, c0:c1], kc == 0, kc == 7,
                           reads=[B_wtm, B_hT], writes=[PSB[pi]])
                    if c0 < 1536:
                        evac_copy(tmo[:, s, c0:c1], PS[pi], [PSB[pi]], [B_tmo])
                    else:
                        evac_copy(gto[:, s, :], PS[pi][:, 0:24], [PSB[pi]], [B_gto])
            dma("sp", s_tm[t0:t0 + 512, :].rearrange("(s p) n -> p s n", p=128), tmo, reads=[B_tmo])
            dma("sp", s_gate[t0:t0 + 512, :].rearrange("(s p) n -> p s n", p=128), gto, reads=[B_gto])
        A.release()


    def phase1_nsa():
        B_c1 = Buf("c1")
        cmpD = A.alloc([128, SEQ], F32, "cmpD")
        dma("sp", cmpD[0:127, :], cd["cmpD"][:, :], writes=[B_c1])
        fmul = A.alloc([128, 16, 32], F32, "fmul"); fadd = A.alloc([128, 16, 32], F32, "fadd")
        dma("sp", fmul, cd["fmul"][:, :, :], writes=[B_c1]); dma("sp", fadd, cd["fadd"][:, :, :], writes=[B_c1])
        tbt = A.alloc([32, SEQ], F32, "tb"); dma("sp", tbt, cd["tb"][:, :], writes=[B_c1])
        ea = A.alloc([128, SEQ], BF16, "ea")
        G_memset(ea, 0.0, [B_c1])
        dma("pool", ea[0:34, :], cd["ea"][:, :], writes=[B_c1])
        cbt = A.alloc([128, 896], BF16, "cb"); dma("pool", cbt, cd["cb"][:, :], writes=[B_c1])
        wbt = A.alloc([128, 8, 1152], BF16, "wb"); dma("pool", wbt, cd["wb"][:, :, :], writes=[B_c1])
        MbAs = [A.alloc([128, 8, 512], BF16, "MbA%d" % i) for i in range(2)]
        B_mbs = [[Buf("mb%d_%d" % (i, h)) for h in range(8)] for i in range(2)]
        for i in range(2):
            G_memset(MbAs[i], 0.0, B_mbs[i])
            dma("pool", MbAs[i][32:34, :, :], cd["crow"][:, :, :], writes=B_mbs[i])
        W1 = {}; W2 = {}; peT = {}; cbias = {}
        B_cw = Buf("cw")
        for nm, w1d, w2d, ped in (("k", w1k_d, w2k_d, pek_d), ("v", w1v_d, w2v_d, pev_d)):
            W1[nm] = A.alloc([128, 32, 128], BF16, "w1" + nm)
            src = w1d.rearrange("(p d) h -> d p h", d=64)
            dma("pool", W1[nm][0:64, :, :], src, writes=[B_cw])
            dma("pool", W1[nm][64:128, :, :], src, writes=[B_cw])
            W2[nm] = A.alloc([128, 128 if nm == "k" else 64], BF16, "w2" + nm)
            dma("pool", W2[nm], w2d[:, :], writes=[B_cw])
            peT[nm] = A.alloc([64, 32], BF16, "pe" + nm)
            dma("pool", peT[nm], ped[:, :], writes=[B_cw])
            cbias[nm] = A.alloc([128, 1], F32, "cbias" + nm)
        B_cb = Buf("cbias")
        for nm in ("k", "v"):
            for p in range(32):
                mm(PS[7][:, 0:1], W1[nm][0:64, p, :], peT[nm][0:64, p:p + 1], p == 0, p == 31, reads=[B_cw], writes=[PSB[7]])
            V_copy(cbias[nm], PS[7][:, 0:1], [PSB[7]], [B_cb])
        gateb = A.alloc([128, 24], F32, "gateb"); dma("sp", gateb, gate_b_d[:, :], writes=[B_c1])
        stage(1)
        qa = A.alloc([128, 4, SEQ], BF16, "qa"); B_qa = Buf("qa")
        kcT = A.alloc([128, SEQ], BF16, "kcT"); vcT = A.alloc([128, SEQ], BF16, "vcT"); B_kvc = Buf("kvc")
        ksT = A.alloc([128, 4, SEQ], BF16, "ksT"); B_ks = Buf("ks")
        kwT = A.alloc([128, 4, SEQ], BF16, "kwT"); B_kw = Buf("kw")
        vsa = A.alloc([128, 16, 2, 65], BF16, "vsa"); B_vs = Buf("vs")
        vwa = A.alloc([128, 16, 2, 65], BF16, "vwa"); B_vw = Buf("vw")
        G_memset(vsa[:, :, :, 64:65], 1.0, [B_vs])
        G_memset(vwa[:, :, :, 64:65], 1.0, [B_vw])
        gsig = A.alloc([128, 16, 24], F32, "gsig"); B_gs = Buf("gs")
        hidT = A.alloc([128, 128], BF16, "hidT"); B_hid = Buf("hid")
        kcmpT = A.alloc([128, 2, 128], BF16, "kcmpT"); B_kcmp = Buf("kcmp")
        Rg = A.alloc([128, 2, 97], BF16, "Rg"); B_R = Buf("R")
        G_memset(Rg[:, :, 64:65], 1.0, [B_R])
        for g in range(2):
            dma("pool", Rg[0:127, g, 65:97], cd["ovl"][:, :], writes=[B_R])
        S_sb = A.alloc([128, 512], F32, "S_sb"); B_S = Buf("S")
        PTs = [A.alloc([128, 512], BF16, "PT%d" % i) for i in range(3)]; B_PT = [Buf("PT%d" % i) for i in range(3)]
        oaccs = [A.alloc([128, 4, 8, 64], F32, "oacc%d" % i) for i in range(2)]; B_oas = [Buf("oacc%d" % i) for i in range(2)]
        otmp = A.alloc([128, 4, 64], F32, "otmp"); B_ot = Buf("otmp")
        onb = A.alloc([128, 4, 512], BF16, "onb"); B_onb = Buf("onb")
        onT = A.alloc([128, 4, 512], BF16, "onT"); B_onT = Buf("onT")
        imp = A.alloc([128, 4, 32], F32, "imp"); B_imp = Buf("imp")
        itmp = A.alloc([128, 4, 32], F32, "itmp"); B_it = Buf("itmp")
        t8 = A.alloc([128, 4, 8], F32, "t8"); B_t8 = Buf("t8")
        selb = A.alloc([128, 4, 32], F32, "selb"); B_selb = Buf("selb")
        selT = A.alloc([32, 512], F32, "selT"); B_selT = Buf("selT")
        sm = A.alloc([128, 16], F32, "sm"); B_sm = Buf("sm")
        pt_i = [0]; sc_i = [0]; acc_i = [0]; ptc_i = [0]
        PTc = [A.alloc([128, 512], BF16, "PTc%d" % i) for i in range(2)]; B_PTc = [Buf("PTc%d" % i) for i in range(2)]

        def pv_evac(pacc, pb, qt, h, br, first, par):
            oacc = oaccs[par]; B_oa = B_oas[par]
            if br == 0:
                V_ts(sm[:, 0:4], pacc[:, :, 64], 1e-30, None, ALU.max, None, [PSB[pb]], [B_sm])
                V_recip(sm[:, 4:8], sm[:, 0:4], [B_sm], [B_sm])
            else:
                V_recip(sm[:, 4:8], pacc[:, :, 64], [PSB[pb]], [B_sm])
            V_tt(sm[:, 8:12], sm[:, 4:8], gsig[:, 4 * qt:4 * qt + 4, 3 * h + br], ALU.mult, [B_sm, B_gs], [B_sm])
            rgb = sm[:, 8:12].unsqueeze(2).to_broadcast([128, 4, 64])
            if first:
                V_tt(oacc[:, :, h, :], pacc[:, :, 0:64], rgb, ALU.mult, [PSB[pb], B_sm], [B_oa])
            else:
                V_tt(otmp, pacc[:, :, 0:64], rgb, ALU.mult, [PSB[pb], B_sm], [B_ot])
                V_tt(oacc[:, :, h, :], oacc[:, :, h, :], otmp, ALU.add, [B_oa, B_ot], [B_oa])

        for sq in range(NSEQ):
            tb0 = sq * SEQ
            dma("sp", qa, s_fm[FM_QA:FM_QA + 4, :, tb0:tb0 + SEQ].rearrange("c p t -> p c t"), writes=[B_qa])
            dma("sp", kcT, s_fm[FM_KC, :, tb0:tb0 + SEQ], writes=[B_kvc])
            dma("sp", vcT, s_fm[FM_VC, :, tb0:tb0 + SEQ], writes=[B_kvc])
            for g in range(2):
                for hf in range(2):
                    dma("sp", ksT[:, 2 * g + hf, :], s_fm[FM_KS + g, :, tb0:tb0 + SEQ], writes=[B_ks])
                    dma("sp", kwT[:, 2 * g + hf, :], s_fm[FM_KW + g, :, tb0:tb0 + SEQ], writes=[B_kw])
                    zlo = 64 * (1 - hf)
                    G_memset(ksT[zlo:zlo + 64, 2 * g + hf, :], 0.0, [B_ks])
                    G_memset(kwT[zlo:zlo + 64, 2 * g + hf, :], 0.0, [B_kw])
            for g in range(2):
                dma("sp", vsa[:, :, g, 0:64],
                    s_tm[tb0:tb0 + SEQ, TM_VS + 64 * g:TM_VS + 64 * g + 64].rearrange("(kt p) d -> p kt d", p=128),
                    writes=[B_vs])
                dma("sp", vwa[:, :, g, 0:64],
                    s_tm[tb0:tb0 + SEQ, TM_VW + 64 * g:TM_VW + 64 * g + 64].rearrange("(kt p) d -> p kt d", p=128),
                    writes=[B_vw])
            dma("sp", gsig, s_gate[tb0:tb0 + SEQ, :].rearrange("(kt p) n -> p kt n", p=128), writes=[B_gs])
            V_tt(gsig, gsig, gateb.unsqueeze(1).to_broadcast([128, 16, 24]), ALU.add, [B_gs, B_c1], [B_gs])
            A_act(gsig, gsig, AF.Sigmoid, [B_gs], [B_gs])
            stage(2)
            for nm, srcT in (("k", kcT), ("v", vcT)):
                s3 = srcT.rearrange("q (n s) -> q n s", s=16)
                for g in range(2):
                    for p in range(32):
                        mm(PS[7][:, 0:127], W1[nm][64 * g:64 * g + 64, p, :], s3[64 * g:64 * g + 64, p // 16:p // 16 + 127, p % 16],
                           p == 0, p == 31, reads=[B_cw, B_kvc], writes=[PSB[7]])
                    A_act(hidT[:, 0:127], PS[7][:, 0:127], AF.Gelu_apprx_tanh, [PSB[7], B_cb], [B_hid], bias=cbias[nm])
                    if nm == "k":
                        mm(PS[6][:, 0:127], W2["k"], hidT[:, 0:127], True, True, reads=[B_cw, B_hid], writes=[PSB[6]])
                        evac_copy(kcmpT[:, g, 0:127], PS[6][:, 0:127], [PSB[6]], [B_kcmp])
                    else:
                        mm(PS[6][0:127, 0:64], hidT[:, 0:127], W2["v"], True, True, reads=[B_cw, B_hid], writes=[PSB[6]])
                        evac_copy(Rg[0:127, g, 0:64], PS[6][0:127, 0:64], [PSB[6]], [B_R])
            stage(3)
            def cmp_stages(qt):
                q0 = qt * 512
                par = qt % 2
                st_list = []
                for g in range(2):
                    for gi in range(4):
                        h = 4 * g + gi
                        hp = 64 * (h % 2)
                        box = {}

                        def s1(h=h, hp=hp, g=g, box=box):
                            pi = sc_i[0] % 3; sc_i[0] += 1
                            mm(PS[pi][0:127, :], kcmpT[hp:hp + 64, g, 0:127], qa[hp:hp + 64, h // 2, q0:q0 + 512], True, True,
                               reads=[B_kcmp, B_qa], writes=[PSB[pi]])
                            V_stt(S_sb[0:127, :], cmpD[0:127, q0:q0 + 512], SLOPES[h], PS[pi][0:127, :], ALU.mult, ALU.add,
                                  [PSB[pi], B_c1], [B_S])
                            k = ptc_i[0] % 2; ptc_i[0] += 1
                            box["k"] = k
                            A_act(PTc[k][0:127, :], S_sb[0:127, :], AF.Exp, [B_S], [B_PTc[k]])

                        def s2(h=h, g=g, gi=gi, box=box):
                            k = box["k"]
                            pu = PS[5][:, 0:388].rearrange("p (s c) -> p s c", s=4)
                            for sub in range(4):
                                mm(pu[:, sub, :], PTc[k][0:127, sub * 128:(sub + 1) * 128], Rg[0:127, g, :], True, True,
                                   reads=[B_PTc[k], B_R], writes=[PSB[5]])
                            pv_evac(pu, 5, qt, h, 0, True, par)
                            rdb = sm[:, 4:8].unsqueeze(2).to_broadcast([128, 4, 32])
                            if gi == 0:
                                V_tt(imp, pu[:, :, 65:97], rdb, ALU.mult, [PSB[5], B_sm], [B_imp])
                            else:
                                V_tt(itmp, pu[:, :, 65:97], rdb, ALU.mult, [PSB[5], B_sm], [B_it])
                                V_tt(imp, imp, itmp, ALU.add, [B_imp, B_it], [B_imp])
                            if gi == 3:
                                V_tt(imp, imp, fmul[:, 4 * qt:4 * qt + 4, :], ALU.mult, [B_imp, B_c1], [B_imp])
                                V_tt(imp, imp, fadd[:, 4 * qt:4 * qt + 4, :], ALU.add, [B_imp, B_c1], [B_imp])
                                for sub in range(4):
                                    V_max(t8[:, sub, :], imp[:, sub, :], [B_imp], [B_t8])
                                for sub in range(4):
                                    V_ts(selb[:, sub, :], imp[:, sub, :], t8[:, sub, 7:8], -NEG, ALU.is_ge, ALU.mult,
                                         [B_imp, B_t8], [B_selb])

                        st_list.append(s1)
                        st_list.append(s2)

                    def s3(g=g):
                        for sub in range(4):
                            transpose(PS[6][0:32, sub * 128:(sub + 1) * 128], selb[:, sub, :], ident_f,
                                      reads=[B_selb, B_ident], writes=[PSB[6]])
                        V_ts(selT, PS[6][0:32, :], NEG, None, ALU.add, None, [PSB[6]], [B_selT])
                        for gi in range(4):
                            h = 4 * g + gi
                            V_stt(MbAs[par][0:32, h, :], tbt[0:32, q0:q0 + 512], -SLOPES[h], selT, ALU.mult, ALU.add,
                                  [B_c1, B_selT], [B_mbs[par][h]])

                    st_list.append(s3)
                return st_list

            for fn in cmp_stages(0):
                fn()
            for qt in range(4):
                q0 = qt * 512
                par = qt % 2
                nxt = cmp_stages(qt + 1) if qt < 3 else []
                items = []
                for h in range(8):
                    g = h // 4; qc = h // 2
                    for br in (1, 2):
                        pb = 3 + acc_i[0] % 2; acc_i[0] += 1
                        pacc = PS[pb][:, 0:260].rearrange("p (s c) -> p s c", s=4)
                        if br == 1:
                            kts = list(range(0, 4 * qt + 4))
                        else:
                            kts = list(range(max(0, 4 * qt - 2), 4 * qt + 4))
                        pairs = []
                        for kt in kts:
                            off = kt * 128 - q0
                            for sub in range(4):
                                dmax = 128 * sub + 127 - off
                                dmin = 128 * sub - 127 - off
                                if dmax < 0:
                                    continue
                                if br == 2 and dmin >= 256:
                                    continue
                                pairs.append((kt, sub))
                        lastkt = {}
                        for kt, sub in pairs:
                            lastkt[sub] = kt
                        for kt in kts:
                            items.append(dict(h=h, g=g, qc=qc, br=br, kt=kt, pb=pb, pacc=pacc, pairs=pairs, lastkt=lastkt,
                                              firstkt=(kt == kts[0]), lastk=(kt == kts[-1])))

                def emit_scores(it):
                    h = it["h"]; g = it["g"]; br = it["br"]; kt = it["kt"]
                    off = kt * 128 - q0
                    pi = sc_i[0] % 3; sc_i[0] += 1
                    kT = ksT if br == 1 else kwT
                    Bk = B_ks if br == 1 else B_kw
                    mm(PS[pi], kT[:, 2 * g + (h % 2), kt * 128:(kt + 1) * 128], qa[:, it["qc"], q0:q0 + 512], True, False,
                       reads=[Bk, B_qa], writes=[PSB[pi]])
                    if br == 1:
                        diag = off >= 0
                        mm(PS[pi], ea[:, kt * 128:(kt + 1) * 128], MbAs[par][:, h, :], False, not diag,
                           reads=[B_c1, B_mbs[par][h]], writes=[PSB[pi]])
                        if diag:
                            mm(PS[pi], ident_b, cbt[:, 384 - off:384 - off + 512], False, True,
                               reads=[B_ident, B_c1], writes=[PSB[pi]])
                    else:
                        mm(PS[pi], ident_b, wbt[:, h, 384 - off:384 - off + 512], False, True,
                           reads=[B_ident, B_c1], writes=[PSB[pi]])
                    k = pt_i[0] % 3; pt_i[0] += 1
                    it["k"] = k
                    A_act(PTs[k], PS[pi], AF.Exp, [PSB[pi]], [B_PT[k]])

                def emit_pv(it):
                    h = it["h"]; g = it["g"]; br = it["br"]; kt = it["kt"]; k = it["k"]; pb = it["pb"]
                    va = vsa if br == 1 else vwa
                    Bv = B_vs if br == 1 else B_vw
                    first = it["firstkt"]
                    for sub in range(4):
                        if (kt, sub) not in it["pairs"]:
                            continue
                        mm(it["pacc"][:, sub, :], PTs[k][:, sub * 128:(sub + 1) * 128], va[:, kt, g, :], first, it["lastkt"][sub] == kt,
                           reads=[B_PT[k], Bv], writes=[PSB[pb]], skip=True)
                        first = False
                    if it["lastk"]:
                        pv_evac(it["pacc"], pb, qt, h, br, False, par)

                LA = 2
                step = max(1, len(items) // (len(nxt) + 1))
                for i in range(len(items) + LA):
                    if i < len(items):
                        emit_scores(items[i])
                    if i >= LA:
                        emit_pv(items[i - LA])
                    if nxt and i % step == step - 1:
                        nxt.pop(0)()
                while nxt:
                    nxt.pop(0)()
                oacc_p = oaccs[par]
                A_act(onb.rearrange("p a b -> p (a b)"), oacc_p.rearrange("p a h d -> p (a h d)"), AF.Copy, [B_oas[par]], [B_onb])
                for fc in range(4):
                    pst = PS[6].bitcast(BF16)
                    for sub in range(4):
                        transpose(pst[:, sub * 128:(sub + 1) * 128], onb[:, sub, fc * 128:(fc + 1) * 128], ident_b,
                                  reads=[B_onb, B_ident], writes=[PSB[6]])
                    evac_copy(onT[:, fc, :], pst[:, 0:512], [PSB[6]], [B_onT])
                dma("sp", s_on[:, :, tb0 + q0:tb0 + q0 + 512].rearrange("c p t -> p c t"), onT, reads=[B_onT])

    def phase1_gla():
        B_gc = Buf("gc")
        wa2 = A.alloc([16, 256], F32, "wa2"); dma("sp", wa2, wa2_d[0:16, :], writes=[B_gc])
        ba = A.alloc([128, 2], F32, "ba"); dma("sp", ba, ba_d[:, :], writes=[B_gc])
        nba = A.alloc([128, 2], F32, "nba"); V_ts(nba, ba, -1.0, None, ALU.mult, None, [B_gc], [B_gc])
        srst = A.alloc([128, SEQ], F32, "srst"); dma("sp", srst, cd["srst"][:, :], writes=[B_gc])
        gmask = A.alloc([128, 128], F32, "gmask"); dma("sp", gmask, cd["gmask"][:, :], writes=[B_gc])
        glag = A.alloc([128, 128], F32, "glag"); dma("sp", glag, gla_g_d[:, :], writes=[B_gc])
        alT = A.alloc([16, SEQ], F32, "alT"); B_al = Buf("al")
        qbT = A.alloc([128, 2, SEQ], BF16, "qbT"); kbT = A.alloc([128, 2, SEQ], BF16, "kbT"); B_qk = Buf("qk")
        vb = A.alloc([128, 16, 512], BF16, "vb"); B_vb = Buf("vb")
        rb = A.alloc([128, 16, 512], BF16, "rb"); B_rb = Buf("rb")
        sr = A.alloc([128, 16, 512], BF16, "sr"); B_sr = Buf("sr")
        rg = A.alloc([128, 16, 512], BF16, "rg"); B_rg = Buf("rg")
        laT = A.alloc([128, 2, SEQ], F32, "laT"); B_la = Buf("la")
        bT = A.alloc([128, 2, SEQ], F32, "bT"); B_b = Buf("b")
        ET = A.alloc([128, 2, SEQ], F32, "ET"); B_E = Buf("E")
        qd4 = A.alloc([128, 4, SEQ], BF16, "qd4"); B_qd = Buf("qd")
        kdT = A.alloc([128, 2, SEQ], BF16, "kdT"); B_kd = Buf("kd")
        dec = A.alloc([128, 2, 32], F32, "dec"); B_dec = Buf("dec")
        G_memset(qd4, 0.0, [B_qd])
        kd_ab = A.alloc([128, 2, 2, 128], BF16, "kd_ab"); B_kab = Buf("kab")
        G_memset(kd_ab, 0.0, [B_kab])
        Sf = [A.alloc([128, 128], F32, "Sf%d" % c) for c in range(2)]; B_Sf = [Buf("Sf%d" % c) for c in range(2)]
        tmpS = [A.alloc([128, 128], F32, "tS%d" % c) for c in range(2)]; B_tS = [Buf("tS%d" % c) for c in range(2)]
        Sbf = [[A.alloc([128, 128], BF16, "Sbf%d%d" % (c, a)) for a in range(2)] for c in range(2)]
        B_Sbf = [[Buf("Sbf%d%d" % (c, a)) for a in range(2)] for c in range(2)]
        att = A.alloc([128, 4, 128], BF16, "att"); B_att = Buf("att")
        ss = A.alloc([128, 16], F32, "ss"); B_ss = Buf("ss")
        junk = A.alloc([128, 128], BF16, "junk"); B_junk = Buf("junk")
        ogb = A.alloc([128, 512], BF16, "ogb"); B_ogb = Buf("ogb")
        ogT = A.alloc([128, 4, 512], BF16, "ogT"); B_ogT = Buf("ogT")
        for sq in range(NSEQ):
            tb0 = sq * SEQ
            dma("sp", alT, s_al[:, tb0:tb0 + SEQ], writes=[B_al])
            dma("sp", qbT, s_fm[FM_QB:FM_QB + 2, :, tb0:tb0 + SEQ].rearrange("c p t -> p c t"), writes=[B_qk])
            dma("sp", kbT, s_fm[FM_KB:FM_KB + 2, :, tb0:tb0 + SEQ].rearrange("c p t -> p c t"), writes=[B_qk])
            dma("sp", vb, s_tm[tb0:tb0 + SEQ, TM_VB:TM_VB + 512].rearrange("(kt p) n -> p kt n", p=128), writes=[B_vb])
            dma("sp", rb, s_tm[tb0:tb0 + SEQ, TM_RB:TM_RB + 512].rearrange("(kt p) n -> p kt n", p=128), writes=[B_rb])
            for c in range(2):
                for tt in range(4):
                    mm(PS[0], wa2[0:16, c * 128:(c + 1) * 128], alT[0:16, tt * 512:(tt + 1) * 512], True, True,
                       reads=[B_gc, B_al], writes=[PSB[0]])
                    A_act(laT[:, c, tt * 512:(tt + 1) * 512], PS[0], AF.Exp, [PSB[0], B_gc], [B_la], scale=-1.0, bias=nba[:, c:c + 1])
                A_act(laT[:, c, :], laT[:, c, :], AF.Ln, [B_la], [B_la], bias=1.0)
                P.op("dve", lambda e, c=c: e.tensor_tensor_scan(out=bT[:, c, :], data0=srst, data1=laT[:, c, :], initial=0.0,
                                                                op0=ALU.mult, op1=ALU.add), reads=[B_gc, B_la], writes=[B_b])
                A_act(ET[:, c, :], bT[:, c, :], AF.Exp, [B_b], [B_E], scale=-1.0 / 16)
                A_act(dec[:, c, :], bT[:, c, :].rearrange("p (n s) -> p n s", s=64)[:, :, 63], AF.Exp, [B_b], [B_dec], scale=-1.0 / 16)
                for hh in range(2):
                    lo = 64 * hh
                    V_stt(qd4[lo:lo + 64, 2 * c + hh, :], qbT[lo:lo + 64, c, :], 0.125, ET[lo:lo + 64, c, :], ALU.mult, ALU.mult,
                          [B_qk, B_E], [B_qd])
            for c in range(2):
                A_act(ET[:, c, :], bT[:, c, :], AF.Exp, [B_b], [B_E], scale=1.0 / 16)
                V_tt(kdT[:, c, :], kbT[:, c, :], ET[:, c, :], ALU.mult, [B_qk, B_E], [B_kd])
            A_act(sr, rb, AF.Sigmoid, [B_rb], [B_sr])
            G_tt(rg, rb, sr, ALU.mult, [B_rb, B_sr], [B_rg])
            rg4 = rg.rearrange("p k (h e) -> p (k h) e", e=128)
            G_tt(rg4, rg4, glag.unsqueeze(1).to_broadcast([128, 64, 128]), ALU.mult, [B_rg, B_gc], [B_rg])
            for c in range(2):
                P.op("dve", lambda e, c=c: e.memset(Sf[c], 0.0), writes=[B_Sf[c]])
            if "cs" in dbg and sq == 0:
                dma("sp", dbg["cs"][:, :, :], bT, reads=[B_b]); dma("sp", dbg["kd"][:, :, :], kdT, reads=[B_kd])
                dma("sp", dbg["qd"][:, :, :], qd4, reads=[B_qd]); dma("sp", dbg["rg"][:, :, :], rg, reads=[B_rg])
                dma("sp", dbg["la"][:, :, :], laT, reads=[B_la])
            for blk in range(16):
                t1 = blk * 128
                pst = PS[4].bitcast(BF16)
                for c in range(2):
                    transpose(pst[:, c * 128:(c + 1) * 128], kdT[:, c, t1:t1 + 128], ident_b, reads=[B_kd, B_ident], writes=[PSB[4]])
                for c in range(2):
                    V_copy(kd_ab[0:64, c, 0, :], pst[0:64, c * 128:(c + 1) * 128], [PSB[4]], [B_kab])
                    V_copy(kd_ab[64:128, c, 1, :], pst[64:128, c * 128:(c + 1) * 128], [PSB[4]], [B_kab])
                PSm = [PS[1][:, 0:256].rearrange("p (a e) -> p a e", a=2), PS[2][:, 0:256].rearrange("p (a e) -> p a e", a=2)]
                for c in range(2):
                    for ab in range(2):
                        for hh in range(2):
                            h = 2 * c + hh
                            mm(PSm[c][64 * hh:64 * hh + 64, ab, :], kd_ab[:, c, ab, 64 * hh:64 * hh + 64], vb[:, blk, h * 128:(h + 1) * 128],
                               True, True, reads=[B_kab, B_vb], writes=[PSB[1 + c]])
                PSa = PS[0].rearrange("p (h i) -> p h i", h=4)
                for h in range(4):
                    mm(PSa[:, h, :], kdT[:, h // 2, t1:t1 + 128], qd4[:, h, t1:t1 + 128], True, True,
                       reads=[B_kd, B_qd], writes=[PSB[0]])
                V_tt(att, PSa, gmask.unsqueeze(1).to_broadcast([128, 4, 128]), ALU.mult, [PSB[0], B_gc], [B_att])
                for c in range(2):
                    V_copy(Sbf[c][0], Sf[c], [B_Sf[c]], [B_Sbf[c][0]])
                    V_tt(tmpS[c], PSm[c][:, 0, :], Sf[c], ALU.add, [PSB[1 + c], B_Sf[c]], [B_tS[c]])
                    V_ts(Sf[c], tmpS[c], dec[:, c, 2 * blk:2 * blk + 1], None, ALU.mult, None, [B_tS[c], B_dec], [B_Sf[c]])
                    V_copy(Sbf[c][1], Sf[c], [B_Sf[c]], [B_Sbf[c][1]])
                    V_tt(tmpS[c], PSm[c][:, 1, :], Sf[c], ALU.add, [PSB[1 + c], B_Sf[c]], [B_tS[c]])
                    V_ts(Sf[c], tmpS[c], dec[:, c, 2 * blk + 1:2 * blk + 2], None, ALU.mult, None, [B_tS[c], B_dec], [B_Sf[c]])
                PSo = PS[3].rearrange("p (h e) -> p h e", h=4)
                for h in range(4):
                    c = h // 2
                    mm(PSo[:, h, :], att[:, h, :], vb[:, blk, h * 128:(h + 1) * 128], True, False,
                       reads=[B_att, B_vb], writes=[PSB[3]], skip=True)
                    mm(PSo[0:64, h, :], qd4[:, h, t1:t1 + 64], Sbf[c][0], False, False,
                       reads=[B_qd, B_Sbf[c][0]], writes=[PSB[3]], skip=True)
                    mm(PSo[64:128, h, :], qd4[:, h, t1 + 64:t1 + 128], Sbf[c][1], False, True,
                       reads=[B_qd, B_Sbf[c][1]], writes=[PSB[3]], skip=True)
                for h in range(4):
                    A_act(junk, PSo[:, h, :], AF.Square, [PSB[3]], [B_junk, B_ss], accum_out=ss[:, h:h + 1])
                A_act(ss[:, 4:8], ss[:, 0:4], AF.Sqrt, [B_ss], [B_ss], bias=EPS, scale=1.0 / 128)
                V_recip(ss[:, 8:12], ss[:, 4:8], [B_ss], [B_ss])
                for h in range(4):
                    V_stt(ogb[:, h * 128:(h + 1) * 128], PSo[:, h, :], ss[:, 8 + h:9 + h], rg[:, blk, h * 128:(h + 1) * 128],
                          ALU.mult, ALU.mult, [PSB[3], B_ss, B_rg], [B_ogb])
                if "ogb" in dbg and sq == 0 and blk == 0:
                    dma("sp", dbg["ogb"][:, :], ogb, reads=[B_ogb]); dma("sp", dbg["att"][:, :, :], att, reads=[B_att])
                pst2 = PS[5].bitcast(BF16)
                for fc in range(4):
                    transpose(pst2[:, fc * 128:(fc + 1) * 128], ogb[:, fc * 128:(fc + 1) * 128], ident_b,
                              reads=[B_ogb, B_ident], writes=[PSB[5]])
                evac_copy(ogT[:, :, (blk % 4) * 128:(blk % 4) * 128 + 128], pst2[:, 0:512].rearrange("p (f t) -> p f t", f=4),
                          [PSB[5]], [B_ogT])
                if blk % 4 == 3:
                    q0 = (blk // 4) * 512
                    dma("sp", s_og[:, :, tb0 + q0:tb0 + q0 + 512].rearrange("c p t -> p c t"), ogT, reads=[B_ogT])
    if "p1" in phases:
        P.fence()
        A.mark()
        try:
            if "nonsa" not in phases:
                phase1_nsa()
        except StopBuild:
            pass
        A.release()
        P.fence()
        A.mark()
        phase1_gla()
        A.release()
    def phase2():
        B_w2 = Buf("w2")
        wbn = A.alloc([128, 4, D], BF16, "wbn"); load_cast(wbn, wbn_d.rearrange("(kc p) n -> p kc n", p=128), B_w2)
        wbg = A.alloc([128, 4, D], BF16, "wbg"); load_cast(wbg, wbg_d.rearrange("(kc p) n -> p kc n", p=128), B_w2)
        wout = A.alloc([128, 8, D], BF16, "wout"); load_cast(wout, wout_d.rearrange("(kc p) n -> p kc n", p=128), B_w2)
        wxq = A.alloc([128, 8, 512], BF16, "wxq"); load_cast(wxq, wxq_d.rearrange("(kc p) n -> p kc n", p=128), B_w2)
        wxkv = A.alloc([128, 8, D], BF16, "wxkv"); load_cast(wxkv, wxkv_d.rearrange("(kc p) n -> p kc n", p=128), B_w2)
        wxo = A.alloc([128, 4, D], BF16, "wxo"); load_cast(wxo, wxo_d.rearrange("(kc p) n -> p kc n", p=128), B_w2)
        gx = A.alloc([128, 8], F32, "gx"); gm = A.alloc([128, 8], F32, "gm"); B_g = Buf("g2")
        dma("sp", gx, g_x_d[:, :], writes=[B_g]); dma("sp", gm, g_mem_d[:, :], writes=[B_g])
        xt = A.alloc([128, 4, D], F32, "xt"); B_xt = Buf("xt")
        xn = A.alloc([128, 4, D], BF16, "xn"); B_xn = Buf("xn")
        st = A.alloc([128, 32], F32, "st"); B_st = Buf("st")
        hxT = A.alloc([128, 8, 512], BF16, "hxT"); B_hx = Buf("hx")
        memt = A.alloc([128, 2, D], F32, "memt"); B_mem = Buf("mem")
        memT = A.alloc([128, 8, 256], BF16, "memT"); B_memT = Buf("memT")
        kxT = A.alloc([128, 4, 256], BF16, "kxT"); B_kx = Buf("kx")
        vxa = A.alloc([128, 2, 4, 129], BF16, "vxa"); B_vx = Buf("vx")
        G_memset(vxa[:, :, :, 128:129], 1.0, [B_vx])
        onT = A.alloc([128, 4, 512], BF16, "onT2"); ogT = A.alloc([128, 4, 512], BF16, "ogT2"); B_o = Buf("o2")
        sg = A.alloc([128, 16, 512], BF16, "sg"); B_sg = Buf("sg")
        mixT = A.alloc([128, 8, 512], BF16, "mixT"); B_mix = Buf("mix")
        tmp1 = [A.alloc([128, 512], F32, "tmp1%d" % i) for i in range(2)]; tmp2 = [A.alloc([128, 512], F32, "tmp2%d" % i) for i in range(2)]
        B_t1 = [Buf("t1%d" % i) for i in range(2)]; B_t2 = [Buf("t2%d" % i) for i in range(2)]
        qxT = A.alloc([128, 4, 512], BF16, "qxT"); B_qx = Buf("qx")
        PTx = [A.alloc([128, 512], BF16, "PTx%d" % i) for i in range(2)]; B_PTx = [Buf("PTx%d" % i) for i in range(2)]
        oxb = A.alloc([128, 4, 512], BF16, "oxb"); B_oxb = Buf("oxb")
        oxT = A.alloc([128, 4, 512], BF16, "oxT"); B_oxT = Buf("oxT")
        rd = A.alloc([128, 8], F32, "rd"); B_rd = Buf("rd")
        bk = [0]
        B_mixc = [Buf("mix%d" % i) for i in range(8)]

        def bank():
            b = 2 + bk[0] % 4; bk[0] += 1
            return b

        def p2_prefetch(it):
            t0 = it * 512
            dma("sp", onT, s_on[:, :, t0:t0 + 512].rearrange("c p t -> p c t"), writes=[B_o])
            dma("sp", ogT, s_og[:, :, t0:t0 + 512].rearrange("c p t -> p c t"), writes=[B_o])
            dma("sp", sg, s_fm[FM_MG:FM_MG + 16, :, t0:t0 + 512].rearrange("c p t -> p c t"), writes=[B_sg])

        for it in range(NTOK // 512):
            t0 = it * 512
            if it % 4 == 0:
                sq = it // 4
                dma("sp", memt, mem_d[sq * MEM:(sq + 1) * MEM, :].rearrange("(s p) d -> p s d", p=128), writes=[B_mem])
                rmsnorm_T(memt, B_mem, 2, gm, B_g, memT, B_memT, xn, B_xn, st, B_st, [0, 1])
                for hd in range(4):
                    pi = bank()
                    for kc in range(8):
                        mm(PS[pi][:, 0:256], wxkv[:, kc, hd * 128:(hd + 1) * 128], memT[:, kc, :], kc == 0, kc == 7,
                           reads=[B_w2, B_memT], writes=[PSB[pi]])
                    evac_copy(kxT[:, hd, :], PS[pi][:, 0:256], [PSB[pi]], [B_kx])
                for ms in range(2):
                    pi = bank()
                    for kc in range(8):
                        mm(PS[pi], memT[:, kc, ms * 128:(ms + 1) * 128], wxkv[:, kc, 512:1024], kc == 0, kc == 7,
                           reads=[B_w2, B_memT], writes=[PSB[pi]])
                    evac_copy(vxa[:, ms, :, 0:128], PS[pi].rearrange("p (h d) -> p h d", h=4), [PSB[pi]], [B_vx])
            dma("sp", xt, x_d[t0:t0 + 512, :].rearrange("(s p) d -> p s d", p=128), writes=[B_xt])
            if it == 0:
                p2_prefetch(0)
            for oc in range(8):
                p1 = bank(); p2 = bank()
                for kc in range(4):
                    mm(PS[p1], wbn[:, kc, oc * 128:(oc + 1) * 128], onT[:, kc, :], kc == 0, kc == 3, reads=[B_w2, B_o], writes=[PSB[p1]])
                for kc in range(4):
                    mm(PS[p2], wbg[:, kc, oc * 128:(oc + 1) * 128], ogT[:, kc, :], kc == 0, kc == 3, reads=[B_w2, B_o], writes=[PSB[p2]])
                j = oc % 2
                V_tt(tmp1[j], PS[p1], sg[:, oc, :], ALU.mult, [PSB[p1], B_sg], [B_t1[j]])
                V_tt(tmp2[j], PS[p2], sg[:, 8 + oc, :], ALU.mult, [PSB[p2], B_sg], [B_t2[j]])
                G_tt(mixT[:, oc, :], tmp1[j], tmp2[j], ALU.add, [B_t1[j], B_t2[j]], [B_mixc[oc]])
            if it + 1 < NTOK // 512:
                p2_prefetch(it + 1)
            for sub in range(4):
                for half in range(2):
                    pi = bank()
                    for kc in range(8):
                        mm(PS[pi], mixT[:, kc, sub * 128:(sub + 1) * 128], wout[:, kc, half * 512:(half + 1) * 512], kc == 0, kc == 7,
                           reads=[B_w2, B_mixc[kc]], writes=[PSB[pi]])
                    V_tt(xt[:, sub, half * 512:(half + 1) * 512], xt[:, sub, half * 512:(half + 1) * 512], PS[pi], ALU.add,
                         [B_xt, PSB[pi]], [B_xt])
            rmsnorm_T(xt, B_xt, 4, gx, B_g, hxT, B_hx, xn, B_xn, st, B_st, [0, 1])
            for hd in range(4):
                pi = bank()
                for kc in range(8):
                    mm(PS[pi], wxq[:, kc, hd * 128:(hd + 1) * 128], hxT[:, kc, :], kc == 0, kc == 7, reads=[B_w2, B_hx], writes=[PSB[pi]])
                evac_copy(qxT[:, hd, :], PS[pi], [PSB[pi]], [B_qx])
            for hd in range(4):
                for ms in range(2):
                    pi = bank()
                    mm(PS[pi], kxT[:, hd, ms * 128:(ms + 1) * 128], qxT[:, hd, :], True, True, reads=[B_kx, B_qx], writes=[PSB[pi]])
                    A_act(PTx[ms], PS[pi], AF.Exp, [PSB[pi]], [B_PTx[ms]], scale=128.0 ** -0.5)
                pa = [PS[6][:, 0:258].rearrange("p (s c) -> p s c", s=2), PS[7][:, 0:258].rearrange("p (s c) -> p s c", s=2)]
                for ms in range(2):
                    for sub in range(4):
                        mm(pa[sub // 2][:, sub % 2, :], PTx[ms][:, sub * 128:(sub + 1) * 128], vxa[:, ms, hd, :],
                           ms == 0 and sub % 2 == 0, ms == 1, reads=[B_PTx[ms], B_vx], writes=[PSB[6 + sub // 2]], skip=True)
                for bq in range(2):
                    V_recip(rd[:, 2 * bq:2 * bq + 2], pa[bq][:, :, 128], [PSB[6 + bq]], [B_rd])
                for sub in range(4):
                    V_ts(oxb[:, sub, hd * 128:(hd + 1) * 128], pa[sub // 2][:, sub % 2, 0:128], rd[:, sub:sub + 1], None, ALU.mult, None,
                         [PSB[6 + sub // 2], B_rd], [B_oxb])
            for fc in range(4):
                pi = bank()
                pst = PS[pi].bitcast(BF16)
                for sub in range(4):
                    transpose(pst[:, sub * 128:(sub + 1) * 128], oxb[:, sub, fc * 128:(fc + 1) * 128], ident_b,
                              reads=[B_oxb, B_ident], writes=[PSB[pi]])
                evac_copy(oxT[:, fc, :], pst[:, 0:512], [PSB[pi]], [B_oxT])
            for sub in range(4):
                for half in range(2):
                    pi = bank()
                    for kc in range(4):
                        mm(PS[pi], oxT[:, kc, sub * 128:(sub + 1) * 128], wxo[:, kc, half * 512:(half + 1) * 512], kc == 0, kc == 3,
                           reads=[B_w2, B_oxT], writes=[PSB[pi]])
                    V_tt(xt[:, sub, half * 512:(half + 1) * 512], xt[:, sub, half * 512:(half + 1) * 512], PS[pi], ALU.add,
                         [B_xt, PSB[pi]], [B_xt])
            dma("sp", s_x2[t0:t0 + 512, :].rearrange("(s p) d -> p s d", p=128), xt, reads=[B_xt])

    def phase3():
        B_w3 = Buf("w3")
        wup = A.alloc([128, 8, 2 * FFN], BF16, "wup"); load_cast(wup, wup_d.rearrange("(kc p) n -> p kc n", p=128), B_w3, nsplit=4)
        wdn = A.alloc([128, 22, D], BF16, "wdn"); load_cast(wdn, wdn_d.rearrange("(kc p) n -> p kc n", p=128), B_w3, nsplit=1)
        gf = A.alloc([128, 8], F32, "gf"); B_g = Buf("g3"); dma("sp", gf, g_ffn_d[:, :], writes=[B_g])
        cw = A.alloc([128, 3, 22], F32, "cw"); cbv = A.alloc([128, 22], F32, "cbv")
        dma("sp", cw, convw_d[:, :, :], writes=[B_g]); dma("sp", cbv, convb_d[:, :], writes=[B_g])
        gfin = A.alloc([128, D], F32, "gfin"); dma("sp", gfin, g_fin_d[:, :], writes=[B_g])
        xt = A.alloc([128, 4, D], F32, "xt"); B_xt = Buf("xt")
        xn = A.alloc([128, 4, D], BF16, "xn"); B_xn = Buf("xn")
        st = A.alloc([128, 32], F32, "st"); B_st = Buf("st")
        hfT = A.alloc([128, 8, 512], BF16, "hfT"); B_hf = Buf("hf")
        aT = A.alloc([128, 22, 512], BF16, "aT"); B_a = Buf("aT")
        usb = [A.alloc([128, 514], F32, "usb%d" % i) for i in range(2)]; B_u = [Buf("u%d" % i) for i in range(2)]
        acc = [A.alloc([128, 512], F32, "acc%d" % i) for i in range(2)]; B_acc = [Buf("acc%d" % i) for i in range(2)]
        carry = A.alloc([128, 22, 2], F32, "carry"); B_car = Buf("carry")
        bk = [0]

        def bank():
            b = (2 + bk[0]) % 8; bk[0] += 1
            return b

        B_ac = [Buf("aT%d" % i) for i in range(22)]
        for it in range(NTOK // 512):
            t0 = it * 512
            dma("sp", xt, s_x2[t0:t0 + 512, :].rearrange("(s p) d -> p s d", p=128), writes=[B_xt])
            if it % 4 == 0:
                P.op("dve", lambda e: e.memset(carry, 0.0), writes=[B_car])
            rmsnorm_T(xt, B_xt, 4, gf, B_g, hfT, B_hf, xn, B_xn, st, B_st, [0, 1])
            pend = []
            for fcn in range(22):
                pu = bank(); pg = bank()
                for kc in range(8):
                    mm(PS[pu], wup[:, kc, fcn * 128:(fcn + 1) * 128], hfT[:, kc, :], kc == 0, kc == 7, reads=[B_w3, B_hf], writes=[PSB[pu]])
                for kc in range(8):
                    mm(PS[pg], wup[:, kc, FFN + fcn * 128:FFN + (fcn + 1) * 128], hfT[:, kc, :], kc == 0, kc == 7,
                       reads=[B_w3, B_hf], writes=[PSB[pg]])
                j = fcn % 2
                V_copy(usb[j][:, 0:2], carry[:, fcn, :], [B_car], [B_u[j]])
                A_act(usb[j][:, 2:514], PS[pu], AF.Copy, [PSB[pu]], [B_u[j]])
                V_copy(carry[:, fcn, :], usb[j][:, 512:514], [B_u[j]], [B_car])
                V_ts(acc[j], usb[j][:, 2:514], cw[:, 2, fcn:fcn + 1], cbv[:, fcn:fcn + 1], ALU.mult, ALU.add, [B_u[j], B_g], [B_acc[j]])
                V_stt(acc[j], usb[j][:, 1:513], cw[:, 1, fcn:fcn + 1], acc[j], ALU.mult, ALU.add, [B_u[j], B_g, B_acc[j]], [B_acc[j]])
                V_stt(acc[j], usb[j][:, 0:512], cw[:, 0, fcn:fcn + 1], acc[j], ALU.mult, ALU.add, [B_u[j], B_g, B_acc[j]], [B_acc[j]])
                A_act(acc[j], acc[j], AF.Gelu_apprx_tanh, [B_acc[j]], [B_acc[j]])
                if pend:
                    pend.pop(0)()
                pend.append(lambda fcn=fcn, pg=pg, j=j: V_tt(aT[:, fcn, :], PS[pg], acc[j], ALU.mult, [PSB[pg], B_acc[j]], [B_ac[fcn]]))
            while pend:
                pend.pop(0)()
            for sub in range(4):
                for half in range(2):
                    pi = bank()
                    for kc in range(22):
                        mm(PS[pi], aT[:, kc, sub * 128:(sub + 1) * 128], wdn[:, kc, half * 512:(half + 1) * 512], kc == 0, kc == 21,
                           reads=[B_w3, B_ac[kc]], writes=[PSB[pi]])
                    V_tt(xt[:, sub, half * 512:(half + 1) * 512], xt[:, sub, half * 512:(half + 1) * 512], PS[pi], ALU.add,
                         [B_xt, PSB[pi]], [B_xt])
            if "aT" in dbg and it == 0:
                dma("sp", dbg["aT"][:, :, :], aT, reads=B_ac); dma("sp", dbg["x3"][:, :, :], xt, reads=[B_xt])
                dma("sp", dbg["hf"][:, :, :], hfT, reads=[B_hf])
            for s in range(4):
                A_act(xn[:, s, :], xt[:, s, :], AF.Square, [B_xt], [B_xn, B_st], accum_out=st[:, s:s + 1])
            A_act(st[:, 8:12], st[:, 0:4], AF.Sqrt, [B_st], [B_st], bias=EPS, scale=1.0 / D)
            V_recip(st[:, 16:20], st[:, 8:12], [B_st], [B_st])
            for s in range(4):
                V_stt(xt[:, s, :], xt[:, s, :], st[:, 16 + s:17 + s], gfin, ALU.mult, ALU.mult, [B_xt, B_st, B_g], [B_xt])
            dma("sp", out_d[t0:t0 + 512, :].rearrange("(s p) d -> p s d", p=128), xt, reads=[B_xt])

    if "p2" in phases:
        P.fence(); A.mark(); phase2(); A.release()
    if "p3" in phases:
        P.fence(); A.mark(); phase3(); A.release()
    for e in ENGS:
        last = {}
        for o in P.ops[e]:
            if o.dma:
                last[id(o.token)] = o
        seen = {}
        nd = 0
        for o in P.ops[e]:
            if o.dma:
                seen[nd % NDMA_SLOTS] = o
                nd += 1
        P.final += list(seen.values())
    P.emit(nc, stack)
    stack.close()
    return nc, consts


def prep_inputs(inp):
    f = lambda a: np.ascontiguousarray(np.asarray(a, dtype=np.float32))
    w_in = f(inp["w_in"][0])
    shared = {
        "w_fm": f(w_in[:, _fm_cols()]),
        "w_tm": f(w_in[:, _tm_cols()]),
        "g_mix": pmajor(inp["ln_mix_g"][0], 8),
        "gate_b": f(np.broadcast_to(np.asarray(inp["nsa_gate_b"][0]).reshape(1, 24), (128, 24))),
        "w1k": f(inp["cmp_w1_k"][0]), "w1v": f(inp["cmp_w1_v"][0]),
        "w2k": f(np.concatenate([inp["cmp_w2_k"][0], inp["cmp_w2_k"][0]], axis=1)),
        "w2v": f(inp["cmp_w2_v"][0]),
        "pek": f(np.asarray(inp["cmp_pos_k"][0]).T), "pev": f(np.asarray(inp["cmp_pos_v"][0]).T),
        "wa2": f(np.concatenate([inp["gla_w_alpha2"][0], np.asarray(inp["gla_b_alpha"][0]).reshape(1, 256)], axis=0)),
        "gla_g": f(np.broadcast_to(np.asarray(inp["gla_norm_g"][0]).reshape(1, 128), (128, 128))),
        "ba": pmajor(inp["gla_b_alpha"][0], 2),
        "wbn": f(inp["w_branch_nsa"][0]), "wbg": f(inp["w_branch_gla"][0]), "wout": f(inp["w_out"][0]),
        "g_x": pmajor(inp["ln_x_g"][0], 8), "g_mem": pmajor(inp["ln_mem_g"][0], 8),
        "wxq": f(inp["w_xq"][0]), "wxkv": f(inp["w_xkv"][0]), "wxo": f(inp["w_xo"][0]),
        "g_ffn": pmajor(inp["ln_ffn_g"][0], 8),
        "wup": f(inp["w_up"][0]), "wdn": f(inp["w_down"][0]),
        "convw": f(np.asarray(inp["conv_w"][0]).reshape(3, 22, 128).transpose(2, 0, 1)),
        "convb": f(np.asarray(inp["conv_b"][0]).reshape(22, 128).T),
        "g_fin": f(np.broadcast_to(np.asarray(inp["ln_final_g"]).reshape(1, D), (128, D))),
    }
    for k, v in host_consts().items():
        shared["c_" + k] = v
    x = np.asarray(inp["x"], dtype=np.float32)
    mem = np.asarray(inp["mem"], dtype=np.float32)
    maps = []
    for c in range(NCORES):
        m = dict(shared)
        m["x"] = np.ascontiguousarray(x[c * NSEQ:(c + 1) * NSEQ].reshape(NTOK, D))
        m["mem"] = np.ascontiguousarray(mem[c * NSEQ:(c + 1) * NSEQ].reshape(NSEQ * MEM, D))
        maps.append(m)
    return maps


_CACHE = {}


def kernel(**inputs):
    if "nc" not in _CACHE:
        _CACHE["nc"] = build_program()[0]
    nc = _CACHE["nc"]
    maps = prep_inputs(inputs)
    res = run_bass_kernel_spmd(nc, maps, core_ids=list(range(NCORES)))
    out = np.stack([np.asarray(r["out"]).reshape(NSEQ, SEQ, D) for r in res.results], axis=0)
    return out.reshape(NCORES * NSEQ, SEQ, D).astype(np.float32)
```

```python
import numpy as np
import concourse.bass as bass
import concourse.mybir as mybir
from concourse.bass_utils import run_bass_kernel_spmd

F32 = mybir.dt.float32
BF16 = mybir.dt.bfloat16
U8 = mybir.dt.uint8
AF = mybir.ActivationFunctionType
ALU = mybir.AluOpType
AX = mybir.AxisListType

NCORES = 8
SEQ = 2048
D = 1024
NSEQ = 4
NTOK = NSEQ * SEQ
MEM = 256
FFN = 2816
NEG = -30000.0
EPS = 1e-6

STAGE = [99]


class StopBuild(Exception):
    pass


def stage(n):
    if STAGE[0] == n:
        raise StopBuild()


DEBUG = {}


class Buf:
    __slots__ = ("name", "last_w", "rd_eng", "rd_dma")

    def __init__(self, name):
        self.name = name
        self.last_w = None
        self.rd_eng = {}
        self.rd_dma = []


class Op:
    __slots__ = ("eng", "fn", "dma", "deps", "signal", "token", "prev_slot")

    def __init__(self, eng, fn, dma):
        self.eng = eng
        self.fn = fn
        self.dma = dma
        self.deps = []
        self.signal = dma
        self.token = None
        self.prev_slot = None


ENGS = ("pe", "act", "dve", "pool", "sp")
NDMA_SLOTS = 8


class Prog:
    def __init__(self):
        self.ops = {e: [] for e in ENGS}
        self.final = []
        self.fence_deps = []

    def fence(self):
        deps = []
        for e in ENGS:
            last = None
            for o in reversed(self.ops[e]):
                if not o.dma:
                    last = o
                    break
            if last is not None:
                last.signal = True
                deps.append(last)
            nd = 0
            slots = {}
            for o in self.ops[e]:
                if o.dma:
                    slots[nd % NDMA_SLOTS] = o
                    nd += 1
            deps += list(slots.values())
        self.fence_deps = deps

    def op(self, eng, fn, reads=(), writes=(), dma=False):
        o = Op(eng, fn, dma)
        raw = set()
        other = set()
        for b in reads:
            if b.last_w is not None:
                raw.add(b.last_w)
        for b in writes:
            if b.last_w is not None:
                other.add(b.last_w)
            other.update(b.rd_eng.values())
            other.update(b.rd_dma)
        for d in raw | other:
            if d is o:
                continue
            same = (not dma) and (not d.dma) and d.eng == eng
            if same and (eng == "pe" or d not in raw):
                continue
            o.deps.append(d)
            d.signal = True
        for d in self.fence_deps:
            if (not dma) and (not d.dma) and d.eng == eng:
                continue
            if d not in o.deps:
                o.deps.append(d)
        for b in reads:
            if dma:
                b.rd_dma.append(o)
            else:
                b.rd_eng[eng] = o
        for b in writes:
            b.last_w = o
            b.rd_eng = {}
            b.rd_dma = []
        self.ops[eng].append(o)
        return o

    def emit(self, nc, stack):
        sems = {e: stack.enter_context(nc.semaphore("s_" + e)) for e in ENGS}
        dsem = {e: [stack.enter_context(nc.semaphore("d_%s%d" % (e, i))) for i in range(NDMA_SLOTS)]
                for e in ("sp", "pool", "act")}
        for e in ENGS:
            cnt = 0
            nd = 0
            slot_cnt = [0] * NDMA_SLOTS
            slot_last = [None] * NDMA_SLOTS
            for o in self.ops[e]:
                if o.dma:
                    s = nd % NDMA_SLOTS
                    nd += 1
                    slot_cnt[s] += 16
                    o.prev_slot = slot_last[s]
                    o.token = (dsem[e][s], slot_cnt[s])
                    slot_last[s] = o
                elif o.signal:
                    cnt += 1
                    o.token = (sems[e], cnt)
        block = stack.enter_context(nc.Block())
        prog = self

        def body(e):
            def run(eng):
                waited = {}

                def wait(tok):
                    sem, val = tok
                    k = id(sem)
                    if waited.get(k, 0) >= val:
                        return
                    eng.wait_ge(sem, val)
                    waited[k] = val

                for o in prog.ops[e]:
                    if o.dma and o.prev_slot is not None:
                        wait(o.prev_slot.token)
                    for d in o.deps:
                        wait(d.token)
                    ins = o.fn(eng)
                    if o.token is not None:
                        ins.then_inc(o.token[0], 16 if o.dma else 1)
                if e == "sp":
                    for o in prog.final:
                        wait(o.token)
            return run

        block.tensor(body("pe"))
        block.scalar(body("act"))
        block.vector(body("dve"))
        block.gpsimd(body("pool"))
        block.sync(body("sp"))


class Arena:
    def __init__(self, ap, size):
        self.ap = ap
        self.size = size
        self.off = 0
        self.marks = []

    def alloc(self, shape, dtype, name="t"):
        esz = 4 if dtype == F32 else 2
        n = 1
        for s in shape[1:]:
            n *= s
        nbytes = (n * esz + 31) // 32 * 32
        assert self.off + nbytes <= self.size, (name, self.off, nbytes, self.size)
        a = self.ap[0:shape[0], self.off:self.off + n * esz].bitcast(dtype)
        self.off += nbytes
        if len(shape) == 3:
            a = a.rearrange("p (a b) -> p a b", a=shape[1])
        elif len(shape) == 4:
            a = a.rearrange("p (a b c) -> p a b c", a=shape[1], b=shape[2])
        return a

    def mark(self):
        self.marks.append(self.off)

    def release(self):
        self.off = self.marks.pop()


SLOPES = [2.0 ** (-(h + 1)) for h in range(8)]

C_QA, C_KC, C_VC, C_KS, C_VS, C_KW, C_VW = 0, 512, 640, 768, 896, 1024, 1152
C_GATE, C_QB, C_KB, C_VB, C_RB, C_AL, C_MG = 1280, 1304, 1560, 1816, 2328, 2840, 2856
FM_QA, FM_KC, FM_VC, FM_KS, FM_KW, FM_QB, FM_KB, FM_MG = 0, 4, 5, 6, 8, 10, 12, 14
NFM = 30
TM_VS, TM_VW, TM_KB, TM_VB, TM_RB = 0, 128, 256, 512, 1024
NTM = 1536


def _fm_cols():
    cols = []
    for c in range(4):
        cols += list(range(C_QA + 128 * c, C_QA + 128 * (c + 1)))
    cols += list(range(C_KC, C_KC + 128))
    cols += list(range(C_VC, C_VC + 128))
    for base in (C_KS, C_KW):
        for g in range(2):
            one = list(range(base + 64 * g, base + 64 * (g + 1)))
            cols += one + one
    cols += list(range(C_QB, C_QB + 256))
    cols += list(range(C_KB, C_KB + 256))
    cols += list(range(C_MG, C_MG + 2048))
    cols += list(range(C_AL, C_AL + 16))
    return np.array(cols)


def _tm_cols():
    cols = list(range(C_VS, C_VS + 128)) + list(range(C_VW, C_VW + 128))
    cols += list(range(C_KB, C_KB + 256)) + list(range(C_VB, C_VB + 512)) + list(range(C_RB, C_RB + 512))
    cols += list(range(C_GATE, C_GATE + 24))
    return np.array(cols)


def pmajor(v, nchunk):
    return np.ascontiguousarray(np.asarray(v, np.float32).reshape(nchunk, 128).T)


def host_consts():
    c = {}
    t = np.arange(SEQ)
    n = np.arange(127)
    dist = t[None, :] - (16 * n[:, None] + 31)
    c["cmpD"] = np.where(dist >= 0, -dist, -1.0e6).astype(np.float32)
    ov = np.zeros((127, 32), np.float32)
    for nn in range(127):
        for p in range(32):
            ov[nn, (16 * nn + p) // 64] += 1.0 / 32
    c["ovl"] = ov
    cur = (t // 64)
    j = np.arange(32)
    forced = (j[None, :] == 0) | (j[None, :] == cur[:, None]) | (j[None, :] == cur[:, None] - 1)
    future = j[None, :] > cur[:, None]
    mul = np.where(forced | future, 0.0, 1.0).astype(np.float32)
    add = np.where(forced, 5.0, np.where(future, -1.0, 0.0)).astype(np.float32)
    c["fmul"] = np.ascontiguousarray(mul.reshape(16, 128, 32).transpose(1, 0, 2))
    c["fadd"] = np.ascontiguousarray(add.reshape(16, 128, 32).transpose(1, 0, 2))
    c["tb"] = (64.0 * (cur[None, :] - j[:, None])).astype(np.float32)
    ea = np.zeros((34, SEQ), np.float32)
    ea[t // 64, t] = 1.0
    ea[32] = t % 64
    ea[33] = 1.0
    c["ea"] = ea
    cr = np.zeros((2, 8, 512), np.float32)
    rq = np.arange(512) % 64
    for h in range(8):
        cr[0, h] = SLOPES[h]
        cr[1, h] = -SLOPES[h] * rq
    c["crow"] = cr
    k = np.arange(128)
    cc = np.arange(896)
    c["cb"] = np.where(cc[None, :] - 384 >= k[:, None], 0.0, NEG).astype(np.float32)
    cc = np.arange(1152)
    dd = cc[None, :] - 384 - k[:, None]
    wb = np.zeros((128, 8, 1152), np.float32)
    for h in range(8):
        wb[:, h, :] = np.where((dd >= 0) & (dd < 256), -SLOPES[h] * dd, NEG)
    c["wb"] = wb
    c["ident"] = np.eye(128, dtype=np.float32)
    jj = np.arange(128)
    same = (jj[:, None] // 64) == (jj[None, :] // 64)
    c["gmask"] = (same & (jj[:, None] <= jj[None, :])).astype(np.float32)
    c["srst"] = np.broadcast_to(np.where(t % 64 == 0, 0.0, 1.0).astype(np.float32), (128, SEQ)).copy()
    c["gup"] = (same & (jj[:, None] > jj[None, :])).astype(np.float32)
    return c


CONST_SHAPES = None


def build_program(phases=("p0", "p1", "p2", "p3")):
    import contextlib
    nc = bass.Bass("TRN2", target_bir_lowering=False)
    P = Prog()
    stack = contextlib.ExitStack()

    def din(name, shape, dt=F32):
        return nc.dram_tensor(name, list(shape), dt, kind="ExternalInput").ap()

    def dscr(name, shape, dt):
        kind = "ExternalOutput" if DEBUG.get(name) else "Internal"
        return nc.dram_tensor(name, list(shape), dt, kind=kind).ap()

    consts = host_consts()
    x_d = din("x", [NTOK, D])
    mem_d = din("mem", [NSEQ * MEM, D])
    wfm_d = din("w_fm", [D, NFM * 128 + 16])
    wtm_d = din("w_tm", [D, NTM + 24])
    g_mix_d = din("g_mix", [128, 8])
    gate_b_d = din("gate_b", [128, 24])
    w1k_d = din("w1k", [2048, 128]); w1v_d = din("w1v", [2048, 128])
    w2k_d = din("w2k", [128, 128]); w2v_d = din("w2v", [128, 64])
    pek_d = din("pek", [64, 32]); pev_d = din("pev", [64, 32])
    wa2_d = din("wa2", [17, 256])
    gla_g_d = din("gla_g", [128, 128])
    ba_d = din("ba", [128, 2])
    wbn_d = din("wbn", [512, D]); wbg_d = din("wbg", [512, D]); wout_d = din("wout", [D, D])
    g_x_d = din("g_x", [128, 8]); g_mem_d = din("g_mem", [128, 8])
    wxq_d = din("wxq", [D, 512]); wxkv_d = din("wxkv", [D, 1024]); wxo_d = din("wxo", [512, D])
    g_ffn_d = din("g_ffn", [128, 8])
    wup_d = din("wup", [D, 2 * FFN]); wdn_d = din("wdn", [FFN, D])
    convw_d = din("convw", [128, 3, 22]); convb_d = din("convb", [128, 22])
    g_fin_d = din("g_fin", [128, D])
    cd = {k: din("c_" + k, v.shape) for k, v in consts.items()}
    out_d = nc.dram_tensor("out", [NTOK, D], F32, kind="ExternalOutput").ap()
    s_fm = dscr("s_fm", [NFM, 128, NTOK], BF16)
    s_al = dscr("s_al", [16, NTOK], F32)
    s_tm = dscr("s_tm", [NTOK, NTM], BF16)
    s_gate = dscr("s_gate", [NTOK, 24], F32)
    s_on = dscr("s_on", [4, 128, NTOK], BF16)
    s_og = dscr("s_og", [4, 128, NTOK], BF16)
    s_x2 = dscr("s_x2", [NTOK, D], F32)
    dbg = {}
    if DEBUG.get("gla"):
        dbg["cs"] = nc.dram_tensor("dbg_cs", [128, 2, SEQ], F32, kind="ExternalOutput").ap()
        dbg["kd"] = nc.dram_tensor("dbg_kd", [128, 2, SEQ], BF16, kind="ExternalOutput").ap()
        dbg["qd"] = nc.dram_tensor("dbg_qd", [128, 4, SEQ], BF16, kind="ExternalOutput").ap()
        dbg["rg"] = nc.dram_tensor("dbg_rg", [128, 16, 512], BF16, kind="ExternalOutput").ap()
        dbg["ogb"] = nc.dram_tensor("dbg_ogb", [128, 512], BF16, kind="ExternalOutput").ap()
        dbg["att"] = nc.dram_tensor("dbg_att", [128, 4, 128], BF16, kind="ExternalOutput").ap()
        dbg["la"] = nc.dram_tensor("dbg_la", [128, 2, SEQ], F32, kind="ExternalOutput").ap()
    if DEBUG.get("ffn"):
        dbg["aT"] = nc.dram_tensor("dbg_aT", [128, 22, 512], BF16, kind="ExternalOutput").ap()
        dbg["x3"] = nc.dram_tensor("dbg_x3", [128, 4, D], F32, kind="ExternalOutput").ap()
        dbg["hf"] = nc.dram_tensor("dbg_hf", [128, 8, 512], BF16, kind="ExternalOutput").ap()

    ARENA = 204 * 1024
    arena_t = stack.enter_context(nc.sbuf_tensor("arena", [128, ARENA], U8))
    psum_t = stack.enter_context(nc.psum_tensor("psum", [128, 4096], F32))
    A = Arena(arena_t, ARENA)
    PS = [psum_t[:, 512 * i:512 * (i + 1)] for i in range(8)]
    PSB = [Buf("ps%d" % i) for i in range(8)]

    rr = {"ev": 0, "q": 0}

    def dma(q, out, in_, reads=(), writes=(), **kw):
        return P.op(q, lambda e: e.dma_start(out=out, in_=in_, **kw), reads=reads, writes=writes, dma=True)

    def load_cast(dst, src, wb, nsplit=1):
        last = dst.shape[-1]
        step = (last + nsplit - 1) // nsplit
        for s0 in range(0, last, step):
            s1 = min(last, s0 + step)
            if len(dst.shape) == 2:
                dma("pool", dst[:, s0:s1], src[:, s0:s1], writes=[wb])
            else:
                dma("pool", dst[:, :, s0:s1], src[:, :, s0:s1], writes=[wb])

    def load_cast_pieces(dst, src, bounds, bufs):
        for (b0, b1), wb in zip(bounds, bufs):
            dma("pool", dst[:, :, b0:b1], src[:, :, b0:b1], writes=[wb])

    def evac_copy(out, in_, reads, writes, scale=None):
        rr["ev"] += 1
        if rr["ev"] % 2 == 0:
            if scale is None:
                P.op("act", lambda e: e.activation(out=out, in_=in_, func=AF.Copy), reads=reads, writes=writes)
            else:
                P.op("act", lambda e: e.activation(out=out, in_=in_, func=AF.Copy, scale=scale), reads=reads, writes=writes)
        else:
            if scale is None:
                P.op("dve", lambda e: e.tensor_copy(out=out, in_=in_), reads=reads, writes=writes)
            else:
                P.op("dve", lambda e: e.tensor_scalar(out=out, in0=in_, scalar1=scale, scalar2=None, op0=ALU.mult),
                     reads=reads, writes=writes)

    def mm(out, lhsT, rhs, start, stop, reads, writes, skip=False):
        P.op("pe", lambda e: e.matmul(out, lhsT=lhsT, rhs=rhs, start=start, stop=stop, skip_group_check=skip),
             reads=reads, writes=writes)

    def transpose(out, in_, ident, reads, writes):
        P.op("pe", lambda e: e.transpose(out, in_, ident), reads=reads, writes=writes)

    ident_f = A.alloc([128, 128], F32, "ident_f")
    ident_b = A.alloc([128, 128], BF16, "ident_b")
    B_ident = Buf("ident")
    dma("sp", ident_f, cd["ident"][:, :], writes=[B_ident])
    dma("pool", ident_b, cd["ident"][:, :], writes=[B_ident])

    def V_tt(out, in0, in1, op, reads, writes):
        P.op("dve", lambda e: e.tensor_tensor(out=out, in0=in0, in1=in1, op=op), reads=reads, writes=writes)

    def V_ts(out, in0, s1, s2, op0, op1, reads, writes):
        if op1 is None:
            P.op("dve", lambda e: e.tensor_scalar(out=out, in0=in0, scalar1=s1, scalar2=None, op0=op0), reads=reads, writes=writes)
        else:
            P.op("dve", lambda e: e.tensor_scalar(out=out, in0=in0, scalar1=s1, scalar2=s2, op0=op0, op1=op1), reads=reads, writes=writes)

    def V_stt(out, in0, scalar, in1, op0, op1, reads, writes):
        P.op("dve", lambda e: e.scalar_tensor_tensor(out=out, in0=in0, scalar=scalar, in1=in1, op0=op0, op1=op1),
             reads=reads, writes=writes)

    def V_copy(out, in_, reads, writes):
        P.op("dve", lambda e: e.tensor_copy(out=out, in_=in_), reads=reads, writes=writes)

    def V_recip(out, in_, reads, writes):
        P.op("dve", lambda e: e.reciprocal(out=out, in_=in_), reads=reads, writes=writes)

    def V_max(out, in_, reads, writes):
        P.op("dve", lambda e: e.max(out=out, in_=in_), reads=reads, writes=writes)

    def A_act(out, in_, func, reads, writes, **kw):
        P.op("act", lambda e: e.activation(out=out, in_=in_, func=func, **kw), reads=reads, writes=writes)

    def G_memset(ap, val, writes):
        P.op("pool", lambda e: e.memset(ap, val), writes=writes)

    def G_tt(out, in0, in1, op, reads, writes):
        P.op("pool", lambda e: e.tensor_tensor(out=out, in0=in0, in1=in1, op=op), reads=reads, writes=writes)

    def rms_a(xt, B_xt, nsub, xn, B_xn, st, B_st):
        for s in range(nsub):
            A_act(xn[:, s, :], xt[:, s, :], AF.Square, [B_xt], [B_xn, B_st], accum_out=st[:, s:s + 1])
        A_act(st[:, 8:8 + nsub], st[:, 0:nsub], AF.Sqrt, [B_st], [B_st], bias=EPS, scale=1.0 / D)
        V_recip(st[:, 16:16 + nsub], st[:, 8:8 + nsub], [B_st], [B_st])
        for s in range(nsub):
            V_ts(xn[:, s, :], xt[:, s, :], st[:, 16 + s:17 + s], None, ALU.mult, None, [B_xt, B_st], [B_xn])

    def rms_b(nsub, g_sb, B_g, hT, B_hT, xn, B_xn, psA):
        for kc in range(8):
            pi = psA[kc % len(psA)]
            pst = PS[pi].bitcast(BF16)
            for s in range(nsub):
                transpose(pst[:, s * 128:(s + 1) * 128], xn[:, s, kc * 128:(kc + 1) * 128], ident_b,
                          reads=[B_xn, B_ident], writes=[PSB[pi]])
            evac_copy(hT[:, kc, 0:nsub * 128], pst[:, 0:nsub * 128], reads=[PSB[pi], B_g], writes=[B_hT],
                      scale=g_sb[:, kc:kc + 1])

    def rmsnorm_T(xt, B_xt, nsub, g_sb, B_g, hT, B_hT, xn, B_xn, st, B_st, psA):
        rms_a(xt, B_xt, nsub, xn, B_xn, st, B_st)
        rms_b(nsub, g_sb, B_g, hT, B_hT, xn, B_xn, psA)

    if "p0" in phases:
        A.mark()
        wfm = A.alloc([128, 8, NFM * 128 + 16], BF16, "wfm"); B_wfm = Buf("wfm")
        wtm = A.alloc([128, 8, NTM + 24], BF16, "wtm"); B_wtm = Buf("wtm")
        load_cast(wfm, wfm_d.rearrange("(kc p) n -> p kc n", p=128), B_wfm, nsplit=4)
        load_cast(wtm, wtm_d.rearrange("(kc p) n -> p kc n", p=128), B_wtm, nsplit=2)
        gmix = A.alloc([128, 8], F32, "gmix"); B_gmix = Buf("gmix")
        dma("sp", gmix, g_mix_d[:, :], writes=[B_gmix])
        xt = A.alloc([128, 4, D], F32, "xt"); B_xt = Buf("xt")
        xn = A.alloc([128, 4, D], BF16, "xn"); B_xn = Buf("xn")
        st = A.alloc([128, 32], F32, "st"); B_st = Buf("st")
        hTs = [A.alloc([128, 8, 512], BF16, "hT%d" % i) for i in range(2)]
        B_hTs = [Buf("hT%d" % i) for i in range(2)]
        fmo = A.alloc([128, NFM, 512], BF16, "fmo")
        B_fmo = [Buf("fmo%d" % i) for i in range(3)]
        alo = A.alloc([16, 512], F32, "alo"); B_alo = Buf("alo")
        tmo = A.alloc([128, 4, NTM], BF16, "tmo"); B_tmo = Buf("tmo")
        gto = A.alloc([128, 4, 24], F32, "gto"); B_gto = Buf("gto")
        mmbank = [2, 3, 4, 5, 6, 7]
        bi = 0
        NT0 = NTOK // 512

        def p0_load_a(it):
            dma("sp", xt, x_d[it * 512:it * 512 + 512, :].rearrange("(s p) d -> p s d", p=128), writes=[B_xt])
            rms_a(xt, B_xt, 4, xn, B_xn, st, B_st)

        def p0_b(it):
            rms_b(4, gmix, B_gmix, hTs[it % 2], B_hTs[it % 2], xn, B_xn, [0, 1])

        p0_load_a(0)
        p0_b(0)
        for it in range(NT0):
            t0 = it * 512
            hT = hTs[it % 2]; B_hT = B_hTs[it % 2]
            for c in range(NFM + 1):
                if c == 8 and it + 1 < NT0:
                    p0_load_a(it + 1)
                if c == 26 and it + 1 < NT0:
                    p0_b(it + 1)
                M = 128 if c < NFM else 16
                pi = mmbank[bi % 6]; bi += 1
                for kc in range(8):
                    mm(PS[pi][0:M, :], wfm[:, kc, c * 128:c * 128 + M], hT[:, kc, :], kc == 0, kc == 7,
                       reads=[B_wfm, B_hT], writes=[PSB[pi]])
                if c == NFM:
                    P.op("dve", lambda e, pi=pi: e.tensor_copy(out=alo, in_=PS[pi][0:16, :]),
                         reads=[PSB[pi]], writes=[B_alo])
                    continue
                bo = B_fmo[c // 10]
                if c < 4:
                    evac_copy(fmo[:, c, :], PS[pi], [PSB[pi]], [bo], scale=0.125)
                elif c >= FM_MG:
                    P.op("act", lambda e, c=c, pi=pi: e.activation(out=fmo[:, c, :], in_=PS[pi], func=AF.Sigmoid),
                         reads=[PSB[pi]], writes=[bo])
                else:
                    evac_copy(fmo[:, c, :], PS[pi], [PSB[pi]], [bo])
                if c % 10 == 9:
                    g0 = c - 9
                    dma("sp", s_fm[g0:g0 + 10, :, t0:t0 + 512].rearrange("c p t -> p c t"), fmo[:, g0:g0 + 10, :],
                        reads=[bo])
            dma("sp", s_al[:, t0:t0 + 512], alo, reads=[B_alo])
            for s in range(4):
                for (c0, c1) in ((0, 512), (512, 1024), (1024, 1536), (1536, 1560)):
                    pi = mmbank[bi % 6]; bi += 1
                    for kc in range(8):
                        mm(PS[pi][:, 0:c1 - c0], hT[:, kc, s * 128:(s + 1) * 128], wtm[:, kc, c0:c1], kc == 0, kc == 7,
                           reads=[B_wtm, B_hT], writes=[PSB[pi]])
                    if c0 < 1536:
                        evac_copy(tmo[:, s, c0:c1], PS[pi], [PSB[pi]], [B_tmo])
                    else:
                        evac_copy(gto[:, s, :], PS[pi][:, 0:24], [PSB[pi]], [B_gto])
            dma("sp", s_tm[t0:t0 + 512, :].rearrange("(s p) n -> p s n", p=128), tmo, reads=[B_tmo])
            dma("sp", s_gate[t0:t0 + 512, :].rearrange("(s p) n -> p s n", p=128), gto, reads=[B_gto])
        A.release()


    def phase1_nsa():
        B_c1 = Buf("c1")
        cmpD = A.alloc([128, SEQ], F32, "cmpD")
        dma("sp", cmpD[0:127, :], cd["cmpD"][:, :], writes=[B_c1])
        fmul = A.alloc([128, 16, 32], F32, "fmul"); fadd = A.alloc([128, 16, 32], F32, "fadd")
        dma("sp", fmul, cd["fmul"][:, :, :], writes=[B_c1]); dma("sp", fadd, cd["fadd"][:, :, :], writes=[B_c1])
        tbt = A.alloc([32, SEQ], F32, "tb"); dma("sp", tbt, cd["tb"][:, :], writes=[B_c1])
        ea = A.alloc([128, SEQ], BF16, "ea")
        G_memset(ea, 0.0, [B_c1])
        dma("pool", ea[0:34, :], cd["ea"][:, :], writes=[B_c1])
        cbt = A.alloc([128, 896], BF16, "cb"); dma("pool", cbt, cd["cb"][:, :], writes=[B_c1])
        wbt = A.alloc([128, 8, 1152], BF16, "wb"); dma("pool", wbt, cd["wb"][:, :, :], writes=[B_c1])
        MbAs = [A.alloc([128, 8, 512], BF16, "MbA%d" % i) for i in range(2)]
        B_mbs = [[Buf("mb%d_%d" % (i, h)) for h in range(8)] for i in range(2)]
        for i in range(2):
            G_memset(MbAs[i], 0.0, B_mbs[i])
            dma("pool", MbAs[i][32:34, :, :], cd["crow"][:, :, :], writes=B_mbs[i])
        W1 = {}; W2 = {}; peT = {}; cbias = {}
        B_cw = Buf("cw")
        for nm, w1d, w2d, ped in (("k", w1k_d, w2k_d, pek_d), ("v", w1v_d, w2v_d, pev_d)):
            W1[nm] = A.alloc([128, 32, 128], BF16, "w1" + nm)
            src = w1d.rearrange("(p d) h -> d p h", d=64)
            dma("pool", W1[nm][0:64, :, :], src, writes=[B_cw])
            dma("pool", W1[nm][64:128, :, :], src, writes=[B_cw])
            W2[nm] = A.alloc([128, 128 if nm == "k" else 64], BF16, "w2" + nm)
            dma("pool", W2[nm], w2d[:, :], writes=[B_cw])
            peT[nm] = A.alloc([64, 32], BF16, "pe" + nm)
            dma("pool", peT[nm], ped[:, :], writes=[B_cw])
            cbias[nm] = A.alloc([128, 1], F32, "cbias" + nm)
        B_cb = Buf("cbias")
        for nm in ("k", "v"):
            for p in range(32):
                mm(PS[7][:, 0:1], W1[nm][0:64, p, :], peT[nm][0:64, p:p + 1], p == 0, p == 31, reads=[B_cw], writes=[PSB[7]])
            V_copy(cbias[nm], PS[7][:, 0:1], [PSB[7]], [B_cb])
        gateb = A.alloc([128, 24], F32, "gateb"); dma("sp", gateb, gate_b_d[:, :], writes=[B_c1])
        stage(1)
        qa = A.alloc([128, 4, SEQ], BF16, "qa"); B_qa = Buf("qa")
        kcT = A.alloc([128, SEQ], BF16, "kcT"); vcT = A.alloc([128, SEQ], BF16, "vcT"); B_kvc = Buf("kvc")
        ksT = A.alloc([128, 4, SEQ], BF16, "ksT"); B_ks = Buf("ks")
        kwT = A.alloc([128, 4, SEQ], BF16, "kwT"); B_kw = Buf("kw")
        vsa = A.alloc([128, 16, 2, 65], BF16, "vsa"); B_vs = Buf("vs")
        vwa = A.alloc([128, 16, 2, 65], BF16, "vwa"); B_vw = Buf("vw")
        G_memset(vsa[:, :, :, 64:65], 1.0, [B_vs])
        G_memset(vwa[:, :, :, 64:65], 1.0, [B_vw])
        gsig = A.alloc([128, 16, 24], F32, "gsig"); B_gs = Buf("gs")
        hidT = A.alloc([128, 128], BF16, "hidT"); B_hid = Buf("hid")
        kcmpT = A.alloc([128, 2, 128], BF16, "kcmpT"); B_kcmp = Buf("kcmp")
        Rg = A.alloc([128, 2, 97], BF16, "Rg"); B_R = Buf("R")
        G_memset(Rg[:, :, 64:65], 1.0, [B_R])
        for g in range(2):
            dma("pool", Rg[0:127, g, 65:97], cd["ovl"][:, :], writes=[B_R])
        S_sb = A.alloc([128, 512], F32, "S_sb"); B_S = Buf("S")
        PTs = [A.alloc([128, 512], BF16, "PT%d" % i) for i in range(3)]; B_PT = [Buf("PT%d" % i) for i in range(3)]
        oaccs = [A.alloc([128, 4, 8, 64], F32, "oacc%d" % i) for i in range(2)]; B_oas = [Buf("oacc%d" % i) for i in range(2)]
        otmp = A.alloc([128, 4, 64], F32, "otmp"); B_ot = Buf("otmp")
        onb = A.alloc([128, 4, 512], BF16, "onb"); B_onb = Buf("onb")
        onT = A.alloc([128, 4, 512], BF16, "onT"); B_onT = Buf("onT")
        imp = A.alloc([128, 4, 32], F32, "imp"); B_imp = Buf("imp")
        itmp = A.alloc([128, 4, 32], F32, "itmp"); B_it = Buf("itmp")
        t8 = A.alloc([128, 4, 8], F32, "t8"); B_t8 = Buf("t8")
        selb = A.alloc([128, 4, 32], F32, "selb"); B_selb = Buf("selb")
        selT = A.alloc([32, 512], F32, "selT"); B_selT = Buf("selT")
        sm = A.alloc([128, 16], F32, "sm"); B_sm = Buf("sm")
        pt_i = [0]; sc_i = [0]; acc_i = [0]; ptc_i = [0]
        PTc = [A.alloc([128, 512], BF16, "PTc%d" % i) for i in range(2)]; B_PTc = [Buf("PTc%d" % i) for i in range(2)]

        def pv_evac(pacc, pb, qt, h, br, first, par):
            oacc = oaccs[par]; B_oa = B_oas[par]
            if br == 0:
                V_ts(sm[:, 0:4], pacc[:, :, 64], 1e-30, None, ALU.max, None, [PSB[pb]], [B_sm])
                V_recip(sm[:, 4:8], sm[:, 0:4], [B_sm], [B_sm])
            else:
                V_recip(sm[:, 4:8], pacc[:, :, 64], [PSB[pb]], [B_sm])
            V_tt(sm[:, 8:12], sm[:, 4:8], gsig[:, 4 * qt:4 * qt + 4, 3 * h + br], ALU.mult, [B_sm, B_gs], [B_sm])
            rgb = sm[:, 8:12].unsqueeze(2).to_broadcast([128, 4, 64])
            if first:
                V_tt(oacc[:, :, h, :], pacc[:, :, 0:64], rgb, ALU.mult, [PSB[pb], B_sm], [B_oa])
            else:
                V_tt(otmp, pacc[:, :, 0:64], rgb, ALU.mult, [PSB[pb], B_sm], [B_ot])
                V_tt(oacc[:, :, h, :], oacc[:, :, h, :], otmp, ALU.add, [B_oa, B_ot], [B_oa])

        for sq in range(NSEQ):
            tb0 = sq * SEQ
            dma("sp", qa, s_fm[FM_QA:FM_QA + 4, :, tb0:tb0 + SEQ].rearrange("c p t -> p c t"), writes=[B_qa])
            dma("sp", kcT, s_fm[FM_KC, :, tb0:tb0 + SEQ], writes=[B_kvc])
            dma("sp", vcT, s_fm[FM_VC, :, tb0:tb0 + SEQ], writes=[B_kvc])
            for g in range(2):
                for hf in range(2):
                    dma("sp", ksT[:, 2 * g + hf, :], s_fm[FM_KS + g, :, tb0:tb0 + SEQ], writes=[B_ks])
                    dma("sp", kwT[:, 2 * g + hf, :], s_fm[FM_KW + g, :, tb0:tb0 + SEQ], writes=[B_kw])
                    zlo = 64 * (1 - hf)
                    G_memset(ksT[zlo:zlo + 64, 2 * g + hf, :], 0.0, [B_ks])
                    G_memset(kwT[zlo:zlo + 64, 2 * g + hf, :], 0.0, [B_kw])
            for g in range(2):
                dma("sp", vsa[:, :, g, 0:64],
                    s_tm[tb0:tb0 + SEQ, TM_VS + 64 * g:TM_VS + 64 * g + 64].rearrange("(kt p) d -> p kt d", p=128),
                    writes=[B_vs])
                dma("sp", vwa[:, :, g, 0:64],
                    s_tm[tb0:tb0 + SEQ, TM_VW + 64 * g:TM_VW + 64 * g + 64].rearrange("(kt p) d -> p kt d", p=128),
                    writes=[B_vw])
            dma("sp", gsig, s_gate[tb0:tb0 + SEQ, :].rearrange("(kt p) n -> p kt n", p=128), writes=[B_gs])
            V_tt(gsig, gsig, gateb.unsqueeze(1).to_broadcast([128, 16, 24]), ALU.add, [B_gs, B_c1], [B_gs])
            A_act(gsig, gsig, AF.Sigmoid, [B_gs], [B_gs])
            stage(2)
            for nm, srcT in (("k", kcT), ("v", vcT)):
                s3 = srcT.rearrange("q (n s) -> q n s", s=16)
                for g in range(2):
                    for p in range(32):
                        mm(PS[7][:, 0:127], W1[nm][64 * g:64 * g + 64, p, :], s3[64 * g:64 * g + 64, p // 16:p // 16 + 127, p % 16],
                           p == 0, p == 31, reads=[B_cw, B_kvc], writes=[PSB[7]])
                    A_act(hidT[:, 0:127], PS[7][:, 0:127], AF.Gelu_apprx_tanh, [PSB[7], B_cb], [B_hid], bias=cbias[nm])
                    if nm == "k":
                        mm(PS[6][:, 0:127], W2["k"], hidT[:, 0:127], True, True, reads=[B_cw, B_hid], writes=[PSB[6]])
                        evac_copy(kcmpT[:, g, 0:127], PS[6][:, 0:127], [PSB[6]], [B_kcmp])
                    else:
                        mm(PS[6][0:127, 0:64], hidT[:, 0:127], W2["v"], True, True, reads=[B_cw, B_hid], writes=[PSB[6]])
                        evac_copy(Rg[0:127, g, 0:64], PS[6][0:127, 0:64], [PSB[6]], [B_R])
            stage(3)
            def cmp_stages(qt):
                q0 = qt * 512
                par = qt % 2
                st_list = []
                for g in range(2):
                    for gi in range(4):
                        h = 4 * g + gi
                        hp = 64 * (h % 2)
                        box = {}

                        def s1(h=h, hp=hp, g=g, box=box):
                            pi = sc_i[0] % 3; sc_i[0] += 1
                            mm(PS[pi][0:127, :], kcmpT[hp:hp + 64, g, 0:127], qa[hp:hp + 64, h // 2, q0:q0 + 512], True, True,
                               reads=[B_kcmp, B_qa], writes=[PSB[pi]])
                            V_stt(S_sb[0:127, :], cmpD[0:127, q0:q0 + 512], SLOPES[h], PS[pi][0:127, :], ALU.mult, ALU.add,
                                  [PSB[pi], B_c1], [B_S])
                            k = ptc_i[0] % 2; ptc_i[0] += 1
                            box["k"] = k
                            A_act(PTc[k][0:127, :], S_sb[0:127, :], AF.Exp, [B_S], [B_PTc[k]])

                        def s2(h=h, g=g, gi=gi, box=box):
                            k = box["k"]
                            pu = PS[5][:, 0:388].rearrange("p (s c) -> p s c", s=4)
                            for sub in range(4):
                                mm(pu[:, sub, :], PTc[k][0:127, sub * 128:(sub + 1) * 128], Rg[0:127, g, :], True, True,
                                   reads=[B_PTc[k], B_R], writes=[PSB[5]])
                            pv_evac(pu, 5, qt, h, 0, True, par)
                            rdb = sm[:, 4:8].unsqueeze(2).to_broadcast([128, 4, 32])
                            if gi == 0:
                                V_tt(imp, pu[:, :, 65:97], rdb, ALU.mult, [PSB[5], B_sm], [B_imp])
                            else:
                                V_tt(itmp, pu[:, :, 65:97], rdb, ALU.mult, [PSB[5], B_sm], [B_it])
                                V_tt(imp, imp, itmp, ALU.add, [B_imp, B_it], [B_imp])
                            if gi == 3:
                                V_tt(imp, imp, fmul[:, 4 * qt:4 * qt + 4, :], ALU.mult, [B_imp, B_c1], [B_imp])
                                V_tt(imp, imp, fadd[:, 4 * qt:4 * qt + 4, :], ALU.add, [B_imp, B_c1], [B_imp])
                                for sub in range(4):
                                    V_max(t8[:, sub, :], imp[:, sub, :], [B_imp], [B_t8])
                                for sub in range(4):
                                    V_ts(selb[:, sub, :], imp[:, sub, :], t8[:, sub, 7:8], -NEG, ALU.is_ge, ALU.mult,
                                         [B_imp, B_t8], [B_selb])

                        st_list.append(s1)
                        st_list.append(s2)

                    def s3(g=g):
                        for sub in range(4):
                            transpose(PS[6][0:32, sub * 128:(sub + 1) * 128], selb[:, sub, :], ident_f,
                                      reads=[B_selb, B_ident], writes=[PSB[6]])
                        V_ts(selT, PS[6][0:32, :], NEG, None, ALU.add, None, [PSB[6]], [B_selT])
                        for gi in range(4):
                            h = 4 * g + gi
                            V_stt(MbAs[par][0:32, h, :], tbt[0:32, q0:q0 + 512], -SLOPES[h], selT, ALU.mult, ALU.add,
                                  [B_c1, B_selT], [B_mbs[par][h]])

                    st_list.append(s3)
                return st_list

            for fn in cmp_stages(0):
                fn()
            for qt in range(4):
                q0 = qt * 512
                par = qt % 2
                nxt = cmp_stages(qt + 1) if qt < 3 else []
                items = []
                for h in range(8):
                    g = h // 4; qc = h // 2
                    for br in (1, 2):
                        pb = 3 + acc_i[0] % 2; acc_i[0] += 1
                        pacc = PS[pb][:, 0:260].rearrange("p (s c) -> p s c", s=4)
                        if br == 1:
                            kts = list(range(0, 4 * qt + 4))
                        else:
                            kts = list(range(max(0, 4 * qt - 2), 4 * qt + 4))
                        pairs = []
                        for kt in kts:
                            off = kt * 128 - q0
                            for sub in range(4):
                                dmax = 128 * sub + 127 - off
                                dmin = 128 * sub - 127 - off
                                if dmax < 0:
                                    continue
                                if br == 2 and dmin >= 256:
                                    continue
                                pairs.append((kt, sub))
                        lastkt = {}
                        for kt, sub in pairs:
                            lastkt[sub] = kt
                        for kt in kts:
                            items.append(dict(h=h, g=g, qc=qc, br=br, kt=kt, pb=pb, pacc=pacc, pairs=pairs, lastkt=lastkt,
                                              firstkt=(kt == kts[0]), lastk=(kt == kts[-1])))

                def emit_scores(it):
                    h = it["h"]; g = it["g"]; br = it["br"]; kt = it["kt"]
                    off = kt * 128 - q0
                    pi = sc_i[0] % 3; sc_i[0] += 1
                    kT = ksT if br == 1 else kwT
                    Bk = B_ks if br == 1 else B_kw
                    mm(PS[pi], kT[:, 2 * g + (h % 2), kt * 128:(kt + 1) * 128], qa[:, it["qc"], q0:q0 + 512], True, False,
                       reads=[Bk, B_qa], writes=[PSB[pi]])
                    if br == 1:
                        diag = off >= 0
                        mm(PS[pi], ea[:, kt * 128:(kt + 1) * 128], MbAs[par][:, h, :], False, not diag,
                           reads=[B_c1, B_mbs[par][h]], writes=[PSB[pi]])
                        if diag:
                            mm(PS[pi], ident_b, cbt[:, 384 - off:384 - off + 512], False, True,
                               reads=[B_ident, B_c1], writes=[PSB[pi]])
                    else:
                        mm(PS[pi], ident_b, wbt[:, h, 384 - off:384 - off + 512], False, True,
                           reads=[B_ident, B_c1], writes=[PSB[pi]])
                    k = pt_i[0] % 3; pt_i[0] += 1
                    it["k"] = k
                    A_act(PTs[k], PS[pi], AF.Exp, [PSB[pi]], [B_PT[k]])

                def emit_pv(it):
                    h = it["h"]; g = it["g"]; br = it["br"]; kt = it["kt"]; k = it["k"]; pb = it["pb"]
                    va = vsa if br == 1 else vwa
                    Bv = B_vs if br == 1 else B_vw
                    first = it["firstkt"]
                    for sub in range(4):
                        if (kt, sub) not in it["pairs"]:
                            continue
                        mm(it["pacc"][:, sub, :], PTs[k][:, sub * 128:(sub + 1) * 128], va[:, kt, g, :], first, it["lastkt"][sub] == kt,
                           reads=[B_PT[k], Bv], writes=[PSB[pb]], skip=True)
                        first = False
                    if it["lastk"]:
                        pv_evac(it["pacc"], pb, qt, h, br, False, par)

                LA = 2
                step = max(1, len(items) // (len(nxt) + 1))
                for i in range(len(items) + LA):
                    if i < len(items):
                        emit_scores(items[i])
                    if i >= LA:
                        emit_pv(items[i - LA])
                    if nxt and i % step == step - 1:
                        nxt.pop(0)()
                while nxt:
                    nxt.pop(0)()
                oacc_p = oaccs[par]
                A_act(onb.rearrange("p a b -> p (a b)"), oacc_p.rearrange("p a h d -> p (a h d)"), AF.Copy, [B_oas[par]], [B_onb])
                for fc in range(4):
                    pst = PS[6].bitcast(BF16)
                    for sub in range(4):
                        transpose(pst[:, sub * 128:(sub + 1) * 128], onb[:, sub, fc * 128:(fc + 1) * 128], ident_b,
                                  reads=[B_onb, B_ident], writes=[PSB[6]])
                    evac_copy(onT[:, fc, :], pst[:, 0:512], [PSB[6]], [B_onT])
                dma("sp", s_on[:, :, tb0 + q0:tb0 + q0 + 512].rearrange("c p t -> p c t"), onT, reads=[B_onT])

    def phase1_gla():
        B_gc = Buf("gc")
        wa2 = A.alloc([16, 256], F32, "wa2"); dma("sp", wa2, wa2_d[0:16, :], writes=[B_gc])
        ba = A.alloc([128, 2], F32, "ba"); dma("sp", ba, ba_d[:, :], writes=[B_gc])
        nba = A.alloc([128, 2], F32, "nba"); V_ts(nba, ba, -1.0, None, ALU.mult, None, [B_gc], [B_gc])
        srst = A.alloc([128, SEQ], F32, "srst"); dma("sp", srst, cd["srst"][:, :], writes=[B_gc])
        gmask = A.alloc([128, 128], F32, "gmask"); dma("sp", gmask, cd["gmask"][:, :], writes=[B_gc])
        glag = A.alloc([128, 128], F32, "glag"); dma("sp", glag, gla_g_d[:, :], writes=[B_gc])
        alT = A.alloc([16, SEQ], F32, "alT"); B_al = Buf("al")
        qbT = A.alloc([128, 2, SEQ], BF16, "qbT"); kbT = A.alloc([128, 2, SEQ], BF16, "kbT"); B_qk = Buf("qk")
        vb = A.alloc([128, 16, 512], BF16, "vb"); B_vb = Buf("vb")
        rb = A.alloc([128, 16, 512], BF16, "rb"); B_rb = Buf("rb")
        sr = A.alloc([128, 16, 512], BF16, "sr"); B_sr = Buf("sr")
        rg = A.alloc([128, 16, 512], BF16, "rg"); B_rg = Buf("rg")
        laT = A.alloc([128, 2, SEQ], F32, "laT"); B_la = Buf("la")
        bT = A.alloc([128, 2, SEQ], F32, "bT"); B_b = Buf("b")
        ET = A.alloc([128, 2, SEQ], F32, "ET"); B_E = Buf("E")
        qd4 = A.alloc([128, 4, SEQ], BF16, "qd4"); B_qd = Buf("qd")
        kdT = A.alloc([128, 2, SEQ], BF16, "kdT"); B_kd = Buf("kd")
        dec = A.alloc([128, 2, 32], F32, "dec"); B_dec = Buf("dec")
        G_memset(qd4, 0.0, [B_qd])
        kd_ab = A.alloc([128, 2, 2, 128], BF16, "kd_ab"); B_kab = Buf("kab")
        G_memset(kd_ab, 0.0, [B_kab])
        Sf = [A.alloc([128, 128], F32, "Sf%d" % c) for c in range(2)]; B_Sf = [Buf("Sf%d" % c) for c in range(2)]
        tmpS = [A.alloc([128, 128], F32, "tS%d" % c) for c in range(2)]; B_tS = [Buf("tS%d" % c) for c in range(2)]
        Sbf = [[A.alloc([128, 128], BF16, "Sbf%d%d" % (c, a)) for a in range(2)] for c in range(2)]
        B_Sbf = [[Buf("Sbf%d%d" % (c, a)) for a in range(2)] for c in range(2)]
        att = A.alloc([128, 4, 128], BF16, "att"); B_att = Buf("att")
        ss = A.alloc([128, 16], F32, "ss"); B_ss = Buf("ss")
        junk = A.alloc([128, 128], BF16, "junk"); B_junk = Buf("junk")
        ogb = A.alloc([128, 512], BF16, "ogb"); B_ogb = Buf("ogb")
        ogT = A.alloc([128, 4, 512], BF16, "ogT"); B_ogT = Buf("ogT")
        for sq in range(NSEQ):
            tb0 = sq * SEQ
            dma("sp", alT, s_al[:, tb0:tb0 + SEQ], writes=[B_al])
            dma("sp", qbT, s_fm[FM_QB:FM_QB + 2, :, tb0:tb0 + SEQ].rearrange("c p t -> p c t"), writes=[B_qk])
            dma("sp", kbT, s_fm[FM_KB:FM_KB + 2, :, tb0:tb0 + SEQ].rearrange("c p t -> p c t"), writes=[B_qk])
            dma("sp", vb, s_tm[tb0:tb0 + SEQ, TM_VB:TM_VB + 512].rearrange("(kt p) n -> p kt n", p=128), writes=[B_vb])
            dma("sp", rb, s_tm[tb0:tb0 + SEQ, TM_RB:TM_RB + 512].rearrange("(kt p) n -> p kt n", p=128), writes=[B_rb])
            for c in range(2):
                for tt in range(4):
                    mm(PS[0], wa2[0:16, c * 128:(c + 1) * 128], alT[0:16, tt * 512:(tt + 1) * 512], True, True,
                       reads=[B_gc, B_al], writes=[PSB[0]])
                    A_act(laT[:, c, tt * 512:(tt + 1) * 512], PS[0], AF.Exp, [PSB[0], B_gc], [B_la], scale=-1.0, bias=nba[:, c:c + 1])
                A_act(laT[:, c, :], laT[:, c, :], AF.Ln, [B_la], [B_la], bias=1.0)
                P.op("dve", lambda e, c=c: e.tensor_tensor_scan(out=bT[:, c, :], data0=srst, data1=laT[:, c, :], initial=0.0,
                                                                op0=ALU.mult, op1=ALU.add), reads=[B_gc, B_la], writes=[B_b])
                A_act(ET[:, c, :], bT[:, c, :], AF.Exp, [B_b], [B_E], scale=-1.0 / 16)
                A_act(dec[:, c, :], bT[:, c, :].rearrange("p (n s) -> p n s", s=64)[:, :, 63], AF.Exp, [B_b], [B_dec], scale=-1.0 / 16)
                for hh in range(2):
                    lo = 64 * hh
                    V_stt(qd4[lo:lo + 64, 2 * c + hh, :], qbT[lo:lo + 64, c, :], 0.125, ET[lo:lo + 64, c, :], ALU.mult, ALU.mult,
                          [B_qk, B_E], [B_qd])
            for c in range(2):
                A_act(ET[:, c, :], bT[:, c, :], AF.Exp, [B_b], [B_E], scale=1.0 / 16)
                V_tt(kdT[:, c, :], kbT[:, c, :], ET[:, c, :], ALU.mult, [B_qk, B_E], [B_kd])
            A_act(sr, rb, AF.Sigmoid, [B_rb], [B_sr])
            G_tt(rg, rb, sr, ALU.mult, [B_rb, B_sr], [B_rg])
            rg4 = rg.rearrange("p k (h e) -> p (k h) e", e=128)
            G_tt(rg4, rg4, glag.unsqueeze(1).to_broadcast([128, 64, 128]), ALU.mult, [B_rg, B_gc], [B_rg])
            for c in range(2):
                P.op("dve", lambda e, c=c: e.memset(Sf[c], 0.0), writes=[B_Sf[c]])
            if "cs" in dbg and sq == 0:
                dma("sp", dbg["cs"][:, :, :], bT, reads=[B_b]); dma("sp", dbg["kd"][:, :, :], kdT, reads=[B_kd])
                dma("sp", dbg["qd"][:, :, :], qd4, reads=[B_qd]); dma("sp", dbg["rg"][:, :, :], rg, reads=[B_rg])
                dma("sp", dbg["la"][:, :, :], laT, reads=[B_la])
            for blk in range(16):
                t1 = blk * 128
                pst = PS[4].bitcast(BF16)
                for c in range(2):
                    transpose(pst[:, c * 128:(c + 1) * 128], kdT[:, c, t1:t1 + 128], ident_b, reads=[B_kd, B_ident], writes=[PSB[4]])
                for c in range(2):
                    V_copy(kd_ab[0:64, c, 0, :], pst[0:64, c * 128:(c + 1) * 128], [PSB[4]], [B_kab])
                    V_copy(kd_ab[64:128, c, 1, :], pst[64:128, c * 128:(c + 1) * 128], [PSB[4]], [B_kab])
                PSm = [PS[1][:, 0:256].rearrange("p (a e) -> p a e", a=2), PS[2][:, 0:256].rearrange("p (a e) -> p a e", a=2)]
                for c in range(2):
                    for ab in range(2):
                        for hh in range(2):
                            h = 2 * c + hh
                            mm(PSm[c][64 * hh:64 * hh + 64, ab, :], kd_ab[:, c, ab, 64 * hh:64 * hh + 64], vb[:, blk, h * 128:(h + 1) * 128],
                               True, True, reads=[B_kab, B_vb], writes=[PSB[1 + c]])
                PSa = PS[0].rearrange("p (h i) -> p h i", h=4)
                for h in range(4):
                    mm(PSa[:, h, :], kdT[:, h // 2, t1:t1 + 128], qd4[:, h, t1:t1 + 128], True, True,
                       reads=[B_kd, B_qd], writes=[PSB[0]])
                V_tt(att, PSa, gmask.unsqueeze(1).to_broadcast([128, 4, 128]), ALU.mult, [PSB[0], B_gc], [B_att])
                for c in range(2):
                    V_copy(Sbf[c][0], Sf[c], [B_Sf[c]], [B_Sbf[c][0]])
                    V_tt(tmpS[c], PSm[c][:, 0, :], Sf[c], ALU.add, [PSB[1 + c], B_Sf[c]], [B_tS[c]])
                    V_ts(Sf[c], tmpS[c], dec[:, c, 2 * blk:2 * blk + 1], None, ALU.mult, None, [B_tS[c], B_dec], [B_Sf[c]])
                    V_copy(Sbf[c][1], Sf[c], [B_Sf[c]], [B_Sbf[c][1]])
                    V_tt(tmpS[c], PSm[c][:, 1, :], Sf[c], ALU.add, [PSB[1 + c], B_Sf[c]], [B_tS[c]])
                    V_ts(Sf[c], tmpS[c], dec[:, c, 2 * blk + 1:2 * blk + 2], None, ALU.mult, None, [B_tS[c], B_dec], [B_Sf[c]])
                PSo = PS[3].rearrange("p (h e) -> p h e", h=4)
                for h in range(4):
                    c = h // 2
                    mm(PSo[:, h, :], att[:, h, :], vb[:, blk, h * 128:(h + 1) * 128], True, False,
                       reads=[B_att, B_vb], writes=[PSB[3]], skip=True)
                    mm(PSo[0:64, h, :], qd4[:, h, t1:t1 + 64], Sbf[c][0], False, False,
                       reads=[B_qd, B_Sbf[c][0]], writes=[PSB[3]], skip=True)
                    mm(PSo[64:128, h, :], qd4[:, h, t1 + 64:t1 + 128], Sbf[c][1], False, True,
                       reads=[B_qd, B_Sbf[c][1]], writes=[PSB[3]], skip=True)
                for h in range(4):
                    A_act(junk, PSo[:, h, :], AF.Square, [PSB[3]], [B_junk, B_ss], accum_out=ss[:, h:h + 1])
                A_act(ss[:, 4:8], ss[:, 0:4], AF.Sqrt, [B_ss], [B_ss], bias=EPS, scale=1.0 / 128)
                V_recip(ss[:, 8:12], ss[:, 4:8], [B_ss], [B_ss])
                for h in range(4):
                    V_stt(ogb[:, h * 128:(h + 1) * 128], PSo[:, h, :], ss[:, 8 + h:9 + h], rg[:, blk, h * 128:(h + 1) * 128],
                          ALU.mult, ALU.mult, [PSB[3], B_ss, B_rg], [B_ogb])
                if "ogb" in dbg and sq == 0 and blk == 0:
                    dma("sp", dbg["ogb"][:, :], ogb, reads=[B_ogb]); dma("sp", dbg["att"][:, :, :], att, reads=[B_att])
                pst2 = PS[5].bitcast(BF16)
                for fc in range(4):
                    transpose(pst2[:, fc * 128:(fc + 1) * 128], ogb[:, fc * 128:(fc + 1) * 128], ident_b,
                              reads=[B_ogb, B_ident], writes=[PSB[5]])
                evac_copy(ogT[:, :, (blk % 4) * 128:(blk % 4) * 128 + 128], pst2[:, 0:512].rearrange("p (f t) -> p f t", f=4),
                          [PSB[5]], [B_ogT])
                if blk % 4 == 3:
                    q0 = (blk // 4) * 512
                    dma("sp", s_og[:, :, tb0 + q0:tb0 + q0 + 512].rearrange("c p t -> p c t"), ogT, reads=[B_ogT])
    if "p1" in phases:
        P.fence()
        A.mark()
        try:
            if "nonsa" not in phases:
                phase1_nsa()
        except StopBuild:
            pass
        A.release()
        P.fence()
        A.mark()
        phase1_gla()
        A.release()
    def phase2():
        B_w2 = Buf("w2")
        B_wkv = Buf("wkv"); B_wb = Buf("wb"); B_wo = Buf("wo"); B_wq = Buf("wq"); B_wxo = Buf("wxo")
        wxkv = A.alloc([128, 8, D], BF16, "wxkv"); load_cast(wxkv, wxkv_d.rearrange("(kc p) n -> p kc n", p=128), B_wkv)
        wbn = A.alloc([128, 4, D], BF16, "wbn"); load_cast(wbn, wbn_d.rearrange("(kc p) n -> p kc n", p=128), B_wb)
        wbg = A.alloc([128, 4, D], BF16, "wbg"); load_cast(wbg, wbg_d.rearrange("(kc p) n -> p kc n", p=128), B_wb)
        wout = A.alloc([128, 8, D], BF16, "wout"); load_cast(wout, wout_d.rearrange("(kc p) n -> p kc n", p=128), B_wo)
        wxq = A.alloc([128, 8, 512], BF16, "wxq"); load_cast(wxq, wxq_d.rearrange("(kc p) n -> p kc n", p=128), B_wq)
        wxo = A.alloc([128, 4, D], BF16, "wxo"); load_cast(wxo, wxo_d.rearrange("(kc p) n -> p kc n", p=128), B_wxo)
        gx = A.alloc([128, 8], F32, "gx"); gm = A.alloc([128, 8], F32, "gm"); B_g = Buf("g2")
        dma("sp", gx, g_x_d[:, :], writes=[B_g]); dma("sp", gm, g_mem_d[:, :], writes=[B_g])
        xt = A.alloc([128, 4, D], F32, "xt"); B_xt = Buf("xt")
        xn = A.alloc([128, 4, D], BF16, "xn"); B_xn = Buf("xn")
        st = A.alloc([128, 32], F32, "st"); B_st = Buf("st")
        hxT = A.alloc([128, 8, 512], BF16, "hxT"); B_hx = Buf("hx")
        memt = A.alloc([128, 2, D], F32, "memt"); B_mem = Buf("mem")
        memT = A.alloc([128, 8, 256], BF16, "memT"); B_memT = Buf("memT")
        kxT = A.alloc([128, 4, 256], BF16, "kxT"); B_kx = Buf("kx")
        vxa = A.alloc([128, 2, 4, 129], BF16, "vxa"); B_vx = Buf("vx")
        G_memset(vxa[:, :, :, 128:129], 1.0, [B_vx])
        onT = A.alloc([128, 4, 512], BF16, "onT2"); ogT = A.alloc([128, 4, 512], BF16, "ogT2"); B_o = Buf("o2")
        sg = A.alloc([128, 16, 512], BF16, "sg"); B_sg = Buf("sg")
        mixT = A.alloc([128, 8, 512], BF16, "mixT"); B_mix = Buf("mix")
        tmp1 = [A.alloc([128, 512], F32, "tmp1%d" % i) for i in range(2)]; tmp2 = [A.alloc([128, 512], F32, "tmp2%d" % i) for i in range(2)]
        B_t1 = [Buf("t1%d" % i) for i in range(2)]; B_t2 = [Buf("t2%d" % i) for i in range(2)]
        qxT = A.alloc([128, 4, 512], BF16, "qxT"); B_qx = Buf("qx")
        PTx = [A.alloc([128, 512], BF16, "PTx%d" % i) for i in range(2)]; B_PTx = [Buf("PTx%d" % i) for i in range(2)]
        oxb = A.alloc([128, 4, 512], BF16, "oxb"); B_oxb = Buf("oxb")
        oxT = A.alloc([128, 4, 512], BF16, "oxT"); B_oxT = Buf("oxT")
        rd = A.alloc([128, 8], F32, "rd"); B_rd = Buf("rd")
        bk = [0]
        B_mixc = [Buf("mix%d" % i) for i in range(8)]

        def bank():
            b = 2 + bk[0] % 4; bk[0] += 1
            return b

        def p2_prefetch(it):
            t0 = it * 512
            dma("sp", onT, s_on[:, :, t0:t0 + 512].rearrange("c p t -> p c t"), writes=[B_o])
            dma("sp", ogT, s_og[:, :, t0:t0 + 512].rearrange("c p t -> p c t"), writes=[B_o])
            dma("sp", sg, s_fm[FM_MG:FM_MG + 16, :, t0:t0 + 512].rearrange("c p t -> p c t"), writes=[B_sg])

        for it in range(NTOK // 512):
            t0 = it * 512
            if it % 4 == 0:
                sq = it // 4
                dma("sp", memt, mem_d[sq * MEM:(sq + 1) * MEM, :].rearrange("(s p) d -> p s d", p=128), writes=[B_mem])
                rmsnorm_T(memt, B_mem, 2, gm, B_g, memT, B_memT, xn, B_xn, st, B_st, [0, 1])
                for hd in range(4):
                    pi = bank()
                    for kc in range(8):
                        mm(PS[pi][:, 0:256], wxkv[:, kc, hd * 128:(hd + 1) * 128], memT[:, kc, :], kc == 0, kc == 7,
                           reads=[B_wkv, B_memT], writes=[PSB[pi]])
                    evac_copy(kxT[:, hd, :], PS[pi][:, 0:256], [PSB[pi]], [B_kx])
                for ms in range(2):
                    pi = bank()
                    for kc in range(8):
                        mm(PS[pi], memT[:, kc, ms * 128:(ms + 1) * 128], wxkv[:, kc, 512:1024], kc == 0, kc == 7,
                           reads=[B_wkv, B_memT], writes=[PSB[pi]])
                    evac_copy(vxa[:, ms, :, 0:128], PS[pi].rearrange("p (h d) -> p h d", h=4), [PSB[pi]], [B_vx])
            dma("sp", xt, x_d[t0:t0 + 512, :].rearrange("(s p) d -> p s d", p=128), writes=[B_xt])
            if it == 0:
                p2_prefetch(0)
            for oc in range(8):
                p1 = bank(); p2 = bank()
                for kc in range(4):
                    mm(PS[p1], wbn[:, kc, oc * 128:(oc + 1) * 128], onT[:, kc, :], kc == 0, kc == 3, reads=[B_wb, B_o], writes=[PSB[p1]])
                for kc in range(4):
                    mm(PS[p2], wbg[:, kc, oc * 128:(oc + 1) * 128], ogT[:, kc, :], kc == 0, kc == 3, reads=[B_wb, B_o], writes=[PSB[p2]])
                j = oc % 2
                V_tt(tmp1[j], PS[p1], sg[:, oc, :], ALU.mult, [PSB[p1], B_sg], [B_t1[j]])
                V_tt(tmp2[j], PS[p2], sg[:, 8 + oc, :], ALU.mult, [PSB[p2], B_sg], [B_t2[j]])
                G_tt(mixT[:, oc, :], tmp1[j], tmp2[j], ALU.add, [B_t1[j], B_t2[j]], [B_mixc[oc]])
            if it + 1 < NTOK // 512:
                p2_prefetch(it + 1)
            for sub in range(4):
                for half in range(2):
                    pi = bank()
                    for kc in range(8):
                        mm(PS[pi], mixT[:, kc, sub * 128:(sub + 1) * 128], wout[:, kc, half * 512:(half + 1) * 512], kc == 0, kc == 7,
                           reads=[B_wo, B_mixc[kc]], writes=[PSB[pi]])
                    V_tt(xt[:, sub, half * 512:(half + 1) * 512], xt[:, sub, half * 512:(half + 1) * 512], PS[pi], ALU.add,
                         [B_xt, PSB[pi]], [B_xt])
            rmsnorm_T(xt, B_xt, 4, gx, B_g, hxT, B_hx, xn, B_xn, st, B_st, [0, 1])
            for hd in range(4):
                pi = bank()
                for kc in range(8):
                    mm(PS[pi], wxq[:, kc, hd * 128:(hd + 1) * 128], hxT[:, kc, :], kc == 0, kc == 7, reads=[B_wq, B_hx], writes=[PSB[pi]])
                evac_copy(qxT[:, hd, :], PS[pi], [PSB[pi]], [B_qx])
            for hd in range(4):
                for ms in range(2):
                    pi = bank()
                    mm(PS[pi], kxT[:, hd, ms * 128:(ms + 1) * 128], qxT[:, hd, :], True, True, reads=[B_kx, B_qx], writes=[PSB[pi]])
                    A_act(PTx[ms], PS[pi], AF.Exp, [PSB[pi]], [B_PTx[ms]], scale=128.0 ** -0.5)
                pa = [PS[6][:, 0:258].rearrange("p (s c) -> p s c", s=2), PS[7][:, 0:258].rearrange("p (s c) -> p s c", s=2)]
                for ms in range(2):
                    for sub in range(4):
                        mm(pa[sub // 2][:, sub % 2, :], PTx[ms][:, sub * 128:(sub + 1) * 128], vxa[:, ms, hd, :],
                           ms == 0 and sub % 2 == 0, ms == 1, reads=[B_PTx[ms], B_vx], writes=[PSB[6 + sub // 2]], skip=True)
                for bq in range(2):
                    V_recip(rd[:, 2 * bq:2 * bq + 2], pa[bq][:, :, 128], [PSB[6 + bq]], [B_rd])
                for sub in range(4):
                    V_ts(oxb[:, sub, hd * 128:(hd + 1) * 128], pa[sub // 2][:, sub % 2, 0:128], rd[:, sub:sub + 1], None, ALU.mult, None,
                         [PSB[6 + sub // 2], B_rd], [B_oxb])
            for fc in range(4):
                pi = bank()
                pst = PS[pi].bitcast(BF16)
                for sub in range(4):
                    transpose(pst[:, sub * 128:(sub + 1) * 128], oxb[:, sub, fc * 128:(fc + 1) * 128], ident_b,
                              reads=[B_oxb, B_ident], writes=[PSB[pi]])
                evac_copy(oxT[:, fc, :], pst[:, 0:512], [PSB[pi]], [B_oxT])
            for sub in range(4):
                for half in range(2):
                    pi = bank()
                    for kc in range(4):
                        mm(PS[pi], oxT[:, kc, sub * 128:(sub + 1) * 128], wxo[:, kc, half * 512:(half + 1) * 512], kc == 0, kc == 3,
                           reads=[B_wxo, B_oxT], writes=[PSB[pi]])
                    V_tt(xt[:, sub, half * 512:(half + 1) * 512], xt[:, sub, half * 512:(half + 1) * 512], PS[pi], ALU.add,
                         [B_xt, PSB[pi]], [B_xt])
            dma("sp", s_x2[t0:t0 + 512, :].rearrange("(s p) d -> p s d", p=128), xt, reads=[B_xt])

    def phase3():
        B_w3 = Buf("w3")
        wup = A.alloc([128, 8, 2 * FFN], BF16, "wup")
        B_wu = [Buf("wu%d" % i) for i in range(4)]
        wsrc = wup_d.rearrange("(kc p) n -> p kc n", p=128)
        for pc in (0, 2, 1, 3):
            load_cast_pieces(wup, wsrc, [(1408 * pc, 1408 * (pc + 1))], [B_wu[pc]])
        wdn = A.alloc([128, 22, D], BF16, "wdn"); load_cast(wdn, wdn_d.rearrange("(kc p) n -> p kc n", p=128), B_w3, nsplit=1)
        gf = A.alloc([128, 8], F32, "gf"); B_g = Buf("g3"); dma("sp", gf, g_ffn_d[:, :], writes=[B_g])
        cw = A.alloc([128, 3, 22], F32, "cw"); cbv = A.alloc([128, 22], F32, "cbv")
        dma("sp", cw, convw_d[:, :, :], writes=[B_g]); dma("sp", cbv, convb_d[:, :], writes=[B_g])
        gfin = A.alloc([128, D], F32, "gfin"); dma("sp", gfin, g_fin_d[:, :], writes=[B_g])
        xt = A.alloc([128, 4, D], F32, "xt"); B_xt = Buf("xt")
        xn = A.alloc([128, 4, D], BF16, "xn"); B_xn = Buf("xn")
        st = A.alloc([128, 32], F32, "st"); B_st = Buf("st")
        hfT = A.alloc([128, 8, 512], BF16, "hfT"); B_hf = Buf("hf")
        aT = A.alloc([128, 22, 512], BF16, "aT"); B_a = Buf("aT")
        usb = [A.alloc([128, 514], F32, "usb%d" % i) for i in range(2)]; B_u = [Buf("u%d" % i) for i in range(2)]
        acc = [A.alloc([128, 512], F32, "acc%d" % i) for i in range(2)]; B_acc = [Buf("acc%d" % i) for i in range(2)]
        carry = A.alloc([128, 22, 2], F32, "carry"); B_car = Buf("carry")
        bk = [0]

        def bank():
            b = (2 + bk[0]) % 8; bk[0] += 1
            return b

        B_ac = [Buf("aT%d" % i) for i in range(22)]
        for it in range(NTOK // 512):
            t0 = it * 512
            dma("sp", xt, s_x2[t0:t0 + 512, :].rearrange("(s p) d -> p s d", p=128), writes=[B_xt])
            if it % 4 == 0:
                P.op("dve", lambda e: e.memset(carry, 0.0), writes=[B_car])
            rmsnorm_T(xt, B_xt, 4, gf, B_g, hfT, B_hf, xn, B_xn, st, B_st, [0, 1])
            pend = []
            for fcn in range(22):
                pu = bank(); pg = bank()
                for kc in range(8):
                    mm(PS[pu], wup[:, kc, fcn * 128:(fcn + 1) * 128], hfT[:, kc, :], kc == 0, kc == 7, reads=[B_wu[fcn // 11], B_hf], writes=[PSB[pu]])
                for kc in range(8):
                    mm(PS[pg], wup[:, kc, FFN + fcn * 128:FFN + (fcn + 1) * 128], hfT[:, kc, :], kc == 0, kc == 7,
                       reads=[B_wu[2 + fcn // 11], B_hf], writes=[PSB[pg]])
                j = fcn % 2
                V_copy(usb[j][:, 0:2], carry[:, fcn, :], [B_car], [B_u[j]])
                A_act(usb[j][:, 2:514], PS[pu], AF.Copy, [PSB[pu]], [B_u[j]])
                V_copy(carry[:, fcn, :], usb[j][:, 512:514], [B_u[j]], [B_car])
                V_ts(acc[j], usb[j][:, 2:514], cw[:, 2, fcn:fcn + 1], cbv[:, fcn:fcn + 1], ALU.mult, ALU.add, [B_u[j], B_g], [B_acc[j]])
                V_stt(acc[j], usb[j][:, 1:513], cw[:, 1, fcn:fcn + 1], acc[j], ALU.mult, ALU.add, [B_u[j], B_g, B_acc[j]], [B_acc[j]])
                V_stt(acc[j], usb[j][:, 0:512], cw[:, 0, fcn:fcn + 1], acc[j], ALU.mult, ALU.add, [B_u[j], B_g, B_acc[j]], [B_acc[j]])
                A_act(acc[j], acc[j], AF.Gelu_apprx_tanh, [B_acc[j]], [B_acc[j]])
                if pend:
                    pend.pop(0)()
                pend.append(lambda fcn=fcn, pg=pg, j=j: V_tt(aT[:, fcn, :], PS[pg], acc[j], ALU.mult, [PSB[pg], B_acc[j]], [B_ac[fcn]]))
            while pend:
                pend.pop(0)()
            for sub in range(4):
                for half in range(2):
                    pi = bank()
                    for kc in range(22):
                        mm(PS[pi], aT[:, kc, sub * 128:(sub + 1) * 128], wdn[:, kc, half * 512:(half + 1) * 512], kc == 0, kc == 21,
                           reads=[B_w3, B_ac[kc]], writes=[PSB[pi]])
                    V_tt(xt[:, sub, half * 512:(half + 1) * 512], xt[:, sub, half * 512:(half + 1) * 512], PS[pi], ALU.add,
                         [B_xt, PSB[pi]], [B_xt])
            if "aT" in dbg and it == 0:
                dma("sp", dbg["aT"][:, :, :], aT, reads=B_ac); dma("sp", dbg["x3"][:, :, :], xt, reads=[B_xt])
                dma("sp", dbg["hf"][:, :, :], hfT, reads=[B_hf])
            for s in range(4):
                A_act(xn[:, s, :], xt[:, s, :], AF.Square, [B_xt], [B_xn, B_st], accum_out=st[:, s:s + 1])
            A_act(st[:, 8:12], st[:, 0:4], AF.Sqrt, [B_st], [B_st], bias=EPS, scale=1.0 / D)
            V_recip(st[:, 16:20], st[:, 8:12], [B_st], [B_st])
            for s in range(4):
                V_stt(xt[:, s, :], xt[:, s, :], st[:, 16 + s:17 + s], gfin, ALU.mult, ALU.mult, [B_xt, B_st, B_g], [B_xt])
            dma("sp", out_d[t0:t0 + 512, :].rearrange("(s p) d -> p s d", p=128), xt, reads=[B_xt])

    if "p2" in phases:
        P.fence(); A.mark(); phase2(); A.release()
    if "p3" in phases:
        P.fence(); A.mark(); phase3(); A.release()
    for e in ENGS:
        last = {}
        for o in P.ops[e]:
            if o.dma:
                last[id(o.token)] = o
        seen = {}
        nd = 0
        for o in P.ops[e]:
            if o.dma:
                seen[nd % NDMA_SLOTS] = o
                nd += 1
        P.final += list(seen.values())
    P.emit(nc, stack)
    stack.close()
    return nc, consts


def prep_inputs(inp):
    f = lambda a: np.ascontiguousarray(np.asarray(a, dtype=np.float32))
    w_in = f(inp["w_in"][0])
    shared = {
        "w_fm": f(w_in[:, _fm_cols()]),
        "w_tm": f(w_in[:, _tm_cols()]),
        "g_mix": pmajor(inp["ln_mix_g"][0], 8),
        "gate_b": f(np.broadcast_to(np.asarray(inp["nsa_gate_b"][0]).reshape(1, 24), (128, 24))),
        "w1k": f(inp["cmp_w1_k"][0]), "w1v": f(inp["cmp_w1_v"][0]),
        "w2k": f(np.concatenate([inp["cmp_w2_k"][0], inp["cmp_w2_k"][0]], axis=1)),
        "w2v": f(inp["cmp_w2_v"][0]),
        "pek": f(np.asarray(inp["cmp_pos_k"][0]).T), "pev": f(np.asarray(inp["cmp_pos_v"][0]).T),
        "wa2": f(np.concatenate([inp["gla_w_alpha2"][0], np.asarray(inp["gla_b_alpha"][0]).reshape(1, 256)], axis=0)),
        "gla_g": f(np.broadcast_to(np.asarray(inp["gla_norm_g"][0]).reshape(1, 128), (128, 128))),
        "ba": pmajor(inp["gla_b_alpha"][0], 2),
        "wbn": f(inp["w_branch_nsa"][0]), "wbg": f(inp["w_branch_gla"][0]), "wout": f(inp["w_out"][0]),
        "g_x": pmajor(inp["ln_x_g"][0], 8), "g_mem": pmajor(inp["ln_mem_g"][0], 8),
        "wxq": f(inp["w_xq"][0]), "wxkv": f(inp["w_xkv"][0]), "wxo": f(inp["w_xo"][0]),
        "g_ffn": pmajor(inp["ln_ffn_g"][0], 8),
        "wup": f(inp["w_up"][0]), "wdn": f(inp["w_down"][0]),
        "convw": f(np.asarray(inp["conv_w"][0]).reshape(3, 22, 128).transpose(2, 0, 1)),
        "convb": f(np.asarray(inp["conv_b"][0]).reshape(22, 128).T),
        "g_fin": f(np.broadcast_to(np.asarray(inp["ln_final_g"]).reshape(1, D), (128, D))),
    }
    for k, v in host_consts().items():
        shared["c_" + k] = v
    x = np.asarray(inp["x"], dtype=np.float32)
    mem = np.asarray(inp["mem"], dtype=np.float32)
    maps = []
    for c in range(NCORES):
        m = dict(shared)
        m["x"] = np.ascontiguousarray(x[c * NSEQ:(c + 1) * NSEQ].reshape(NTOK, D))
        m["mem"] = np.ascontiguousarray(mem[c * NSEQ:(c + 1) * NSEQ].reshape(NSEQ * MEM, D))
        maps.append(m)
    return maps


_CACHE = {}


def kernel(**inputs):
    if "nc" not in _CACHE:
        _CACHE["nc"] = build_program()[0]
    nc = _CACHE["nc"]
    maps = prep_inputs(inputs)
    res = run_bass_kernel_spmd(nc, maps, core_ids=list(range(NCORES)))
    out = np.stack([np.asarray(r["out"]).reshape(NSEQ, SEQ, D) for r in res.results], axis=0)
    return out.reshape(NCORES * NSEQ, SEQ, D).astype(np.float32)
```
